# Optimizing a Trainium2 kernel written in Bass

```python
import math
import jax
import jax.numpy as jnp
from jax import lax
import numpy as np

D_MODEL = 1024
BATCH = 8
SEQ = 8192
DEPTH = 2
DEC_BATCH = 32
DEC_SEQ = 2048
PAST_LEN = 128

HEAD_DIM = 64
GRID_W = 64
ROPE_THETA = 10000.0
EPS = 1e-6
A_HEADS = D_MODEL // (2 * HEAD_DIM)
A_PATTERNS = ((128, 1), (512, 4), (2048, 16))
B_Q_HEADS = D_MODEL // (2 * HEAD_DIM)
B_KV_HEADS = B_Q_HEADS // 4
Q_BLOCK = 128
C_HEADS = D_MODEL // (2 * HEAD_DIM)
C_CHUNK = 64
D_GROUPS = D_MODEL // (2 * HEAD_DIM)
D_GROUP_DIM = HEAD_DIM
D_CHUNK = 128
D_FF = 4 * D_MODEL
N_EVEN = (DEPTH + 1) // 2
N_ODD = DEPTH // 2
A_W = A_HEADS * HEAD_DIM
B_QW = B_Q_HEADS * HEAD_DIM
B_KVW = B_KV_HEADS * HEAD_DIM
AB_IN = 3 * A_W + B_QW + 2 * B_KVW
AB_OUT = A_W + B_QW
C_W = C_HEADS * HEAD_DIM
D_W = D_GROUPS * D_GROUP_DIM
CD_IN = 4 * C_W + 4 * C_HEADS + 2 * D_W
CD_OUT = C_W + D_W

kernel_name = 'hybrid_bidir_encoder_dilated_gqa_mlstm_sgu'


def rmsnorm(x, g):
    xf = x.astype(jnp.float32)
    y = xf * lax.rsqrt(jnp.mean(xf * xf, axis=-1, keepdims=True) + EPS)
    return (y * g.astype(jnp.float32)).astype(x.dtype)


def layernorm(x, g):
    xf = x.astype(jnp.float32)
    mu = jnp.mean(xf, axis=-1, keepdims=True)
    var = jnp.mean(jnp.square(xf - mu), axis=-1, keepdims=True)
    return ((xf - mu) * lax.rsqrt(var + EPS) * g.astype(jnp.float32)).astype(x.dtype)


def rope_angles(pos, dim):
    inv_freq = ROPE_THETA ** (-jnp.arange(0, dim, 2, dtype=jnp.float32) / dim)
    return pos.astype(jnp.float32)[:, None] * inv_freq[None, :]


def apply_rotary(x, ang):
    cos = jnp.cos(ang)[:, None, :]
    sin = jnp.sin(ang)[:, None, :]
    x1, x2 = jnp.split(x.astype(jnp.float32), 2, axis=-1)
    return jnp.concatenate([x1 * cos - x2 * sin, x2 * cos + x1 * sin], axis=-1).astype(x.dtype)


def apply_axial_rotary(x, ang_row, ang_col):
    half = x.shape[-1] // 2
    return jnp.concatenate([apply_rotary(x[..., :half], ang_row),
                            apply_rotary(x[..., half:], ang_col)], axis=-1)


def dilated_branch(q, k, v, window, dilation):
    B, S, H, Dh = q.shape
    R = window // (2 * dilation)
    L = S // dilation
    nb = -(-L // R)
    Lp = nb * R

    def to_sub(t):
        t = t.reshape(B, L, dilation, H, Dh).transpose(0, 2, 3, 1, 4)
        return jnp.pad(t, ((0, 0), (0, 0), (0, 0), (0, Lp - L), (0, 0)))

    def key_blocks(t):
        tp = jnp.pad(to_sub(t), ((0, 0), (0, 0), (0, 0), (R, R), (0, 0)))
        tp = tp.reshape(B, dilation, H, nb + 2, R, Dh)
        return jnp.concatenate([tp[:, :, :, :-2], tp[:, :, :, 1:-1], tp[:, :, :, 2:]], axis=4)

    qb = to_sub(q).reshape(B, dilation, H, nb, R, Dh)
    kb = key_blocks(k)
    vb = key_blocks(v)
    qi = jnp.arange(Lp).reshape(nb, R)
    kj = jnp.arange(-R, Lp + R).reshape(nb + 2, R)
    kj = jnp.concatenate([kj[:-2], kj[1:-1], kj[2:]], axis=1)
    dist = jnp.abs(qi[:, :, None] - kj[:, None, :])
    valid = ((dist <= R) & (kj >= 0)[:, None, :] & (kj < L)[:, None, :]) | (dist == 0)
    s = jnp.einsum('bdhnqe,bdhnke->bdhnqk', qb, kb,
                   preferred_element_type=jnp.float32) * (Dh ** -0.5)
    s = jnp.where(valid, s, -jnp.inf)
    lse = jax.nn.logsumexp(s, axis=-1)
    p = jnp.exp(s - lse[..., None])
    o = jnp.einsum('bdhnqk,bdhnke->bdhnqe', p, vb.astype(jnp.float32))
    o = o.reshape(B, dilation, H, Lp, Dh)[:, :, :, :L].transpose(0, 3, 1, 2, 4).reshape(B, S, H, Dh)
    lse = lse.reshape(B, dilation, H, Lp)[:, :, :, :L].transpose(0, 3, 1, 2).reshape(B, S, H)
    return o, lse


def gqa_full(q, k, v):
    B, S, Hq, Dh = q.shape
    Hkv = k.shape[2]
    G = Hq // Hkv
    nq = S // Q_BLOCK
    qb = q.reshape(B, nq, Q_BLOCK, Hkv, G, Dh).transpose(1, 0, 2, 3, 4, 5)
    vf = v.astype(jnp.float32)

    def block(qblk):
        s = jnp.einsum('bqhgd,bkhd->bhgqk', qblk, k,
                       preferred_element_type=jnp.float32) * (Dh ** -0.5)
        p = jax.nn.softmax(s, axis=-1)
        return jnp.einsum('bhgqk,bkhd->bqhgd', p, vf)

    o = lax.map(block, qb)
    return o.transpose(1, 0, 2, 3, 4, 5).reshape(B, S, Hq, Dh)


def mlstm_chunkwise(q, k, v, i_pre, log_f):
    B, H, S, Dh = q.shape
    L = C_CHUNK
    nc = S // L
    k = k * (Dh ** -0.5)

    def chunks(t):
        return jnp.moveaxis(t.reshape((B, H, nc, L) + t.shape[3:]), 2, 0)

    causal = jnp.tril(jnp.ones((L, L), dtype=bool))

    def step(carry, inp):
        C, n, m = carry
        qq, kk, vv, ii, ff = inp
        b = jnp.cumsum(ff, axis=-1)
        log_d = b[..., :, None] - b[..., None, :] + ii[..., None, :]
        log_d = jnp.where(causal, log_d, -jnp.inf)
        inter = b + m[..., None]
        m_t = jnp.maximum(inter, jnp.max(log_d, axis=-1))
        dmat = jnp.exp(log_d - m_t[..., None])
        w_inter = jnp.exp(inter - m_t)
        s = jnp.einsum('bhtd,bhsd->bhts', qq, kk) * dmat
        num = jnp.einsum('bhts,bhse->bhte', s, vv) + \
            w_inter[..., None] * jnp.einsum('bhed,bhtd->bhte', C, qq)
        den = jnp.sum(s, axis=-1) + w_inter * jnp.einsum('bhd,bhtd->bht', n, qq)
        h = num / jnp.maximum(jnp.abs(den), jnp.exp(-m_t))[..., None]
        b_last = b[..., -1]
        log_w = b_last[..., None] - b + ii
        m_new = jnp.maximum(b_last + m, jnp.max(log_w, axis=-1))
        wk = jnp.exp(log_w - m_new[..., None])
        decay = jnp.exp(b_last + m - m_new)
        C_new = decay[..., None, None] * C + jnp.einsum('bhs,bhse,bhsd->bhed', wk, vv, kk)
        n_new = decay[..., None] * n + jnp.einsum('bhs,bhsd->bhd', wk, kk)
        return (C_new, n_new, m_new), h

    init = (jnp.zeros((B, H, Dh, Dh), jnp.float32),
            jnp.zeros((B, H, Dh), jnp.float32),
            jnp.zeros((B, H), jnp.float32))
    _, h = lax.scan(step, init, (chunks(q), chunks(k), chunks(v), chunks(i_pre), chunks(log_f)))
    return jnp.moveaxis(h, 0, 2).reshape(B, H, S, Dh)


def spatial_gating(u, v, ln_g, w_s, b_s):
    B, S, _ = u.shape
    nc = S // D_CHUNK
    u = jax.nn.gelu(u)
    v = layernorm(jax.nn.gelu(v), ln_g)
    vb = v.reshape(B, nc, D_CHUNK, D_GROUPS, D_GROUP_DIM)
    s = jnp.einsum('gpq,bcqge->bcpge', w_s, vb) + b_s.T[None, None, :, :, None]
    return u * s.reshape(B, S, D_W)


def even_mixer(h, w_in, w_out, qk_g, ang_1d, ang_row, ang_col):
    B, S, _ = h.shape
    z = h @ w_in
    o1 = A_W
    o2 = 2 * A_W
    o3 = 3 * A_W
    o4 = o3 + B_QW
    o5 = o4 + B_KVW
    qa, ka, va, qb, kb, vb = jnp.split(z, [o1, o2, o3, o4, o5], axis=-1)
    qa = apply_rotary(qa.reshape(B, S, A_HEADS, HEAD_DIM), ang_1d)
    ka = apply_rotary(ka.reshape(B, S, A_HEADS, HEAD_DIM), ang_1d)
    va = va.reshape(B, S, A_HEADS, HEAD_DIM)
    outs = []
    lses = []
    for window, dil in A_PATTERNS:
        o_g, l_g = dilated_branch(qa, ka, va, window, dil)
        outs.append(o_g)
        lses.append(l_g)
    alpha = jax.nn.softmax(jnp.stack(lses, axis=0), axis=0)
    oa = jnp.sum(alpha[..., None] * jnp.stack(outs, axis=0), axis=0)
    qb = rmsnorm(qb.reshape(B, S, B_Q_HEADS, HEAD_DIM), qk_g[0])
    kb = rmsnorm(kb.reshape(B, S, B_KV_HEADS, HEAD_DIM), qk_g[1])
    qb = apply_axial_rotary(qb, ang_row, ang_col)
    kb = apply_axial_rotary(kb, ang_row, ang_col)
    ob = gqa_full(qb, kb, vb.reshape(B, S, B_KV_HEADS, HEAD_DIM))
    o = jnp.concatenate([oa.reshape(B, S, A_W), ob.reshape(B, S, B_QW)], axis=-1).astype(h.dtype)
    return o @ w_out


def odd_mixer(h, w_in, w_out, gate_b, mh_g, sg_g, w_s, b_s):
    B, S, _ = h.shape
    z = h @ w_in
    o1 = C_W
    o2 = 2 * C_W
    o3 = 3 * C_W
    o4 = 4 * C_W
    o5 = o4 + 4 * C_HEADS
    o6 = o5 + D_W
    q, k, v, og, gates, u, vd = jnp.split(z, [o1, o2, o3, o4, o5, o6], axis=-1)

    def heads(t):
        return t.reshape(B, S, C_HEADS, HEAD_DIM).transpose(0, 2, 1, 3).astype(jnp.float32)

    q, k, v = heads(q), heads(k), heads(v)
    g = gates.astype(jnp.float32) + gate_b.reshape(-1).astype(jnp.float32)
    g = g.reshape(B, S, 4, C_HEADS).transpose(2, 0, 3, 1)
    i_fw, f_fw, i_bw, f_bw = g[0], g[1], g[2], g[3]
    h_fw = mlstm_chunkwise(q, k, v, i_fw, jax.nn.log_sigmoid(f_fw))
    flip = lambda t: jnp.flip(t, axis=2)
    h_bw = flip(mlstm_chunkwise(flip(q), flip(k), flip(v), flip(i_bw), flip(jax.nn.log_sigmoid(f_bw))))
    hc = (h_fw + h_bw).transpose(0, 2, 1, 3)
    hc = rmsnorm(hc, mh_g) * jax.nn.sigmoid(og.reshape(B, S, C_HEADS, HEAD_DIM).astype(jnp.float32))
    d_out = spatial_gating(u, vd, sg_g, w_s, b_s)
    o = jnp.concatenate([hc.reshape(B, S, C_W).astype(h.dtype), d_out.astype(h.dtype)], axis=-1)
    return o @ w_out


def trunk(x, c, w_in_ab, w_out_ab, qk_norm_g, w_in_cd, w_out_cd, gate_bias, mh_norm_g,
          sg_norm_g, w_spatial, b_spatial, w_ada, b_ada, norm_g, w_ff1, w_ff2):
    S = x.shape[1]
    rows = S // GRID_W
    pos = jnp.arange(S)
    ang_1d = rope_angles(pos, HEAD_DIM)
    row_ids = jnp.repeat(jnp.arange(rows), GRID_W)
    col_ids = jnp.tile(jnp.arange(GRID_W), rows)
    ang_row = rope_angles(row_ids, HEAD_DIM // 2)
    ang_col = rope_angles(col_ids, HEAD_DIM // 2)
    c_act = jax.nn.silu(c)
    for layer in range(DEPTH):
        mod = (c_act @ w_ada[layer] + b_ada[layer])[:, None, :]
        sh1, sc1, g1, sh2, sc2, g2 = jnp.split(mod, 6, axis=-1)
        hm = rmsnorm(x, norm_g[layer, 0]) * (1.0 + sc1) + sh1
        if layer % 2 == 0:
            e = layer // 2
            y = even_mixer(hm, w_in_ab[e], w_out_ab[e], qk_norm_g[e], ang_1d, ang_row, ang_col)
        else:
            o = layer // 2
            y = odd_mixer(hm, w_in_cd[o], w_out_cd[o], gate_bias[o], mh_norm_g[o],
                          sg_norm_g[o], w_spatial[o], b_spatial[o])
        x = x + g1 * rmsnorm(y, norm_g[layer, 1])
        hf = rmsnorm(x, norm_g[layer, 2]) * (1.0 + sc2) + sh2
        y = jnp.square(jax.nn.relu(hf @ w_ff1[layer])) @ w_ff2[layer]
        x = x + g2 * rmsnorm(y, norm_g[layer, 3])
    return x


def setup_inputs(seed: int = 0) -> dict:
    key = jax.random.key(seed)
    ks = jax.random.split(key, 24)

    def nrm(k, shape, scale):
        return jax.random.normal(k, shape, jnp.float32) * scale

    f_bias = jnp.linspace(3.0, 6.0, C_HEADS, dtype=jnp.float32)
    gate_bias = jnp.stack([nrm(ks[8], (N_ODD, C_HEADS), 0.1),
                           f_bias + nrm(ks[9], (N_ODD, C_HEADS), 0.1),
                           nrm(ks[10], (N_ODD, C_HEADS), 0.1),
                           f_bias + nrm(ks[11], (N_ODD, C_HEADS), 0.1)], axis=1)
    return {
        'x_prompt': nrm(ks[0], (BATCH, SEQ, D_MODEL), 1.0),
        'x_sample': nrm(ks[1], (DEC_BATCH, DEC_SEQ, D_MODEL), 1.0),
        'c_prompt': nrm(ks[2], (BATCH, D_MODEL), 1.0),
        'c_sample': nrm(ks[3], (DEC_BATCH, D_MODEL), 1.0),
        'w_in_ab': nrm(ks[4], (N_EVEN, D_MODEL, AB_IN), D_MODEL ** -0.5),
        'w_out_ab': nrm(ks[5], (N_EVEN, AB_OUT, D_MODEL), AB_OUT ** -0.5),
        'qk_norm_g': 1.0 + nrm(ks[6], (N_EVEN, 2, HEAD_DIM), 0.02),
        'w_in_cd': nrm(ks[7], (N_ODD, D_MODEL, CD_IN), D_MODEL ** -0.5),
        'w_out_cd': nrm(ks[12], (N_ODD, CD_OUT, D_MODEL), CD_OUT ** -0.5),
        'gate_bias': gate_bias,
        'mh_norm_g': 1.0 + nrm(ks[13], (N_ODD, C_HEADS, HEAD_DIM), 0.02),
        'sg_norm_g': 1.0 + nrm(ks[14], (N_ODD, D_W), 0.02),
        'w_spatial': nrm(ks[15], (N_ODD, D_GROUPS, D_CHUNK, D_CHUNK), D_CHUNK ** -0.5),
        'b_spatial': nrm(ks[16], (N_ODD, D_GROUPS, D_CHUNK), 0.02),
        'w_ada': nrm(ks[17], (DEPTH, D_MODEL, 6 * D_MODEL), D_MODEL ** -0.5),
        'b_ada': nrm(ks[18], (DEPTH, 6 * D_MODEL), 0.02),
        'norm_g': 1.0 + nrm(ks[19], (DEPTH, 4, D_MODEL), 0.02),
        'w_ff1': nrm(ks[20], (DEPTH, D_MODEL, D_FF), D_MODEL ** -0.5),
        'w_ff2': nrm(ks[21], (DEPTH, D_FF, D_MODEL), D_FF ** -0.5),
    }


def reference(x_prompt, x_sample, c_prompt, c_sample, w_in_ab, w_out_ab, qk_norm_g, w_in_cd,
              w_out_cd, gate_bias, mh_norm_g, sg_norm_g, w_spatial, b_spatial, w_ada, b_ada,
              norm_g, w_ff1, w_ff2):
    y_prompt = trunk(x_prompt, c_prompt, w_in_ab, w_out_ab, qk_norm_g, w_in_cd, w_out_cd,
                     gate_bias, mh_norm_g, sg_norm_g, w_spatial, b_spatial, w_ada, b_ada,
                     norm_g, w_ff1, w_ff2)
    y_sample = trunk(x_sample, c_sample, w_in_ab, w_out_ab, qk_norm_g, w_in_cd, w_out_cd,
                     gate_bias, mh_norm_g, sg_norm_g, w_spatial, b_spatial, w_ada, b_ada,
                     norm_g, w_ff1, w_ff2)
    return (y_prompt, y_sample)
```

```python
import numpy as np
import os
from contextlib import ExitStack
import concourse.bass as bass
import concourse.mybir as mybir
from concourse.bass_utils import run_bass_kernel_spmd

F32 = mybir.dt.float32
BF16 = mybir.dt.bfloat16
AF = mybir.ActivationFunctionType
ALU = mybir.AluOpType
AX = mybir.AxisListType
ENGS = ('pe', 'act', 'dve', 'pool', 'sp')
D = 1024
EPS = 1e-6


class Sched:
    def __init__(self, nc, stack):
        self.nc = nc
        self.stack = stack
        self.streams = {e: [] for e in ENGS}
        self.sems = {}
        self.cnt = {}
        self.seen = {e: {} for e in ENGS}
        self.bufs = {}
        self.snap = {}
        self.nwaits = 0
        self.nops = 0

    def sem(self, name):
        if name not in self.sems:
            self.sems[name] = self.stack.enter_context(
                self.nc.semaphore(name.replace(':', '_').replace('/', '_')))
            self.cnt[name] = 0
        return self.sems[name]

    def _need(self, E, ev, waits):
        if ev is None:
            return
        name, val = ev
        if self.seen[E].get(name, 0) >= val:
            return
        if waits.get(name, 0) < val:
            waits[name] = val

    def _collect(self, E, reads, writes):
        waits = {}
        for k in reads:
            b = self.bufs.get(k)
            if b is not None:
                self._need(E, b['w'], waits)
        own = 'e:' + E
        for k in writes:
            b = self.bufs.get(k)
            if b is not None:
                self._need(E, b['w'], waits)
                for ev in b['r']:
                    if ev[0] != own:
                        self._need(E, ev, waits)
        return waits

    def _apply_waits(self, E, waits):
        seen = self.seen[E]
        wl = []
        for name, val in waits.items():
            if seen.get(name, 0) >= val:
                continue
            wl.append((self.sem(name), val))
            sn = self.snap.get((name, val))
            if sn:
                for n2, v2 in sn.items():
                    if seen.get(n2, 0) < v2:
                        seen[n2] = v2
            seen[name] = val
        self.nwaits += len(wl)
        return wl

    def _record(self, ev, reads, writes):
        for k in reads:
            b = self.bufs.get(k)
            if b is None:
                b = self.bufs[k] = {'w': None, 'r': []}
            b['r'].append(ev)
            if len(b['r']) > 64:
                b['r'] = b['r'][-48:]
        for k in writes:
            self.bufs[k] = {'w': ev, 'r': []}

    def op(self, E, fn, reads=(), writes=()):
        waits = self._collect(E, reads, writes)
        if E == 'pe':
            waits.pop('e:pe', None)
        wl = self._apply_waits(E, waits)
        name = 'e:' + E
        s = self.sem(name)
        self.cnt[name] += 1
        val = self.cnt[name]
        ev = (name, val)
        if E == 'pe':
            self.seen[E][name] = val
        self.snap[ev] = dict(self.seen[E])
        self.streams[E].append((wl, fn, s, 1))
        self._record(ev, reads, writes)
        self.nops += 1
        return ev

    def dma(self, Q, semname, out, in_, reads=(), writes=(), **kw):
        waits = self._collect(Q, reads, writes)
        wl = self._apply_waits(Q, waits)
        name = 'd:' + semname
        s = self.sem(name)
        self.cnt[name] += 16
        val = self.cnt[name]
        ev = (name, val)
        self.snap[ev] = dict(self.seen[Q])

        def fn(eng, out=out, in_=in_, kw=kw):
            return eng.dma_start(out=out, in_=in_, **kw)
        self.streams[Q].append((wl, fn, s, 16))
        self._record(ev, reads, writes)
        self.nops += 1
        return ev

    def barrier(self):
        allev = dict(self.cnt)
        for E in ENGS:
            waits = {}
            for name, val in allev.items():
                if val > 0 and self.seen[E].get(name, 0) < val and name != 'e:' + E:
                    waits[name] = val
            wl = self._apply_waits(E, waits)
            if wl:
                self.streams[E].append((wl, None, None, 0))
        self.bufs = {}

    def emit(self):
        nc = self.nc
        self.barrier()
        streams = self.streams
        with nc.Block() as block:
            def run(E):
                def body(eng):
                    for wl, fn, s, inc in streams[E]:
                        for (ws, wv) in wl:
                            eng.wait_ge(ws, wv)
                        if fn is not None:
                            fn(eng).then_inc(s, inc)
                return body
            block.sync(run('sp'))
            block.scalar(run('act'))
            block.vector(run('dve'))
            block.gpsimd(run('pool'))
            block.tensor(run('pe'))


class Ctx:
    pass


def build(seqs, phases, debug=False):
    nc = bass.Bass("TRN2", target_bir_lowering=False)
    NS = len(seqs)
    NT = sum(seqs)
    offs = [sum(seqs[:i]) for i in range(NS)]
    SMAX = max(seqs)
    g = Ctx()
    g.nc, g.seqs, g.NS, g.NT, g.offs, g.debug = nc, seqs, NS, NT, offs, debug

    def din(name, shape, dt=F32):
        return nc.dram_tensor(name, list(shape), dt, kind="ExternalInput").ap()

    def dscr(name, shape, dt):
        return nc.dram_tensor(name, list(shape), dt, kind=("ExternalOutput" if debug else "Internal")).ap()
    g.dscr = dscr
    I = Ctx()
    g.I = I
    I.x = din("x", [NT, D])
    I.cT = din("cT", [128, 8 * NS])
    I.w_in_ab = din("w_in_ab", [D, 2304])
    I.w_out_ab = din("w_out_ab", [D, D])
    I.w_in_cd = din("w_in_cd", [D, 3104])
    I.w_out_cd = din("w_out_cd", [D, D])
    I.w_ada = din("w_ada", [2, D, 6 * D])
    I.b_ada = din("b_ada", [1, 2 * 6 * D])
    I.normgT = din("normgT", [128, 2 * 4 * 8])
    I.norm_g = din("norm_g", [8, D])
    I.w_ff1 = din("w_ff1", [2, D, 4 * D])
    I.w_ff2 = din("w_ff2", [2, 4 * D, D])
    I.qkg = din("qkg", [1, 128])
    I.ropeA = din("ropeA", [SMAX, 64])
    I.ropeB = din("ropeB", [SMAX, 64])
    I.gate_bias = din("gate_bias", [32, 1])
    I.mhg = din("mhg", [1, 512])
    I.sgg = din("sgg", [1, 512])
    I.wsT = din("wsT", [128, 8 * 128])
    I.bsT = din("bsT", [128, 8])
    y = nc.dram_tensor("y", [NT, D], F32, kind="ExternalOutput").ap()
    g.y = y

    Sx = Ctx()
    g.S = Sx
    Sx.gates = dscr("s_gates", [2, 2, NS, D], F32)
    Sx.qaT = dscr("s_qaT", [512, NT], BF16)
    Sx.kaT = dscr("s_kaT", [512, NT], BF16)
    Sx.qbT = dscr("s_qbT", [512, NT], BF16)
    Sx.kbT = dscr("s_kbT", [128, NT], BF16)
    Sx.vA = dscr("s_vA", [NT, 512], BF16)
    Sx.vB = dscr("s_vB", [NT, 128], BF16)
    Sx.oT = dscr("s_oT", [D, NT], BF16)
    Sx.xmid = dscr("s_xmid", [NT, D], F32)
    Sx.x1 = dscr("s_x1", [NT, D], F32)
    Sx.qcT = dscr("s_qcT", [512, NT], BF16)
    Sx.kcT = dscr("s_kcT", [512, NT], BF16)
    Sx.kc = dscr("s_kc", [NT, 512], BF16)
    Sx.vc = dscr("s_vc", [NT, 512], BF16)
    Sx.ogs = dscr("s_ogs", [NT, 512], F32)
    Sx.gtok = dscr("s_gtok", [NT, 32], F32)
    Sx.o2T = dscr("s_o2T", [D, NT], BF16)

    with ExitStack() as st:
        s = Sched(nc, st)
        g.s = s
        g.identB = st.enter_context(nc.sbuf_tensor("identB", [128, 128], BF16))
        g.identF = st.enter_context(nc.sbuf_tensor("identF", [128, 128], F32))
        g.GS = st.enter_context(nc.sbuf_tensor("GS", [128, 2, 2, 8, NS], F32))
        g.SH = st.enter_context(nc.sbuf_tensor("SH", [128, 2, 2, 8, NS], F32))
        g.epsc = st.enter_context(nc.sbuf_tensor("epsc", [128, 2], F32))
        s.op('pool', lambda e: e.memset(g.epsc[:], EPS), writes=['epsc'])
        for t, nm in ((g.identB, 'identB'), (g.identF, 'identF')):
            s.op('pool', lambda e, t=t: e.memset(t[:], 1.0), writes=[nm])
            s.op('pool', lambda e, t=t: e.affine_select(out=t[:], in_=t[:], pattern=[[-1, 128]],
                                                        compare_op=ALU.is_equal, fill=0.0, base=0,
                                                        channel_multiplier=1), reads=[nm], writes=[nm])
        if 0 in phases:
            phase0(g)
        if 1 in phases:
            phase1(g)
        if 2 in phases:
            import os
            if not os.environ.get('SKIPB'):
                phase2(g)
            if not os.environ.get('SKIPA'):
                phase2a(g)
        if 3 in phases:
            phase3(g, 0, Sx.oT, I.w_out_ab, I.x, Sx.xmid)
        if 4 in phases:
            phase4(g, 0, Sx.xmid, Sx.x1 if (5 in phases or debug) else g.y)
        if 5 in phases:
            phase5(g)
        if 6 in phases:
            phase6(g)
        if 7 in phases:
            phase3(g, 1, Sx.o2T, I.w_out_cd, Sx.x1, Sx.xmid)
        if 8 in phases:
            phase4(g, 1, Sx.xmid, g.y)
        s.emit()
    g.stats = (s.nops, s.nwaits)
    return nc, g


def load_w_bf16(g, st_name, wt, w_dram, K, N, n0=0, colmap=None):
    s = g.s
    for k in range(K):
        c = 0
        while c < N:
            w = min(1024, N - c)
            s.dma('pool', st_name, wt[:, k, c:c + w], w_dram[k * 128:(k + 1) * 128, n0 + c:n0 + c + w],
                  writes=[st_name])
            c += w


def phase0(g):
    nc, s, NS, I = g.nc, g.s, g.NS, g.I
    with ExitStack() as st:
        sb = lambda n, sh, dt: st.enter_context(nc.sbuf_tensor(n, sh, dt))
        cTf = sb("p0_cTf", [128, 8 * NS], F32)
        cTb = sb("p0_cTb", [128, 8, NS], BF16)
        wt = [sb(f"p0_w{i}", [128, 8, 3072], BF16) for i in range(2)]
        bada = sb("p0_bada", [NS, 2 * 6 * D], F32)
        modrow = sb("p0_modrow", [NS, 2, 6 * D], F32)
        ngT = sb("p0_ngT", [128, 2, 4, 8], F32)
        modT = sb("p0_modT", [128, 2, 4, 8, NS], F32)
        ps = [st.enter_context(nc.psum_tensor(f"p0_ps{i}", [128, 512], F32)) for i in range(2)]
        pT = st.enter_context(nc.psum_tensor("p0_pT", [128, 4 * 8 * NS], F32))
        s.dma('sp', 'p0_cTf', cTf[:], I.cT[:, :], writes=['cTf'])
        s.dma('sp', 'p0_bada', bada[:], I.b_ada[0:1, :].partition_broadcast(NS), writes=['bada'])
        s.dma('sp', 'p0_ngT', ngT[:], I.normgT[:, :], writes=['ngT'])
        s.op('act', lambda e: e.activation(out=cTb[:].rearrange("p k s -> p (k s)"), in_=cTf[:], func=AF.Silu),
             reads=['cTf'], writes=['cTb'])
        it = 0
        for l in range(2):
            for hf in range(2):
                w = wt[it % 2]
                wn = f"p0_w{it % 2}"
                load_w_bf16(g, wn, w, I.w_ada[l], 8, 3072, n0=hf * 3072)
                for nch in range(6):
                    p = ps[nch % 2]
                    pn = f"p0_ps{nch % 2}"

                    def mm(e, p=p, w=w, nch=nch):
                        for k in range(8):
                            r = e.matmul(p[0:NS, :], lhsT=cTb[:, k, :], rhs=w[:, k, nch * 512:(nch + 1) * 512],
                                         start=(k == 0), stop=(k == 7))
                        return r
                    s.op('pe', mm, reads=['cTb', wn], writes=[pn])
                    c0 = hf * 3072 + nch * 512
                    s.op('dve', lambda e, p=p, l=l, c0=c0: e.tensor_tensor(
                        out=modrow[:, l, c0:c0 + 512], in0=p[0:NS, :],
                        in1=bada[:, l * 6 * D + c0:l * 6 * D + c0 + 512], op=ALU.add),
                        reads=[pn, 'bada'], writes=['modrow'])
                it += 1
        for l in range(2):
            for gi, part in enumerate((2, 5)):
                s.dma('sp', 'p0_gst', g.S.gates[l, gi], modrow[:, l, part * D:(part + 1) * D],
                      reads=['modrow'])
        for l in range(2):
            def tr(e, l=l):
                for pi, part in enumerate((0, 1, 3, 4)):
                    for k in range(8):
                        c0 = part * D + k * 128
                        o = (pi * 8 + k) * NS
                        r = e.transpose(out=pT[:, o:o + NS], in_=modrow[:, l, c0:c0 + 128],
                                        identity=g.identF[0:NS, 0:NS])
                return r
            s.op('pe', tr, reads=['modrow', 'identF'], writes=['p0_pT'])
            s.op('dve', lambda e, l=l: e.tensor_copy(out=modT[:, l].rearrange("p a k s -> p (a k s)"), in_=pT[:, :]),
                 reads=['p0_pT'], writes=['modT'])
        for l in range(2):
            for m in range(2):
                sc = modT[:, l, 2 * m + 1]
                sh = modT[:, l, 2 * m]
                ngb = ngT[:, l, 2 * m, :].unsqueeze(2).to_broadcast([128, 8, NS])
                s.op('dve', lambda e, sc=sc, ngb=ngb, l=l, m=m: e.scalar_tensor_tensor(
                    out=g.GS[:, l, m], in0=sc, scalar=1.0, in1=ngb, op0=ALU.add, op1=ALU.mult),
                    reads=['modT', 'ngT'], writes=['GS'])
                s.op('dve', lambda e, sh=sh, l=l, m=m: e.tensor_copy(out=g.SH[:, l, m], in_=sh),
                     reads=['modT'], writes=['SH'])
        s.barrier()


def bc(ap2d, shape):
    return ap2d.unsqueeze(1).to_broadcast(shape)


def rsqrt_act(g, out, in_, scale, rk, wk):
    s = g.s
    s.op('act', lambda e: e.activation(out=out, in_=in_, func=AF.Ln, scale=scale, bias=g.epsc[:, 0:1]),
         reads=rk + ['epsc'], writes=wk)
    s.op('act', lambda e: e.activation(out=out, in_=out, func=AF.Exp, scale=-0.5), reads=wk, writes=wk)


def rms_prep(g, pre, xt, xn, ss, rstd, junk, nblk, xkey, outkey):
    s = g.s
    for b in range(nblk):
        s.op('act', lambda e, b=b: e.activation(out=junk[:], in_=xt[:, b, :], func=AF.Square,
                                                accum_out=ss[:, b:b + 1]),
             reads=[xkey], writes=[pre + 'junk', pre + 'ss'])
    rsqrt_act(g, rstd[:, 0:nblk], ss[:, 0:nblk], 1.0 / D, [pre + 'ss'], [pre + 'rstd'])
    for b in range(nblk):
        s.op('act', lambda e, b=b: e.activation(out=xn[:, b, :], in_=xt[:, b, :], func=AF.Copy,
                                                scale=rstd[:, b:b + 1]),
             reads=[xkey, pre + 'rstd'], writes=[outkey])


def to_featmajor(g, pre, xn, hmT, tps, l, m, j, nblk, xnkey, hkey):
    s = g.s
    for half in range(nblk // 2):
        tp = tps[half % len(tps)]
        tk = pre + f'tp{half % len(tps)}'

        def tr(e, half=half, tp=tp):
            for k in range(8):
                for bb in range(2):
                    r = e.transpose(out=tp[:, k, bb * 128:(bb + 1) * 128],
                                    in_=xn[:, half * 2 + bb, k * 128:(k + 1) * 128], identity=g.identB[:])
            return r
        s.op('pe', tr, reads=[xnkey, 'identB'], writes=[tk])
        for k in range(8):
            eng = 'dve' if k % 2 == 0 else 'pool'
            eng = 'dve'
            s.op(eng, lambda e, k=k, half=half, tp=tp: e.tensor_scalar(
                out=hmT[:, k, half * 256:(half + 1) * 256], in0=tp[:, k, :],
                scalar1=g.GS[:, l, m, k, j:j + 1], scalar2=g.SH[:, l, m, k, j:j + 1],
                op0=ALU.mult, op1=ALU.add), reads=[tk, 'GS', 'SH'], writes=[hkey])


def rotary(g, eng, out, zin, cos, sin, H, hd, tmp, rk, wk, tk):
    s = g.s
    zv = zin.rearrange("p (h two d) -> p h two d", two=2, d=hd)
    ov = out.rearrange("p (h two d) -> p h two d", two=2, d=hd)
    x1, x2 = zv[:, :, 0, :], zv[:, :, 1, :]
    o1, o2 = ov[:, :, 0, :], ov[:, :, 1, :]
    cb = bc(cos, [128, H, hd])
    sb_ = bc(sin, [128, H, hd])
    t = [tmp[:, i, 0:H * hd].rearrange("p (h d) -> p h d", d=hd) for i in range(4)]
    s.op(eng, lambda e: e.tensor_tensor(out=t[0], in0=x1, in1=cb, op=ALU.mult), reads=rk, writes=[tk + '0'])
    s.op(eng, lambda e: e.tensor_tensor(out=t[1], in0=x2, in1=sb_, op=ALU.mult), reads=rk, writes=[tk + '1'])
    s.op(eng, lambda e: e.tensor_tensor(out=o1, in0=t[0], in1=t[1], op=ALU.subtract),
         reads=[tk + '0', tk + '1'], writes=wk)
    s.op(eng, lambda e: e.tensor_tensor(out=t[2], in0=x2, in1=cb, op=ALU.mult), reads=rk, writes=[tk + '2'])
    s.op(eng, lambda e: e.tensor_tensor(out=t[3], in0=x1, in1=sb_, op=ALU.mult), reads=rk, writes=[tk + '3'])
    s.op(eng, lambda e: e.tensor_tensor(out=o2, in0=t[2], in1=t[3], op=ALU.add),
         reads=[tk + '2', tk + '3'], writes=wk)


def phase1(g):
    nc, s, NS, I, S = g.nc, g.s, g.NS, g.I, g.S
    with ExitStack() as st:
        sb = lambda n, sh, dt: st.enter_context(nc.sbuf_tensor(n, sh, dt))
        pm = lambda n, sh, dt: st.enter_context(nc.psum_tensor(n, sh, dt))
        wab = sb("p1_wab", [128, 8, 2304], BF16)
        xt = [sb(f"p1_xt{i}", [128, 4, D], F32) for i in range(2)]
        xn = [sb(f"p1_xn{i}", [128, 4, D], BF16) for i in range(2)]
        junk = sb("p1_junk", [128, D], BF16)
        ss = [sb(f"p1_ss{i}", [128, 4], F32) for i in range(2)]
        rstd = [sb(f"p1_rstd{i}", [128, 4], F32) for i in range(2)]
        hmT = [sb(f"p1_hmT{i}", [128, 8, 512], BF16) for i in range(2)]
        zs = [sb(f"p1_zs{i}", [128, 2304], F32) for i in range(2)]
        rA = [sb(f"p1_rA{i}", [128, 4, 64], F32) for i in range(2)]
        rB = [sb(f"p1_rB{i}", [128, 4, 64], F32) for i in range(2)]
        qkg = sb("p1_qkg", [128, 128], F32)
        tmpA = sb("p1_tmpA", [128, 4, 256], F32)
        tmpB = sb("p1_tmpB", [128, 4, 256], F32)
        sq = sb("p1_sq", [128, 640], F32)
        ssq = sb("p1_ssq", [128, 10], F32)
        qn = sb("p1_qn", [128, 640], F32)
        qk = [sb(f"p1_qk{i}", [128, 1664], BF16) for i in range(2)]
        stg = [sb(f"p1_stg{i}", [128, 13, 512], BF16) for i in range(2)]
        vst = [sb(f"p1_vst{i}", [128, 4, 640], BF16) for i in range(2)]
        tps = [pm(f"p1_tp{i}", [128, 8, 256], BF16) for i in range(2)]
        zp = [pm(f"p1_zp{i}", [128, 512], F32) for i in range(2)]
        qT = pm("p1_qT", [128, 16, 128], BF16)

        load_w_bf16(g, 'p1_wab', wab, I.w_in_ab, 8, 2304)
        s.dma('sp', 'p1_qkg', qkg[:], I.qkg[0:1, :].partition_broadcast(128), writes=['qkg'])
        tiles = []
        for j, Sj in enumerate(g.seqs):
            for t in range(Sj // 512):
                tiles.append((j, t * 512, g.offs[j] + t * 512))

        def load(i):
            j, p0, t0 = tiles[i]
            b = i % 2
            s.dma('sp', f'p1_xt{b}', xt[b][:], I.x[t0:t0 + 512, :].rearrange("(b p) d -> p b d", p=128),
                  writes=[f'xt{b}'])
            s.dma('sp', f'p1_rA{b}', rA[b][:], I.ropeA[p0:p0 + 512, :].rearrange("(b p) d -> p b d", p=128),
                  writes=[f'rA{b}'])
            s.dma('sp', f'p1_rB{b}', rB[b][:], I.ropeB[p0:p0 + 512, :].rearrange("(b p) d -> p b d", p=128),
                  writes=[f'rB{b}'])
        load(0)
        zi = 0
        for i, (j, p0, t0) in enumerate(tiles):
            b = i % 2
            if i + 1 < len(tiles):
                load(i + 1)
            rms_prep(g, f'p1{b}', xt[b], xn[b], ss[b], rstd[b], junk, 4, f'xt{b}', f'xn{b}')
            to_featmajor(g, 'p1', xn[b], hmT[b], tps, 0, 0, j, 4, f'xn{b}', f'hmT{b}')
            for blk in range(4):
                zb = zs[blk % 2]
                zk = f'zs{blk % 2}'
                for c in range(5):
                    n0 = c * 512
                    nw = min(512, 2304 - n0)
                    p = zp[zi % 2]
                    pn = f'zp{zi % 2}'
                    zi += 1

                    def mm(e, p=p, n0=n0, nw=nw, blk=blk, b=b):
                        for k in range(8):
                            r = e.matmul(p[:, 0:nw], lhsT=hmT[b][:, k, blk * 128:(blk + 1) * 128],
                                         rhs=wab[:, k, n0:n0 + nw], start=(k == 0), stop=(k == 7))
                        return r
                    s.op('pe', mm, reads=[f'hmT{b}', 'p1_wab'], writes=[pn])
                    if c == 2:
                        s.op('act', lambda e, p=p, blk=blk, b=b: e.activation(
                            out=vst[b][:, blk, 0:512], in_=p[:, 0:512], func=AF.Copy),
                            reads=[pn], writes=[f'vst{b}'])
                    else:
                        s.op('act', lambda e, p=p, n0=n0, nw=nw, zb=zb: e.activation(
                            out=zb[:, n0:n0 + nw], in_=p[:, 0:nw], func=AF.Copy),
                            reads=[pn], writes=[zk + f'c{c}'])
                q = qk[blk % 2]
                qkk = f'qk{blk % 2}'
                cosA, sinA = rA[b][:, blk, 0:32], rA[b][:, blk, 32:64]
                cosB, sinB = rB[b][:, blk, 0:32], rB[b][:, blk, 32:64]
                rotary(g, 'dve', q[:, 0:512], zb[:, 0:512], cosA, sinA, 8, 32, tmpA,
                       [zk + 'c0', f'rA{b}'], [qkk + 'qa'], 'tmpA')
                rotary(g, 'pool', q[:, 512:1024], zb[:, 512:1024], cosA, sinA, 8, 32, tmpB,
                       [zk + 'c1', f'rA{b}'], [qkk + 'ka'], 'tmpB')
                s.op('act', lambda e, zb=zb: e.activation(out=sq[:], in_=zb[:, 1536:2176], func=AF.Square),
                     reads=[zk + 'c3', zk + 'c4'], writes=['sq'])
                s.op('dve', lambda e: e.tensor_reduce(out=ssq[:], in_=sq[:].rearrange("p (h d) -> p h d", d=64),
                                                      axis=AX.X, op=ALU.add), reads=['sq'], writes=['ssq'])
                rsqrt_act(g, ssq[:], ssq[:], 1.0 / 64, ['ssq'], ['ssq'])
                s.op('dve', lambda e, zb=zb: e.tensor_tensor(
                    out=qn[:].rearrange("p (h d) -> p h d", d=64),
                    in0=zb[:, 1536:2176].rearrange("p (h d) -> p h d", d=64),
                    in1=ssq[:].unsqueeze(2).to_broadcast([128, 10, 64]), op=ALU.mult),
                    reads=[zk + 'c3', zk + 'c4', 'ssq'], writes=['qn'])
                s.op('dve', lambda e: e.tensor_tensor(
                    out=qn[:, 0:512].rearrange("p (h d) -> p h d", d=64),
                    in0=qn[:, 0:512].rearrange("p (h d) -> p h d", d=64),
                    in1=bc(qkg[:, 0:64], [128, 8, 64]), op=ALU.mult), reads=['qn', 'qkg'], writes=['qn'])
                s.op('dve', lambda e: e.tensor_tensor(
                    out=qn[:, 512:640].rearrange("p (h d) -> p h d", d=64),
                    in0=qn[:, 512:640].rearrange("p (h d) -> p h d", d=64),
                    in1=bc(qkg[:, 64:128], [128, 2, 64]), op=ALU.mult), reads=['qn', 'qkg'], writes=['qn'])
                for half in range(2):
                    for (c0, H, o0) in ((0, 8, 1024), (512, 2, 1536)):
                        zin = qn[:, c0:c0 + H * 64].rearrange("p (h x) -> p h x", x=64)[:, :, half * 32:(half + 1) * 32]
                        oo = q[:, o0:o0 + H * 64].rearrange("p (h x) -> p h x", x=64)[:, :, half * 32:(half + 1) * 32]
                        rotary_v(g, 'dve', oo, zin, cosB[:, half * 16:(half + 1) * 16],
                                 sinB[:, half * 16:(half + 1) * 16], H, 16, tmpA, ['qn', f'rB{b}'],
                                 [qkk + 'b'], 'tmpA')
                s.op('pool', lambda e, zb=zb, blk=blk, b=b: e.tensor_copy(out=vst[b][:, blk, 512:640],
                                                                          in_=zb[:, 2176:2304]),
                     reads=[zk + 'c4'], writes=[f'vst{b}'])
                def trq(e, q=q):
                    for c in range(13):
                        r = e.transpose(out=qT[:, c, :], in_=q[:, c * 128:(c + 1) * 128], identity=g.identB[:])
                    return r
                s.op('pe', trq, reads=[qkk + 'qa', qkk + 'ka', qkk + 'b', 'identB'], writes=['qT'])
                s.op('act', lambda e, blk=blk, b=b: e.activation(out=stg[b][:, :, blk * 128:(blk + 1) * 128],
                                                                 in_=qT[:, 0:13, :], func=AF.Copy),
                     reads=['qT'], writes=[f'stg{b}'])
            for (dst, c0, n) in ((S.qaT, 0, 4), (S.kaT, 4, 4), (S.qbT, 8, 4), (S.kbT, 12, 1)):
                s.dma('sp', f'p1_stg{b}', dst[:, t0:t0 + 512].rearrange("(c p) t -> p c t", p=128),
                      stg[b][:, c0:c0 + n, :], reads=[f'stg{b}'])
            s.dma('sp', f'p1_vst{b}', S.vA[t0:t0 + 512, :].rearrange("(b p) d -> p b d", p=128), vst[b][:, :, 0:512],
                  reads=[f'vst{b}'])
            s.dma('sp', f'p1_vst{b}', S.vB[t0:t0 + 512, :].rearrange("(b p) d -> p b d", p=128), vst[b][:, :, 512:640],
                  reads=[f'vst{b}'])
        s.barrier()


def rotary_v(g, eng, ov, zv, cos, sin, H, hd, tmp, rk, wk, tk):
    s = g.s
    x1, x2 = zv[:, :, 0:hd], zv[:, :, hd:2 * hd]
    o1, o2 = ov[:, :, 0:hd], ov[:, :, hd:2 * hd]
    cb = bc(cos, [128, H, hd])
    sb_ = bc(sin, [128, H, hd])
    t = [tmp[:, i, 0:H * hd].rearrange("p (h d) -> p h d", d=hd) for i in range(4)]
    s.op(eng, lambda e: e.tensor_tensor(out=t[0], in0=x1, in1=cb, op=ALU.mult), reads=rk, writes=[tk + '0'])
    s.op(eng, lambda e: e.tensor_tensor(out=t[1], in0=x2, in1=sb_, op=ALU.mult), reads=rk, writes=[tk + '1'])
    s.op(eng, lambda e: e.tensor_tensor(out=o1, in0=t[0], in1=t[1], op=ALU.subtract),
         reads=[tk + '0', tk + '1'], writes=wk)
    s.op(eng, lambda e: e.tensor_tensor(out=t[2], in0=x2, in1=cb, op=ALU.mult), reads=rk, writes=[tk + '2'])
    s.op(eng, lambda e: e.tensor_tensor(out=t[3], in0=x1, in1=sb_, op=ALU.mult), reads=rk, writes=[tk + '3'])
    s.op(eng, lambda e: e.tensor_tensor(out=o2, in0=t[2], in1=t[3], op=ALU.add),
         reads=[tk + '2', tk + '3'], writes=wk)


def phase2(g):
    nc, s, S = g.nc, g.s, g.S
    SMAX = max(g.seqs)
    with ExitStack() as st:
        sb = lambda n, sh, dt: st.enter_context(nc.sbuf_tensor(n, sh, dt))
        pm = lambda n, sh, dt: st.enter_context(nc.psum_tensor(n, sh, dt))
        kT = [sb(f"p2_kT{i}", [128, SMAX], BF16) for i in range(2)]
        V1 = [sb(f"p2_V1{i}", [128, SMAX // 128, 128], BF16) for i in range(2)]
        qT = [sb(f"p2_qT{i}", [128, SMAX], BF16) for i in range(2)]
        pT = [sb(f"p2_pT{i}", [128, 1024], BF16) for i in range(3)]
        rd = [sb(f"p2_rd{i}", [64, 512], F32) for i in range(2)]
        ob = [sb(f"p2_ob{i}", [64, 512], BF16) for i in range(2)]
        sp_ = [pm(f"p2_sp{i}", [128, 1024], F32) for i in range(3)]
        ot = [pm(f"p2_ot{i}", [128, 512], F32) for i in range(2)]
        for i in range(2):
            s.op('pool', lambda e, i=i: e.memset(V1[i][:, :, 64:128], 1.0), writes=[f'V1{i}'])
            s.op('pool', lambda e, i=i: e.memset(kT[i][64:128, :], 0.0), writes=[f'kT{i}'])
            s.op('pool', lambda e, i=i: e.memset(qT[i][64:128, :], 0.0), writes=[f'qT{i}'])
        ikv = 0
        ih = 0
        ip = 0
        io = 0
        LAG = 2
        pend = []

        def flush(n):
            while len(pend) > n:
                pend.pop(0)()
        for j, Sj in enumerate(g.seqs):
            base = g.offs[j]
            nkb, nqg = Sj // 128, Sj // 512
            for kv in range(2):
                bk = ikv % 2
                ikv += 1
                s.dma('sp', f'p2_kT{bk}', kT[bk][0:64, 0:Sj], S.kbT[kv * 64:(kv + 1) * 64, base:base + Sj],
                      writes=[f'kT{bk}'])
                s.dma('sp', f'p2_V1{bk}', V1[bk][:, 0:nkb, 0:64],
                      S.vB[base:base + Sj, kv * 64:(kv + 1) * 64].rearrange("(b p) d -> p b d", p=128),
                      writes=[f'V1{bk}'])
                for hq in range(4):
                    h = kv * 4 + hq
                    bq = ih % 2
                    ih += 1
                    s.dma('sp', f'p2_qT{bq}', qT[bq][0:64, 0:Sj], S.qbT[h * 64:(h + 1) * 64, base:base + Sj],
                          writes=[f'qT{bq}'])
                    for qg in range(nqg):
                        o = ot[io % 2]
                        on = f'ot{io % 2}'
                        bo = io % 2
                        io += 1
                        for kb in range(0, nkb, 2):
                            p = sp_[ip % 3]
                            pn = f'sp{ip % 3}'
                            pt = pT[ip % 3]
                            ptn = f'pT{ip % 3}'
                            ip += 1

                            def smm(e, p=p, bk=bk, bq=bq, kb=kb, qg=qg):
                                for i in range(2):
                                    r = e.matmul(p[:, i * 512:(i + 1) * 512],
                                                 lhsT=kT[bk][:, (kb + i) * 128:(kb + i + 1) * 128],
                                                 rhs=qT[bq][:, qg * 512:(qg + 1) * 512], start=True, stop=True)
                                return r
                            s.op('pe', smm, reads=[f'kT{bk}', f'qT{bq}'], writes=[pn])
                            s.op('act', lambda e, p=p, pt=pt: e.activation(out=pt[:], in_=p[:], func=AF.Exp,
                                                                           scale=0.125),
                                 reads=[pn], writes=[ptn])

                            def stage2(o=o, on=on, bo=bo, bk=bk, kb=kb, pt=pt, ptn=ptn, nkb=nkb, h=h, qg=qg,
                                       base=base):
                                def pvm(e):
                                    for i in range(2):
                                        r = e.matmul(o[:, :], lhsT=V1[bk][:, kb + i, :], rhs=pt[:, i * 512:(i + 1) * 512],
                                                     start=(kb + i == 0), stop=(kb + i == nkb - 1))
                                    return r
                                s.op('pe', pvm, reads=[f'V1{bk}', ptn], writes=[on])
                                if kb + 2 == nkb:
                                    s.op('dve', lambda e: e.reciprocal(out=rd[bo][:], in_=o[64:128, :]),
                                         reads=[on], writes=[f'rd{bo}'])
                                    s.op('dve', lambda e: e.tensor_tensor(out=ob[bo][:], in0=o[0:64, :],
                                                                          in1=rd[bo][:], op=ALU.mult),
                                         reads=[on, f'rd{bo}'], writes=[f'ob{bo}'])
                                    t0 = base + qg * 512
                                    s.dma('sp', f'p2_ob{bo}', S.oT[512 + h * 64:512 + (h + 1) * 64, t0:t0 + 512],
                                          ob[bo][:], reads=[f'ob{bo}'])
                            pend.append(stage2)
                            flush(LAG)
        flush(0)
        s.barrier()


def phase2a(g):
    nc, s, S = g.nc, g.s, g.S
    with ExitStack() as st:
        sb = lambda n, sh, dt: st.enter_context(nc.sbuf_tensor(n, sh, dt))
        pm = lambda n, sh, dt: st.enter_context(nc.psum_tensor(n, sh, dt))
        kTw = sb("p2a_kTw", [128, 4, 4096], BF16)
        qTsh = [sb(f"p2a_qTs{i}", [128, 4, 2048], BF16) for i in range(2)]
        ACC = sb("p2a_ACC", [128, 8, 2048], F32)
        V1 = [sb(f"p2a_V1{i}", [128, 9, 8, 128], BF16) for i in range(2)]
        band = sb("p2a_band", [128, 3, 256], BF16)
        pT = [sb(f"p2a_pT{i}", [128, 256], BF16) for i in range(4)]
        pTm = [sb(f"p2a_pTm{i}", [128, 256], BF16) for i in range(4)]
        rd = [sb(f"p2a_rd{i}", [64, 2048], F32) for i in range(2)]
        ob = [sb(f"p2a_ob{i}", [64, 2048], BF16) for i in range(2)]
        sp_ = [pm(f"p2a_sp{i}", [128, 512], F32) for i in range(4)]
        ot = [pm(f"p2a_ot{i}", [128, 512], F32) for i in range(4)]
        s.op('pool', lambda e: e.memset(qTsh[0][64:128, :, :], 0.0), writes=['qTs'])
        s.op('pool', lambda e: e.memset(qTsh[1][0:64, :, :], 0.0), writes=['qTs'])
        NEG = -30000.0
        for bi in range(3):
            bt = band[:, bi, :]
            s.op('pool', lambda e, bt=bt: e.memset(bt, 0.0), writes=['band'])
            s.op('pool', lambda e, bt=bt: e.affine_select(out=bt, in_=bt, pattern=[[1, 256]], compare_op=ALU.is_ge,
                                                          fill=NEG, base=0, channel_multiplier=-1),
                 reads=['band'], writes=['band'])
            s.op('pool', lambda e, bt=bt: e.affine_select(out=bt, in_=bt, pattern=[[-1, 256]], compare_op=ALU.is_ge,
                                                          fill=NEG, base=128, channel_multiplier=1),
                 reads=['band'], writes=['band'])
        s.op('pool', lambda e: e.memset(band[0:64, 1, :], NEG), reads=['band'], writes=['band'])
        s.op('pool', lambda e: e.memset(band[64:128, 2, :], NEG), reads=['band'], writes=['band'])
        for i in range(2):
            s.op('pool', lambda e, i=i: e.memset(V1[i][:, :, :, 0:64], 0.0), writes=[f'aV1{i}'])
            s.op('pool', lambda e, i=i: e.memset(V1[i][:, :, :, 64:128], 1.0), writes=[f'aV1{i}'])
        iu = 0
        ip = 0
        import os
        LAG = int(os.environ.get('LAGA', '2'))
        pend = []

        def flush(n):
            while len(pend) > n:
                pend.pop(0)()
        for j, Sj in enumerate(g.seqs):
            base = g.offs[j]
            for seg in range(Sj // 2048):
                seg0 = seg * 2048
                lo, hi = max(0, seg0 - 1024), min(Sj, seg0 + 3072)
                if lo > seg0 - 1024:
                    s.op('pool', lambda e: e.memset(kTw[:, :, 0:1024], 0.0), writes=['kTw'])
                if hi < seg0 + 3072:
                    s.op('pool', lambda e: e.memset(kTw[:, :, 3072:4096], 0.0), writes=['kTw'])
                s.dma('sp', 'p2a_kTw', kTw[:, :, lo - (seg0 - 1024):hi - (seg0 - 1024)],
                      S.kaT[:, base + lo:base + hi].rearrange("(c p) t -> p c t", p=128), writes=['kTw'])
                qsrc = S.qaT[:, base + seg0:base + seg0 + 2048].rearrange("(c p) t -> p c t", p=128)
                s.dma('sp', 'p2a_qTs', qTsh[0][0:64, :, :], qsrc[0:64], writes=['qTs'])
                s.dma('sp', 'p2a_qTs', qTsh[1][64:128, :, :], qsrc[64:128], writes=['qTs'])
                units = [(1, 0, 0, 8), (1, 0, 8, 8)] + [(4, r, 0, 4) for r in range(4)] + \
                        [(16, r, 0, 1) for r in range(16)]
                first_write = {}
                for (d, r, qb0, nqb) in units:
                    L = Sj // d
                    lq0 = seg0 // d + 128 * qb0
                    bv = iu % 2
                    iu += 1
                    vk = f'aV1{bv}'
                    v1 = V1[bv]
                    nkb = nqb + 1
                    m_lo, m_hi = 0, nkb
                    at_start = (lq0 == 0)
                    at_end = (lq0 + 128 * nqb == L)
                    if lq0 == 0:
                        tok = base + 0 * d + r
                        s.dma('sp', f'p2a_V1{bv}', v1[64:128, 0, :, 0:64],
                              S.vA[tok:tok + 63 * d + 1:d, :].rearrange("p (h e) -> p h e", e=64), writes=[vk])
                        m_lo = 1
                    if lq0 + 128 * nqb == L:
                        tok = base + (lq0 - 64 + 128 * nqb) * d + r
                        s.dma('sp', f'p2a_V1{bv}', v1[0:64, nqb, :, 0:64],
                              S.vA[tok:tok + 63 * d + 1:d, :].rearrange("p (h e) -> p h e", e=64), writes=[vk])
                        m_hi = nkb - 1
                    for m in range(m_lo, m_hi):
                        tok = base + (lq0 - 64 + 128 * m) * d + r
                        s.dma('sp', f'p2a_V1{bv}', v1[:, m, :, 0:64],
                              S.vA[tok:tok + 127 * d + 1:d, :].rearrange("p (h e) -> p h e", e=64), writes=[vk])
                    for h in range(8):
                        c, hh = h // 2, (h % 2) * 64
                        for m in range(nkb):
                            n_lo, n_hi = max(m - 1, 0), min(m, nqb - 1)
                            nq = n_hi - n_lo + 1
                            kc0 = 1024 + (128 * (qb0 + m) - 64) * d + r
                            qc0 = 128 * (qb0 + n_lo) * d + r
                            p = sp_[ip % 4][:, 0:256]
                            pn = f'asp{ip % 4}'
                            pt, ptm = pT[ip % 4], pTm[ip % 4]
                            ptn = f'apT{ip % 4}'
                            ip += 1
                            W = nq * 128
                            b0 = 128 if m == 0 else 0
                            bi = 1 if (m == 0 and at_start) else (2 if (m == nkb - 1 and at_end) else 0)

                            def smm(e, p=p, c=c, hh=hh, kc0=kc0, qc0=qc0, W=W, d=d, b0=b0, bi=bi):
                                e.matmul(p[:, 0:W], lhsT=kTw[:, c, kc0:kc0 + 127 * d + 1:d],
                                         rhs=qTsh[hh // 64][:, c, qc0:qc0 + (W - 1) * d + 1:d], start=True, stop=False)
                                return e.matmul(p[:, 0:W], lhsT=g.identB[:], rhs=band[:, bi, b0:b0 + W],
                                                start=False, stop=True)
                            s.op('pe', smm, reads=['kTw', 'qTs', 'band', 'identB'], writes=[pn])
                            s.op('act', lambda e, p=p, ptm=ptm, W=W: e.activation(out=ptm[:, 0:W], in_=p[:, 0:W],
                                                                                 func=AF.Exp, scale=0.125),
                                 reads=[pn], writes=[ptn + 'm'])

                            def stage2(n_lo=n_lo, n_hi=n_hi, h=h, v1=v1, vk=vk, m=m, ptm=ptm, ptn=ptn, d=d, r=r,
                                       qb0=qb0):
                                for n in range(n_lo, n_hi + 1):
                                    o = ot[n % 4][:, 0:128]
                                    on = f'aot{n % 4}'
                                    col = (n - n_lo) * 128
                                    s.op('pe', lambda e, o=o, col=col, n=n: e.matmul(
                                        o, lhsT=v1[:, m, h, :], rhs=ptm[:, col:col + 128],
                                        start=(m == n), stop=(m == n + 1)),
                                        reads=[vk, ptn + 'm'], writes=[on])
                                    if m == n + 1:
                                        a0 = 128 * (qb0 + n) * d + r
                                        av = ACC[:, h, a0:a0 + 127 * d + 1:d]
                                        if d == 1:
                                            s.op('dve', lambda e, o=o, av=av: e.tensor_copy(out=av, in_=o),
                                                 reads=[on], writes=[f'ACC{h}'])
                                        else:
                                            s.op('dve', lambda e, o=o, av=av: e.tensor_tensor(out=av, in0=o, in1=av,
                                                                                              op=ALU.add),
                                                 reads=[on, f'ACC{h}'], writes=[f'ACC{h}'])
                            pend.append(stage2)
                            flush(LAG)
                flush(0)
                for h in range(8):
                    bo = h % 2
                    s.op('act', lambda e, h=h, bo=bo: e.activation(out=rd[bo][:], in_=ACC[64:128, h, :], func=AF.Ln),
                         reads=[f'ACC{h}'], writes=[f'ard{bo}'])
                    s.op('act', lambda e, bo=bo: e.activation(out=rd[bo][:], in_=rd[bo][:], func=AF.Exp, scale=-1.0),
                         reads=[f'ard{bo}'], writes=[f'ard{bo}'])
                    s.op('pool', lambda e, h=h, bo=bo: e.tensor_tensor(out=ob[bo][:], in0=ACC[0:64, h, :],
                                                                      in1=rd[bo][:], op=ALU.mult),
                         reads=[f'ACC{h}', f'ard{bo}'], writes=[f'aob{bo}'])
                    t0 = base + seg0
                    s.dma('sp', f'p2a_ob{bo}', S.oT[h * 64:(h + 1) * 64, t0:t0 + 2048], ob[bo][:],
                          reads=[f'aob{bo}'])
        s.barrier()


def post_norm_residual(g, pre, yp, ypk, xres, xkey, GB, gbk, out, outkey, ss, rstd, junk, tmp, par=0):
    s = g.s
    sp = str(par)
    tm = tmp[par] if isinstance(tmp, (list, tuple)) else tmp
    s.op('act', lambda e: e.activation(out=junk[:], in_=yp[:, :], func=AF.Square, accum_out=ss[:, par:par + 1]),
         reads=[ypk], writes=[pre + 'junk', pre + 'ss' + sp])
    rsqrt_act(g, rstd[:, par:par + 1], ss[:, par:par + 1], 1.0 / D, [pre + 'ss' + sp], [pre + 'rstd' + sp])
    s.op('dve', lambda e: e.scalar_tensor_tensor(out=tm[:], in0=yp[:, :], scalar=rstd[:, par:par + 1], in1=GB[:],
                                                 op0=ALU.mult, op1=ALU.mult),
         reads=[ypk, pre + 'rstd' + sp, gbk], writes=[pre + 'tmp' + sp])
    s.op('pool' if par == 0 else 'dve', lambda e: e.tensor_tensor(out=out, in0=tm[:], in1=xres, op=ALU.add),
         reads=[pre + 'tmp' + sp, xkey], writes=[outkey])


def load_GB(g, pre, GB, gb, ngb, l, gi, j):
    s = g.s
    s.dma('sp', pre + 'gb', gb[:], g.S.gates[l, gi, j:j + 1, :].partition_broadcast(128), writes=[pre + 'gb'])
    s.op('dve', lambda e: e.tensor_tensor(out=GB[:], in0=gb[:], in1=ngb[:], op=ALU.mult),
         reads=[pre + 'gb', pre + 'ngb'], writes=[pre + 'GB'])


def phase3(g, l, oT_d, w_out_d, xin_d, xout_d):
    nc, s = g.nc, g.s
    pre = f'p3{l}'
    with ExitStack() as st:
        sb = lambda n, sh, dt: st.enter_context(nc.sbuf_tensor(pre + n, sh, dt))
        pm = lambda n, sh, dt: st.enter_context(nc.psum_tensor(pre + n, sh, dt))
        wo = sb("wo", [128, 8, D], BF16)
        oT = [sb(f"oT{i}", [128, 8, 512], BF16) for i in range(2)]
        xt = [sb(f"xt{i}", [128, 4, D], F32) for i in range(2)]
        xo = [sb(f"xo{i}", [128, 4, D], F32) for i in range(2)]
        gb = sb("gb", [128, D], F32)
        ngb = sb("ngb", [128, D], F32)
        GB = sb("GB", [128, D], F32)
        junk = sb("junk", [128, D], BF16)
        tmp = [sb(f"tmp{i}", [128, D], F32) for i in range(2)]
        ss = sb("ss", [128, 2], F32)
        rstd = sb("rstd", [128, 2], F32)
        yp = [pm(f"yp{i}", [128, D], F32) for i in range(2)]
        load_w_bf16(g, pre + 'wo', wo, w_out_d, 8, D)
        s.dma('sp', pre + 'ngb', ngb[:], g.I.norm_g[l * 4 + 1:l * 4 + 2, :].partition_broadcast(128),
              writes=[pre + 'ngb'])
        tiles = []
        for j, Sj in enumerate(g.seqs):
            for t in range(Sj // 512):
                tiles.append((j, g.offs[j] + t * 512))

        def load(i):
            j, t0 = tiles[i]
            b = i % 2
            s.dma('sp', pre + f'oT{b}', oT[b][:], oT_d[:, t0:t0 + 512].rearrange("(c p) t -> p c t", p=128),
                  writes=[pre + f'oT{b}'])
            s.dma('sp', pre + f'xt{b}', xt[b][:], xin_d[t0:t0 + 512, :].rearrange("(b p) d -> p b d", p=128),
                  writes=[pre + f'xt{b}'])
        load(0)
        curj = -1
        iy = 0
        for i, (j, t0) in enumerate(tiles):
            b = i % 2
            if i + 1 < len(tiles):
                load(i + 1)
            if j != curj:
                load_GB(g, pre, GB, gb, ngb, l, 0, j)
                curj = j
            for blk in range(4):
                y_ = yp[iy % 2]
                yk = pre + f'yp{iy % 2}'
                iy += 1

                def mm(e, y_=y_, b=b, blk=blk):
                    for nh in range(2):
                        for k in range(8):
                            r = e.matmul(y_[:, nh * 512:(nh + 1) * 512], lhsT=oT[b][:, k, blk * 128:(blk + 1) * 128],
                                         rhs=wo[:, k, nh * 512:(nh + 1) * 512], start=(k == 0), stop=(k == 7))
                    return r
                s.op('pe', mm, reads=[pre + f'oT{b}', pre + 'wo'], writes=[yk])
                post_norm_residual(g, pre, y_, yk, xt[b][:, blk, :], pre + f'xt{b}', GB, pre + 'GB',
                                   xo[b][:, blk, :], pre + f'xo{b}', ss, rstd, junk, tmp, par=(iy % 2))
            s.dma('sp', pre + f'xo{b}', xout_d[t0:t0 + 512, :].rearrange("(b p) d -> p b d", p=128), xo[b][:],
                  reads=[pre + f'xo{b}'])
        s.barrier()


def phase4(g, l, xin_d, xout_d):
    nc, s = g.nc, g.s
    pre = f'p4{l}'
    with ExitStack() as st:
        sb = lambda n, sh, dt: st.enter_context(nc.sbuf_tensor(pre + n, sh, dt))
        pm = lambda n, sh, dt: st.enter_context(nc.psum_tensor(pre + n, sh, dt))
        w1 = sb("w1", [128, 8, 4 * D], BF16)
        w2 = sb("w2", [128, 32, D], BF16)
        xt = [sb(f"xt{i}", [128, 2, D], F32) for i in range(2)]
        xn = sb("xn", [128, 2, D], BF16)
        xo = [sb(f"xo{i}", [128, 2, D], F32) for i in range(2)]
        hfT = sb("hfT", [128, 8, 256], BF16)
        h1a = sb("h1a", [128, 2, 256], BF16)
        h1T = sb("h1T", [128, 32, 256], BF16)
        ngb = sb("ngb", [128, D], F32)
        GB = sb("GB", [128, D], F32)
        junk = sb("junk", [128, D], BF16)
        tmp = [sb(f"tmp{i}", [128, D], F32) for i in range(2)]
        ss = sb("ss", [128, 4], F32)
        rstd = sb("rstd", [128, 4], F32)
        ss2 = sb("ss2", [128, 2], F32)
        rstd2 = sb("rstd2", [128, 2], F32)
        tps = [pm("tp0", [128, 8, 256], BF16)]
        hp = [pm(f"hp{i}", [128, 512], F32) for i in range(2)]
        yp = [pm(f"yp{i}", [128, D], F32) for i in range(2)]
        load_w_bf16(g, pre + 'w1', w1, g.I.w_ff1[l], 8, 4 * D)
        load_w_bf16(g, pre + 'w2', w2, g.I.w_ff2[l], 32, D)
        s.dma('sp', pre + 'ngb', ngb[:], g.I.norm_g[l * 4 + 3:l * 4 + 4, :].partition_broadcast(128),
              writes=[pre + 'ngb'])
        tiles = []
        for j, Sj in enumerate(g.seqs):
            for t in range(Sj // 256):
                tiles.append((j, g.offs[j] + t * 256))

        def load(i):
            j, t0 = tiles[i]
            b = i % 2
            s.dma('sp', pre + f'xt{b}', xt[b][:], xin_d[t0:t0 + 256, :].rearrange("(b p) d -> p b d", p=128),
                  writes=[pre + f'xt{b}'])
        load(0)
        curj = -1
        ih = 0
        iy = 0
        def prep(i):
            j, t0 = tiles[i]
            b = i % 2
            rms_prep(g, pre, xt[b], xn, ss, rstd, junk, 2, pre + f'xt{b}', pre + 'xn')
            to_featmajor(g, pre, xn, hfT, tps, l, 1, j, 2, pre + 'xn', pre + 'hfT')
        prep(0)
        for i, (j, t0) in enumerate(tiles):
            b = i % 2
            if i + 1 < len(tiles):
                load(i + 1)
            if j != curj:
                s.dma('sp', pre + 'GB', GB[:], g.S.gates[l, 1, j:j + 1, :].partition_broadcast(128),
                      writes=[pre + 'GB'])
                s.op('pool', lambda e: e.tensor_tensor(out=GB[:], in0=GB[:], in1=ngb[:], op=ALU.mult),
                     reads=[pre + 'GB', pre + 'ngb'], writes=[pre + 'GB'])
                curj = j
            for fc in range(32):
                p = hp[ih % 2]
                pn = pre + f'hp{ih % 2}'
                ha = h1a[:, ih % 2, :]
                hak = pre + f'h1a{ih % 2}'
                ih += 1

                def mm(e, p=p, fc=fc):
                    for k in range(8):
                        r = e.matmul(p[:, 0:256], lhsT=w1[:, k, fc * 128:(fc + 1) * 128], rhs=hfT[:, k, :],
                                     start=(k == 0), stop=(k == 7))
                    return r
                s.op('pe', mm, reads=[pre + 'w1', pre + 'hfT'], writes=[pn])
                s.op('act', lambda e, p=p, ha=ha: e.activation(out=ha, in_=p[:, 0:256], func=AF.Relu),
                     reads=[pn], writes=[hak])
                eng = 'pool' if fc % 2 == 0 else 'dve'
                s.op(eng, lambda e, ha=ha, fc=fc: e.tensor_tensor(out=h1T[:, fc, :], in0=ha, in1=ha, op=ALU.mult),
                     reads=[hak], writes=[pre + f'h1T{fc}'])
            if i + 1 < len(tiles):
                prep(i + 1)
            for blk in range(2):
                y_ = yp[iy % 2]
                yk = pre + f'yp{iy % 2}'
                iy += 1

                def mm2(e, y_=y_, blk=blk):
                    for nh in range(2):
                        for k in range(32):
                            r = e.matmul(y_[:, nh * 512:(nh + 1) * 512], lhsT=h1T[:, k, blk * 128:(blk + 1) * 128],
                                         rhs=w2[:, k, nh * 512:(nh + 1) * 512], start=(k == 0), stop=(k == 31))
                    return r
                s.op('pe', mm2, reads=[pre + f'h1T{fc}' for fc in range(32)] + [pre + 'w2'], writes=[yk])
                post_norm_residual(g, pre + 'b', y_, yk, xt[b][:, blk, :], pre + f'xt{b}', GB, pre + 'GB',
                                   xo[b][:, blk, :], pre + f'xo{b}', ss2, rstd2, junk, tmp, par=(iy % 2))
            s.dma('sp', pre + f'xo{b}', xout_d[t0:t0 + 256, :].rearrange("(b p) d -> p b d", p=128), xo[b][:],
                  reads=[pre + f'xo{b}'])
        s.barrier()


def _rope_angles(pos, dim):
    inv_freq = (np.float32(10000.0) ** (-np.arange(0, dim, 2, dtype=np.float32) / np.float32(dim))).astype(np.float32)
    return (pos.astype(np.float32)[:, None] * inv_freq[None, :]).astype(np.float32)


def rope_tables(S):
    pos = np.arange(S)
    angA = _rope_angles(pos, 64)
    ar = _rope_angles(pos // 64, 32)
    ac = _rope_angles(pos % 64, 32)
    ropeA = np.concatenate([np.cos(angA), np.sin(angA)], axis=1).astype(np.float32)
    ropeB = np.concatenate([np.cos(ar), np.cos(ac), np.sin(ar), np.sin(ac)], axis=1).astype(np.float32)
    return ropeA, ropeB


def core_map(inp, xs, cs):
    NS = len(xs)
    SMAX = max(x.shape[0] for x in xs)
    ropeA, ropeB = rope_tables(SMAX)
    c = np.stack(cs)
    cT = c.reshape(NS, 8, 128).transpose(2, 1, 0).reshape(128, 8 * NS)
    ng = np.asarray(inp['norm_g'])
    m = {
        'x': np.concatenate(xs, 0), 'cT': cT,
        'w_in_ab': inp['w_in_ab'][0], 'w_out_ab': inp['w_out_ab'][0], 'w_in_cd': inp['w_in_cd'][0],
        'w_out_cd': inp['w_out_cd'][0], 'w_ada': inp['w_ada'], 'b_ada': np.asarray(inp['b_ada']).reshape(1, -1),
        'normgT': ng.reshape(2, 4, 8, 128).transpose(3, 0, 1, 2).reshape(128, 64),
        'norm_g': ng.reshape(8, 1024), 'w_ff1': inp['w_ff1'], 'w_ff2': inp['w_ff2'],
        'qkg': np.asarray(inp['qk_norm_g']).reshape(1, 128), 'ropeA': ropeA, 'ropeB': ropeB,
        'gate_bias': np.asarray(inp['gate_bias']).reshape(32, 1), 'mhg': np.asarray(inp['mh_norm_g']).reshape(1, 512),
        'sgg': np.asarray(inp['sg_norm_g']).reshape(1, 512),
        'wsT': np.asarray(inp['w_spatial'])[0].transpose(2, 0, 1).reshape(128, 1024),
        'bsT': np.asarray(inp['b_spatial'])[0].T,
    }
    return {k: np.ascontiguousarray(np.asarray(v), dtype=np.float32) for k, v in m.items()}


ALL_PHASES = (0, 1, 2, 3, 4, 5, 6, 7, 8)
_CACHE = {}


def kernel(**inputs):
    inp = {k: np.asarray(v) for k, v in inputs.items()}
    n = 8
    seqs = [8192, 2048, 2048, 2048, 2048]
    if 'nc' not in _CACHE:
        _CACHE['nc'] = build(seqs, set(ALL_PHASES), debug=False)
    nc, g = _CACHE['nc']
    in_maps = []
    for i in range(n):
        xs = [inp['x_prompt'][i]] + [inp['x_sample'][4 * i + k] for k in range(4)]
        cs = [inp['c_prompt'][i]] + [inp['c_sample'][4 * i + k] for k in range(4)]
        in_maps.append(core_map(inp, xs, cs))
    res = run_bass_kernel_spmd(nc, in_maps, core_ids=list(range(n)))
    y_prompt = np.empty((8, 8192, 1024), np.float32)
    y_sample = np.empty((32, 2048, 1024), np.float32)
    for i in range(n):
        y = np.asarray(res.results[i]['y'])
        y_prompt[i] = y[0:8192]
        for k in range(4):
            y_sample[4 * i + k] = y[8192 + 2048 * k:8192 + 2048 * (k + 1)]
    return (y_prompt, y_sample)


def phase5(g):
    nc, s, NS, I, S = g.nc, g.s, g.NS, g.I, g.S
    with ExitStack() as st:
        sb = lambda n, sh, dt: st.enter_context(nc.sbuf_tensor("p5_" + n, sh, dt))
        pm = lambda n, sh, dt: st.enter_context(nc.psum_tensor("p5_" + n, sh, dt))
        wcd = sb("wcd", [128, 8, 3104], BF16)
        wsT = sb("wsT", [128, 8, 128], BF16)
        bsT = sb("bsT", [128, 8], F32)
        sgg = sb("sgg", [128, 512], F32)
        gbias = sb("gbias", [128, 32], F32)
        xt = [sb(f"xt{i}", [128, 4, D], F32) for i in range(2)]
        xn = sb("xn", [128, 4, D], BF16)
        junk = sb("junk", [128, D], BF16)
        ss = sb("ss", [128, 4], F32)
        rstd = sb("rstd", [128, 4], F32)
        hmT = sb("hmT", [128, 8, 512], BF16)
        qk = [sb(f"qk{i}", [128, 1536], BF16) for i in range(2)]
        vst = [sb(f"vst{i}", [128, 4, 512], BF16) for i in range(2)]
        kst = [sb(f"kst{i}", [128, 4, 512], BF16) for i in range(2)]
        ogst = [sb(f"ogst{i}", [128, 4, 512], F32) for i in range(2)]
        gst = [sb(f"gst{i}", [128, 4, 32], F32) for i in range(2)]
        stg = [sb(f"stg{i}", [128, 12, 512], BF16) for i in range(2)]
        ug = sb("ug", [128, 512], F32)
        vg = sb("vg", [128, 512], F32)
        vc = sb("vc", [128, 512], F32)
        vln = sb("vln", [128, 512], BF16)
        sgt = sb("sgt", [128, 512], F32)
        lns = sb("lns", [128, 4], F32)
        tps = [pm("tp0", [128, 8, 256], BF16)]
        zp = [pm(f"zp{i}", [128, 512], F32) for i in range(2)]
        qT = pm("qT", [128, 16, 128], BF16)
        sgp = pm("sgp", [128, 512], F32)
        for k in range(8):
            for (d0, s0, w) in ((0, 0, 1024), (1024, 1024, 1024), (2048, 2080, 512), (2560, 2592, 512),
                                (3072, 2048, 32)):
                s.dma('pool', 'p5_wcd', wcd[:, k, d0:d0 + w], I.w_in_cd[k * 128:(k + 1) * 128, s0:s0 + w],
                      writes=['wcd'])
        s.dma('pool', 'p5_wsT', wsT[:].rearrange("q g p -> q (g p)"), I.wsT[:, :], writes=['wsT'])
        s.dma('sp', 'p5_bsT', bsT[:], I.bsT[:, :], writes=['bsT'])
        s.dma('sp', 'p5_sgg', sgg[:], I.sgg[0:1, :].partition_broadcast(128), writes=['sgg'])
        s.dma('sp', 'p5_gbias', gbias[:], I.gate_bias.rearrange("a b -> b a")[0:1, :].partition_broadcast(128),
              writes=['gbias'])
        tiles = []
        for j, Sj in enumerate(g.seqs):
            for t in range(Sj // 512):
                tiles.append((j, g.offs[j] + t * 512))

        def load(i):
            j, t0 = tiles[i]
            b = i % 2
            s.dma('sp', f'p5_xt{b}', xt[b][:], S.x1[t0:t0 + 512, :].rearrange("(b p) d -> p b d", p=128),
                  writes=[f'xt{b}'])
        load(0)
        zi = 0
        for i, (j, t0) in enumerate(tiles):
            b = i % 2
            if i + 1 < len(tiles):
                load(i + 1)
            rms_prep(g, 'p5', xt[b], xn, ss, rstd, junk, 4, f'xt{b}', 'xn')
            to_featmajor(g, 'p5', xn, hmT, tps, 1, 0, j, 4, 'xn', 'hmT')
            for blk in range(4):
                q = qk[blk % 2]
                qkk = f'qk{blk % 2}'
                for c in range(7):
                    n0 = c * 512
                    nw = min(512, 3104 - n0)
                    p = zp[zi % 2]
                    pn = f'zp{zi % 2}'
                    zi += 1

                    def mm(e, p=p, n0=n0, nw=nw, blk=blk):
                        for k in range(8):
                            r = e.matmul(p[:, 0:nw], lhsT=hmT[:, k, blk * 128:(blk + 1) * 128],
                                         rhs=wcd[:, k, n0:n0 + nw], start=(k == 0), stop=(k == 7))
                        return r
                    s.op('pe', mm, reads=['hmT', 'wcd'], writes=[pn])
                    if c == 0:
                        s.op('act', lambda e, p=p, q=q: e.activation(out=q[:, 0:512], in_=p[:, :], func=AF.Copy),
                             reads=[pn], writes=[qkk + 'q'])
                    elif c == 1:
                        s.op('act', lambda e, p=p, q=q: e.activation(out=q[:, 512:1024], in_=p[:, :], func=AF.Copy,
                                                                     scale=0.125), reads=[pn], writes=[qkk + 'k'])
                        s.op('pool', lambda e, q=q, blk=blk, b=b: e.tensor_copy(out=kst[b][:, blk, :],
                                                                               in_=q[:, 512:1024]),
                             reads=[qkk + 'k'], writes=[f'kst{b}'])
                    elif c == 2:
                        s.op('act', lambda e, p=p, blk=blk, b=b: e.activation(out=vst[b][:, blk, :], in_=p[:, :],
                                                                              func=AF.Copy),
                             reads=[pn], writes=[f'vst{b}'])
                    elif c == 3:
                        s.op('act', lambda e, p=p, blk=blk, b=b: e.activation(out=ogst[b][:, blk, :], in_=p[:, :],
                                                                              func=AF.Sigmoid),
                             reads=[pn], writes=[f'ogst{b}'])
                    elif c == 4:
                        s.op('act', lambda e, p=p: e.activation(out=ug[:], in_=p[:, :], func=AF.Gelu_apprx_tanh),
                             reads=[pn], writes=['ug'])
                    elif c == 5:
                        s.op('act', lambda e, p=p: e.activation(out=vg[:], in_=p[:, :], func=AF.Gelu_apprx_tanh,
                                                                accum_out=lns[:, 0:1]),
                             reads=[pn], writes=['vg', 'lns0'])
                    else:
                        s.op('dve', lambda e, p=p, blk=blk, b=b: e.tensor_tensor(
                            out=gst[b][:, blk, :], in0=p[:, 0:32], in1=gbias[:], op=ALU.add),
                            reads=[pn, 'gbias'], writes=[f'gst{b}'])
                s.op('dve', lambda e: e.tensor_scalar(out=lns[:, 1:2], in0=lns[:, 0:1], scalar1=-1.0 / 512,
                                                      scalar2=None, op0=ALU.mult), reads=['lns0'], writes=['lns1'])
                s.op('act', lambda e: e.activation(out=vc[:], in_=vg[:], func=AF.Identity, bias=lns[:, 1:2]),
                     reads=['vg', 'lns1'], writes=['vc'])
                s.op('act', lambda e: e.activation(out=junk[:, 0:512], in_=vc[:], func=AF.Square,
                                                   accum_out=lns[:, 2:3]), reads=['vc'], writes=['lns2', 'p5junk'])
                rsqrt_act(g, lns[:, 3:4], lns[:, 2:3], 1.0 / 512, ['lns2'], ['lns3'])
                s.op('dve', lambda e: e.scalar_tensor_tensor(out=vln[:], in0=vc[:], scalar=lns[:, 3:4], in1=sgg[:],
                                                             op0=ALU.mult, op1=ALU.mult),
                     reads=['vc', 'lns3', 'sgg'], writes=['vln'])

                def sgm(e):
                    for grp in range(8):
                        r = e.matmul(sgp[:, grp * 64:(grp + 1) * 64], lhsT=wsT[:, grp, :],
                                     rhs=vln[:, grp * 64:(grp + 1) * 64], start=True, stop=True)
                    return r
                s.op('pe', sgm, reads=['wsT', 'vln'], writes=['sgp'])
                s.op('dve', lambda e: e.tensor_tensor(out=sgt[:].rearrange("p (g e) -> p g e", e=64),
                                                      in0=sgp[:, :].rearrange("p (g e) -> p g e", e=64),
                                                      in1=bsT[:, :].unsqueeze(2).to_broadcast([128, 8, 64]),
                                                      op=ALU.add), reads=['sgp', 'bsT'], writes=['sgt'])
                s.op('pool', lambda e, q=q: e.tensor_tensor(out=q[:, 1024:1536], in0=sgt[:], in1=ug[:], op=ALU.mult),
                     reads=['sgt', 'ug'], writes=[qkk + 'd'])

                def trq(e, q=q):
                    for c in range(12):
                        r = e.transpose(out=qT[:, c, :], in_=q[:, c * 128:(c + 1) * 128], identity=g.identB[:])
                    return r
                s.op('pe', trq, reads=[qkk + 'q', qkk + 'k', qkk + 'd', 'identB'], writes=['qT'])
                s.op('act', lambda e, blk=blk, b=b: e.activation(out=stg[b][:, :, blk * 128:(blk + 1) * 128],
                                                                 in_=qT[:, 0:12, :], func=AF.Copy),
                     reads=['qT'], writes=[f'stg{b}'])
            for (dst, r0, c0, n) in ((S.qcT, 0, 0, 4), (S.kcT, 0, 4, 4), (S.o2T, 512, 8, 4)):
                s.dma('sp', f'p5_stg{b}', dst[r0:r0 + 512, t0:t0 + 512].rearrange("(c p) t -> p c t", p=128),
                      stg[b][:, c0:c0 + n, :], reads=[f'stg{b}'])
            tm = lambda d: d[t0:t0 + 512, :].rearrange("(b p) d -> p b d", p=128)
            s.dma('sp', f'p5_vst{b}', tm(S.vc), vst[b][:], reads=[f'vst{b}'])
            s.dma('sp', f'p5_kst{b}', tm(S.kc), kst[b][:], reads=[f'kst{b}'])
            s.dma('sp', f'p5_ogst{b}', tm(S.ogs), ogst[b][:], reads=[f'ogst{b}'])
            s.dma('sp', f'p5_gst{b}', tm(S.gtok), gst[b][:], reads=[f'gst{b}'])
        s.barrier()


def phase6(g):
    nc, s, I, S = g.nc, g.s, g.I, g.S
    SMAX = max(g.seqs)
    NCH = SMAX // 128
    with ExitStack() as st:
        sb = lambda n, sh, dt: st.enter_context(nc.sbuf_tensor("p6_" + n, sh, dt))
        pm = lambda n, sh, dt: st.enter_context(nc.psum_tensor("p6_" + n, sh, dt))
        SU = sb("SU", [128, 128], F32)
        SL = sb("SL", [128, 128], F32)
        ONES = sb("ONES", [128, 128], F32)
        maskF = sb("maskF", [128, 128], BF16)
        maskB = sb("maskB", [128, 128], BF16)
        mhg = sb("mhg", [128, 512], F32)
        gz = sb("gz", [128, NCH, 32], F32)
        l1 = [sb(f"l1{d}", [128, NCH * 8], F32) for d in range(2)]
        tmpg = sb("tmpg", [128, NCH * 8], F32)
        E = [sb(f"E{d}", [128, NCH * 8], F32) for d in range(2)]
        THR = [sb(f"THR{d}", [128, NCH * 8], F32) for d in range(2)]
        DEC = [sb(f"DEC{d}", [128, NCH * 8], F32) for d in range(2)]
        QTh = [sb(f"QTh{i}", [128, SMAX], BF16) for i in range(2)]
        KT = sb("KT", [128, SMAX], BF16)
        Ktok = sb("Ktok", [128, NCH, 128], BF16)
        V1 = sb("V1", [128, NCH, 2, 65], BF16)
        OGS = sb("OGS", [128, NCH, 128], F32)
        HF = sb("HF", [128, NCH, 128], F32)
        Wst = [sb(f"W{d}", [128, 65], F32) for d in range(2)]
        U = [sb(f"U{i}", [128, 65], BF16) for i in range(4)]
        Kw = [sb(f"Kw{i}", [128, 64], BF16) for i in range(4)]
        PT = [sb(f"PT{i}", [128, 128], BF16) for i in range(4)]
        rr = [sb(f"rr{i}", [128, 2], F32) for i in range(4)]
        sqt = sb("sqt", [128, 4, 128], F32)
        ssq = sb("ssq", [128, 8], F32)
        hn = sb("hn", [128, 4, 128], F32)
        hb = sb("hb", [128, 4, 128], BF16)
        stg = [sb(f"stg{i}", [128, 512], BF16) for i in range(2)]
        Bk = [pm(f"Bk{i}", [128, 512], F32) for i in range(6)]
        GP = Bk[0:4]
        trP = pm("trP", [128, 4, 128], BF16)
        DECp = [sb(f"DECp{d}", [128, NCH, 4], F32) for d in range(2)]
        Upair = [sb(f"Up{i}", [128, 65], BF16) for i in range(4)]
        Kwp = [sb(f"Kwp{i}", [128, 2, 64], BF16) for i in range(4)]
        PTp = [sb(f"PTp{i}", [128, 2, 128], BF16) for i in range(4)]
        rp = [sb(f"rp{i}", [128, 4], F32) for i in range(4)]
        htmp = [sb(f"htmp{i}", [128, 2, 64], F32) for i in range(2)]

        def tri(t, nm, cm, step, base):
            s.op('pool', lambda e: e.memset(t[:], 1.0), writes=[nm])
            s.op('pool', lambda e: e.affine_select(out=t[:], in_=t[:], pattern=[[step, 128]], compare_op=ALU.is_ge,
                                                   fill=0.0, base=base, channel_multiplier=cm),
                 reads=[nm], writes=[nm])
        tri(SU, 'SU', 1, -1, -1)
        tri(SL, 'SL', -1, 1, -1)
        tri(maskF, 'maskF', -1, 1, 0)
        tri(maskB, 'maskB', 1, -1, 0)
        s.op('pool', lambda e: e.memset(ONES[:], 1.0), writes=['ONES'])
        s.op('pool', lambda e: e.memset(QTh[0][64:128, :], 0.0), writes=['QT'])
        s.op('pool', lambda e: e.memset(QTh[1][0:64, :], 0.0), writes=['QT'])
        s.op('pool', lambda e: e.memset(V1[:, :, :, 64:65], 1.0), writes=['V1'])
        s.dma('sp', 'p6_mhg', mhg[:], I.mhg[0:1, :].partition_broadcast(128), writes=['mhg'])
        islot = 0
        istg = 0
        for j, Sj in enumerate(g.seqs):
            base = g.offs[j]
            nch = Sj // 128
            N8 = nch * 8
            s.dma('sp', 'p6_gz', gz[:, 0:nch, :], S.gtok[base:base + Sj, :].rearrange("(c p) d -> p c d", p=128),
                  writes=['gz'])
            for d in range(2):
                fcol = 8 + 16 * d
                icol = 16 * d
                l1v = l1[d][:, 0:N8].rearrange("p (c h) -> p c h", h=8)
                s.op('act', lambda e, nch=nch, N8=N8, l1v=l1v, fcol=fcol: e.activation(out=l1v, in_=gz[:, 0:nch, fcol:fcol + 8],
                                                                       func=AF.Exp, scale=-1.0),
                     reads=['gz'], writes=[f'l1{d}'])
                s.op('act', lambda e, nch=nch, N8=N8, d=d: e.activation(out=l1[d][:, 0:N8], in_=l1[d][:, 0:N8], func=AF.Ln, bias=1.0),
                     reads=[f'l1{d}'], writes=[f'l1{d}'])
                Gp, Tp = GP[2 * d], GP[2 * d + 1]
                tri_m = SU if d == 0 else SL
                s.op('pe', lambda e, nch=nch, N8=N8, Gp=Gp, tri_m=tri_m, d=d: e.matmul(Gp[:, 0:N8], lhsT=tri_m[:], rhs=l1[d][:, 0:N8],
                                                                      start=True, stop=True),
                     reads=['SU', 'SL', f'l1{d}'], writes=[f'B{2 * d}'])
                s.op('pe', lambda e, nch=nch, N8=N8, Tp=Tp, d=d: e.matmul(Tp[:, 0:N8], lhsT=ONES[:], rhs=l1[d][:, 0:N8],
                                                         start=True, stop=True),
                     reads=['ONES', f'l1{d}'], writes=[f'B{2 * d + 1}'])
                s.op('dve', lambda e, nch=nch, N8=N8, Gp=Gp, icol=icol: e.tensor_tensor(
                    out=tmpg[:, 0:N8].rearrange("p (c h) -> p c h", h=8), in0=gz[:, 0:nch, icol:icol + 8],
                    in1=Gp[:, 0:N8].rearrange("p (c h) -> p c h", h=8), op=ALU.subtract),
                    reads=['gz', f'B{2 * d}'], writes=['tmpg'])
                s.op('act', lambda e, nch=nch, N8=N8, d=d: e.activation(out=E[d][:, 0:N8], in_=tmpg[:, 0:N8], func=AF.Exp),
                     reads=['tmpg'], writes=[f'E{d}'])
                s.op('act', lambda e, nch=nch, N8=N8, d=d, Gp=Gp: e.activation(out=THR[d][:, 0:N8], in_=Gp[:, 0:N8], func=AF.Exp,
                                                              scale=-1.0), reads=[f'B{2 * d}'], writes=[f'THR{d}'])
                s.op('act', lambda e, nch=nch, N8=N8, d=d, Tp=Tp: e.activation(out=DEC[d][:, 0:N8], in_=Tp[:, 0:N8], func=AF.Exp,
                                                              scale=-1.0), reads=[f'B{2 * d + 1}'], writes=[f'DEC{d}'])
            for d in range(2):
                for hh in range(2):
                    p0 = hh * 64
                    s.op('pool', lambda e, nch=nch, N8=N8, d=d, hh=hh, p0=p0: e.tensor_copy(
                        out=DECp[d][p0:p0 + 64, 0:nch, :],
                        in_=DEC[d][p0:p0 + 64, 0:N8].rearrange("p (c hp two) -> p c hp two", hp=4, two=2)[:, :, :, hh]),
                        reads=[f'DEC{d}'], writes=[f'DECp{d}'])
            for hp in range(4):
                r0 = hp * 128
                s.dma('sp', 'p6_QT', QTh[0][0:64, 0:Sj], S.qcT[r0:r0 + 64, base:base + Sj], writes=['QT'])
                s.dma('sp', 'p6_QT', QTh[1][64:128, 0:Sj], S.qcT[r0 + 64:r0 + 128, base:base + Sj], writes=['QT'])
                s.dma('sp', 'p6_KT', KT[:, 0:Sj], S.kcT[r0:r0 + 128, base:base + Sj], writes=['KT'])
                s.dma('sp', 'p6_Ktok', Ktok[:, 0:nch, :],
                      S.kc[base:base + Sj, r0:r0 + 128].rearrange("(c p) d -> p c d", p=128), writes=['Ktok'])
                for hh in range(2):
                    s.dma('sp', 'p6_V1', V1[:, 0:nch, hh, 0:64],
                          S.vc[base:base + Sj, r0 + hh * 64:r0 + hh * 64 + 64].rearrange("(c p) e -> p c e", p=128),
                          writes=['V1'])
                s.dma('sp', 'p6_OGS', OGS[:, 0:nch, :],
                      S.ogs[base:base + Sj, r0:r0 + 128].rearrange("(c p) d -> p c d", p=128), writes=['OGS'])
                pend = []

                def flush(n):
                    while len(pend) > n:
                        pend.pop(0)()
                for ci in range(nch):
                    for d in range(2):
                        c = ci if d == 0 else nch - 1 - ci
                        first = (ci == 0)
                        last = (ci == nch - 1)
                        mask = maskF if d == 0 else maskB
                        sl = islot % 4
                        bkp = islot % 2
                        islot += 1
                        col0 = c * 8 + hp * 2
                        cs = slice(c * 128, (c + 1) * 128)
                        spb, Ob, dUb = Bk[bkp], Bk[2 + bkp], Bk[4 + bkp]
                        spk, Ok, dUk = f'B{bkp}', f'B{2 + bkp}', f'B{4 + bkp}'

                        def st1(spb=spb, spk=spk, cs=cs, sl=sl, c=c, d=d, col0=col0, mask=mask):
                            def mm(e):
                                for hh in range(2):
                                    p0 = hh * 64
                                    r = e.matmul(spb[:, hh * 128:(hh + 1) * 128], lhsT=KT[:, cs],
                                                 rhs=QTh[hh][:, cs], start=True, stop=True)
                                return r
                            s.op('pe', mm, reads=['KT', 'QT'], writes=[spk])
                            for hh in range(2):
                                s.op('act', lambda e, hh=hh: e.activation(
                                    out=Kwp[sl][:, hh, :], in_=Ktok[:, c, hh * 64:(hh + 1) * 64], func=AF.Copy,
                                    scale=E[d][:, col0 + hh:col0 + hh + 1]),
                                    reads=['Ktok', f'E{d}'], writes=[f'Kwp{sl}'])
                            for hh in range(2):
                                s.op('dve', lambda e, hh=hh: e.scalar_tensor_tensor(
                                    out=PTp[sl][:, hh, :], in0=spb[:, hh * 128:(hh + 1) * 128],
                                    scalar=E[d][:, col0 + hh:col0 + hh + 1], in1=mask[:], op0=ALU.mult, op1=ALU.mult),
                                    reads=[spk, f'E{d}', 'maskF', 'maskB'], writes=[f'PTp{sl}'])

                        firstw = (ci < nch // 2)

                        def st2(Ob=Ob, Ok=Ok, dUb=dUb, dUk=dUk, cs=cs, sl=sl, c=c, d=d, col0=col0, first=first,
                                last=last, hp=hp, firstw=firstw):
                            wk = f'W{d}'
                            u_ = Upair[sl]
                            if not first:
                                s.op('act', lambda e: e.activation(out=u_[:], in_=Wst[d][:], func=AF.Copy,
                                                                   scale=DECp[d][:, c, hp:hp + 1]),
                                     reads=[wk, f'DECp{d}'], writes=[f'Up{sl}'])

                            def omm(e):
                                for hh in range(2):
                                    p0 = hh * 64
                                    r = e.matmul(Ob[:, hh * 128:hh * 128 + 65], lhsT=PTp[sl][:, hh, :],
                                                 rhs=V1[:, c, hh, :], start=True, stop=first)
                                    if not first:
                                        r = e.matmul(Ob[:, hh * 128:hh * 128 + 65], lhsT=QTh[hh][:, cs],
                                                     rhs=u_[:, :], start=False, stop=True)
                                return r
                            s.op('pe', omm, reads=[f'PTp{sl}', 'V1', 'QT', f'Up{sl}'], writes=[Ok])
                            if not last:
                                def dmm(e):
                                    for hh in range(2):
                                        p0 = hh * 64
                                        r = e.matmul(dUb[p0:p0 + 64, 0:65], lhsT=Kwp[sl][:, hh, :],
                                                     rhs=V1[:, c, hh, :], start=True, stop=True)
                                    return r
                                s.op('pe', dmm, reads=[f'Kwp{sl}', 'V1'], writes=[dUk])
                                if first:
                                    s.op('dve', lambda e: e.tensor_copy(out=Wst[d][:], in_=dUb[:, 0:65]),
                                         reads=[dUk], writes=[wk])
                                else:
                                    s.op('dve', lambda e: e.scalar_tensor_tensor(
                                        out=Wst[d][:], in0=Wst[d][:], scalar=DECp[d][:, c, hp:hp + 1],
                                        in1=dUb[:, 0:65], op0=ALU.mult, op1=ALU.add),
                                        reads=[wk, f'DECp{d}', dUk], writes=[wk])
                            if os.environ.get('P6A'):
                                return
                            r_ = rp[sl]
                            Ov = Ob[:, 0:256].rearrange("p (h x) -> p h x", x=128)
                            s.op('act', lambda e: e.activation(out=r_[:, 0:2].unsqueeze(2), in_=Ov[:, :, 64:65],
                                                               func=AF.Abs), reads=[Ok], writes=[f'rp{sl}'])
                            s.op('dve', lambda e: e.tensor_tensor(out=r_[:, 0:2], in0=r_[:, 0:2],
                                                                  in1=THR[d][:, col0:col0 + 2], op=ALU.max),
                                 reads=[f'rp{sl}', f'THR{d}'], writes=[f'rp{sl}'])
                            s.op('dve', lambda e: e.reciprocal(out=r_[:, 2:4], in_=r_[:, 0:2]),
                                 reads=[f'rp{sl}'], writes=[f'rp{sl}b'])
                            hfv = HF[:, c, :].rearrange("p (h x) -> p h x", x=64)
                            rb = r_[:, 2:4].unsqueeze(2).to_broadcast([128, 2, 64])
                            if firstw:
                                s.op('dve', lambda e: e.tensor_tensor(out=hfv, in0=Ov[:, :, 0:64], in1=rb, op=ALU.mult),
                                     reads=[Ok, f'rp{sl}b'], writes=[f'HF{c}'])
                            else:
                                ht = htmp[sl % 2]
                                s.op('dve', lambda e: e.tensor_tensor(out=ht[:], in0=Ov[:, :, 0:64], in1=rb, op=ALU.mult),
                                     reads=[Ok, f'rp{sl}b'], writes=[f'htmp{sl % 2}'])
                                s.op('pool', lambda e: e.tensor_tensor(out=hfv, in0=hfv, in1=ht[:], op=ALU.add),
                                     reads=[f'htmp{sl % 2}', f'HF{c}'], writes=[f'HF{c}'])
                        st1()
                        pend.append(st2)
                        flush(int(os.environ.get('LAG6', '1')))
                flush(0)
                for cg in range(nch // 4):
                    hv = HF[:, cg * 4:(cg + 1) * 4, :]
                    hkeys = [f'HF{c}' for c in range(cg * 4, cg * 4 + 4)]
                    s.op('act', lambda e, hv=hv: e.activation(out=sqt[:], in_=hv, func=AF.Square),
                         reads=hkeys, writes=['sqt'])
                    s.op('dve', lambda e: e.tensor_reduce(out=ssq[:], in_=sqt[:].rearrange("p c (h e) -> p (c h) e", e=64),
                                                          axis=AX.X, op=ALU.add), reads=['sqt'], writes=['ssq'])
                    rsqrt_act(g, ssq[:], ssq[:], 1.0 / 64, ['ssq'], ['ssq'])
                    s.op('dve', lambda e, hv=hv: e.tensor_tensor(
                        out=hn[:].rearrange("p c (h e) -> p (c h) e", e=64),
                        in0=hv.rearrange("p c (h e) -> p (c h) e", e=64),
                        in1=ssq[:].unsqueeze(2).to_broadcast([128, 8, 64]), op=ALU.mult),
                        reads=hkeys + ['ssq'], writes=['hn'])
                    s.op('pool', lambda e, r0=r0: e.tensor_tensor(out=hn[:], in0=hn[:],
                                                                  in1=bc(mhg[:, r0:r0 + 128], [128, 4, 128]), op=ALU.mult),
                         reads=['hn', 'mhg'], writes=['hn'])
                    s.op('pool', lambda e, cg=cg: e.tensor_tensor(out=hb[:], in0=hn[:], in1=OGS[:, cg * 4:(cg + 1) * 4, :],
                                                                  op=ALU.mult), reads=['hn', 'OGS'], writes=['hb'])

                    def trh(e):
                        for i in range(4):
                            r = e.transpose(out=trP[:, i, :], in_=hb[:, i, :], identity=g.identB[:])
                        return r
                    s.op('pe', trh, reads=['hb', 'identB'], writes=['trP'])
                    bs = istg % 2
                    istg += 1
                    s.op('act', lambda e, bs=bs: e.activation(out=stg[bs][:], in_=trP[:, :, :].rearrange("p c t -> p (c t)"),
                                                              func=AF.Copy), reads=['trP'], writes=[f'stg{bs}'])
                    t0 = base + cg * 512
                    s.dma('sp', f'p6_stg{bs}', S.o2T[r0:r0 + 128, t0:t0 + 512], stg[bs][:], reads=[f'stg{bs}'])
        s.barrier()
```

```python
import numpy as np
import os
from contextlib import ExitStack
import concourse.bass as bass
import concourse.mybir as mybir
from concourse.bass_utils import run_bass_kernel_spmd

F32 = mybir.dt.float32
BF16 = mybir.dt.bfloat16
AF = mybir.ActivationFunctionType
ALU = mybir.AluOpType
AX = mybir.AxisListType
ENGS = ('pe', 'act', 'dve', 'pool', 'sp')
D = 1024
EPS = 1e-6


class Sched:
    def __init__(self, nc, stack):
        self.nc = nc
        self.stack = stack
        self.streams = {e: [] for e in ENGS}
        self.sems = {}
        self.cnt = {}
        self.seen = {e: {} for e in ENGS}
        self.bufs = {}
        self.snap = {}
        self.nwaits = 0
        self.nops = 0

    def sem(self, name):
        if name not in self.sems:
            self.sems[name] = self.stack.enter_context(
                self.nc.semaphore(name.replace(':', '_').replace('/', '_')))
            self.cnt[name] = 0
        return self.sems[name]

    def _need(self, E, ev, waits):
        if ev is None:
            return
        name, val = ev
        if self.seen[E].get(name, 0) >= val:
            return
        if waits.get(name, 0) < val:
            waits[name] = val

    def _collect(self, E, reads, writes):
        waits = {}
        for k in reads:
            b = self.bufs.get(k)
            if b is not None:
                self._need(E, b['w'], waits)
        own = 'e:' + E
        for k in writes:
            b = self.bufs.get(k)
            if b is not None:
                self._need(E, b['w'], waits)
                for ev in b['r']:
                    if ev[0] != own:
                        self._need(E, ev, waits)
        return waits

    def _apply_waits(self, E, waits):
        seen = self.seen[E]
        wl = []
        for name, val in waits.items():
            if seen.get(name, 0) >= val:
                continue
            wl.append((self.sem(name), val))
            sn = self.snap.get((name, val))
            if sn:
                for n2, v2 in sn.items():
                    if seen.get(n2, 0) < v2:
                        seen[n2] = v2
            seen[name] = val
        self.nwaits += len(wl)
        return wl

    def _record(self, ev, reads, writes):
        for k in reads:
            b = self.bufs.get(k)
            if b is None:
                b = self.bufs[k] = {'w': None, 'r': []}
            b['r'].append(ev)
            if len(b['r']) > 64:
                b['r'] = b['r'][-48:]
        for k in writes:
            self.bufs[k] = {'w': ev, 'r': []}

    def op(self, E, fn, reads=(), writes=()):
        waits = self._collect(E, reads, writes)
        if E == 'pe':
            waits.pop('e:pe', None)
        wl = self._apply_waits(E, waits)
        name = 'e:' + E
        s = self.sem(name)
        self.cnt[name] += 1
        val = self.cnt[name]
        ev = (name, val)
        if E == 'pe':
            self.seen[E][name] = val
        self.snap[ev] = dict(self.seen[E])
        self.streams[E].append((wl, fn, s, 1))
        self._record(ev, reads, writes)
        self.nops += 1
        return ev

    def dma(self, Q, semname, out, in_, reads=(), writes=(), **kw):
        waits = self._collect(Q, reads, writes)
        wl = self._apply_waits(Q, waits)
        name = 'd:' + semname
        s = self.sem(name)
        self.cnt[name] += 16
        val = self.cnt[name]
        ev = (name, val)
        self.snap[ev] = dict(self.seen[Q])

        def fn(eng, out=out, in_=in_, kw=kw):
            return eng.dma_start(out=out, in_=in_, **kw)
        self.streams[Q].append((wl, fn, s, 16))
        self._record(ev, reads, writes)
        self.nops += 1
        return ev

    def barrier(self):
        allev = dict(self.cnt)
        for E in ENGS:
            waits = {}
            for name, val in allev.items():
                if val > 0 and self.seen[E].get(name, 0) < val and name != 'e:' + E:
                    waits[name] = val
            wl = self._apply_waits(E, waits)
            if wl:
                self.streams[E].append((wl, None, None, 0))
        self.bufs = {}

    def emit(self):
        nc = self.nc
        self.barrier()
        streams = self.streams
        with nc.Block() as block:
            def run(E):
                def body(eng):
                    for wl, fn, s, inc in streams[E]:
                        for (ws, wv) in wl:
                            eng.wait_ge(ws, wv)
                        if fn is not None:
                            fn(eng).then_inc(s, inc)
                return body
            block.sync(run('sp'))
            block.scalar(run('act'))
            block.vector(run('dve'))
            block.gpsimd(run('pool'))
            block.tensor(run('pe'))


class Ctx:
    pass


def build(seqs, phases, debug=False):
    nc = bass.Bass("TRN2", target_bir_lowering=False)
    NS = len(seqs)
    NT = sum(seqs)
    offs = [sum(seqs[:i]) for i in range(NS)]
    SMAX = max(seqs)
    g = Ctx()
    g.nc, g.seqs, g.NS, g.NT, g.offs, g.debug = nc, seqs, NS, NT, offs, debug

    def din(name, shape, dt=F32):
        return nc.dram_tensor(name, list(shape), dt, kind="ExternalInput").ap()

    def dscr(name, shape, dt):
        return nc.dram_tensor(name, list(shape), dt, kind=("ExternalOutput" if debug else "Internal")).ap()
    g.dscr = dscr
    I = Ctx()
    g.I = I
    I.x = din("x", [NT, D])
    I.cT = din("cT", [128, 8 * NS])
    I.w_in_ab = din("w_in_ab", [D, 2304])
    I.w_out_ab = din("w_out_ab", [D, D])
    I.w_in_cd = din("w_in_cd", [D, 3104])
    I.w_out_cd = din("w_out_cd", [D, D])
    I.w_ada = din("w_ada", [2, D, 6 * D])
    I.b_ada = din("b_ada", [1, 2 * 6 * D])
    I.normgT = din("normgT", [128, 2 * 4 * 8])
    I.norm_g = din("norm_g", [8, D])
    I.w_ff1 = din("w_ff1", [2, D, 4 * D])
    I.w_ff2 = din("w_ff2", [2, 4 * D, D])
    I.qkg = din("qkg", [1, 128])
    I.ropeA = din("ropeA", [SMAX, 64])
    I.ropeB = din("ropeB", [SMAX, 64])
    I.gate_bias = din("gate_bias", [32, 1])
    I.mhg = din("mhg", [1, 512])
    I.sgg = din("sgg", [1, 512])
    I.wsT = din("wsT", [128, 8 * 128])
    I.bsT = din("bsT", [128, 8])
    y = nc.dram_tensor("y", [NT, D], F32, kind="ExternalOutput").ap()
    g.y = y

    Sx = Ctx()
    g.S = Sx
    Sx.gates = dscr("s_gates", [2, 2, NS, D], F32)
    Sx.qaT = dscr("s_qaT", [512, NT], BF16)
    Sx.kaT = dscr("s_kaT", [512, NT], BF16)
    Sx.qbT = dscr("s_qbT", [512, NT], BF16)
    Sx.kbT = dscr("s_kbT", [128, NT], BF16)
    Sx.vA = dscr("s_vA", [NT, 512], BF16)
    Sx.vB = dscr("s_vB", [NT, 128], BF16)
    Sx.oT = dscr("s_oT", [D, NT], BF16)
    Sx.xmid = dscr("s_xmid", [NT, D], F32)
    Sx.x1 = dscr("s_x1", [NT, D], F32)
    Sx.qcT = dscr("s_qcT", [512, NT], BF16)
    Sx.kcT = dscr("s_kcT", [512, NT], BF16)
    Sx.kc = dscr("s_kc", [NT, 512], BF16)
    Sx.vc = dscr("s_vc", [NT, 512], BF16)
    Sx.ogs = dscr("s_ogs", [NT, 512], F32)
    Sx.gtok = dscr("s_gtok", [NT, 32], F32)
    Sx.o2T = dscr("s_o2T", [D, NT], BF16)

    with ExitStack() as st:
        s = Sched(nc, st)
        g.s = s
        g.identB = st.enter_context(nc.sbuf_tensor("identB", [128, 128], BF16))
        g.identF = st.enter_context(nc.sbuf_tensor("identF", [128, 128], F32))
        g.GS = st.enter_context(nc.sbuf_tensor("GS", [128, 2, 2, 8, NS], F32))
        g.SH = st.enter_context(nc.sbuf_tensor("SH", [128, 2, 2, 8, NS], F32))
        g.epsc = st.enter_context(nc.sbuf_tensor("epsc", [128, 2], F32))
        s.op('pool', lambda e: e.memset(g.epsc[:], EPS), writes=['epsc'])
        for t, nm in ((g.identB, 'identB'), (g.identF, 'identF')):
            s.op('pool', lambda e, t=t: e.memset(t[:], 1.0), writes=[nm])
            s.op('pool', lambda e, t=t: e.affine_select(out=t[:], in_=t[:], pattern=[[-1, 128]],
                                                        compare_op=ALU.is_equal, fill=0.0, base=0,
                                                        channel_multiplier=1), reads=[nm], writes=[nm])
        if 0 in phases:
            phase0(g)
        if 1 in phases:
            phase1(g)
        if 2 in phases:
            import os
            if not os.environ.get('SKIPB'):
                phase2(g)
            if not os.environ.get('SKIPA'):
                phase2a(g)
        if 3 in phases:
            phase3(g, 0, Sx.oT, I.w_out_ab, I.x, Sx.xmid)
        if 4 in phases:
            phase4(g, 0, Sx.xmid, Sx.x1 if (5 in phases or debug) else g.y)
        if 5 in phases:
            phase5(g)
        if 6 in phases:
            phase6(g)
        if 7 in phases:
            phase3(g, 1, Sx.o2T, I.w_out_cd, Sx.x1, Sx.xmid)
        if 8 in phases:
            phase4(g, 1, Sx.xmid, g.y)
        s.emit()
    g.stats = (s.nops, s.nwaits)
    return nc, g


def load_w_bf16(g, st_name, wt, w_dram, K, N, n0=0, colmap=None):
    s = g.s
    for k in range(K):
        c = 0
        while c < N:
            w = min(1024, N - c)
            s.dma('pool', st_name, wt[:, k, c:c + w], w_dram[k * 128:(k + 1) * 128, n0 + c:n0 + c + w],
                  writes=[st_name])
            c += w


def phase0(g):
    nc, s, NS, I = g.nc, g.s, g.NS, g.I
    with ExitStack() as st:
        sb = lambda n, sh, dt: st.enter_context(nc.sbuf_tensor(n, sh, dt))
        cTf = sb("p0_cTf", [128, 8 * NS], F32)
        cTb = sb("p0_cTb", [128, 8, NS], BF16)
        wt = [sb(f"p0_w{i}", [128, 8, 3072], BF16) for i in range(2)]
        bada = sb("p0_bada", [NS, 2 * 6 * D], F32)
        modrow = sb("p0_modrow", [NS, 2, 6 * D], F32)
        ngT = sb("p0_ngT", [128, 2, 4, 8], F32)
        modT = sb("p0_modT", [128, 2, 4, 8, NS], F32)
        ps = [st.enter_context(nc.psum_tensor(f"p0_ps{i}", [128, 512], F32)) for i in range(2)]
        pT = st.enter_context(nc.psum_tensor("p0_pT", [128, 4 * 8 * NS], F32))
        s.dma('sp', 'p0_cTf', cTf[:], I.cT[:, :], writes=['cTf'])
        s.dma('sp', 'p0_bada', bada[:], I.b_ada[0:1, :].partition_broadcast(NS), writes=['bada'])
        s.dma('sp', 'p0_ngT', ngT[:], I.normgT[:, :], writes=['ngT'])
        s.op('act', lambda e: e.activation(out=cTb[:].rearrange("p k s -> p (k s)"), in_=cTf[:], func=AF.Silu),
             reads=['cTf'], writes=['cTb'])
        it = 0
        for l in range(2):
            for hf in range(2):
                w = wt[it % 2]
                wn = f"p0_w{it % 2}"
                load_w_bf16(g, wn, w, I.w_ada[l], 8, 3072, n0=hf * 3072)
                for nch in range(6):
                    p = ps[nch % 2]
                    pn = f"p0_ps{nch % 2}"

                    def mm(e, p=p, w=w, nch=nch):
                        for k in range(8):
                            r = e.matmul(p[0:NS, :], lhsT=cTb[:, k, :], rhs=w[:, k, nch * 512:(nch + 1) * 512],
                                         start=(k == 0), stop=(k == 7))
                        return r
                    s.op('pe', mm, reads=['cTb', wn], writes=[pn])
                    c0 = hf * 3072 + nch * 512
                    s.op('dve', lambda e, p=p, l=l, c0=c0: e.tensor_tensor(
                        out=modrow[:, l, c0:c0 + 512], in0=p[0:NS, :],
                        in1=bada[:, l * 6 * D + c0:l * 6 * D + c0 + 512], op=ALU.add),
                        reads=[pn, 'bada'], writes=['modrow'])
                it += 1
        for l in range(2):
            for gi, part in enumerate((2, 5)):
                s.dma('sp', 'p0_gst', g.S.gates[l, gi], modrow[:, l, part * D:(part + 1) * D],
                      reads=['modrow'])
        for l in range(2):
            def tr(e, l=l):
                for pi, part in enumerate((0, 1, 3, 4)):
                    for k in range(8):
                        c0 = part * D + k * 128
                        o = (pi * 8 + k) * NS
                        r = e.transpose(out=pT[:, o:o + NS], in_=modrow[:, l, c0:c0 + 128],
                                        identity=g.identF[0:NS, 0:NS])
                return r
            s.op('pe', tr, reads=['modrow', 'identF'], writes=['p0_pT'])
            s.op('dve', lambda e, l=l: e.tensor_copy(out=modT[:, l].rearrange("p a k s -> p (a k s)"), in_=pT[:, :]),
                 reads=['p0_pT'], writes=['modT'])
        for l in range(2):
            for m in range(2):
                sc = modT[:, l, 2 * m + 1]
                sh = modT[:, l, 2 * m]
                ngb = ngT[:, l, 2 * m, :].unsqueeze(2).to_broadcast([128, 8, NS])
                s.op('dve', lambda e, sc=sc, ngb=ngb, l=l, m=m: e.scalar_tensor_tensor(
                    out=g.GS[:, l, m], in0=sc, scalar=1.0, in1=ngb, op0=ALU.add, op1=ALU.mult),
                    reads=['modT', 'ngT'], writes=['GS'])
                s.op('dve', lambda e, sh=sh, l=l, m=m: e.tensor_copy(out=g.SH[:, l, m], in_=sh),
                     reads=['modT'], writes=['SH'])
        s.barrier()


def bc(ap2d, shape):
    return ap2d.unsqueeze(1).to_broadcast(shape)


def rsqrt_act(g, out, in_, scale, rk, wk):
    s = g.s
    s.op('act', lambda e: e.activation(out=out, in_=in_, func=AF.Ln, scale=scale, bias=g.epsc[:, 0:1]),
         reads=rk + ['epsc'], writes=wk)
    s.op('act', lambda e: e.activation(out=out, in_=out, func=AF.Exp, scale=-0.5), reads=wk, writes=wk)


def rms_prep(g, pre, xt, xn, ss, rstd, junk, nblk, xkey, outkey):
    s = g.s
    for b in range(nblk):
        s.op('act', lambda e, b=b: e.activation(out=junk[:], in_=xt[:, b, :], func=AF.Square,
                                                accum_out=ss[:, b:b + 1]),
             reads=[xkey], writes=[pre + 'junk', pre + 'ss'])
    rsqrt_act(g, rstd[:, 0:nblk], ss[:, 0:nblk], 1.0 / D, [pre + 'ss'], [pre + 'rstd'])
    for b in range(nblk):
        s.op('act', lambda e, b=b: e.activation(out=xn[:, b, :], in_=xt[:, b, :], func=AF.Copy,
                                                scale=rstd[:, b:b + 1]),
             reads=[xkey, pre + 'rstd'], writes=[outkey])


def to_featmajor(g, pre, xn, hmT, tps, l, m, j, nblk, xnkey, hkey):
    s = g.s
    for half in range(nblk // 2):
        tp = tps[half % len(tps)]
        tk = pre + f'tp{half % len(tps)}'

        def tr(e, half=half, tp=tp):
            for k in range(8):
                for bb in range(2):
                    r = e.transpose(out=tp[:, k, bb * 128:(bb + 1) * 128],
                                    in_=xn[:, half * 2 + bb, k * 128:(k + 1) * 128], identity=g.identB[:])
            return r
        s.op('pe', tr, reads=[xnkey, 'identB'], writes=[tk])
        for k in range(8):
            eng = 'dve' if k % 2 == 0 else 'pool'
            eng = 'dve'
            s.op(eng, lambda e, k=k, half=half, tp=tp: e.tensor_scalar(
                out=hmT[:, k, half * 256:(half + 1) * 256], in0=tp[:, k, :],
                scalar1=g.GS[:, l, m, k, j:j + 1], scalar2=g.SH[:, l, m, k, j:j + 1],
                op0=ALU.mult, op1=ALU.add), reads=[tk, 'GS', 'SH'], writes=[hkey])


def rotary(g, eng, out, zin, cos, sin, H, hd, tmp, rk, wk, tk):
    s = g.s
    zv = zin.rearrange("p (h two d) -> p h two d", two=2, d=hd)
    ov = out.rearrange("p (h two d) -> p h two d", two=2, d=hd)
    x1, x2 = zv[:, :, 0, :], zv[:, :, 1, :]
    o1, o2 = ov[:, :, 0, :], ov[:, :, 1, :]
    cb = bc(cos, [128, H, hd])
    sb_ = bc(sin, [128, H, hd])
    t = [tmp[:, i, 0:H * hd].rearrange("p (h d) -> p h d", d=hd) for i in range(4)]
    s.op(eng, lambda e: e.tensor_tensor(out=t[0], in0=x1, in1=cb, op=ALU.mult), reads=rk, writes=[tk + '0'])
    s.op(eng, lambda e: e.tensor_tensor(out=t[1], in0=x2, in1=sb_, op=ALU.mult), reads=rk, writes=[tk + '1'])
    s.op(eng, lambda e: e.tensor_tensor(out=o1, in0=t[0], in1=t[1], op=ALU.subtract),
         reads=[tk + '0', tk + '1'], writes=wk)
    s.op(eng, lambda e: e.tensor_tensor(out=t[2], in0=x2, in1=cb, op=ALU.mult), reads=rk, writes=[tk + '2'])
    s.op(eng, lambda e: e.tensor_tensor(out=t[3], in0=x1, in1=sb_, op=ALU.mult), reads=rk, writes=[tk + '3'])
    s.op(eng, lambda e: e.tensor_tensor(out=o2, in0=t[2], in1=t[3], op=ALU.add),
         reads=[tk + '2', tk + '3'], writes=wk)


def phase1(g):
    nc, s, NS, I, S = g.nc, g.s, g.NS, g.I, g.S
    with ExitStack() as st:
        sb = lambda n, sh, dt: st.enter_context(nc.sbuf_tensor(n, sh, dt))
        pm = lambda n, sh, dt: st.enter_context(nc.psum_tensor(n, sh, dt))
        wab = sb("p1_wab", [128, 8, 2304], BF16)
        xt = [sb(f"p1_xt{i}", [128, 4, D], F32) for i in range(2)]
        xn = [sb(f"p1_xn{i}", [128, 4, D], BF16) for i in range(2)]
        junk = sb("p1_junk", [128, D], BF16)
        ss = [sb(f"p1_ss{i}", [128, 4], F32) for i in range(2)]
        rstd = [sb(f"p1_rstd{i}", [128, 4], F32) for i in range(2)]
        hmT = [sb(f"p1_hmT{i}", [128, 8, 512], BF16) for i in range(2)]
        zs = [sb(f"p1_zs{i}", [128, 2304], F32) for i in range(2)]
        rA = [sb(f"p1_rA{i}", [128, 4, 64], F32) for i in range(2)]
        rB = [sb(f"p1_rB{i}", [128, 4, 64], F32) for i in range(2)]
        qkg = sb("p1_qkg", [128, 128], F32)
        tmpA = sb("p1_tmpA", [128, 4, 256], F32)
        tmpB = sb("p1_tmpB", [128, 4, 256], F32)
        sq = sb("p1_sq", [128, 640], F32)
        ssq = sb("p1_ssq", [128, 10], F32)
        qn = sb("p1_qn", [128, 640], F32)
        qk = [sb(f"p1_qk{i}", [128, 1664], BF16) for i in range(2)]
        stg = [sb(f"p1_stg{i}", [128, 13, 512], BF16) for i in range(2)]
        vst = [sb(f"p1_vst{i}", [128, 4, 640], BF16) for i in range(2)]
        tps = [pm(f"p1_tp{i}", [128, 8, 256], BF16) for i in range(2)]
        zp = [pm(f"p1_zp{i}", [128, 512], F32) for i in range(2)]
        qT = pm("p1_qT", [128, 16, 128], BF16)

        load_w_bf16(g, 'p1_wab', wab, I.w_in_ab, 8, 2304)
        s.dma('sp', 'p1_qkg', qkg[:], I.qkg[0:1, :].partition_broadcast(128), writes=['qkg'])
        tiles = []
        for j, Sj in enumerate(g.seqs):
            for t in range(Sj // 512):
                tiles.append((j, t * 512, g.offs[j] + t * 512))

        def load(i):
            j, p0, t0 = tiles[i]
            b = i % 2
            s.dma('sp', f'p1_xt{b}', xt[b][:], I.x[t0:t0 + 512, :].rearrange("(b p) d -> p b d", p=128),
                  writes=[f'xt{b}'])
            s.dma('sp', f'p1_rA{b}', rA[b][:], I.ropeA[p0:p0 + 512, :].rearrange("(b p) d -> p b d", p=128),
                  writes=[f'rA{b}'])
            s.dma('sp', f'p1_rB{b}', rB[b][:], I.ropeB[p0:p0 + 512, :].rearrange("(b p) d -> p b d", p=128),
                  writes=[f'rB{b}'])
        load(0)
        zi = 0
        pend = []

        def flush():
            while pend:
                pend.pop(0)()

        def prep(i):
            j, p0, t0 = tiles[i]
            b = i % 2
            rms_prep(g, f'p1{b}', xt[b], xn[b], ss[b], rstd[b], junk, 4, f'xt{b}', f'xn{b}')
            to_featmajor(g, 'p1', xn[b], hmT[b], tps, 0, 0, j, 4, f'xn{b}', f'hmT{b}')
        prep(0)
        for i, (j, p0, t0) in enumerate(tiles):
            b = i % 2
            if i + 1 < len(tiles):
                load(i + 1)
            for blk in range(4):
                if blk == 3 and i + 1 < len(tiles):
                    prep(i + 1)
                zb = zs[blk % 2]
                zk = f'zs{blk % 2}'
                for c in range(5):
                    n0 = c * 512
                    nw = min(512, 2304 - n0)
                    p = zp[zi % 2]
                    pn = f'zp{zi % 2}'
                    zi += 1

                    def mm(e, p=p, n0=n0, nw=nw, blk=blk, b=b):
                        for k in range(8):
                            r = e.matmul(p[:, 0:nw], lhsT=hmT[b][:, k, blk * 128:(blk + 1) * 128],
                                         rhs=wab[:, k, n0:n0 + nw], start=(k == 0), stop=(k == 7))
                        return r
                    s.op('pe', mm, reads=[f'hmT{b}', 'p1_wab'], writes=[pn])
                    if c == 2:
                        s.op('act', lambda e, p=p, blk=blk, b=b: e.activation(
                            out=vst[b][:, blk, 0:512], in_=p[:, 0:512], func=AF.Copy),
                            reads=[pn], writes=[f'vst{b}'])
                    else:
                        s.op('act', lambda e, p=p, n0=n0, nw=nw, zb=zb: e.activation(
                            out=zb[:, n0:n0 + nw], in_=p[:, 0:nw], func=AF.Copy),
                            reads=[pn], writes=[zk + f'c{c}'])
                flush()
                q = qk[blk % 2]
                qkk = f'qk{blk % 2}'
                cosA, sinA = rA[b][:, blk, 0:32], rA[b][:, blk, 32:64]
                cosB, sinB = rB[b][:, blk, 0:32], rB[b][:, blk, 32:64]
                rotary(g, 'dve', q[:, 0:512], zb[:, 0:512], cosA, sinA, 8, 32, tmpA,
                       [zk + 'c0', f'rA{b}'], [qkk + 'qa'], 'tmpA')
                rotary(g, 'pool', q[:, 512:1024], zb[:, 512:1024], cosA, sinA, 8, 32, tmpB,
                       [zk + 'c1', f'rA{b}'], [qkk + 'ka'], 'tmpB')
                s.op('act', lambda e, zb=zb: e.activation(out=sq[:], in_=zb[:, 1536:2176], func=AF.Square),
                     reads=[zk + 'c3', zk + 'c4'], writes=['sq'])
                s.op('dve', lambda e: e.tensor_reduce(out=ssq[:], in_=sq[:].rearrange("p (h d) -> p h d", d=64),
                                                      axis=AX.X, op=ALU.add), reads=['sq'], writes=['ssq'])
                rsqrt_act(g, ssq[:], ssq[:], 1.0 / 64, ['ssq'], ['ssq'])
                s.op('dve', lambda e, zb=zb: e.tensor_tensor(
                    out=qn[:].rearrange("p (h d) -> p h d", d=64),
                    in0=zb[:, 1536:2176].rearrange("p (h d) -> p h d", d=64),
                    in1=ssq[:].unsqueeze(2).to_broadcast([128, 10, 64]), op=ALU.mult),
                    reads=[zk + 'c3', zk + 'c4', 'ssq'], writes=['qn'])
                s.op('dve', lambda e: e.tensor_tensor(
                    out=qn[:, 0:512].rearrange("p (h d) -> p h d", d=64),
                    in0=qn[:, 0:512].rearrange("p (h d) -> p h d", d=64),
                    in1=bc(qkg[:, 0:64], [128, 8, 64]), op=ALU.mult), reads=['qn', 'qkg'], writes=['qn'])
                s.op('dve', lambda e: e.tensor_tensor(
                    out=qn[:, 512:640].rearrange("p (h d) -> p h d", d=64),
                    in0=qn[:, 512:640].rearrange("p (h d) -> p h d", d=64),
                    in1=bc(qkg[:, 64:128], [128, 2, 64]), op=ALU.mult), reads=['qn', 'qkg'], writes=['qn'])
                for half in range(2):
                    for (c0, H, o0) in ((0, 8, 1024), (512, 2, 1536)):
                        zin = qn[:, c0:c0 + H * 64].rearrange("p (h x) -> p h x", x=64)[:, :, half * 32:(half + 1) * 32]
                        oo = q[:, o0:o0 + H * 64].rearrange("p (h x) -> p h x", x=64)[:, :, half * 32:(half + 1) * 32]
                        rotary_v(g, 'dve', oo, zin, cosB[:, half * 16:(half + 1) * 16],
                                 sinB[:, half * 16:(half + 1) * 16], H, 16, tmpA, ['qn', f'rB{b}'],
                                 [qkk + 'b'], 'tmpA')
                s.op('pool', lambda e, zb=zb, blk=blk, b=b: e.tensor_copy(out=vst[b][:, blk, 512:640],
                                                                          in_=zb[:, 2176:2304]),
                     reads=[zk + 'c4'], writes=[f'vst{b}'])
                def tail(q=q, qkk=qkk, blk=blk, b=b):
                    def trq(e):
                        for c in range(13):
                            r = e.transpose(out=qT[:, c, :], in_=q[:, c * 128:(c + 1) * 128], identity=g.identB[:])
                        return r
                    s.op('pe', trq, reads=[qkk + 'qa', qkk + 'ka', qkk + 'b', 'identB'], writes=['qT'])
                    s.op('act', lambda e: e.activation(out=stg[b][:, :, blk * 128:(blk + 1) * 128],
                                                       in_=qT[:, 0:13, :], func=AF.Copy),
                         reads=['qT'], writes=[f'stg{b}'])
                pend.append(tail)
            flush()
            for (dst, c0, n) in ((S.qaT, 0, 4), (S.kaT, 4, 4), (S.qbT, 8, 4), (S.kbT, 12, 1)):
                s.dma('sp', f'p1_stg{b}', dst[:, t0:t0 + 512].rearrange("(c p) t -> p c t", p=128),
                      stg[b][:, c0:c0 + n, :], reads=[f'stg{b}'])
            s.dma('sp', f'p1_vst{b}', S.vA[t0:t0 + 512, :].rearrange("(b p) d -> p b d", p=128), vst[b][:, :, 0:512],
                  reads=[f'vst{b}'])
            s.dma('sp', f'p1_vst{b}', S.vB[t0:t0 + 512, :].rearrange("(b p) d -> p b d", p=128), vst[b][:, :, 512:640],
                  reads=[f'vst{b}'])
        s.barrier()


def rotary_v(g, eng, ov, zv, cos, sin, H, hd, tmp, rk, wk, tk):
    s = g.s
    x1, x2 = zv[:, :, 0:hd], zv[:, :, hd:2 * hd]
    o1, o2 = ov[:, :, 0:hd], ov[:, :, hd:2 * hd]
    cb = bc(cos, [128, H, hd])
    sb_ = bc(sin, [128, H, hd])
    t = [tmp[:, i, 0:H * hd].rearrange("p (h d) -> p h d", d=hd) for i in range(4)]
    s.op(eng, lambda e: e.tensor_tensor(out=t[0], in0=x1, in1=cb, op=ALU.mult), reads=rk, writes=[tk + '0'])
    s.op(eng, lambda e: e.tensor_tensor(out=t[1], in0=x2, in1=sb_, op=ALU.mult), reads=rk, writes=[tk + '1'])
    s.op(eng, lambda e: e.tensor_tensor(out=o1, in0=t[0], in1=t[1], op=ALU.subtract),
         reads=[tk + '0', tk + '1'], writes=wk)
    s.op(eng, lambda e: e.tensor_tensor(out=t[2], in0=x2, in1=cb, op=ALU.mult), reads=rk, writes=[tk + '2'])
    s.op(eng, lambda e: e.tensor_tensor(out=t[3], in0=x1, in1=sb_, op=ALU.mult), reads=rk, writes=[tk + '3'])
    s.op(eng, lambda e: e.tensor_tensor(out=o2, in0=t[2], in1=t[3], op=ALU.add),
         reads=[tk + '2', tk + '3'], writes=wk)


def phase2(g):
    nc, s, S = g.nc, g.s, g.S
    SMAX = max(g.seqs)
    with ExitStack() as st:
        sb = lambda n, sh, dt: st.enter_context(nc.sbuf_tensor(n, sh, dt))
        pm = lambda n, sh, dt: st.enter_context(nc.psum_tensor(n, sh, dt))
        kT = [sb(f"p2_kT{i}", [128, SMAX], BF16) for i in range(2)]
        V1 = [sb(f"p2_V1{i}", [128, SMAX // 128, 128], BF16) for i in range(2)]
        qT = [sb(f"p2_qT{i}", [128, SMAX], BF16) for i in range(2)]
        pT = [sb(f"p2_pT{i}", [128, 1024], BF16) for i in range(3)]
        rd = [sb(f"p2_rd{i}", [64, 512], F32) for i in range(2)]
        ob = [sb(f"p2_ob{i}", [64, 512], BF16) for i in range(2)]
        sp_ = [pm(f"p2_sp{i}", [128, 1024], F32) for i in range(3)]
        ot = [pm(f"p2_ot{i}", [128, 512], F32) for i in range(2)]
        for i in range(2):
            s.op('pool', lambda e, i=i: e.memset(V1[i][:, :, 64:128], 1.0), writes=[f'V1{i}'])
            s.op('pool', lambda e, i=i: e.memset(kT[i][64:128, :], 0.0), writes=[f'kT{i}'])
            s.op('pool', lambda e, i=i: e.memset(qT[i][64:128, :], 0.0), writes=[f'qT{i}'])
        ikv = 0
        ih = 0
        ip = 0
        io = 0
        LAG = 2
        pend = []

        def flush(n):
            while len(pend) > n:
                pend.pop(0)()
        for j, Sj in enumerate(g.seqs):
            base = g.offs[j]
            nkb, nqg = Sj // 128, Sj // 512
            for kv in range(2):
                bk = ikv % 2
                ikv += 1
                s.dma('sp', f'p2_kT{bk}', kT[bk][0:64, 0:Sj], S.kbT[kv * 64:(kv + 1) * 64, base:base + Sj],
                      writes=[f'kT{bk}'])
                s.dma('sp', f'p2_V1{bk}', V1[bk][:, 0:nkb, 0:64],
                      S.vB[base:base + Sj, kv * 64:(kv + 1) * 64].rearrange("(b p) d -> p b d", p=128),
                      writes=[f'V1{bk}'])
                for hq in range(4):
                    h = kv * 4 + hq
                    bq = ih % 2
                    ih += 1
                    s.dma('sp', f'p2_qT{bq}', qT[bq][0:64, 0:Sj], S.qbT[h * 64:(h + 1) * 64, base:base + Sj],
                          writes=[f'qT{bq}'])
                    for qg in range(nqg):
                        o = ot[io % 2]
                        on = f'ot{io % 2}'
                        bo = io % 2
                        io += 1
                        for kb in range(0, nkb, 2):
                            p = sp_[ip % 3]
                            pn = f'sp{ip % 3}'
                            pt = pT[ip % 3]
                            ptn = f'pT{ip % 3}'
                            ip += 1

                            def smm(e, p=p, bk=bk, bq=bq, kb=kb, qg=qg):
                                for i in range(2):
                                    r = e.matmul(p[:, i * 512:(i + 1) * 512],
                                                 lhsT=kT[bk][:, (kb + i) * 128:(kb + i + 1) * 128],
                                                 rhs=qT[bq][:, qg * 512:(qg + 1) * 512], start=True, stop=True)
                                return r
                            s.op('pe', smm, reads=[f'kT{bk}', f'qT{bq}'], writes=[pn])
                            s.op('act', lambda e, p=p, pt=pt: e.activation(out=pt[:], in_=p[:], func=AF.Exp,
                                                                           scale=0.125),
                                 reads=[pn], writes=[ptn])

                            def stage2(o=o, on=on, bo=bo, bk=bk, kb=kb, pt=pt, ptn=ptn, nkb=nkb, h=h, qg=qg,
                                       base=base):
                                def pvm(e):
                                    for i in range(2):
                                        r = e.matmul(o[:, :], lhsT=V1[bk][:, kb + i, :], rhs=pt[:, i * 512:(i + 1) * 512],
                                                     start=(kb + i == 0), stop=(kb + i == nkb - 1))
                                    return r
                                s.op('pe', pvm, reads=[f'V1{bk}', ptn], writes=[on])
                                if kb + 2 == nkb:
                                    s.op('dve', lambda e: e.reciprocal(out=rd[bo][:], in_=o[64:128, :]),
                                         reads=[on], writes=[f'rd{bo}'])
                                    s.op('dve', lambda e: e.tensor_tensor(out=ob[bo][:], in0=o[0:64, :],
                                                                          in1=rd[bo][:], op=ALU.mult),
                                         reads=[on, f'rd{bo}'], writes=[f'ob{bo}'])
                                    t0 = base + qg * 512
                                    s.dma('sp', f'p2_ob{bo}', S.oT[512 + h * 64:512 + (h + 1) * 64, t0:t0 + 512],
                                          ob[bo][:], reads=[f'ob{bo}'])
                            pend.append(stage2)
                            flush(LAG)
        flush(0)
        s.barrier()


def phase2a(g):
    nc, s, S = g.nc, g.s, g.S
    with ExitStack() as st:
        sb = lambda n, sh, dt: st.enter_context(nc.sbuf_tensor(n, sh, dt))
        pm = lambda n, sh, dt: st.enter_context(nc.psum_tensor(n, sh, dt))
        kTw = sb("p2a_kTw", [128, 4, 4096], BF16)
        qTsh = [sb(f"p2a_qTs{i}", [128, 4, 2048], BF16) for i in range(2)]
        ACC = sb("p2a_ACC", [128, 8, 2048], F32)
        V1 = [sb(f"p2a_V1{i}", [128, 9, 8, 128], BF16) for i in range(2)]
        band = sb("p2a_band", [128, 3, 256], BF16)
        pT = [sb(f"p2a_pT{i}", [128, 256], BF16) for i in range(4)]
        pTm = [sb(f"p2a_pTm{i}", [128, 256], BF16) for i in range(4)]
        rd = [sb(f"p2a_rd{i}", [64, 2048], F32) for i in range(2)]
        ob = [sb(f"p2a_ob{i}", [64, 2048], BF16) for i in range(2)]
        sp_ = [pm(f"p2a_sp{i}", [128, 512], F32) for i in range(4)]
        ot = [pm(f"p2a_ot{i}", [128, 512], F32) for i in range(4)]
        s.op('pool', lambda e: e.memset(qTsh[0][64:128, :, :], 0.0), writes=['qTs'])
        s.op('pool', lambda e: e.memset(qTsh[1][0:64, :, :], 0.0), writes=['qTs'])
        NEG = -30000.0
        for bi in range(3):
            bt = band[:, bi, :]
            s.op('pool', lambda e, bt=bt: e.memset(bt, 0.0), writes=['band'])
            s.op('pool', lambda e, bt=bt: e.affine_select(out=bt, in_=bt, pattern=[[1, 256]], compare_op=ALU.is_ge,
                                                          fill=NEG, base=0, channel_multiplier=-1),
                 reads=['band'], writes=['band'])
            s.op('pool', lambda e, bt=bt: e.affine_select(out=bt, in_=bt, pattern=[[-1, 256]], compare_op=ALU.is_ge,
                                                          fill=NEG, base=128, channel_multiplier=1),
                 reads=['band'], writes=['band'])
        s.op('pool', lambda e: e.memset(band[0:64, 1, :], NEG), reads=['band'], writes=['band'])
        s.op('pool', lambda e: e.memset(band[64:128, 2, :], NEG), reads=['band'], writes=['band'])
        for i in range(2):
            s.op('pool', lambda e, i=i: e.memset(V1[i][:, :, :, 0:64], 0.0), writes=[f'aV1{i}'])
            s.op('pool', lambda e, i=i: e.memset(V1[i][:, :, :, 64:128], 1.0), writes=[f'aV1{i}'])
        iu = 0
        ip = 0
        import os
        LAG = int(os.environ.get('LAGA', '2'))
        pend = []

        def flush(n):
            while len(pend) > n:
                pend.pop(0)()
        for j, Sj in enumerate(g.seqs):
            base = g.offs[j]
            for seg in range(Sj // 2048):
                seg0 = seg * 2048
                lo, hi = max(0, seg0 - 1024), min(Sj, seg0 + 3072)
                if lo > seg0 - 1024:
                    s.op('pool', lambda e: e.memset(kTw[:, :, 0:1024], 0.0), writes=['kTw'])
                if hi < seg0 + 3072:
                    s.op('pool', lambda e: e.memset(kTw[:, :, 3072:4096], 0.0), writes=['kTw'])
                s.dma('sp', 'p2a_kTw', kTw[:, :, lo - (seg0 - 1024):hi - (seg0 - 1024)],
                      S.kaT[:, base + lo:base + hi].rearrange("(c p) t -> p c t", p=128), writes=['kTw'])
                qsrc = S.qaT[:, base + seg0:base + seg0 + 2048].rearrange("(c p) t -> p c t", p=128)
                s.dma('sp', 'p2a_qTs', qTsh[0][0:64, :, :], qsrc[0:64], writes=['qTs'])
                s.dma('sp', 'p2a_qTs', qTsh[1][64:128, :, :], qsrc[64:128], writes=['qTs'])
                units = [(1, 0, 0, 8), (1, 0, 8, 8)] + [(4, r, 0, 4) for r in range(4)] + \
                        [(16, r, 0, 1) for r in range(16)]
                first_write = {}
                for (d, r, qb0, nqb) in units:
                    L = Sj // d
                    lq0 = seg0 // d + 128 * qb0
                    bv = iu % 2
                    iu += 1
                    vk = f'aV1{bv}'
                    v1 = V1[bv]
                    nkb = nqb + 1
                    m_lo, m_hi = 0, nkb
                    at_start = (lq0 == 0)
                    at_end = (lq0 + 128 * nqb == L)
                    if lq0 == 0:
                        tok = base + 0 * d + r
                        s.dma('sp', f'p2a_V1{bv}', v1[64:128, 0, :, 0:64],
                              S.vA[tok:tok + 63 * d + 1:d, :].rearrange("p (h e) -> p h e", e=64), writes=[vk])
                        m_lo = 1
                    if lq0 + 128 * nqb == L:
                        tok = base + (lq0 - 64 + 128 * nqb) * d + r
                        s.dma('sp', f'p2a_V1{bv}', v1[0:64, nqb, :, 0:64],
                              S.vA[tok:tok + 63 * d + 1:d, :].rearrange("p (h e) -> p h e", e=64), writes=[vk])
                        m_hi = nkb - 1
                    for m in range(m_lo, m_hi):
                        tok = base + (lq0 - 64 + 128 * m) * d + r
                        s.dma('sp', f'p2a_V1{bv}', v1[:, m, :, 0:64],
                              S.vA[tok:tok + 127 * d + 1:d, :].rearrange("p (h e) -> p h e", e=64), writes=[vk])
                    for h in range(8):
                        c, hh = h // 2, (h % 2) * 64
                        for m in range(nkb):
                            n_lo, n_hi = max(m - 1, 0), min(m, nqb - 1)
                            nq = n_hi - n_lo + 1
                            kc0 = 1024 + (128 * (qb0 + m) - 64) * d + r
                            qc0 = 128 * (qb0 + n_lo) * d + r
                            p = sp_[ip % 4][:, 0:256]
                            pn = f'asp{ip % 4}'
                            pt, ptm = pT[ip % 4], pTm[ip % 4]
                            ptn = f'apT{ip % 4}'
                            ip += 1
                            W = nq * 128
                            b0 = 128 if m == 0 else 0
                            bi = 1 if (m == 0 and at_start) else (2 if (m == nkb - 1 and at_end) else 0)

                            def smm(e, p=p, c=c, hh=hh, kc0=kc0, qc0=qc0, W=W, d=d, b0=b0, bi=bi):
                                e.matmul(p[:, 0:W], lhsT=kTw[:, c, kc0:kc0 + 127 * d + 1:d],
                                         rhs=qTsh[hh // 64][:, c, qc0:qc0 + (W - 1) * d + 1:d], start=True, stop=False)
                                return e.matmul(p[:, 0:W], lhsT=g.identB[:], rhs=band[:, bi, b0:b0 + W],
                                                start=False, stop=True)
                            s.op('pe', smm, reads=['kTw', 'qTs', 'band', 'identB'], writes=[pn])
                            s.op('act', lambda e, p=p, ptm=ptm, W=W: e.activation(out=ptm[:, 0:W], in_=p[:, 0:W],
                                                                                 func=AF.Exp, scale=0.125),
                                 reads=[pn], writes=[ptn + 'm'])

                            def stage2(n_lo=n_lo, n_hi=n_hi, h=h, v1=v1, vk=vk, m=m, ptm=ptm, ptn=ptn, d=d, r=r,
                                       qb0=qb0):
                                for n in range(n_lo, n_hi + 1):
                                    o = ot[n % 4][:, 0:128]
                                    on = f'aot{n % 4}'
                                    col = (n - n_lo) * 128
                                    s.op('pe', lambda e, o=o, col=col, n=n: e.matmul(
                                        o, lhsT=v1[:, m, h, :], rhs=ptm[:, col:col + 128],
                                        start=(m == n), stop=(m == n + 1)),
                                        reads=[vk, ptn + 'm'], writes=[on])
                                    if m == n + 1:
                                        a0 = 128 * (qb0 + n) * d + r
                                        av = ACC[:, h, a0:a0 + 127 * d + 1:d]
                                        if d == 1:
                                            s.op('dve', lambda e, o=o, av=av: e.tensor_copy(out=av, in_=o),
                                                 reads=[on], writes=[f'ACC{h}'])
                                        else:
                                            s.op('dve', lambda e, o=o, av=av: e.tensor_tensor(out=av, in0=o, in1=av,
                                                                                              op=ALU.add),
                                                 reads=[on, f'ACC{h}'], writes=[f'ACC{h}'])
                            pend.append(stage2)
                            flush(LAG)
                flush(0)
                for h in range(8):
                    bo = h % 2
                    s.op('act', lambda e, h=h, bo=bo: e.activation(out=rd[bo][:], in_=ACC[64:128, h, :], func=AF.Ln),
                         reads=[f'ACC{h}'], writes=[f'ard{bo}'])
                    s.op('act', lambda e, bo=bo: e.activation(out=rd[bo][:], in_=rd[bo][:], func=AF.Exp, scale=-1.0),
                         reads=[f'ard{bo}'], writes=[f'ard{bo}'])
                    s.op('pool', lambda e, h=h, bo=bo: e.tensor_tensor(out=ob[bo][:], in0=ACC[0:64, h, :],
                                                                      in1=rd[bo][:], op=ALU.mult),
                         reads=[f'ACC{h}', f'ard{bo}'], writes=[f'aob{bo}'])
                    t0 = base + seg0
                    s.dma('sp', f'p2a_ob{bo}', S.oT[h * 64:(h + 1) * 64, t0:t0 + 2048], ob[bo][:],
                          reads=[f'aob{bo}'])
        s.barrier()


def post_norm_residual(g, pre, yp, ypk, xres, xkey, GB, gbk, out, outkey, ss, rstd, junk, tmp, par=0):
    s = g.s
    sp = str(par)
    tm = tmp[par] if isinstance(tmp, (list, tuple)) else tmp
    s.op('act', lambda e: e.activation(out=junk[:], in_=yp[:, :], func=AF.Square, accum_out=ss[:, par:par + 1]),
         reads=[ypk], writes=[pre + 'junk', pre + 'ss' + sp])
    rsqrt_act(g, rstd[:, par:par + 1], ss[:, par:par + 1], 1.0 / D, [pre + 'ss' + sp], [pre + 'rstd' + sp])
    s.op('dve', lambda e: e.scalar_tensor_tensor(out=tm[:], in0=yp[:, :], scalar=rstd[:, par:par + 1], in1=GB[:],
                                                 op0=ALU.mult, op1=ALU.mult),
         reads=[ypk, pre + 'rstd' + sp, gbk], writes=[pre + 'tmp' + sp])
    s.op('pool' if par == 0 else 'dve', lambda e: e.tensor_tensor(out=out, in0=tm[:], in1=xres, op=ALU.add),
         reads=[pre + 'tmp' + sp, xkey], writes=[outkey])


def load_GB(g, pre, GB, gb, ngb, l, gi, j):
    s = g.s
    s.dma('sp', pre + 'gb', gb[:], g.S.gates[l, gi, j:j + 1, :].partition_broadcast(128), writes=[pre + 'gb'])
    s.op('dve', lambda e: e.tensor_tensor(out=GB[:], in0=gb[:], in1=ngb[:], op=ALU.mult),
         reads=[pre + 'gb', pre + 'ngb'], writes=[pre + 'GB'])


def phase3(g, l, oT_d, w_out_d, xin_d, xout_d):
    nc, s = g.nc, g.s
    pre = f'p3{l}'
    with ExitStack() as st:
        sb = lambda n, sh, dt: st.enter_context(nc.sbuf_tensor(pre + n, sh, dt))
        pm = lambda n, sh, dt: st.enter_context(nc.psum_tensor(pre + n, sh, dt))
        wo = sb("wo", [128, 8, D], BF16)
        oT = [sb(f"oT{i}", [128, 8, 512], BF16) for i in range(2)]
        xt = [sb(f"xt{i}", [128, 4, D], F32) for i in range(2)]
        xo = [sb(f"xo{i}", [128, 4, D], F32) for i in range(2)]
        gb = sb("gb", [128, D], F32)
        ngb = sb("ngb", [128, D], F32)
        GB = sb("GB", [128, D], F32)
        junk = sb("junk", [128, D], BF16)
        tmp = [sb(f"tmp{i}", [128, D], F32) for i in range(2)]
        ss = sb("ss", [128, 2], F32)
        rstd = sb("rstd", [128, 2], F32)
        yp = [pm(f"yp{i}", [128, D], F32) for i in range(2)]
        load_w_bf16(g, pre + 'wo', wo, w_out_d, 8, D)
        s.dma('sp', pre + 'ngb', ngb[:], g.I.norm_g[l * 4 + 1:l * 4 + 2, :].partition_broadcast(128),
              writes=[pre + 'ngb'])
        tiles = []
        for j, Sj in enumerate(g.seqs):
            for t in range(Sj // 512):
                tiles.append((j, g.offs[j] + t * 512))

        def load(i):
            j, t0 = tiles[i]
            b = i % 2
            s.dma('sp', pre + f'oT{b}', oT[b][:], oT_d[:, t0:t0 + 512].rearrange("(c p) t -> p c t", p=128),
                  writes=[pre + f'oT{b}'])
            s.dma('sp', pre + f'xt{b}', xt[b][:], xin_d[t0:t0 + 512, :].rearrange("(b p) d -> p b d", p=128),
                  writes=[pre + f'xt{b}'])
        load(0)
        curj = -1
        iy = 0
        for i, (j, t0) in enumerate(tiles):
            b = i % 2
            if i + 1 < len(tiles):
                load(i + 1)
            if j != curj:
                load_GB(g, pre, GB, gb, ngb, l, 0, j)
                curj = j
            for blk in range(4):
                y_ = yp[iy % 2]
                yk = pre + f'yp{iy % 2}'
                iy += 1

                def mm(e, y_=y_, b=b, blk=blk):
                    for nh in range(2):
                        for k in range(8):
                            r = e.matmul(y_[:, nh * 512:(nh + 1) * 512], lhsT=oT[b][:, k, blk * 128:(blk + 1) * 128],
                                         rhs=wo[:, k, nh * 512:(nh + 1) * 512], start=(k == 0), stop=(k == 7))
                    return r
                s.op('pe', mm, reads=[pre + f'oT{b}', pre + 'wo'], writes=[yk])
                post_norm_residual(g, pre, y_, yk, xt[b][:, blk, :], pre + f'xt{b}', GB, pre + 'GB',
                                   xo[b][:, blk, :], pre + f'xo{b}', ss, rstd, junk, tmp, par=(iy % 2))
            s.dma('sp', pre + f'xo{b}', xout_d[t0:t0 + 512, :].rearrange("(b p) d -> p b d", p=128), xo[b][:],
                  reads=[pre + f'xo{b}'])
        s.barrier()


def phase4(g, l, xin_d, xout_d):
    nc, s = g.nc, g.s
    pre = f'p4{l}'
    with ExitStack() as st:
        sb = lambda n, sh, dt: st.enter_context(nc.sbuf_tensor(pre + n, sh, dt))
        pm = lambda n, sh, dt: st.enter_context(nc.psum_tensor(pre + n, sh, dt))
        w1 = sb("w1", [128, 8, 4 * D], BF16)
        w2 = sb("w2", [128, 32, D], BF16)
        xt = [sb(f"xt{i}", [128, 2, D], F32) for i in range(2)]
        xn = sb("xn", [128, 2, D], BF16)
        xo = [sb(f"xo{i}", [128, 2, D], F32) for i in range(2)]
        hfT = sb("hfT", [128, 8, 256], BF16)
        h1a = sb("h1a", [128, 2, 256], BF16)
        h1T = sb("h1T", [128, 32, 256], BF16)
        ngb = sb("ngb", [128, D], F32)
        GB = sb("GB", [128, D], F32)
        junk = sb("junk", [128, D], BF16)
        tmp = [sb(f"tmp{i}", [128, D], F32) for i in range(2)]
        ss = sb("ss", [128, 4], F32)
        rstd = sb("rstd", [128, 4], F32)
        ss2 = sb("ss2", [128, 2], F32)
        rstd2 = sb("rstd2", [128, 2], F32)
        tps = [pm("tp0", [128, 8, 256], BF16)]
        hp = [pm(f"hp{i}", [128, 512], F32) for i in range(2)]
        yp = [pm(f"yp{i}", [128, D], F32) for i in range(2)]
        load_w_bf16(g, pre + 'w1', w1, g.I.w_ff1[l], 8, 4 * D)
        load_w_bf16(g, pre + 'w2', w2, g.I.w_ff2[l], 32, D)
        s.dma('sp', pre + 'ngb', ngb[:], g.I.norm_g[l * 4 + 3:l * 4 + 4, :].partition_broadcast(128),
              writes=[pre + 'ngb'])
        tiles = []
        for j, Sj in enumerate(g.seqs):
            for t in range(Sj // 256):
                tiles.append((j, g.offs[j] + t * 256))

        def load(i):
            j, t0 = tiles[i]
            b = i % 2
            s.dma('sp', pre + f'xt{b}', xt[b][:], xin_d[t0:t0 + 256, :].rearrange("(b p) d -> p b d", p=128),
                  writes=[pre + f'xt{b}'])
        load(0)
        curj = -1
        ih = 0
        iy = 0
        def prep(i):
            j, t0 = tiles[i]
            b = i % 2
            rms_prep(g, pre, xt[b], xn, ss, rstd, junk, 2, pre + f'xt{b}', pre + 'xn')
            to_featmajor(g, pre, xn, hfT, tps, l, 1, j, 2, pre + 'xn', pre + 'hfT')
        prep(0)
        for i, (j, t0) in enumerate(tiles):
            b = i % 2
            if i + 1 < len(tiles):
                load(i + 1)
            if j != curj:
                s.dma('sp', pre + 'GB', GB[:], g.S.gates[l, 1, j:j + 1, :].partition_broadcast(128),
                      writes=[pre + 'GB'])
                s.op('pool', lambda e: e.tensor_tensor(out=GB[:], in0=GB[:], in1=ngb[:], op=ALU.mult),
                     reads=[pre + 'GB', pre + 'ngb'], writes=[pre + 'GB'])
                curj = j
            for fc in range(32):
                p = hp[ih % 2]
                pn = pre + f'hp{ih % 2}'
                ha = h1a[:, ih % 2, :]
                hak = pre + f'h1a{ih % 2}'
                ih += 1

                def mm(e, p=p, fc=fc):
                    for k in range(8):
                        r = e.matmul(p[:, 0:256], lhsT=w1[:, k, fc * 128:(fc + 1) * 128], rhs=hfT[:, k, :],
                                     start=(k == 0), stop=(k == 7))
                    return r
                s.op('pe', mm, reads=[pre + 'w1', pre + 'hfT'], writes=[pn])
                s.op('act', lambda e, p=p, ha=ha: e.activation(out=ha, in_=p[:, 0:256], func=AF.Relu),
                     reads=[pn], writes=[hak])
                eng = 'pool' if fc % 2 == 0 else 'dve'
                s.op(eng, lambda e, ha=ha, fc=fc: e.tensor_tensor(out=h1T[:, fc, :], in0=ha, in1=ha, op=ALU.mult),
                     reads=[hak], writes=[pre + f'h1T{fc}'])
            if i + 1 < len(tiles):
                prep(i + 1)
            for blk in range(2):
                y_ = yp[iy % 2]
                yk = pre + f'yp{iy % 2}'
                iy += 1

                def mm2(e, y_=y_, blk=blk):
                    for nh in range(2):
                        for k in range(32):
                            r = e.matmul(y_[:, nh * 512:(nh + 1) * 512], lhsT=h1T[:, k, blk * 128:(blk + 1) * 128],
                                         rhs=w2[:, k, nh * 512:(nh + 1) * 512], start=(k == 0), stop=(k == 31))
                    return r
                s.op('pe', mm2, reads=[pre + f'h1T{fc}' for fc in range(32)] + [pre + 'w2'], writes=[yk])
                post_norm_residual(g, pre + 'b', y_, yk, xt[b][:, blk, :], pre + f'xt{b}', GB, pre + 'GB',
                                   xo[b][:, blk, :], pre + f'xo{b}', ss2, rstd2, junk, tmp, par=(iy % 2))
            s.dma('sp', pre + f'xo{b}', xout_d[t0:t0 + 256, :].rearrange("(b p) d -> p b d", p=128), xo[b][:],
                  reads=[pre + f'xo{b}'])
        s.barrier()


def _rope_angles(pos, dim):
    inv_freq = (np.float32(10000.0) ** (-np.arange(0, dim, 2, dtype=np.float32) / np.float32(dim))).astype(np.float32)
    return (pos.astype(np.float32)[:, None] * inv_freq[None, :]).astype(np.float32)


def rope_tables(S):
    pos = np.arange(S)
    angA = _rope_angles(pos, 64)
    ar = _rope_angles(pos // 64, 32)
    ac = _rope_angles(pos % 64, 32)
    ropeA = np.concatenate([np.cos(angA), np.sin(angA)], axis=1).astype(np.float32)
    ropeB = np.concatenate([np.cos(ar), np.cos(ac), np.sin(ar), np.sin(ac)], axis=1).astype(np.float32)
    return ropeA, ropeB


def core_map(inp, xs, cs):
    NS = len(xs)
    SMAX = max(x.shape[0] for x in xs)
    ropeA, ropeB = rope_tables(SMAX)
    c = np.stack(cs)
    cT = c.reshape(NS, 8, 128).transpose(2, 1, 0).reshape(128, 8 * NS)
    ng = np.asarray(inp['norm_g'])
    m = {
        'x': np.concatenate(xs, 0), 'cT': cT,
        'w_in_ab': inp['w_in_ab'][0], 'w_out_ab': inp['w_out_ab'][0], 'w_in_cd': inp['w_in_cd'][0],
        'w_out_cd': inp['w_out_cd'][0], 'w_ada': inp['w_ada'], 'b_ada': np.asarray(inp['b_ada']).reshape(1, -1),
        'normgT': ng.reshape(2, 4, 8, 128).transpose(3, 0, 1, 2).reshape(128, 64),
        'norm_g': ng.reshape(8, 1024), 'w_ff1': inp['w_ff1'], 'w_ff2': inp['w_ff2'],
        'qkg': np.asarray(inp['qk_norm_g']).reshape(1, 128), 'ropeA': ropeA, 'ropeB': ropeB,
        'gate_bias': np.asarray(inp['gate_bias']).reshape(32, 1), 'mhg': np.asarray(inp['mh_norm_g']).reshape(1, 512),
        'sgg': np.asarray(inp['sg_norm_g']).reshape(1, 512),
        'wsT': np.asarray(inp['w_spatial'])[0].transpose(2, 0, 1).reshape(128, 1024),
        'bsT': np.asarray(inp['b_spatial'])[0].T,
    }
    return {k: np.ascontiguousarray(np.asarray(v), dtype=np.float32) for k, v in m.items()}


ALL_PHASES = (0, 1, 2, 3, 4, 5, 6, 7, 8)
_CACHE = {}


def kernel(**inputs):
    inp = {k: np.asarray(v) for k, v in inputs.items()}
    n = 8
    seqs = [8192, 2048, 2048, 2048, 2048]
    if 'nc' not in _CACHE:
        _CACHE['nc'] = build(seqs, set(ALL_PHASES), debug=False)
    nc, g = _CACHE['nc']
    in_maps = []
    for i in range(n):
        xs = [inp['x_prompt'][i]] + [inp['x_sample'][4 * i + k] for k in range(4)]
        cs = [inp['c_prompt'][i]] + [inp['c_sample'][4 * i + k] for k in range(4)]
        in_maps.append(core_map(inp, xs, cs))
    res = run_bass_kernel_spmd(nc, in_maps, core_ids=list(range(n)))
    y_prompt = np.empty((8, 8192, 1024), np.float32)
    y_sample = np.empty((32, 2048, 1024), np.float32)
    for i in range(n):
        y = np.asarray(res.results[i]['y'])
        y_prompt[i] = y[0:8192]
        for k in range(4):
            y_sample[4 * i + k] = y[8192 + 2048 * k:8192 + 2048 * (k + 1)]
    return (y_prompt, y_sample)


def phase5(g):
    nc, s, NS, I, S = g.nc, g.s, g.NS, g.I, g.S
    with ExitStack() as st:
        sb = lambda n, sh, dt: st.enter_context(nc.sbuf_tensor("p5_" + n, sh, dt))
        pm = lambda n, sh, dt: st.enter_context(nc.psum_tensor("p5_" + n, sh, dt))
        wcd = sb("wcd", [128, 8, 3104], BF16)
        wsT = sb("wsT", [128, 8, 128], BF16)
        bsT = sb("bsT", [128, 8], F32)
        sgg = sb("sgg", [128, 512], F32)
        gbias = sb("gbias", [128, 32], F32)
        xt = [sb(f"xt{i}", [128, 4, D], F32) for i in range(2)]
        xn = sb("xn", [128, 4, D], BF16)
        junk = sb("junk", [128, D], BF16)
        ss = sb("ss", [128, 4], F32)
        rstd = sb("rstd", [128, 4], F32)
        hmT = sb("hmT", [128, 8, 512], BF16)
        qk = [sb(f"qk{i}", [128, 1536], BF16) for i in range(2)]
        vst = [sb(f"vst{i}", [128, 4, 512], BF16) for i in range(2)]
        kst = [sb(f"kst{i}", [128, 4, 512], BF16) for i in range(2)]
        ogst = [sb(f"ogst{i}", [128, 4, 512], F32) for i in range(2)]
        gst = [sb(f"gst{i}", [128, 4, 32], F32) for i in range(2)]
        stg = [sb(f"stg{i}", [128, 12, 512], BF16) for i in range(2)]
        ugs = [sb(f"ug{i}", [128, 512], F32) for i in range(2)]
        vg = sb("vg", [128, 512], F32)
        vc = sb("vc", [128, 512], F32)
        vln = sb("vln", [128, 512], BF16)
        sgt = sb("sgt", [128, 512], F32)
        lns = sb("lns", [128, 4], F32)
        tps = [pm("tp0", [128, 8, 256], BF16)]
        zp = [pm(f"zp{i}", [128, 512], F32) for i in range(2)]
        qT = pm("qT", [128, 16, 128], BF16)
        sgp = pm("sgp", [128, 512], F32)
        for k in range(8):
            for (d0, s0, w) in ((0, 0, 1024), (1024, 1024, 1024), (2048, 2080, 512), (2560, 2592, 512),
                                (3072, 2048, 32)):
                s.dma('pool', 'p5_wcd', wcd[:, k, d0:d0 + w], I.w_in_cd[k * 128:(k + 1) * 128, s0:s0 + w],
                      writes=['wcd'])
        s.dma('pool', 'p5_wsT', wsT[:].rearrange("q g p -> q (g p)"), I.wsT[:, :], writes=['wsT'])
        s.dma('sp', 'p5_bsT', bsT[:], I.bsT[:, :], writes=['bsT'])
        s.dma('sp', 'p5_sgg', sgg[:], I.sgg[0:1, :].partition_broadcast(128), writes=['sgg'])
        s.dma('sp', 'p5_gbias', gbias[:], I.gate_bias.rearrange("a b -> b a")[0:1, :].partition_broadcast(128),
              writes=['gbias'])
        tiles = []
        for j, Sj in enumerate(g.seqs):
            for t in range(Sj // 512):
                tiles.append((j, g.offs[j] + t * 512))

        def load(i):
            j, t0 = tiles[i]
            b = i % 2
            s.dma('sp', f'p5_xt{b}', xt[b][:], S.x1[t0:t0 + 512, :].rearrange("(b p) d -> p b d", p=128),
                  writes=[f'xt{b}'])
        load(0)
        zi = 0
        pend = []
        for i, (j, t0) in enumerate(tiles):
            b = i % 2
            if i + 1 < len(tiles):
                load(i + 1)
            rms_prep(g, 'p5', xt[b], xn, ss, rstd, junk, 4, f'xt{b}', 'xn')
            to_featmajor(g, 'p5', xn, hmT, tps, 1, 0, j, 4, 'xn', 'hmT')
            for blk in range(4):
                q = qk[blk % 2]
                qkk = f'qk{blk % 2}'
                if blk > 0:
                    pass
                for c in range(7):
                    n0 = c * 512
                    nw = min(512, 3104 - n0)
                    p = zp[zi % 2]
                    pn = f'zp{zi % 2}'
                    zi += 1

                    def mm(e, p=p, n0=n0, nw=nw, blk=blk):
                        for k in range(8):
                            r = e.matmul(p[:, 0:nw], lhsT=hmT[:, k, blk * 128:(blk + 1) * 128],
                                         rhs=wcd[:, k, n0:n0 + nw], start=(k == 0), stop=(k == 7))
                        return r
                    s.op('pe', mm, reads=['hmT', 'wcd'], writes=[pn])
                    if c == 0:
                        s.op('act', lambda e, p=p, q=q: e.activation(out=q[:, 0:512], in_=p[:, :], func=AF.Copy),
                             reads=[pn], writes=[qkk + 'q'])
                    elif c == 1:
                        s.op('act', lambda e, p=p, q=q: e.activation(out=q[:, 512:1024], in_=p[:, :], func=AF.Copy,
                                                                     scale=0.125), reads=[pn], writes=[qkk + 'k'])
                        s.op('pool', lambda e, q=q, blk=blk, b=b: e.tensor_copy(out=kst[b][:, blk, :],
                                                                               in_=q[:, 512:1024]),
                             reads=[qkk + 'k'], writes=[f'kst{b}'])
                    elif c == 2:
                        s.op('act', lambda e, p=p, blk=blk, b=b: e.activation(out=vst[b][:, blk, :], in_=p[:, :],
                                                                              func=AF.Copy),
                             reads=[pn], writes=[f'vst{b}'])
                    elif c == 3:
                        s.op('act', lambda e, p=p, blk=blk, b=b: e.activation(out=ogst[b][:, blk, :], in_=p[:, :],
                                                                              func=AF.Sigmoid),
                             reads=[pn], writes=[f'ogst{b}'])
                    elif c == 4:
                        s.op('act', lambda e, p=p, blk=blk: e.activation(out=ugs[blk % 2][:], in_=p[:, :],
                                                                         func=AF.Gelu_apprx_tanh),
                             reads=[pn], writes=[f'ug{blk % 2}'])
                    elif c == 5:
                        s.op('act', lambda e, p=p: e.activation(out=vg[:], in_=p[:, :], func=AF.Gelu_apprx_tanh,
                                                                accum_out=lns[:, 0:1]),
                             reads=[pn], writes=['vg', 'lns0'])
                    else:
                        s.op('dve', lambda e, p=p, blk=blk, b=b: e.tensor_tensor(
                            out=gst[b][:, blk, :], in0=p[:, 0:32], in1=gbias[:], op=ALU.add),
                            reads=[pn, 'gbias'], writes=[f'gst{b}'])
                while pend:
                    pend.pop(0)()
                s.op('dve', lambda e: e.tensor_scalar(out=lns[:, 1:2], in0=lns[:, 0:1], scalar1=-1.0 / 512,
                                                      scalar2=None, op0=ALU.mult), reads=['lns0'], writes=['lns1'])
                s.op('act', lambda e: e.activation(out=vc[:], in_=vg[:], func=AF.Identity, bias=lns[:, 1:2]),
                     reads=['vg', 'lns1'], writes=['vc'])
                s.op('act', lambda e: e.activation(out=junk[:, 0:512], in_=vc[:], func=AF.Square,
                                                   accum_out=lns[:, 2:3]), reads=['vc'], writes=['lns2', 'p5junk'])
                rsqrt_act(g, lns[:, 3:4], lns[:, 2:3], 1.0 / 512, ['lns2'], ['lns3'])
                s.op('dve', lambda e: e.scalar_tensor_tensor(out=vln[:], in0=vc[:], scalar=lns[:, 3:4], in1=sgg[:],
                                                             op0=ALU.mult, op1=ALU.mult),
                     reads=['vc', 'lns3', 'sgg'], writes=['vln'])

                def tail(q=q, qkk=qkk, blk=blk, b=b):
                    def sgm(e):
                        for grp in range(8):
                            r = e.matmul(sgp[:, grp * 64:(grp + 1) * 64], lhsT=wsT[:, grp, :],
                                         rhs=vln[:, grp * 64:(grp + 1) * 64], start=True, stop=True)
                        return r
                    s.op('pe', sgm, reads=['wsT', 'vln'], writes=['sgp'])
                    s.op('dve', lambda e: e.tensor_tensor(out=sgt[:].rearrange("p (g e) -> p g e", e=64),
                                                          in0=sgp[:, :].rearrange("p (g e) -> p g e", e=64),
                                                          in1=bsT[:, :].unsqueeze(2).to_broadcast([128, 8, 64]),
                                                          op=ALU.add), reads=['sgp', 'bsT'], writes=['sgt'])
                    s.op('pool', lambda e, q=q: e.tensor_tensor(out=q[:, 1024:1536], in0=sgt[:], in1=ugs[blk % 2][:],
                                                                op=ALU.mult),
                         reads=['sgt', f'ug{blk % 2}'], writes=[qkk + 'd'])

                    def trq(e, q=q):
                        for c in range(12):
                            r = e.transpose(out=qT[:, c, :], in_=q[:, c * 128:(c + 1) * 128], identity=g.identB[:])
                        return r
                    s.op('pe', trq, reads=[qkk + 'q', qkk + 'k', qkk + 'd', 'identB'], writes=['qT'])
                    s.op('act', lambda e, blk=blk, b=b: e.activation(out=stg[b][:, :, blk * 128:(blk + 1) * 128],
                                                                     in_=qT[:, 0:12, :], func=AF.Copy),
                         reads=['qT'], writes=[f'stg{b}'])

                pend.append(tail)
            while pend:
                pend.pop(0)()
            for (dst, r0, c0, n) in ((S.qcT, 0, 0, 4), (S.kcT, 0, 4, 4), (S.o2T, 512, 8, 4)):
                s.dma('sp', f'p5_stg{b}', dst[r0:r0 + 512, t0:t0 + 512].rearrange("(c p) t -> p c t", p=128),
                      stg[b][:, c0:c0 + n, :], reads=[f'stg{b}'])
            tm = lambda d: d[t0:t0 + 512, :].rearrange("(b p) d -> p b d", p=128)
            s.dma('sp', f'p5_vst{b}', tm(S.vc), vst[b][:], reads=[f'vst{b}'])
            s.dma('sp', f'p5_kst{b}', tm(S.kc), kst[b][:], reads=[f'kst{b}'])
            s.dma('sp', f'p5_ogst{b}', tm(S.ogs), ogst[b][:], reads=[f'ogst{b}'])
            s.dma('sp', f'p5_gst{b}', tm(S.gtok), gst[b][:], reads=[f'gst{b}'])
        s.barrier()


def phase6(g):
    nc, s, I, S = g.nc, g.s, g.I, g.S
    SMAX = max(g.seqs)
    NCH = SMAX // 128
    with ExitStack() as st:
        sb = lambda n, sh, dt: st.enter_context(nc.sbuf_tensor("p6_" + n, sh, dt))
        pm = lambda n, sh, dt: st.enter_context(nc.psum_tensor("p6_" + n, sh, dt))
        SU = sb("SU", [128, 128], F32)
        SL = sb("SL", [128, 128], F32)
        ONES = sb("ONES", [128, 128], F32)
        maskF = sb("maskF", [128, 128], BF16)
        maskB = sb("maskB", [128, 128], BF16)
        mhg = sb("mhg", [128, 512], F32)
        gz = sb("gz", [128, NCH, 32], F32)
        l1 = [sb(f"l1{d}", [128, NCH * 8], F32) for d in range(2)]
        tmpg = sb("tmpg", [128, NCH * 8], F32)
        E = [sb(f"E{d}", [128, NCH * 8], F32) for d in range(2)]
        THR = [sb(f"THR{d}", [128, NCH * 8], F32) for d in range(2)]
        DEC = [sb(f"DEC{d}", [128, NCH * 8], F32) for d in range(2)]
        QTh = [sb(f"QTh{i}", [128, SMAX], BF16) for i in range(2)]
        KT = sb("KT", [128, SMAX], BF16)
        Ktok = sb("Ktok", [128, NCH, 128], BF16)
        V1 = sb("V1", [128, NCH, 2, 65], BF16)
        OGS = sb("OGS", [128, NCH, 128], F32)
        HF = sb("HF", [128, NCH, 128], F32)
        Wst = [sb(f"W{d}", [128, 65], F32) for d in range(2)]
        U = [sb(f"U{i}", [128, 65], BF16) for i in range(4)]
        Kw = [sb(f"Kw{i}", [128, 64], BF16) for i in range(4)]
        PT = [sb(f"PT{i}", [128, 128], BF16) for i in range(4)]
        rr = [sb(f"rr{i}", [128, 2], F32) for i in range(4)]
        sqt = sb("sqt", [128, 4, 128], F32)
        ssq = sb("ssq", [128, 8], F32)
        hn = sb("hn", [128, 4, 128], F32)
        hb = sb("hb", [128, 4, 128], BF16)
        stg = [sb(f"stg{i}", [128, 512], BF16) for i in range(2)]
        Bk = [pm(f"Bk{i}", [128, 512], F32) for i in range(6)]
        GP = Bk[0:4]
        trP = pm("trP", [128, 4, 128], BF16)
        DECp = [sb(f"DECp{d}", [128, NCH, 4], F32) for d in range(2)]
        Upair = [sb(f"Up{i}", [128, 65], BF16) for i in range(4)]
        Kwp = [sb(f"Kwp{i}", [128, 2, 64], BF16) for i in range(4)]
        PTp = [sb(f"PTp{i}", [128, 2, 128], BF16) for i in range(4)]
        rp = [sb(f"rp{i}", [128, 4], F32) for i in range(4)]
        htmp = [sb(f"htmp{i}", [128, 2, 64], F32) for i in range(2)]

        def tri(t, nm, cm, step, base):
            s.op('pool', lambda e: e.memset(t[:], 1.0), writes=[nm])
            s.op('pool', lambda e: e.affine_select(out=t[:], in_=t[:], pattern=[[step, 128]], compare_op=ALU.is_ge,
                                                   fill=0.0, base=base, channel_multiplier=cm),
                 reads=[nm], writes=[nm])
        tri(SU, 'SU', 1, -1, -1)
        tri(SL, 'SL', -1, 1, -1)
        tri(maskF, 'maskF', -1, 1, 0)
        tri(maskB, 'maskB', 1, -1, 0)
        s.op('pool', lambda e: e.memset(ONES[:], 1.0), writes=['ONES'])
        s.op('pool', lambda e: e.memset(QTh[0][64:128, :], 0.0), writes=['QT'])
        s.op('pool', lambda e: e.memset(QTh[1][0:64, :], 0.0), writes=['QT'])
        s.op('pool', lambda e: e.memset(V1[:, :, :, 64:65], 1.0), writes=['V1'])
        s.dma('sp', 'p6_mhg', mhg[:], I.mhg[0:1, :].partition_broadcast(128), writes=['mhg'])
        islot = 0
        istg = 0
        for j, Sj in enumerate(g.seqs):
            base = g.offs[j]
            nch = Sj // 128
            N8 = nch * 8
            s.dma('sp', 'p6_gz', gz[:, 0:nch, :], S.gtok[base:base + Sj, :].rearrange("(c p) d -> p c d", p=128),
                  writes=['gz'])
            for d in range(2):
                fcol = 8 + 16 * d
                icol = 16 * d
                l1v = l1[d][:, 0:N8].rearrange("p (c h) -> p c h", h=8)
                s.op('act', lambda e, nch=nch, N8=N8, l1v=l1v, fcol=fcol: e.activation(out=l1v, in_=gz[:, 0:nch, fcol:fcol + 8],
                                                                       func=AF.Exp, scale=-1.0),
                     reads=['gz'], writes=[f'l1{d}'])
                s.op('act', lambda e, nch=nch, N8=N8, d=d: e.activation(out=l1[d][:, 0:N8], in_=l1[d][:, 0:N8], func=AF.Ln, bias=1.0),
                     reads=[f'l1{d}'], writes=[f'l1{d}'])
                Gp, Tp = GP[2 * d], GP[2 * d + 1]
                tri_m = SU if d == 0 else SL
                s.op('pe', lambda e, nch=nch, N8=N8, Gp=Gp, tri_m=tri_m, d=d: e.matmul(Gp[:, 0:N8], lhsT=tri_m[:], rhs=l1[d][:, 0:N8],
                                                                      start=True, stop=True),
                     reads=['SU', 'SL', f'l1{d}'], writes=[f'B{2 * d}'])
                s.op('pe', lambda e, nch=nch, N8=N8, Tp=Tp, d=d: e.matmul(Tp[:, 0:N8], lhsT=ONES[:], rhs=l1[d][:, 0:N8],
                                                         start=True, stop=True),
                     reads=['ONES', f'l1{d}'], writes=[f'B{2 * d + 1}'])
                s.op('dve', lambda e, nch=nch, N8=N8, Gp=Gp, icol=icol: e.tensor_tensor(
                    out=tmpg[:, 0:N8].rearrange("p (c h) -> p c h", h=8), in0=gz[:, 0:nch, icol:icol + 8],
                    in1=Gp[:, 0:N8].rearrange("p (c h) -> p c h", h=8), op=ALU.subtract),
                    reads=['gz', f'B{2 * d}'], writes=['tmpg'])
                s.op('act', lambda e, nch=nch, N8=N8, d=d: e.activation(out=E[d][:, 0:N8], in_=tmpg[:, 0:N8], func=AF.Exp),
                     reads=['tmpg'], writes=[f'E{d}'])
                s.op('act', lambda e, nch=nch, N8=N8, d=d, Gp=Gp: e.activation(out=THR[d][:, 0:N8], in_=Gp[:, 0:N8], func=AF.Exp,
                                                              scale=-1.0), reads=[f'B{2 * d}'], writes=[f'THR{d}'])
                s.op('act', lambda e, nch=nch, N8=N8, d=d, Tp=Tp: e.activation(out=DEC[d][:, 0:N8], in_=Tp[:, 0:N8], func=AF.Exp,
                                                              scale=-1.0), reads=[f'B{2 * d + 1}'], writes=[f'DEC{d}'])
            for d in range(2):
                for hh in range(2):
                    p0 = hh * 64
                    s.op('pool', lambda e, nch=nch, N8=N8, d=d, hh=hh, p0=p0: e.tensor_copy(
                        out=DECp[d][p0:p0 + 64, 0:nch, :],
                        in_=DEC[d][p0:p0 + 64, 0:N8].rearrange("p (c hp two) -> p c hp two", hp=4, two=2)[:, :, :, hh]),
                        reads=[f'DEC{d}'], writes=[f'DECp{d}'])
            for hp in range(4):
                r0 = hp * 128
                s.dma('sp', 'p6_QT', QTh[0][0:64, 0:Sj], S.qcT[r0:r0 + 64, base:base + Sj], writes=['QT'])
                s.dma('sp', 'p6_QT', QTh[1][64:128, 0:Sj], S.qcT[r0 + 64:r0 + 128, base:base + Sj], writes=['QT'])
                s.dma('sp', 'p6_KT', KT[:, 0:Sj], S.kcT[r0:r0 + 128, base:base + Sj], writes=['KT'])
                s.dma('sp', 'p6_Ktok', Ktok[:, 0:nch, :],
                      S.kc[base:base + Sj, r0:r0 + 128].rearrange("(c p) d -> p c d", p=128), writes=['Ktok'])
                for hh in range(2):
                    s.dma('sp', 'p6_V1', V1[:, 0:nch, hh, 0:64],
                          S.vc[base:base + Sj, r0 + hh * 64:r0 + hh * 64 + 64].rearrange("(c p) e -> p c e", p=128),
                          writes=['V1'])
                s.dma('sp', 'p6_OGS', OGS[:, 0:nch, :],
                      S.ogs[base:base + Sj, r0:r0 + 128].rearrange("(c p) d -> p c d", p=128), writes=['OGS'])
                pend = []

                def flush(n):
                    while len(pend) > n:
                        pend.pop(0)()
                for ci in range(nch):
                    for d in range(2):
                        c = ci if d == 0 else nch - 1 - ci
                        first = (ci == 0)
                        last = (ci == nch - 1)
                        mask = maskF if d == 0 else maskB
                        sl = islot % 4
                        bkp = islot % 2
                        islot += 1
                        col0 = c * 8 + hp * 2
                        cs = slice(c * 128, (c + 1) * 128)
                        spb, Ob, dUb = Bk[bkp], Bk[2 + bkp], Bk[4 + bkp]
                        spk, Ok, dUk = f'B{bkp}', f'B{2 + bkp}', f'B{4 + bkp}'

                        def st1(spb=spb, spk=spk, cs=cs, sl=sl, c=c, d=d, col0=col0, mask=mask):
                            def mm(e):
                                for hh in range(2):
                                    p0 = hh * 64
                                    r = e.matmul(spb[:, hh * 128:(hh + 1) * 128], lhsT=KT[:, cs],
                                                 rhs=QTh[hh][:, cs], start=True, stop=True)
                                return r
                            s.op('pe', mm, reads=['KT', 'QT'], writes=[spk])
                            for hh in range(2):
                                s.op('act', lambda e, hh=hh: e.activation(
                                    out=Kwp[sl][:, hh, :], in_=Ktok[:, c, hh * 64:(hh + 1) * 64], func=AF.Copy,
                                    scale=E[d][:, col0 + hh:col0 + hh + 1]),
                                    reads=['Ktok', f'E{d}'], writes=[f'Kwp{sl}'])
                            for hh in range(2):
                                s.op('dve', lambda e, hh=hh: e.scalar_tensor_tensor(
                                    out=PTp[sl][:, hh, :], in0=spb[:, hh * 128:(hh + 1) * 128],
                                    scalar=E[d][:, col0 + hh:col0 + hh + 1], in1=mask[:], op0=ALU.mult, op1=ALU.mult),
                                    reads=[spk, f'E{d}', 'maskF', 'maskB'], writes=[f'PTp{sl}'])

                        firstw = (ci < nch // 2)

                        def st2(Ob=Ob, Ok=Ok, dUb=dUb, dUk=dUk, cs=cs, sl=sl, c=c, d=d, col0=col0, first=first,
                                last=last, hp=hp, firstw=firstw):
                            wk = f'W{d}'
                            u_ = Upair[sl]
                            if not first:
                                s.op('act', lambda e: e.activation(out=u_[:], in_=Wst[d][:], func=AF.Copy,
                                                                   scale=DECp[d][:, c, hp:hp + 1]),
                                     reads=[wk, f'DECp{d}'], writes=[f'Up{sl}'])

                            def omm(e):
                                for hh in range(2):
                                    p0 = hh * 64
                                    r = e.matmul(Ob[:, hh * 128:hh * 128 + 65], lhsT=PTp[sl][:, hh, :],
                                                 rhs=V1[:, c, hh, :], start=True, stop=first)
                                    if not first:
                                        r = e.matmul(Ob[:, hh * 128:hh * 128 + 65], lhsT=QTh[hh][:, cs],
                                                     rhs=u_[:, :], start=False, stop=True)
                                return r
                            s.op('pe', omm, reads=[f'PTp{sl}', 'V1', 'QT', f'Up{sl}'], writes=[Ok])
                            if not last:
                                def dmm(e):
                                    for hh in range(2):
                                        p0 = hh * 64
                                        r = e.matmul(dUb[p0:p0 + 64, 0:65], lhsT=Kwp[sl][:, hh, :],
                                                     rhs=V1[:, c, hh, :], start=True, stop=True)
                                    return r
                                s.op('pe', dmm, reads=[f'Kwp{sl}', 'V1'], writes=[dUk])
                                if first:
                                    s.op('dve', lambda e: e.tensor_copy(out=Wst[d][:], in_=dUb[:, 0:65]),
                                         reads=[dUk], writes=[wk])
                                else:
                                    s.op('dve', lambda e: e.scalar_tensor_tensor(
                                        out=Wst[d][:], in0=Wst[d][:], scalar=DECp[d][:, c, hp:hp + 1],
                                        in1=dUb[:, 0:65], op0=ALU.mult, op1=ALU.add),
                                        reads=[wk, f'DECp{d}', dUk], writes=[wk])
                            if os.environ.get('P6A'):
                                return
                            r_ = rp[sl]
                            Ov = Ob[:, 0:256].rearrange("p (h x) -> p h x", x=128)
                            s.op('act', lambda e: e.activation(out=r_[:, 0:2].unsqueeze(2), in_=Ov[:, :, 64:65],
                                                               func=AF.Abs), reads=[Ok], writes=[f'rp{sl}'])
                            s.op('dve', lambda e: e.tensor_tensor(out=r_[:, 0:2], in0=r_[:, 0:2],
                                                                  in1=THR[d][:, col0:col0 + 2], op=ALU.max),
                                 reads=[f'rp{sl}', f'THR{d}'], writes=[f'rp{sl}'])
                            s.op('dve', lambda e: e.reciprocal(out=r_[:, 2:4], in_=r_[:, 0:2]),
                                 reads=[f'rp{sl}'], writes=[f'rp{sl}b'])
                            hfv = HF[:, c, :].rearrange("p (h x) -> p h x", x=64)
                            rb = r_[:, 2:4].unsqueeze(2).to_broadcast([128, 2, 64])
                            if firstw:
                                s.op('dve', lambda e: e.tensor_tensor(out=hfv, in0=Ov[:, :, 0:64], in1=rb, op=ALU.mult),
                                     reads=[Ok, f'rp{sl}b'], writes=[f'HF{c}'])
                            else:
                                ht = htmp[sl % 2]
                                s.op('dve', lambda e: e.tensor_tensor(out=ht[:], in0=Ov[:, :, 0:64], in1=rb, op=ALU.mult),
                                     reads=[Ok, f'rp{sl}b'], writes=[f'htmp{sl % 2}'])
                                s.op('pool', lambda e: e.tensor_tensor(out=hfv, in0=hfv, in1=ht[:], op=ALU.add),
                                     reads=[f'htmp{sl % 2}', f'HF{c}'], writes=[f'HF{c}'])
                        st1()
                        pend.append(st2)
                        flush(int(os.environ.get('LAG6', '1')))
                flush(0)
                for cg in range(nch // 4):
                    hv = HF[:, cg * 4:(cg + 1) * 4, :]
                    hkeys = [f'HF{c}' for c in range(cg * 4, cg * 4 + 4)]
                    s.op('act', lambda e, hv=hv: e.activation(out=sqt[:], in_=hv, func=AF.Square),
                         reads=hkeys, writes=['sqt'])
                    s.op('dve', lambda e: e.tensor_reduce(out=ssq[:], in_=sqt[:].rearrange("p c (h e) -> p (c h) e", e=64),
                                                          axis=AX.X, op=ALU.add), reads=['sqt'], writes=['ssq'])
                    rsqrt_act(g, ssq[:], ssq[:], 1.0 / 64, ['ssq'], ['ssq'])
                    s.op('dve', lambda e, hv=hv: e.tensor_tensor(
                        out=hn[:].rearrange("p c (h e) -> p (c h) e", e=64),
                        in0=hv.rearrange("p c (h e) -> p (c h) e", e=64),
                        in1=ssq[:].unsqueeze(2).to_broadcast([128, 8, 64]), op=ALU.mult),
                        reads=hkeys + ['ssq'], writes=['hn'])
                    s.op('pool', lambda e, r0=r0: e.tensor_tensor(out=hn[:], in0=hn[:],
                                                                  in1=bc(mhg[:, r0:r0 + 128], [128, 4, 128]), op=ALU.mult),
                         reads=['hn', 'mhg'], writes=['hn'])
                    s.op('pool', lambda e, cg=cg: e.tensor_tensor(out=hb[:], in0=hn[:], in1=OGS[:, cg * 4:(cg + 1) * 4, :],
                                                                  op=ALU.mult), reads=['hn', 'OGS'], writes=['hb'])

                    def trh(e):
                        for i in range(4):
                            r = e.transpose(out=trP[:, i, :], in_=hb[:, i, :], identity=g.identB[:])
                        return r
                    s.op('pe', trh, reads=['hb', 'identB'], writes=['trP'])
                    bs = istg % 2
                    istg += 1
                    s.op('act', lambda e, bs=bs: e.activation(out=stg[bs][:], in_=trP[:, :, :].rearrange("p c t -> p (c t)"),
                                                              func=AF.Copy), reads=['trP'], writes=[f'stg{bs}'])
                    t0 = base + cg * 512
                    s.dma('sp', f'p6_stg{bs}', S.o2T[r0:r0 + 128, t0:t0 + 512], stg[bs][:], reads=[f'stg{bs}'])
        s.barrier()
```

```python
import numpy as np
import os
from contextlib import ExitStack
import concourse.bass as bass
import concourse.mybir as mybir
from concourse.bass_utils import run_bass_kernel_spmd

F32 = mybir.dt.float32
BF16 = mybir.dt.bfloat16
AF = mybir.ActivationFunctionType
ALU = mybir.AluOpType
AX = mybir.AxisListType
ENGS = ('pe', 'act', 'dve', 'pool', 'sp')
D = 1024
EPS = 1e-6


class Sched:
    def __init__(self, nc, stack):
        self.nc = nc
        self.stack = stack
        self.streams = {e: [] for e in ENGS}
        self.sems = {}
        self.cnt = {}
        self.seen = {e: {} for e in ENGS}
        self.bufs = {}
        self.snap = {}
        self.nwaits = 0
        self.nops = 0

    def sem(self, name):
        if name not in self.sems:
            self.sems[name] = self.stack.enter_context(
                self.nc.semaphore(name.replace(':', '_').replace('/', '_')))
            self.cnt[name] = 0
        return self.sems[name]

    def _need(self, E, ev, waits):
        if ev is None:
            return
        name, val = ev
        if self.seen[E].get(name, 0) >= val:
            return
        if waits.get(name, 0) < val:
            waits[name] = val

    def _collect(self, E, reads, writes):
        waits = {}
        for k in reads:
            b = self.bufs.get(k)
            if b is not None:
                self._need(E, b['w'], waits)
        own = 'e:' + E
        for k in writes:
            b = self.bufs.get(k)
            if b is not None:
                self._need(E, b['w'], waits)
                for ev in b['r']:
                    if ev[0] != own:
                        self._need(E, ev, waits)
        return waits

    def _apply_waits(self, E, waits):
        seen = self.seen[E]
        wl = []
        for name, val in waits.items():
            if seen.get(name, 0) >= val:
                continue
            wl.append((self.sem(name), val))
            sn = self.snap.get((name, val))
            if sn:
                for n2, v2 in sn.items():
                    if seen.get(n2, 0) < v2:
                        seen[n2] = v2
            seen[name] = val
        self.nwaits += len(wl)
        return wl

    def _record(self, ev, reads, writes):
        for k in reads:
            b = self.bufs.get(k)
            if b is None:
                b = self.bufs[k] = {'w': None, 'r': []}
            b['r'].append(ev)
            if len(b['r']) > 64:
                b['r'] = b['r'][-48:]
        for k in writes:
            self.bufs[k] = {'w': ev, 'r': []}

    def op(self, E, fn, reads=(), writes=()):
        waits = self._collect(E, reads, writes)
        if E == 'pe':
            waits.pop('e:pe', None)
        wl = self._apply_waits(E, waits)
        name = 'e:' + E
        s = self.sem(name)
        self.cnt[name] += 1
        val = self.cnt[name]
        ev = (name, val)
        if E == 'pe':
            self.seen[E][name] = val
        self.snap[ev] = dict(self.seen[E])
        self.streams[E].append((wl, fn, s, 1))
        self._record(ev, reads, writes)
        self.nops += 1
        return ev

    def dma(self, Q, semname, out, in_, reads=(), writes=(), **kw):
        waits = self._collect(Q, reads, writes)
        wl = self._apply_waits(Q, waits)
        name = 'd:' + semname
        s = self.sem(name)
        self.cnt[name] += 16
        val = self.cnt[name]
        ev = (name, val)
        self.snap[ev] = dict(self.seen[Q])

        def fn(eng, out=out, in_=in_, kw=kw):
            return eng.dma_start(out=out, in_=in_, **kw)
        self.streams[Q].append((wl, fn, s, 16))
        self._record(ev, reads, writes)
        self.nops += 1
        return ev

    def barrier(self):
        allev = dict(self.cnt)
        for E in ENGS:
            waits = {}
            for name, val in allev.items():
                if val > 0 and self.seen[E].get(name, 0) < val and name != 'e:' + E:
                    waits[name] = val
            wl = self._apply_waits(E, waits)
            if wl:
                self.streams[E].append((wl, None, None, 0))
        self.bufs = {}

    def emit(self):
        nc = self.nc
        self.barrier()
        streams = self.streams
        with nc.Block() as block:
            def run(E):
                def body(eng):
                    for wl, fn, s, inc in streams[E]:
                        for (ws, wv) in wl:
                            eng.wait_ge(ws, wv)
                        if fn is not None:
                            fn(eng).then_inc(s, inc)
                return body
            block.sync(run('sp'))
            block.scalar(run('act'))
            block.vector(run('dve'))
            block.gpsimd(run('pool'))
            block.tensor(run('pe'))


class Ctx:
    pass


def build(seqs, phases, debug=False):
    nc = bass.Bass("TRN2", target_bir_lowering=False)
    NS = len(seqs)
    NT = sum(seqs)
    offs = [sum(seqs[:i]) for i in range(NS)]
    SMAX = max(seqs)
    g = Ctx()
    g.nc, g.seqs, g.NS, g.NT, g.offs, g.debug = nc, seqs, NS, NT, offs, debug

    def din(name, shape, dt=F32):
        return nc.dram_tensor(name, list(shape), dt, kind="ExternalInput").ap()

    def dscr(name, shape, dt):
        return nc.dram_tensor(name, list(shape), dt, kind=("ExternalOutput" if debug else "Internal")).ap()
    g.dscr = dscr
    I = Ctx()
    g.I = I
    I.x = din("x", [NT, D])
    I.cT = din("cT", [128, 8 * NS])
    I.w_in_ab = din("w_in_ab", [D, 2304])
    I.w_out_ab = din("w_out_ab", [D, D])
    I.w_in_cd = din("w_in_cd", [D, 3104])
    I.w_out_cd = din("w_out_cd", [D, D])
    I.w_ada = din("w_ada", [2, D, 6 * D])
    I.b_ada = din("b_ada", [1, 2 * 6 * D])
    I.normgT = din("normgT", [128, 2 * 4 * 8])
    I.norm_g = din("norm_g", [8, D])
    I.w_ff1 = din("w_ff1", [2, D, 4 * D])
    I.w_ff2 = din("w_ff2", [2, 4 * D, D])
    I.qkg = din("qkg", [1, 128])
    I.ropeA = din("ropeA", [SMAX, 64])
    I.ropeB = din("ropeB", [SMAX, 64])
    I.gate_bias = din("gate_bias", [32, 1])
    I.mhg = din("mhg", [1, 512])
    I.sgg = din("sgg", [1, 512])
    I.wsT = din("wsT", [128, 8 * 128])
    I.bsT = din("bsT", [128, 8])
    y = nc.dram_tensor("y", [NT, D], F32, kind="ExternalOutput").ap()
    g.y = y

    Sx = Ctx()
    g.S = Sx
    Sx.gates = dscr("s_gates", [2, 2, NS, D], F32)
    Sx.qaT = dscr("s_qaT", [512, NT], BF16)
    Sx.kaT = dscr("s_kaT", [512, NT], BF16)
    Sx.qbT = dscr("s_qbT", [512, NT], BF16)
    Sx.kbT = dscr("s_kbT", [128, NT], BF16)
    Sx.vA = dscr("s_vA", [NT, 512], BF16)
    Sx.vB = dscr("s_vB", [NT, 128], BF16)
    Sx.oT = dscr("s_oT", [D, NT], BF16)
    Sx.xmid = dscr("s_xmid", [NT, D], F32)
    Sx.x1 = dscr("s_x1", [NT, D], F32)
    Sx.qcT = dscr("s_qcT", [512, NT], BF16)
    Sx.kcT = dscr("s_kcT", [512, NT], BF16)
    Sx.kc = dscr("s_kc", [NT, 512], BF16)
    Sx.vc = dscr("s_vc", [NT, 512], BF16)
    Sx.ogs = dscr("s_ogs", [NT, 512], F32)
    Sx.gtok = dscr("s_gtok", [NT, 32], F32)
    Sx.o2T = dscr("s_o2T", [D, NT], BF16)

    with ExitStack() as st:
        s = Sched(nc, st)
        g.s = s
        g.identB = st.enter_context(nc.sbuf_tensor("identB", [128, 128], BF16))
        g.identF = st.enter_context(nc.sbuf_tensor("identF", [128, 128], F32))
        g.GS = st.enter_context(nc.sbuf_tensor("GS", [128, 2, 2, 8, NS], F32))
        g.SH = st.enter_context(nc.sbuf_tensor("SH", [128, 2, 2, 8, NS], F32))
        g.epsc = st.enter_context(nc.sbuf_tensor("epsc", [128, 2], F32))
        s.op('pool', lambda e: e.memset(g.epsc[:], EPS), writes=['epsc'])
        for t, nm in ((g.identB, 'identB'), (g.identF, 'identF')):
            s.op('pool', lambda e, t=t: e.memset(t[:], 1.0), writes=[nm])
            s.op('pool', lambda e, t=t: e.affine_select(out=t[:], in_=t[:], pattern=[[-1, 128]],
                                                        compare_op=ALU.is_equal, fill=0.0, base=0,
                                                        channel_multiplier=1), reads=[nm], writes=[nm])
        if 0 in phases:
            phase0(g)
        if 1 in phases:
            phase1(g)
        if 2 in phases:
            import os
            if not os.environ.get('SKIPB'):
                phase2(g)
            if not os.environ.get('SKIPA'):
                phase2a(g)
        if 3 in phases:
            phase3(g, 0, Sx.oT, I.w_out_ab, I.x, Sx.xmid)
        if 4 in phases:
            phase4(g, 0, Sx.xmid, Sx.x1 if (5 in phases or debug) else g.y)
        if 5 in phases:
            phase5(g)
        if 6 in phases:
            phase6(g)
        if 7 in phases:
            phase3(g, 1, Sx.o2T, I.w_out_cd, Sx.x1, Sx.xmid)
        if 8 in phases:
            phase4(g, 1, Sx.xmid, g.y)
        s.emit()
    g.stats = (s.nops, s.nwaits)
    return nc, g


def load_w_bf16(g, st_name, wt, w_dram, K, N, n0=0, colmap=None):
    s = g.s
    for k in range(K):
        c = 0
        while c < N:
            w = min(1024, N - c)
            s.dma('pool', st_name, wt[:, k, c:c + w], w_dram[k * 128:(k + 1) * 128, n0 + c:n0 + c + w],
                  writes=[st_name])
            c += w


def phase0(g):
    nc, s, NS, I = g.nc, g.s, g.NS, g.I
    with ExitStack() as st:
        sb = lambda n, sh, dt: st.enter_context(nc.sbuf_tensor(n, sh, dt))
        cTf = sb("p0_cTf", [128, 8 * NS], F32)
        cTb = sb("p0_cTb", [128, 8, NS], BF16)
        wt = [sb(f"p0_w{i}", [128, 8, 3072], BF16) for i in range(2)]
        bada = sb("p0_bada", [NS, 2 * 6 * D], F32)
        modrow = sb("p0_modrow", [NS, 2, 6 * D], F32)
        ngT = sb("p0_ngT", [128, 2, 4, 8], F32)
        modT = sb("p0_modT", [128, 2, 4, 8, NS], F32)
        ps = [st.enter_context(nc.psum_tensor(f"p0_ps{i}", [128, 512], F32)) for i in range(2)]
        pT = st.enter_context(nc.psum_tensor("p0_pT", [128, 4 * 8 * NS], F32))
        s.dma('sp', 'p0_cTf', cTf[:], I.cT[:, :], writes=['cTf'])
        s.dma('sp', 'p0_bada', bada[:], I.b_ada[0:1, :].partition_broadcast(NS), writes=['bada'])
        s.dma('sp', 'p0_ngT', ngT[:], I.normgT[:, :], writes=['ngT'])
        s.op('act', lambda e: e.activation(out=cTb[:].rearrange("p k s -> p (k s)"), in_=cTf[:], func=AF.Silu),
             reads=['cTf'], writes=['cTb'])
        it = 0
        for l in range(2):
            for hf in range(2):
                w = wt[it % 2]
                wn = f"p0_w{it % 2}"
                load_w_bf16(g, wn, w, I.w_ada[l], 8, 3072, n0=hf * 3072)
                for nch in range(6):
                    p = ps[nch % 2]
                    pn = f"p0_ps{nch % 2}"

                    def mm(e, p=p, w=w, nch=nch):
                        for k in range(8):
                            r = e.matmul(p[0:NS, :], lhsT=cTb[:, k, :], rhs=w[:, k, nch * 512:(nch + 1) * 512],
                                         start=(k == 0), stop=(k == 7))
                        return r
                    s.op('pe', mm, reads=['cTb', wn], writes=[pn])
                    c0 = hf * 3072 + nch * 512
                    s.op('dve', lambda e, p=p, l=l, c0=c0: e.tensor_tensor(
                        out=modrow[:, l, c0:c0 + 512], in0=p[0:NS, :],
                        in1=bada[:, l * 6 * D + c0:l * 6 * D + c0 + 512], op=ALU.add),
                        reads=[pn, 'bada'], writes=['modrow'])
                it += 1
        for l in range(2):
            for gi, part in enumerate((2, 5)):
                s.dma('sp', 'p0_gst', g.S.gates[l, gi], modrow[:, l, part * D:(part + 1) * D],
                      reads=['modrow'])
        for l in range(2):
            def tr(e, l=l):
                for pi, part in enumerate((0, 1, 3, 4)):
                    for k in range(8):
                        c0 = part * D + k * 128
                        o = (pi * 8 + k) * NS
                        r = e.transpose(out=pT[:, o:o + NS], in_=modrow[:, l, c0:c0 + 128],
                                        identity=g.identF[0:NS, 0:NS])
                return r
            s.op('pe', tr, reads=['modrow', 'identF'], writes=['p0_pT'])
            s.op('dve', lambda e, l=l: e.tensor_copy(out=modT[:, l].rearrange("p a k s -> p (a k s)"), in_=pT[:, :]),
                 reads=['p0_pT'], writes=['modT'])
        for l in range(2):
            for m in range(2):
                sc = modT[:, l, 2 * m + 1]
                sh = modT[:, l, 2 * m]
                ngb = ngT[:, l, 2 * m, :].unsqueeze(2).to_broadcast([128, 8, NS])
                s.op('dve', lambda e, sc=sc, ngb=ngb, l=l, m=m: e.scalar_tensor_tensor(
                    out=g.GS[:, l, m], in0=sc, scalar=1.0, in1=ngb, op0=ALU.add, op1=ALU.mult),
                    reads=['modT', 'ngT'], writes=['GS'])
                s.op('dve', lambda e, sh=sh, l=l, m=m: e.tensor_copy(out=g.SH[:, l, m], in_=sh),
                     reads=['modT'], writes=['SH'])
        s.barrier()


def bc(ap2d, shape):
    return ap2d.unsqueeze(1).to_broadcast(shape)


def rsqrt_act(g, out, in_, scale, rk, wk):
    s = g.s
    s.op('act', lambda e: e.activation(out=out, in_=in_, func=AF.Ln, scale=scale, bias=g.epsc[:, 0:1]),
         reads=rk + ['epsc'], writes=wk)
    s.op('act', lambda e: e.activation(out=out, in_=out, func=AF.Exp, scale=-0.5), reads=wk, writes=wk)


def rms_prep(g, pre, xt, xn, ss, rstd, junk, nblk, xkey, outkey):
    s = g.s
    for b in range(nblk):
        s.op('act', lambda e, b=b: e.activation(out=junk[:], in_=xt[:, b, :], func=AF.Square,
                                                accum_out=ss[:, b:b + 1]),
             reads=[xkey], writes=[pre + 'junk', pre + 'ss'])
    rsqrt_act(g, rstd[:, 0:nblk], ss[:, 0:nblk], 1.0 / D, [pre + 'ss'], [pre + 'rstd'])
    for b in range(nblk):
        s.op('act', lambda e, b=b: e.activation(out=xn[:, b, :], in_=xt[:, b, :], func=AF.Copy,
                                                scale=rstd[:, b:b + 1]),
             reads=[xkey, pre + 'rstd'], writes=[outkey])


def to_featmajor(g, pre, xn, hmT, tps, l, m, j, nblk, xnkey, hkey):
    s = g.s
    for half in range(nblk // 2):
        tp = tps[half % len(tps)]
        tk = pre + f'tp{half % len(tps)}'

        def tr(e, half=half, tp=tp):
            for k in range(8):
                for bb in range(2):
                    r = e.transpose(out=tp[:, k, bb * 128:(bb + 1) * 128],
                                    in_=xn[:, half * 2 + bb, k * 128:(k + 1) * 128], identity=g.identB[:])
            return r
        s.op('pe', tr, reads=[xnkey, 'identB'], writes=[tk])
        for k in range(8):
            eng = 'dve' if k % 2 == 0 else 'pool'
            eng = 'dve'
            s.op(eng, lambda e, k=k, half=half, tp=tp: e.tensor_scalar(
                out=hmT[:, k, half * 256:(half + 1) * 256], in0=tp[:, k, :],
                scalar1=g.GS[:, l, m, k, j:j + 1], scalar2=g.SH[:, l, m, k, j:j + 1],
                op0=ALU.mult, op1=ALU.add), reads=[tk, 'GS', 'SH'], writes=[hkey])


def rotary(g, eng, out, zin, cos, sin, H, hd, tmp, rk, wk, tk):
    s = g.s
    zv = zin.rearrange("p (h two d) -> p h two d", two=2, d=hd)
    ov = out.rearrange("p (h two d) -> p h two d", two=2, d=hd)
    x1, x2 = zv[:, :, 0, :], zv[:, :, 1, :]
    o1, o2 = ov[:, :, 0, :], ov[:, :, 1, :]
    cb = bc(cos, [128, H, hd])
    sb_ = bc(sin, [128, H, hd])
    t = [tmp[:, i, 0:H * hd].rearrange("p (h d) -> p h d", d=hd) for i in range(4)]
    s.op(eng, lambda e: e.tensor_tensor(out=t[0], in0=x1, in1=cb, op=ALU.mult), reads=rk, writes=[tk + '0'])
    s.op(eng, lambda e: e.tensor_tensor(out=t[1], in0=x2, in1=sb_, op=ALU.mult), reads=rk, writes=[tk + '1'])
    s.op(eng, lambda e: e.tensor_tensor(out=o1, in0=t[0], in1=t[1], op=ALU.subtract),
         reads=[tk + '0', tk + '1'], writes=wk)
    s.op(eng, lambda e: e.tensor_tensor(out=t[2], in0=x2, in1=cb, op=ALU.mult), reads=rk, writes=[tk + '2'])
    s.op(eng, lambda e: e.tensor_tensor(out=t[3], in0=x1, in1=sb_, op=ALU.mult), reads=rk, writes=[tk + '3'])
    s.op(eng, lambda e: e.tensor_tensor(out=o2, in0=t[2], in1=t[3], op=ALU.add),
         reads=[tk + '2', tk + '3'], writes=wk)


def phase1(g):
    nc, s, NS, I, S = g.nc, g.s, g.NS, g.I, g.S
    with ExitStack() as st:
        sb = lambda n, sh, dt: st.enter_context(nc.sbuf_tensor(n, sh, dt))
        pm = lambda n, sh, dt: st.enter_context(nc.psum_tensor(n, sh, dt))
        wab = sb("p1_wab", [128, 8, 2304], BF16)
        xt = [sb(f"p1_xt{i}", [128, 4, D], F32) for i in range(2)]
        xn = [sb(f"p1_xn{i}", [128, 4, D], BF16) for i in range(2)]
        junk = sb("p1_junk", [128, D], BF16)
        ss = [sb(f"p1_ss{i}", [128, 4], F32) for i in range(2)]
        rstd = [sb(f"p1_rstd{i}", [128, 4], F32) for i in range(2)]
        hmT = [sb(f"p1_hmT{i}", [128, 8, 512], BF16) for i in range(2)]
        zs = [sb(f"p1_zs{i}", [128, 2304], F32) for i in range(2)]
        rA = [sb(f"p1_rA{i}", [128, 4, 64], F32) for i in range(2)]
        rB = [sb(f"p1_rB{i}", [128, 4, 64], F32) for i in range(2)]
        qkg = sb("p1_qkg", [128, 128], F32)
        tmpA = sb("p1_tmpA", [128, 4, 256], F32)
        tmpB = sb("p1_tmpB", [128, 4, 256], F32)
        sq = sb("p1_sq", [128, 640], F32)
        ssq = sb("p1_ssq", [128, 10], F32)
        qn = sb("p1_qn", [128, 640], F32)
        qk = [sb(f"p1_qk{i}", [128, 1664], BF16) for i in range(2)]
        stg = [sb(f"p1_stg{i}", [128, 13, 512], BF16) for i in range(2)]
        vst = [sb(f"p1_vst{i}", [128, 4, 640], BF16) for i in range(2)]
        tps = [pm(f"p1_tp{i}", [128, 8, 256], BF16) for i in range(2)]
        zp = [pm(f"p1_zp{i}", [128, 512], F32) for i in range(2)]
        qT = pm("p1_qT", [128, 16, 128], BF16)

        load_w_bf16(g, 'p1_wab', wab, I.w_in_ab, 8, 2304)
        s.dma('sp', 'p1_qkg', qkg[:], I.qkg[0:1, :].partition_broadcast(128), writes=['qkg'])
        tiles = []
        for j, Sj in enumerate(g.seqs):
            for t in range(Sj // 512):
                tiles.append((j, t * 512, g.offs[j] + t * 512))

        def load(i):
            j, p0, t0 = tiles[i]
            b = i % 2
            s.dma('sp', f'p1_xt{b}', xt[b][:], I.x[t0:t0 + 512, :].rearrange("(b p) d -> p b d", p=128),
                  writes=[f'xt{b}'])
            s.dma('sp', f'p1_rA{b}', rA[b][:], I.ropeA[p0:p0 + 512, :].rearrange("(b p) d -> p b d", p=128),
                  writes=[f'rA{b}'])
            s.dma('sp', f'p1_rB{b}', rB[b][:], I.ropeB[p0:p0 + 512, :].rearrange("(b p) d -> p b d", p=128),
                  writes=[f'rB{b}'])
        load(0)
        zi = 0
        pend = []

        def flush():
            while pend:
                pend.pop(0)()

        def prep(i):
            j, p0, t0 = tiles[i]
            b = i % 2
            rms_prep(g, f'p1{b}', xt[b], xn[b], ss[b], rstd[b], junk, 4, f'xt{b}', f'xn{b}')
            to_featmajor(g, 'p1', xn[b], hmT[b], tps, 0, 0, j, 4, f'xn{b}', f'hmT{b}')
        prep(0)
        for i, (j, p0, t0) in enumerate(tiles):
            b = i % 2
            if i + 1 < len(tiles):
                load(i + 1)
            for blk in range(4):
                if blk == 3 and i + 1 < len(tiles):
                    prep(i + 1)
                zb = zs[blk % 2]
                zk = f'zs{blk % 2}'
                for c in range(5):
                    n0 = c * 512
                    nw = min(512, 2304 - n0)
                    p = zp[zi % 2]
                    pn = f'zp{zi % 2}'
                    zi += 1

                    def mm(e, p=p, n0=n0, nw=nw, blk=blk, b=b):
                        for k in range(8):
                            r = e.matmul(p[:, 0:nw], lhsT=hmT[b][:, k, blk * 128:(blk + 1) * 128],
                                         rhs=wab[:, k, n0:n0 + nw], start=(k == 0), stop=(k == 7))
                        return r
                    s.op('pe', mm, reads=[f'hmT{b}', 'p1_wab'], writes=[pn])
                    if c == 2:
                        s.op('act', lambda e, p=p, blk=blk, b=b: e.activation(
                            out=vst[b][:, blk, 0:512], in_=p[:, 0:512], func=AF.Copy),
                            reads=[pn], writes=[f'vst{b}'])
                    else:
                        s.op('act', lambda e, p=p, n0=n0, nw=nw, zb=zb: e.activation(
                            out=zb[:, n0:n0 + nw], in_=p[:, 0:nw], func=AF.Copy),
                            reads=[pn], writes=[zk + f'c{c}'])
                flush()
                q = qk[blk % 2]
                qkk = f'qk{blk % 2}'
                cosA, sinA = rA[b][:, blk, 0:32], rA[b][:, blk, 32:64]
                cosB, sinB = rB[b][:, blk, 0:32], rB[b][:, blk, 32:64]
                rotary(g, 'dve', q[:, 0:512], zb[:, 0:512], cosA, sinA, 8, 32, tmpA,
                       [zk + 'c0', f'rA{b}'], [qkk + 'qa'], 'tmpA')
                rotary(g, 'pool', q[:, 512:1024], zb[:, 512:1024], cosA, sinA, 8, 32, tmpB,
                       [zk + 'c1', f'rA{b}'], [qkk + 'ka'], 'tmpB')
                s.op('act', lambda e, zb=zb: e.activation(out=sq[:], in_=zb[:, 1536:2176], func=AF.Square),
                     reads=[zk + 'c3', zk + 'c4'], writes=['sq'])
                s.op('dve', lambda e: e.tensor_reduce(out=ssq[:], in_=sq[:].rearrange("p (h d) -> p h d", d=64),
                                                      axis=AX.X, op=ALU.add), reads=['sq'], writes=['ssq'])
                rsqrt_act(g, ssq[:], ssq[:], 1.0 / 64, ['ssq'], ['ssq'])
                s.op('dve', lambda e, zb=zb: e.tensor_tensor(
                    out=qn[:].rearrange("p (h d) -> p h d", d=64),
                    in0=zb[:, 1536:2176].rearrange("p (h d) -> p h d", d=64),
                    in1=ssq[:].unsqueeze(2).to_broadcast([128, 10, 64]), op=ALU.mult),
                    reads=[zk + 'c3', zk + 'c4', 'ssq'], writes=['qn'])
                s.op('dve', lambda e: e.tensor_tensor(
                    out=qn[:, 0:512].rearrange("p (h d) -> p h d", d=64),
                    in0=qn[:, 0:512].rearrange("p (h d) -> p h d", d=64),
                    in1=bc(qkg[:, 0:64], [128, 8, 64]), op=ALU.mult), reads=['qn', 'qkg'], writes=['qn'])
                s.op('dve', lambda e: e.tensor_tensor(
                    out=qn[:, 512:640].rearrange("p (h d) -> p h d", d=64),
                    in0=qn[:, 512:640].rearrange("p (h d) -> p h d", d=64),
                    in1=bc(qkg[:, 64:128], [128, 2, 64]), op=ALU.mult), reads=['qn', 'qkg'], writes=['qn'])
                for half in range(2):
                    for (c0, H, o0) in ((0, 8, 1024), (512, 2, 1536)):
                        zin = qn[:, c0:c0 + H * 64].rearrange("p (h x) -> p h x", x=64)[:, :, half * 32:(half + 1) * 32]
                        oo = q[:, o0:o0 + H * 64].rearrange("p (h x) -> p h x", x=64)[:, :, half * 32:(half + 1) * 32]
                        rotary_v(g, 'dve', oo, zin, cosB[:, half * 16:(half + 1) * 16],
                                 sinB[:, half * 16:(half + 1) * 16], H, 16, tmpA, ['qn', f'rB{b}'],
                                 [qkk + 'b'], 'tmpA')
                s.op('pool', lambda e, zb=zb, blk=blk, b=b: e.tensor_copy(out=vst[b][:, blk, 512:640],
                                                                          in_=zb[:, 2176:2304]),
                     reads=[zk + 'c4'], writes=[f'vst{b}'])
                def tail(q=q, qkk=qkk, blk=blk, b=b):
                    def trq(e):
                        for c in range(13):
                            r = e.transpose(out=qT[:, c, :], in_=q[:, c * 128:(c + 1) * 128], identity=g.identB[:])
                        return r
                    s.op('pe', trq, reads=[qkk + 'qa', qkk + 'ka', qkk + 'b', 'identB'], writes=['qT'])
                    s.op('act', lambda e: e.activation(out=stg[b][:, :, blk * 128:(blk + 1) * 128],
                                                       in_=qT[:, 0:13, :], func=AF.Copy),
                         reads=['qT'], writes=[f'stg{b}'])
                pend.append(tail)
            flush()
            for (dst, c0, n) in ((S.qaT, 0, 4), (S.kaT, 4, 4), (S.qbT, 8, 4), (S.kbT, 12, 1)):
                s.dma('sp', f'p1_stg{b}', dst[:, t0:t0 + 512].rearrange("(c p) t -> p c t", p=128),
                      stg[b][:, c0:c0 + n, :], reads=[f'stg{b}'])
            s.dma('sp', f'p1_vst{b}', S.vA[t0:t0 + 512, :].rearrange("(b p) d -> p b d", p=128), vst[b][:, :, 0:512],
                  reads=[f'vst{b}'])
            s.dma('sp', f'p1_vst{b}', S.vB[t0:t0 + 512, :].rearrange("(b p) d -> p b d", p=128), vst[b][:, :, 512:640],
                  reads=[f'vst{b}'])
        s.barrier()


def rotary_v(g, eng, ov, zv, cos, sin, H, hd, tmp, rk, wk, tk):
    s = g.s
    x1, x2 = zv[:, :, 0:hd], zv[:, :, hd:2 * hd]
    o1, o2 = ov[:, :, 0:hd], ov[:, :, hd:2 * hd]
    cb = bc(cos, [128, H, hd])
    sb_ = bc(sin, [128, H, hd])
    t = [tmp[:, i, 0:H * hd].rearrange("p (h d) -> p h d", d=hd) for i in range(4)]
    s.op(eng, lambda e: e.tensor_tensor(out=t[0], in0=x1, in1=cb, op=ALU.mult), reads=rk, writes=[tk + '0'])
    s.op(eng, lambda e: e.tensor_tensor(out=t[1], in0=x2, in1=sb_, op=ALU.mult), reads=rk, writes=[tk + '1'])
    s.op(eng, lambda e: e.tensor_tensor(out=o1, in0=t[0], in1=t[1], op=ALU.subtract),
         reads=[tk + '0', tk + '1'], writes=wk)
    s.op(eng, lambda e: e.tensor_tensor(out=t[2], in0=x2, in1=cb, op=ALU.mult), reads=rk, writes=[tk + '2'])
    s.op(eng, lambda e: e.tensor_tensor(out=t[3], in0=x1, in1=sb_, op=ALU.mult), reads=rk, writes=[tk + '3'])
    s.op(eng, lambda e: e.tensor_tensor(out=o2, in0=t[2], in1=t[3], op=ALU.add),
         reads=[tk + '2', tk + '3'], writes=wk)


def phase2(g):
    nc, s, S = g.nc, g.s, g.S
    SMAX = max(g.seqs)
    with ExitStack() as st:
        sb = lambda n, sh, dt: st.enter_context(nc.sbuf_tensor(n, sh, dt))
        pm = lambda n, sh, dt: st.enter_context(nc.psum_tensor(n, sh, dt))
        kT = [sb(f"p2_kT{i}", [128, SMAX], BF16) for i in range(2)]
        V1 = [sb(f"p2_V1{i}", [128, SMAX // 128, 128], BF16) for i in range(2)]
        qT = [sb(f"p2_qT{i}", [128, SMAX], BF16) for i in range(2)]
        pT = [sb(f"p2_pT{i}", [128, 1024], BF16) for i in range(3)]
        rd = [sb(f"p2_rd{i}", [64, 512], F32) for i in range(2)]
        ob = [sb(f"p2_ob{i}", [64, 512], BF16) for i in range(2)]
        sp_ = [pm(f"p2_sp{i}", [128, 1024], F32) for i in range(3)]
        ot = [pm(f"p2_ot{i}", [128, 512], F32) for i in range(2)]
        for i in range(2):
            s.op('pool', lambda e, i=i: e.memset(V1[i][:, :, 64:128], 1.0), writes=[f'V1{i}'])
            s.op('pool', lambda e, i=i: e.memset(kT[i][64:128, :], 0.0), writes=[f'kT{i}'])
            s.op('pool', lambda e, i=i: e.memset(qT[i][64:128, :], 0.0), writes=[f'qT{i}'])
        ikv = 0
        ih = 0
        ip = 0
        io = 0
        LAG = 2
        pend = []

        def flush(n):
            while len(pend) > n:
                pend.pop(0)()
        for j, Sj in enumerate(g.seqs):
            base = g.offs[j]
            nkb, nqg = Sj // 128, Sj // 512
            for kv in range(2):
                bk = ikv % 2
                ikv += 1
                s.dma('sp', f'p2_kT{bk}', kT[bk][0:64, 0:Sj], S.kbT[kv * 64:(kv + 1) * 64, base:base + Sj],
                      writes=[f'kT{bk}'])
                s.dma('sp', f'p2_V1{bk}', V1[bk][:, 0:nkb, 0:64],
                      S.vB[base:base + Sj, kv * 64:(kv + 1) * 64].rearrange("(b p) d -> p b d", p=128),
                      writes=[f'V1{bk}'])
                for hq in range(4):
                    h = kv * 4 + hq
                    bq = ih % 2
                    ih += 1
                    s.dma('sp', f'p2_qT{bq}', qT[bq][0:64, 0:Sj], S.qbT[h * 64:(h + 1) * 64, base:base + Sj],
                          writes=[f'qT{bq}'])
                    for qg in range(nqg):
                        o = ot[io % 2]
                        on = f'ot{io % 2}'
                        bo = io % 2
                        io += 1
                        for kb in range(0, nkb, 2):
                            p = sp_[ip % 3]
                            pn = f'sp{ip % 3}'
                            pt = pT[ip % 3]
                            ptn = f'pT{ip % 3}'
                            ip += 1

                            def smm(e, p=p, bk=bk, bq=bq, kb=kb, qg=qg):
                                for i in range(2):
                                    r = e.matmul(p[:, i * 512:(i + 1) * 512],
                                                 lhsT=kT[bk][:, (kb + i) * 128:(kb + i + 1) * 128],
                                                 rhs=qT[bq][:, qg * 512:(qg + 1) * 512], start=True, stop=True)
                                return r
                            s.op('pe', smm, reads=[f'kT{bk}', f'qT{bq}'], writes=[pn])
                            s.op('act', lambda e, p=p, pt=pt: e.activation(out=pt[:], in_=p[:], func=AF.Exp,
                                                                           scale=0.125),
                                 reads=[pn], writes=[ptn])

                            def stage2(o=o, on=on, bo=bo, bk=bk, kb=kb, pt=pt, ptn=ptn, nkb=nkb, h=h, qg=qg,
                                       base=base):
                                def pvm(e):
                                    for i in range(2):
                                        r = e.matmul(o[:, :], lhsT=V1[bk][:, kb + i, :], rhs=pt[:, i * 512:(i + 1) * 512],
                                                     start=(kb + i == 0), stop=(kb + i == nkb - 1))
                                    return r
                                s.op('pe', pvm, reads=[f'V1{bk}', ptn], writes=[on])
                                if kb + 2 == nkb:
                                    s.op('dve', lambda e: e.reciprocal(out=rd[bo][:], in_=o[64:128, :]),
                                         reads=[on], writes=[f'rd{bo}'])
                                    s.op('dve', lambda e: e.tensor_tensor(out=ob[bo][:], in0=o[0:64, :],
                                                                          in1=rd[bo][:], op=ALU.mult),
                                         reads=[on, f'rd{bo}'], writes=[f'ob{bo}'])
                                    t0 = base + qg * 512
                                    s.dma('sp', f'p2_ob{bo}', S.oT[512 + h * 64:512 + (h + 1) * 64, t0:t0 + 512],
                                          ob[bo][:], reads=[f'ob{bo}'])
                            pend.append(stage2)
                            flush(LAG)
        flush(0)
        s.barrier()


def phase2a(g):
    nc, s, S = g.nc, g.s, g.S
    with ExitStack() as st:
        sb = lambda n, sh, dt: st.enter_context(nc.sbuf_tensor(n, sh, dt))
        pm = lambda n, sh, dt: st.enter_context(nc.psum_tensor(n, sh, dt))
        kTw = sb("p2a_kTw", [128, 4, 4096], BF16)
        qTsh = [sb(f"p2a_qTs{i}", [128, 4, 2048], BF16) for i in range(2)]
        ACC = sb("p2a_ACC", [128, 8, 2048], F32)
        V1 = [sb(f"p2a_V1{i}", [128, 9, 8, 128], BF16) for i in range(2)]
        band = sb("p2a_band", [128, 3, 256], BF16)
        pT = [sb(f"p2a_pT{i}", [128, 256], BF16) for i in range(4)]
        pTm = [sb(f"p2a_pTm{i}", [128, 256], BF16) for i in range(4)]
        rd = [sb(f"p2a_rd{i}", [64, 2048], F32) for i in range(2)]
        ob = [sb(f"p2a_ob{i}", [64, 2048], BF16) for i in range(2)]
        sp_ = [pm(f"p2a_sp{i}", [128, 512], F32) for i in range(4)]
        ot = [pm(f"p2a_ot{i}", [128, 512], F32) for i in range(4)]
        s.op('pool', lambda e: e.memset(qTsh[0][64:128, :, :], 0.0), writes=['qTs'])
        s.op('pool', lambda e: e.memset(qTsh[1][0:64, :, :], 0.0), writes=['qTs'])
        NEG = -30000.0
        for bi in range(3):
            bt = band[:, bi, :]
            s.op('pool', lambda e, bt=bt: e.memset(bt, 0.0), writes=['band'])
            s.op('pool', lambda e, bt=bt: e.affine_select(out=bt, in_=bt, pattern=[[1, 256]], compare_op=ALU.is_ge,
                                                          fill=NEG, base=0, channel_multiplier=-1),
                 reads=['band'], writes=['band'])
            s.op('pool', lambda e, bt=bt: e.affine_select(out=bt, in_=bt, pattern=[[-1, 256]], compare_op=ALU.is_ge,
                                                          fill=NEG, base=128, channel_multiplier=1),
                 reads=['band'], writes=['band'])
        s.op('pool', lambda e: e.memset(band[0:64, 1, :], NEG), reads=['band'], writes=['band'])
        s.op('pool', lambda e: e.memset(band[64:128, 2, :], NEG), reads=['band'], writes=['band'])
        for i in range(2):
            s.op('pool', lambda e, i=i: e.memset(V1[i][:, :, :, 0:64], 0.0), writes=[f'aV1{i}'])
            s.op('pool', lambda e, i=i: e.memset(V1[i][:, :, :, 64:128], 1.0), writes=[f'aV1{i}'])
        iu = 0
        ip = 0
        import os
        LAG = int(os.environ.get('LAGA', '2'))
        pend = []

        def flush(n):
            while len(pend) > n:
                pend.pop(0)()
        for j, Sj in enumerate(g.seqs):
            base = g.offs[j]
            for seg in range(Sj // 2048):
                seg0 = seg * 2048
                lo, hi = max(0, seg0 - 1024), min(Sj, seg0 + 3072)
                if lo > seg0 - 1024:
                    s.op('pool', lambda e: e.memset(kTw[:, :, 0:1024], 0.0), writes=['kTw'])
                if hi < seg0 + 3072:
                    s.op('pool', lambda e: e.memset(kTw[:, :, 3072:4096], 0.0), writes=['kTw'])
                s.dma('sp', 'p2a_kTw', kTw[:, :, lo - (seg0 - 1024):hi - (seg0 - 1024)],
                      S.kaT[:, base + lo:base + hi].rearrange("(c p) t -> p c t", p=128), writes=['kTw'])
                qsrc = S.qaT[:, base + seg0:base + seg0 + 2048].rearrange("(c p) t -> p c t", p=128)
                s.dma('sp', 'p2a_qTs', qTsh[0][0:64, :, :], qsrc[0:64], writes=['qTs'])
                s.dma('sp', 'p2a_qTs', qTsh[1][64:128, :, :], qsrc[64:128], writes=['qTs'])
                units = [(1, 0, 0, 8), (1, 0, 8, 8)] + [(4, r, 0, 4) for r in range(4)] + \
                        [(16, r, 0, 1) for r in range(16)]
                first_write = {}
                for (d, r, qb0, nqb) in units:
                    L = Sj // d
                    lq0 = seg0 // d + 128 * qb0
                    bv = iu % 2
                    iu += 1
                    vk = f'aV1{bv}'
                    v1 = V1[bv]
                    nkb = nqb + 1
                    m_lo, m_hi = 0, nkb
                    at_start = (lq0 == 0)
                    at_end = (lq0 + 128 * nqb == L)
                    if lq0 == 0:
                        tok = base + 0 * d + r
                        s.dma('sp', f'p2a_V1{bv}', v1[64:128, 0, :, 0:64],
                              S.vA[tok:tok + 63 * d + 1:d, :].rearrange("p (h e) -> p h e", e=64), writes=[vk])
                        m_lo = 1
                    if lq0 + 128 * nqb == L:
                        tok = base + (lq0 - 64 + 128 * nqb) * d + r
                        s.dma('sp', f'p2a_V1{bv}', v1[0:64, nqb, :, 0:64],
                              S.vA[tok:tok + 63 * d + 1:d, :].rearrange("p (h e) -> p h e", e=64), writes=[vk])
                        m_hi = nkb - 1
                    for m in range(m_lo, m_hi):
                        tok = base + (lq0 - 64 + 128 * m) * d + r
                        s.dma('sp', f'p2a_V1{bv}', v1[:, m, :, 0:64],
                              S.vA[tok:tok + 127 * d + 1:d, :].rearrange("p (h e) -> p h e", e=64), writes=[vk])
                    for h in range(8):
                        c, hh = h // 2, (h % 2) * 64
                        for m in range(nkb):
                            n_lo, n_hi = max(m - 1, 0), min(m, nqb - 1)
                            nq = n_hi - n_lo + 1
                            kc0 = 1024 + (128 * (qb0 + m) - 64) * d + r
                            qc0 = 128 * (qb0 + n_lo) * d + r
                            p = sp_[ip % 4][:, 0:256]
                            pn = f'asp{ip % 4}'
                            pt, ptm = pT[ip % 4], pTm[ip % 4]
                            ptn = f'apT{ip % 4}'
                            ip += 1
                            W = nq * 128
                            b0 = 128 if m == 0 else 0
                            bi = 1 if (m == 0 and at_start) else (2 if (m == nkb - 1 and at_end) else 0)

                            def smm(e, p=p, c=c, hh=hh, kc0=kc0, qc0=qc0, W=W, d=d, b0=b0, bi=bi):
                                e.matmul(p[:, 0:W], lhsT=kTw[:, c, kc0:kc0 + 127 * d + 1:d],
                                         rhs=qTsh[hh // 64][:, c, qc0:qc0 + (W - 1) * d + 1:d], start=True, stop=False)
                                return e.matmul(p[:, 0:W], lhsT=g.identB[:], rhs=band[:, bi, b0:b0 + W],
                                                start=False, stop=True)
                            s.op('pe', smm, reads=['kTw', 'qTs', 'band', 'identB'], writes=[pn])
                            s.op('act', lambda e, p=p, ptm=ptm, W=W: e.activation(out=ptm[:, 0:W], in_=p[:, 0:W],
                                                                                 func=AF.Exp, scale=0.125),
                                 reads=[pn], writes=[ptn + 'm'])

                            def stage2(n_lo=n_lo, n_hi=n_hi, h=h, v1=v1, vk=vk, m=m, ptm=ptm, ptn=ptn, d=d, r=r,
                                       qb0=qb0):
                                for n in range(n_lo, n_hi + 1):
                                    o = ot[n % 4][:, 0:128]
                                    on = f'aot{n % 4}'
                                    col = (n - n_lo) * 128
                                    s.op('pe', lambda e, o=o, col=col, n=n: e.matmul(
                                        o, lhsT=v1[:, m, h, :], rhs=ptm[:, col:col + 128],
                                        start=(m == n), stop=(m == n + 1)),
                                        reads=[vk, ptn + 'm'], writes=[on])
                                    if m == n + 1:
                                        a0 = 128 * (qb0 + n) * d + r
                                        av = ACC[:, h, a0:a0 + 127 * d + 1:d]
                                        if d == 1:
                                            s.op('dve', lambda e, o=o, av=av: e.tensor_copy(out=av, in_=o),
                                                 reads=[on], writes=[f'ACC{h}'])
                                        else:
                                            s.op('dve', lambda e, o=o, av=av: e.tensor_tensor(out=av, in0=o, in1=av,
                                                                                              op=ALU.add),
                                                 reads=[on, f'ACC{h}'], writes=[f'ACC{h}'])
                            pend.append(stage2)
                            flush(LAG)
                flush(0)
                for h in range(8):
                    bo = h % 2
                    s.op('act', lambda e, h=h, bo=bo: e.activation(out=rd[bo][:], in_=ACC[64:128, h, :], func=AF.Ln),
                         reads=[f'ACC{h}'], writes=[f'ard{bo}'])
                    s.op('act', lambda e, bo=bo: e.activation(out=rd[bo][:], in_=rd[bo][:], func=AF.Exp, scale=-1.0),
                         reads=[f'ard{bo}'], writes=[f'ard{bo}'])
                    s.op('pool', lambda e, h=h, bo=bo: e.tensor_tensor(out=ob[bo][:], in0=ACC[0:64, h, :],
                                                                      in1=rd[bo][:], op=ALU.mult),
                         reads=[f'ACC{h}', f'ard{bo}'], writes=[f'aob{bo}'])
                    t0 = base + seg0
                    s.dma('sp', f'p2a_ob{bo}', S.oT[h * 64:(h + 1) * 64, t0:t0 + 2048], ob[bo][:],
                          reads=[f'aob{bo}'])
        s.barrier()


def post_norm_residual(g, pre, yp, ypk, xres, xkey, GB, gbk, out, outkey, ss, rstd, junk, tmp, par=0):
    s = g.s
    sp = str(par)
    tm = tmp[par] if isinstance(tmp, (list, tuple)) else tmp
    s.op('act', lambda e: e.activation(out=junk[:], in_=yp[:, :], func=AF.Square, accum_out=ss[:, par:par + 1]),
         reads=[ypk], writes=[pre + 'junk', pre + 'ss' + sp])
    rsqrt_act(g, rstd[:, par:par + 1], ss[:, par:par + 1], 1.0 / D, [pre + 'ss' + sp], [pre + 'rstd' + sp])
    s.op('dve', lambda e: e.scalar_tensor_tensor(out=tm[:], in0=yp[:, :], scalar=rstd[:, par:par + 1], in1=GB[:],
                                                 op0=ALU.mult, op1=ALU.mult),
         reads=[ypk, pre + 'rstd' + sp, gbk], writes=[pre + 'tmp' + sp])
    s.op('pool' if par == 0 else 'dve', lambda e: e.tensor_tensor(out=out, in0=tm[:], in1=xres, op=ALU.add),
         reads=[pre + 'tmp' + sp, xkey], writes=[outkey])


def load_GB(g, pre, GB, gb, ngb, l, gi, j):
    s = g.s
    s.dma('sp', pre + 'gb', gb[:], g.S.gates[l, gi, j:j + 1, :].partition_broadcast(128), writes=[pre + 'gb'])
    s.op('dve', lambda e: e.tensor_tensor(out=GB[:], in0=gb[:], in1=ngb[:], op=ALU.mult),
         reads=[pre + 'gb', pre + 'ngb'], writes=[pre + 'GB'])


def phase3(g, l, oT_d, w_out_d, xin_d, xout_d):
    nc, s = g.nc, g.s
    pre = f'p3{l}'
    with ExitStack() as st:
        sb = lambda n, sh, dt: st.enter_context(nc.sbuf_tensor(pre + n, sh, dt))
        pm = lambda n, sh, dt: st.enter_context(nc.psum_tensor(pre + n, sh, dt))
        wo = sb("wo", [128, 8, D], BF16)
        oT = [sb(f"oT{i}", [128, 8, 512], BF16) for i in range(2)]
        xt = [sb(f"xt{i}", [128, 4, D], F32) for i in range(2)]
        xo = [sb(f"xo{i}", [128, 4, D], F32) for i in range(2)]
        gb = sb("gb", [128, D], F32)
        ngb = sb("ngb", [128, D], F32)
        GB = sb("GB", [128, D], F32)
        junk = sb("junk", [128, D], BF16)
        tmp = [sb(f"tmp{i}", [128, D], F32) for i in range(2)]
        ss = sb("ss", [128, 2], F32)
        rstd = sb("rstd", [128, 2], F32)
        yp = [pm(f"yp{i}", [128, D], F32) for i in range(2)]
        load_w_bf16(g, pre + 'wo', wo, w_out_d, 8, D)
        s.dma('sp', pre + 'ngb', ngb[:], g.I.norm_g[l * 4 + 1:l * 4 + 2, :].partition_broadcast(128),
              writes=[pre + 'ngb'])
        tiles = []
        for j, Sj in enumerate(g.seqs):
            for t in range(Sj // 512):
                tiles.append((j, g.offs[j] + t * 512))

        def load(i):
            j, t0 = tiles[i]
            b = i % 2
            s.dma('sp', pre + f'oT{b}', oT[b][:], oT_d[:, t0:t0 + 512].rearrange("(c p) t -> p c t", p=128),
                  writes=[pre + f'oT{b}'])
            s.dma('sp', pre + f'xt{b}', xt[b][:], xin_d[t0:t0 + 512, :].rearrange("(b p) d -> p b d", p=128),
                  writes=[pre + f'xt{b}'])
        load(0)
        curj = -1
        iy = 0
        for i, (j, t0) in enumerate(tiles):
            b = i % 2
            if i + 1 < len(tiles):
                load(i + 1)
            if j != curj:
                load_GB(g, pre, GB, gb, ngb, l, 0, j)
                curj = j
            for blk in range(4):
                y_ = yp[iy % 2]
                yk = pre + f'yp{iy % 2}'
                iy += 1

                def mm(e, y_=y_, b=b, blk=blk):
                    for nh in range(2):
                        for k in range(8):
                            r = e.matmul(y_[:, nh * 512:(nh + 1) * 512], lhsT=oT[b][:, k, blk * 128:(blk + 1) * 128],
                                         rhs=wo[:, k, nh * 512:(nh + 1) * 512], start=(k == 0), stop=(k == 7))
                    return r
                s.op('pe', mm, reads=[pre + f'oT{b}', pre + 'wo'], writes=[yk])
                post_norm_residual(g, pre, y_, yk, xt[b][:, blk, :], pre + f'xt{b}', GB, pre + 'GB',
                                   xo[b][:, blk, :], pre + f'xo{b}', ss, rstd, junk, tmp, par=(iy % 2))
            s.dma('sp', pre + f'xo{b}', xout_d[t0:t0 + 512, :].rearrange("(b p) d -> p b d", p=128), xo[b][:],
                  reads=[pre + f'xo{b}'])
        s.barrier()


def phase4(g, l, xin_d, xout_d):
    nc, s = g.nc, g.s
    pre = f'p4{l}'
    with ExitStack() as st:
        sb = lambda n, sh, dt: st.enter_context(nc.sbuf_tensor(pre + n, sh, dt))
        pm = lambda n, sh, dt: st.enter_context(nc.psum_tensor(pre + n, sh, dt))
        w1 = sb("w1", [128, 8, 4 * D], BF16)
        w2 = sb("w2", [128, 32, D], BF16)
        xt = [sb(f"xt{i}", [128, 2, D], F32) for i in range(2)]
        xn = sb("xn", [128, 2, D], BF16)
        xo = [sb(f"xo{i}", [128, 2, D], F32) for i in range(2)]
        hfT = sb("hfT", [128, 8, 256], BF16)
        h1a = sb("h1a", [128, 2, 256], BF16)
        h1T = sb("h1T", [128, 32, 256], BF16)
        ngb = sb("ngb", [128, D], F32)
        GB = sb("GB", [128, D], F32)
        junk = sb("junk", [128, D], BF16)
        tmp = [sb(f"tmp{i}", [128, D], F32) for i in range(2)]
        ss = sb("ss", [128, 4], F32)
        rstd = sb("rstd", [128, 4], F32)
        ss2 = sb("ss2", [128, 2], F32)
        rstd2 = sb("rstd2", [128, 2], F32)
        tps = [pm("tp0", [128, 8, 256], BF16)]
        hp = [pm(f"hp{i}", [128, 512], F32) for i in range(2)]
        yp = [pm(f"yp{i}", [128, D], F32) for i in range(2)]
        load_w_bf16(g, pre + 'w1', w1, g.I.w_ff1[l], 8, 4 * D)
        load_w_bf16(g, pre + 'w2', w2, g.I.w_ff2[l], 32, D)
        s.dma('sp', pre + 'ngb', ngb[:], g.I.norm_g[l * 4 + 3:l * 4 + 4, :].partition_broadcast(128),
              writes=[pre + 'ngb'])
        tiles = []
        for j, Sj in enumerate(g.seqs):
            for t in range(Sj // 256):
                tiles.append((j, g.offs[j] + t * 256))

        def load(i):
            j, t0 = tiles[i]
            b = i % 2
            s.dma('sp', pre + f'xt{b}', xt[b][:], xin_d[t0:t0 + 256, :].rearrange("(b p) d -> p b d", p=128),
                  writes=[pre + f'xt{b}'])
        load(0)
        curj = -1
        ih = 0
        iy = 0
        def prep_a(i):
            j, t0 = tiles[i]
            b = i % 2
            rms_prep(g, pre, xt[b], xn, ss, rstd, junk, 2, pre + f'xt{b}', pre + 'xn')

        def prep_b(i):
            j, t0 = tiles[i]
            to_featmajor(g, pre, xn, hfT, tps, l, 1, j, 2, pre + 'xn', pre + 'hfT')
        prep_a(0)
        prep_b(0)
        for i, (j, t0) in enumerate(tiles):
            b = i % 2
            if i + 1 < len(tiles):
                load(i + 1)
            if j != curj:
                s.dma('sp', pre + 'GB', GB[:], g.S.gates[l, 1, j:j + 1, :].partition_broadcast(128),
                      writes=[pre + 'GB'])
                s.op('pool', lambda e: e.tensor_tensor(out=GB[:], in0=GB[:], in1=ngb[:], op=ALU.mult),
                     reads=[pre + 'GB', pre + 'ngb'], writes=[pre + 'GB'])
                curj = j
            for fc in range(32):
                if fc == 10 and i + 1 < len(tiles):
                    prep_a(i + 1)
                p = hp[ih % 2]
                pn = pre + f'hp{ih % 2}'
                ha = h1a[:, ih % 2, :]
                hak = pre + f'h1a{ih % 2}'
                ih += 1

                def mm(e, p=p, fc=fc):
                    for k in range(8):
                        r = e.matmul(p[:, 0:256], lhsT=w1[:, k, fc * 128:(fc + 1) * 128], rhs=hfT[:, k, :],
                                     start=(k == 0), stop=(k == 7))
                    return r
                s.op('pe', mm, reads=[pre + 'w1', pre + 'hfT'], writes=[pn])
                s.op('act', lambda e, p=p, ha=ha: e.activation(out=ha, in_=p[:, 0:256], func=AF.Relu),
                     reads=[pn], writes=[hak])
                eng = 'pool' if fc % 2 == 0 else 'dve'
                s.op(eng, lambda e, ha=ha, fc=fc: e.tensor_tensor(out=h1T[:, fc, :], in0=ha, in1=ha, op=ALU.mult),
                     reads=[hak], writes=[pre + f'h1T{fc}'])
            if i + 1 < len(tiles):
                prep_b(i + 1)
            for blk in range(2):
                y_ = yp[iy % 2]
                yk = pre + f'yp{iy % 2}'
                iy += 1

                def mm2(e, y_=y_, blk=blk):
                    for nh in range(2):
                        for k in range(32):
                            r = e.matmul(y_[:, nh * 512:(nh + 1) * 512], lhsT=h1T[:, k, blk * 128:(blk + 1) * 128],
                                         rhs=w2[:, k, nh * 512:(nh + 1) * 512], start=(k == 0), stop=(k == 31))
                    return r
                s.op('pe', mm2, reads=[pre + f'h1T{fc}' for fc in range(32)] + [pre + 'w2'], writes=[yk])
                post_norm_residual(g, pre + 'b', y_, yk, xt[b][:, blk, :], pre + f'xt{b}', GB, pre + 'GB',
                                   xo[b][:, blk, :], pre + f'xo{b}', ss2, rstd2, junk, tmp, par=(iy % 2))
            s.dma('sp', pre + f'xo{b}', xout_d[t0:t0 + 256, :].rearrange("(b p) d -> p b d", p=128), xo[b][:],
                  reads=[pre + f'xo{b}'])
        s.barrier()


def _rope_angles(pos, dim):
    inv_freq = (np.float32(10000.0) ** (-np.arange(0, dim, 2, dtype=np.float32) / np.float32(dim))).astype(np.float32)
    return (pos.astype(np.float32)[:, None] * inv_freq[None, :]).astype(np.float32)


def rope_tables(S):
    pos = np.arange(S)
    angA = _rope_angles(pos, 64)
    ar = _rope_angles(pos // 64, 32)
    ac = _rope_angles(pos % 64, 32)
    ropeA = np.concatenate([np.cos(angA), np.sin(angA)], axis=1).astype(np.float32)
    ropeB = np.concatenate([np.cos(ar), np.cos(ac), np.sin(ar), np.sin(ac)], axis=1).astype(np.float32)
    return ropeA, ropeB


def core_map(inp, xs, cs):
    NS = len(xs)
    SMAX = max(x.shape[0] for x in xs)
    ropeA, ropeB = rope_tables(SMAX)
    c = np.stack(cs)
    cT = c.reshape(NS, 8, 128).transpose(2, 1, 0).reshape(128, 8 * NS)
    ng = np.asarray(inp['norm_g'])
    m = {
        'x': np.concatenate(xs, 0), 'cT': cT,
        'w_in_ab': inp['w_in_ab'][0], 'w_out_ab': inp['w_out_ab'][0], 'w_in_cd': inp['w_in_cd'][0],
        'w_out_cd': inp['w_out_cd'][0], 'w_ada': inp['w_ada'], 'b_ada': np.asarray(inp['b_ada']).reshape(1, -1),
        'normgT': ng.reshape(2, 4, 8, 128).transpose(3, 0, 1, 2).reshape(128, 64),
        'norm_g': ng.reshape(8, 1024), 'w_ff1': inp['w_ff1'], 'w_ff2': inp['w_ff2'],
        'qkg': np.asarray(inp['qk_norm_g']).reshape(1, 128), 'ropeA': ropeA, 'ropeB': ropeB,
        'gate_bias': np.asarray(inp['gate_bias']).reshape(32, 1), 'mhg': np.asarray(inp['mh_norm_g']).reshape(1, 512),
        'sgg': np.asarray(inp['sg_norm_g']).reshape(1, 512),
        'wsT': np.asarray(inp['w_spatial'])[0].transpose(2, 0, 1).reshape(128, 1024),
        'bsT': np.asarray(inp['b_spatial'])[0].T,
    }
    return {k: np.ascontiguousarray(np.asarray(v), dtype=np.float32) for k, v in m.items()}


ALL_PHASES = (0, 1, 2, 3, 4, 5, 6, 7, 8)
_CACHE = {}


def kernel(**inputs):
    inp = {k: np.asarray(v) for k, v in inputs.items()}
    n = 8
    seqs = [8192, 2048, 2048, 2048, 2048]
    if 'nc' not in _CACHE:
        _CACHE['nc'] = build(seqs, set(ALL_PHASES), debug=False)
    nc, g = _CACHE['nc']
    in_maps = []
    for i in range(n):
        xs = [inp['x_prompt'][i]] + [inp['x_sample'][4 * i + k] for k in range(4)]
        cs = [inp['c_prompt'][i]] + [inp['c_sample'][4 * i + k] for k in range(4)]
        in_maps.append(core_map(inp, xs, cs))
    res = run_bass_kernel_spmd(nc, in_maps, core_ids=list(range(n)))
    y_prompt = np.empty((8, 8192, 1024), np.float32)
    y_sample = np.empty((32, 2048, 1024), np.float32)
    for i in range(n):
        y = np.asarray(res.results[i]['y'])
        y_prompt[i] = y[0:8192]
        for k in range(4):
            y_sample[4 * i + k] = y[8192 + 2048 * k:8192 + 2048 * (k + 1)]
    return (y_prompt, y_sample)


def phase5(g):
    nc, s, NS, I, S = g.nc, g.s, g.NS, g.I, g.S
    with ExitStack() as st:
        sb = lambda n, sh, dt: st.enter_context(nc.sbuf_tensor("p5_" + n, sh, dt))
        pm = lambda n, sh, dt: st.enter_context(nc.psum_tensor("p5_" + n, sh, dt))
        wcd = sb("wcd", [128, 8, 3104], BF16)
        wsT = sb("wsT", [128, 8, 128], BF16)
        bsT = sb("bsT", [128, 8], F32)
        sgg = sb("sgg", [128, 512], F32)
        gbias = sb("gbias", [128, 32], F32)
        xt = [sb(f"xt{i}", [128, 4, D], F32) for i in range(2)]
        xn = sb("xn", [128, 4, D], BF16)
        junk = sb("junk", [128, D], BF16)
        ss = sb("ss", [128, 4], F32)
        rstd = sb("rstd", [128, 4], F32)
        hmT = sb("hmT", [128, 8, 512], BF16)
        qk = [sb(f"qk{i}", [128, 1536], BF16) for i in range(2)]
        vst = [sb(f"vst{i}", [128, 4, 512], BF16) for i in range(2)]
        kst = [sb(f"kst{i}", [128, 4, 512], BF16) for i in range(2)]
        ogst = [sb(f"ogst{i}", [128, 4, 512], F32) for i in range(2)]
        gst = [sb(f"gst{i}", [128, 4, 32], F32) for i in range(2)]
        stg = [sb(f"stg{i}", [128, 12, 512], BF16) for i in range(2)]
        ugs = [sb(f"ug{i}", [128, 512], F32) for i in range(2)]
        vg = sb("vg", [128, 512], F32)
        vc = sb("vc", [128, 512], F32)
        vln = sb("vln", [128, 512], BF16)
        sgt = sb("sgt", [128, 512], F32)
        lns = sb("lns", [128, 4], F32)
        tps = [pm("tp0", [128, 8, 256], BF16)]
        zp = [pm(f"zp{i}", [128, 512], F32) for i in range(2)]
        qT = pm("qT", [128, 16, 128], BF16)
        sgp = pm("sgp", [128, 512], F32)
        for k in range(8):
            for (d0, s0, w) in ((0, 0, 1024), (1024, 1024, 1024), (2048, 2080, 512), (2560, 2592, 512),
                                (3072, 2048, 32)):
                s.dma('pool', 'p5_wcd', wcd[:, k, d0:d0 + w], I.w_in_cd[k * 128:(k + 1) * 128, s0:s0 + w],
                      writes=['wcd'])
        s.dma('pool', 'p5_wsT', wsT[:].rearrange("q g p -> q (g p)"), I.wsT[:, :], writes=['wsT'])
        s.dma('sp', 'p5_bsT', bsT[:], I.bsT[:, :], writes=['bsT'])
        s.dma('sp', 'p5_sgg', sgg[:], I.sgg[0:1, :].partition_broadcast(128), writes=['sgg'])
        s.dma('sp', 'p5_gbias', gbias[:], I.gate_bias.rearrange("a b -> b a")[0:1, :].partition_broadcast(128),
              writes=['gbias'])
        tiles = []
        for j, Sj in enumerate(g.seqs):
            for t in range(Sj // 512):
                tiles.append((j, g.offs[j] + t * 512))

        def load(i):
            j, t0 = tiles[i]
            b = i % 2
            s.dma('sp', f'p5_xt{b}', xt[b][:], S.x1[t0:t0 + 512, :].rearrange("(b p) d -> p b d", p=128),
                  writes=[f'xt{b}'])
        load(0)
        zi = 0
        pend = []
        for i, (j, t0) in enumerate(tiles):
            b = i % 2
            if i + 1 < len(tiles):
                load(i + 1)
            rms_prep(g, 'p5', xt[b], xn, ss, rstd, junk, 4, f'xt{b}', 'xn')
            to_featmajor(g, 'p5', xn, hmT, tps, 1, 0, j, 4, 'xn', 'hmT')
            for blk in range(4):
                q = qk[blk % 2]
                qkk = f'qk{blk % 2}'
                if blk > 0:
                    pass
                for c in range(7):
                    n0 = c * 512
                    nw = min(512, 3104 - n0)
                    p = zp[zi % 2]
                    pn = f'zp{zi % 2}'
                    zi += 1

                    def mm(e, p=p, n0=n0, nw=nw, blk=blk):
                        for k in range(8):
                            r = e.matmul(p[:, 0:nw], lhsT=hmT[:, k, blk * 128:(blk + 1) * 128],
                                         rhs=wcd[:, k, n0:n0 + nw], start=(k == 0), stop=(k == 7))
                        return r
                    s.op('pe', mm, reads=['hmT', 'wcd'], writes=[pn])
                    if c == 0:
                        s.op('act', lambda e, p=p, q=q: e.activation(out=q[:, 0:512], in_=p[:, :], func=AF.Copy),
                             reads=[pn], writes=[qkk + 'q'])
                    elif c == 1:
                        s.op('act', lambda e, p=p, q=q: e.activation(out=q[:, 512:1024], in_=p[:, :], func=AF.Copy,
                                                                     scale=0.125), reads=[pn], writes=[qkk + 'k'])
                        s.op('pool', lambda e, q=q, blk=blk, b=b: e.tensor_copy(out=kst[b][:, blk, :],
                                                                               in_=q[:, 512:1024]),
                             reads=[qkk + 'k'], writes=[f'kst{b}'])
                    elif c == 2:
                        s.op('act', lambda e, p=p, blk=blk, b=b: e.activation(out=vst[b][:, blk, :], in_=p[:, :],
                                                                              func=AF.Copy),
                             reads=[pn], writes=[f'vst{b}'])
                    elif c == 3:
                        s.op('act', lambda e, p=p, blk=blk, b=b: e.activation(out=ogst[b][:, blk, :], in_=p[:, :],
                                                                              func=AF.Sigmoid),
                             reads=[pn], writes=[f'ogst{b}'])
                    elif c == 4:
                        s.op('act', lambda e, p=p, blk=blk: e.activation(out=ugs[blk % 2][:], in_=p[:, :],
                                                                         func=AF.Gelu_apprx_tanh),
                             reads=[pn], writes=[f'ug{blk % 2}'])
                    elif c == 5:
                        s.op('act', lambda e, p=p: e.activation(out=vg[:], in_=p[:, :], func=AF.Gelu_apprx_tanh,
                                                                accum_out=lns[:, 0:1]),
                             reads=[pn], writes=['vg', 'lns0'])
                    else:
                        s.op('dve', lambda e, p=p, blk=blk, b=b: e.tensor_tensor(
                            out=gst[b][:, blk, :], in0=p[:, 0:32], in1=gbias[:], op=ALU.add),
                            reads=[pn, 'gbias'], writes=[f'gst{b}'])
                while pend:
                    pend.pop(0)()
                s.op('dve', lambda e: e.tensor_scalar(out=lns[:, 1:2], in0=lns[:, 0:1], scalar1=-1.0 / 512,
                                                      scalar2=None, op0=ALU.mult), reads=['lns0'], writes=['lns1'])
                s.op('act', lambda e: e.activation(out=vc[:], in_=vg[:], func=AF.Identity, bias=lns[:, 1:2]),
                     reads=['vg', 'lns1'], writes=['vc'])
                s.op('act', lambda e: e.activation(out=junk[:, 0:512], in_=vc[:], func=AF.Square,
                                                   accum_out=lns[:, 2:3]), reads=['vc'], writes=['lns2', 'p5junk'])
                rsqrt_act(g, lns[:, 3:4], lns[:, 2:3], 1.0 / 512, ['lns2'], ['lns3'])
                s.op('dve', lambda e: e.scalar_tensor_tensor(out=vln[:], in0=vc[:], scalar=lns[:, 3:4], in1=sgg[:],
                                                             op0=ALU.mult, op1=ALU.mult),
                     reads=['vc', 'lns3', 'sgg'], writes=['vln'])

                def tail(q=q, qkk=qkk, blk=blk, b=b):
                    def sgm(e):
                        for grp in range(8):
                            r = e.matmul(sgp[:, grp * 64:(grp + 1) * 64], lhsT=wsT[:, grp, :],
                                         rhs=vln[:, grp * 64:(grp + 1) * 64], start=True, stop=True)
                        return r
                    s.op('pe', sgm, reads=['wsT', 'vln'], writes=['sgp'])
                    s.op('dve', lambda e: e.tensor_tensor(out=sgt[:].rearrange("p (g e) -> p g e", e=64),
                                                          in0=sgp[:, :].rearrange("p (g e) -> p g e", e=64),
                                                          in1=bsT[:, :].unsqueeze(2).to_broadcast([128, 8, 64]),
                                                          op=ALU.add), reads=['sgp', 'bsT'], writes=['sgt'])
                    s.op('pool', lambda e, q=q: e.tensor_tensor(out=q[:, 1024:1536], in0=sgt[:], in1=ugs[blk % 2][:],
                                                                op=ALU.mult),
                         reads=['sgt', f'ug{blk % 2}'], writes=[qkk + 'd'])

                    def trq(e, q=q):
                        for c in range(12):
                            r = e.transpose(out=qT[:, c, :], in_=q[:, c * 128:(c + 1) * 128], identity=g.identB[:])
                        return r
                    s.op('pe', trq, reads=[qkk + 'q', qkk + 'k', qkk + 'd', 'identB'], writes=['qT'])
                    s.op('act', lambda e, blk=blk, b=b: e.activation(out=stg[b][:, :, blk * 128:(blk + 1) * 128],
                                                                     in_=qT[:, 0:12, :], func=AF.Copy),
                         reads=['qT'], writes=[f'stg{b}'])

                pend.append(tail)
            while pend:
                pend.pop(0)()
            for (dst, r0, c0, n) in ((S.qcT, 0, 0, 4), (S.kcT, 0, 4, 4), (S.o2T, 512, 8, 4)):
                s.dma('sp', f'p5_stg{b}', dst[r0:r0 + 512, t0:t0 + 512].rearrange("(c p) t -> p c t", p=128),
                      stg[b][:, c0:c0 + n, :], reads=[f'stg{b}'])
            tm = lambda d: d[t0:t0 + 512, :].rearrange("(b p) d -> p b d", p=128)
            s.dma('sp', f'p5_vst{b}', tm(S.vc), vst[b][:], reads=[f'vst{b}'])
            s.dma('sp', f'p5_kst{b}', tm(S.kc), kst[b][:], reads=[f'kst{b}'])
            s.dma('sp', f'p5_ogst{b}', tm(S.ogs), ogst[b][:], reads=[f'ogst{b}'])
            s.dma('sp', f'p5_gst{b}', tm(S.gtok), gst[b][:], reads=[f'gst{b}'])
        s.barrier()


def phase6(g):
    nc, s, I, S = g.nc, g.s, g.I, g.S
    SMAX = max(g.seqs)
    NCH = SMAX // 128
    with ExitStack() as st:
        sb = lambda n, sh, dt: st.enter_context(nc.sbuf_tensor("p6_" + n, sh, dt))
        pm = lambda n, sh, dt: st.enter_context(nc.psum_tensor("p6_" + n, sh, dt))
        SU = sb("SU", [128, 128], F32)
        SL = sb("SL", [128, 128], F32)
        ONES = sb("ONES", [128, 128], F32)
        maskF = sb("maskF", [128, 128], BF16)
        maskB = sb("maskB", [128, 128], BF16)
        mhg = sb("mhg", [128, 512], F32)
        gz = sb("gz", [128, NCH, 32], F32)
        l1 = [sb(f"l1{d}", [128, NCH * 8], F32) for d in range(2)]
        tmpg = sb("tmpg", [128, NCH * 8], F32)
        E = [sb(f"E{d}", [128, NCH * 8], F32) for d in range(2)]
        THR = [sb(f"THR{d}", [128, NCH * 8], F32) for d in range(2)]
        DEC = [sb(f"DEC{d}", [128, NCH * 8], F32) for d in range(2)]
        QTh = [sb(f"QTh{i}", [128, SMAX], BF16) for i in range(2)]
        KT = sb("KT", [128, SMAX], BF16)
        Ktok = sb("Ktok", [128, NCH, 128], BF16)
        V1 = sb("V1", [128, NCH, 2, 65], BF16)
        OGS = sb("OGS", [128, NCH, 128], F32)
        HF = sb("HF", [128, NCH, 128], F32)
        Wst = [sb(f"W{d}", [128, 65], F32) for d in range(2)]
        U = [sb(f"U{i}", [128, 65], BF16) for i in range(4)]
        Kw = [sb(f"Kw{i}", [128, 64], BF16) for i in range(4)]
        PT = [sb(f"PT{i}", [128, 128], BF16) for i in range(4)]
        rr = [sb(f"rr{i}", [128, 2], F32) for i in range(4)]
        sqt = sb("sqt", [128, 4, 128], F32)
        ssq = sb("ssq", [128, 8], F32)
        hn = sb("hn", [128, 4, 128], F32)
        hb = sb("hb", [128, 4, 128], BF16)
        stg = [sb(f"stg{i}", [128, 512], BF16) for i in range(2)]
        Bk = [pm(f"Bk{i}", [128, 512], F32) for i in range(6)]
        GP = Bk[0:4]
        trP = pm("trP", [128, 4, 128], BF16)
        DECp = [sb(f"DECp{d}", [128, NCH, 4], F32) for d in range(2)]
        Upair = [sb(f"Up{i}", [128, 65], BF16) for i in range(4)]
        Kwp = [sb(f"Kwp{i}", [128, 2, 64], BF16) for i in range(4)]
        PTp = [sb(f"PTp{i}", [128, 2, 128], BF16) for i in range(4)]
        rp = [sb(f"rp{i}", [128, 4], F32) for i in range(4)]
        htmp = [sb(f"htmp{i}", [128, 2, 64], F32) for i in range(2)]

        def tri(t, nm, cm, step, base):
            s.op('pool', lambda e: e.memset(t[:], 1.0), writes=[nm])
            s.op('pool', lambda e: e.affine_select(out=t[:], in_=t[:], pattern=[[step, 128]], compare_op=ALU.is_ge,
                                                   fill=0.0, base=base, channel_multiplier=cm),
                 reads=[nm], writes=[nm])
        tri(SU, 'SU', 1, -1, -1)
        tri(SL, 'SL', -1, 1, -1)
        tri(maskF, 'maskF', -1, 1, 0)
        tri(maskB, 'maskB', 1, -1, 0)
        s.op('pool', lambda e: e.memset(ONES[:], 1.0), writes=['ONES'])
        s.op('pool', lambda e: e.memset(QTh[0][64:128, :], 0.0), writes=['QT'])
        s.op('pool', lambda e: e.memset(QTh[1][0:64, :], 0.0), writes=['QT'])
        s.op('pool', lambda e: e.memset(V1[:, :, :, 64:65], 1.0), writes=['V1'])
        s.dma('sp', 'p6_mhg', mhg[:], I.mhg[0:1, :].partition_broadcast(128), writes=['mhg'])
        islot = 0
        istg = 0
        for j, Sj in enumerate(g.seqs):
            base = g.offs[j]
            nch = Sj // 128
            N8 = nch * 8
            s.dma('sp', 'p6_gz', gz[:, 0:nch, :], S.gtok[base:base + Sj, :].rearrange("(c p) d -> p c d", p=128),
                  writes=['gz'])
            for d in range(2):
                fcol = 8 + 16 * d
                icol = 16 * d
                l1v = l1[d][:, 0:N8].rearrange("p (c h) -> p c h", h=8)
                s.op('act', lambda e, nch=nch, N8=N8, l1v=l1v, fcol=fcol: e.activation(out=l1v, in_=gz[:, 0:nch, fcol:fcol + 8],
                                                                       func=AF.Exp, scale=-1.0),
                     reads=['gz'], writes=[f'l1{d}'])
                s.op('act', lambda e, nch=nch, N8=N8, d=d: e.activation(out=l1[d][:, 0:N8], in_=l1[d][:, 0:N8], func=AF.Ln, bias=1.0),
                     reads=[f'l1{d}'], writes=[f'l1{d}'])
                Gp, Tp = GP[2 * d], GP[2 * d + 1]
                tri_m = SU if d == 0 else SL
                s.op('pe', lambda e, nch=nch, N8=N8, Gp=Gp, tri_m=tri_m, d=d: e.matmul(Gp[:, 0:N8], lhsT=tri_m[:], rhs=l1[d][:, 0:N8],
                                                                      start=True, stop=True),
                     reads=['SU', 'SL', f'l1{d}'], writes=[f'B{2 * d}'])
                s.op('pe', lambda e, nch=nch, N8=N8, Tp=Tp, d=d: e.matmul(Tp[:, 0:N8], lhsT=ONES[:], rhs=l1[d][:, 0:N8],
                                                         start=True, stop=True),
                     reads=['ONES', f'l1{d}'], writes=[f'B{2 * d + 1}'])
                s.op('dve', lambda e, nch=nch, N8=N8, Gp=Gp, icol=icol: e.tensor_tensor(
                    out=tmpg[:, 0:N8].rearrange("p (c h) -> p c h", h=8), in0=gz[:, 0:nch, icol:icol + 8],
                    in1=Gp[:, 0:N8].rearrange("p (c h) -> p c h", h=8), op=ALU.subtract),
                    reads=['gz', f'B{2 * d}'], writes=['tmpg'])
                s.op('act', lambda e, nch=nch, N8=N8, d=d: e.activation(out=E[d][:, 0:N8], in_=tmpg[:, 0:N8], func=AF.Exp),
                     reads=['tmpg'], writes=[f'E{d}'])
                s.op('act', lambda e, nch=nch, N8=N8, d=d, Gp=Gp: e.activation(out=THR[d][:, 0:N8], in_=Gp[:, 0:N8], func=AF.Exp,
                                                              scale=-1.0), reads=[f'B{2 * d}'], writes=[f'THR{d}'])
                s.op('act', lambda e, nch=nch, N8=N8, d=d, Tp=Tp: e.activation(out=DEC[d][:, 0:N8], in_=Tp[:, 0:N8], func=AF.Exp,
                                                              scale=-1.0), reads=[f'B{2 * d + 1}'], writes=[f'DEC{d}'])
            for d in range(2):
                for hh in range(2):
                    p0 = hh * 64
                    s.op('pool', lambda e, nch=nch, N8=N8, d=d, hh=hh, p0=p0: e.tensor_copy(
                        out=DECp[d][p0:p0 + 64, 0:nch, :],
                        in_=DEC[d][p0:p0 + 64, 0:N8].rearrange("p (c hp two) -> p c hp two", hp=4, two=2)[:, :, :, hh]),
                        reads=[f'DEC{d}'], writes=[f'DECp{d}'])
            for hp in range(4):
                r0 = hp * 128
                s.dma('sp', 'p6_QT', QTh[0][0:64, 0:Sj], S.qcT[r0:r0 + 64, base:base + Sj], writes=['QT'])
                s.dma('sp', 'p6_QT', QTh[1][64:128, 0:Sj], S.qcT[r0 + 64:r0 + 128, base:base + Sj], writes=['QT'])
                s.dma('sp', 'p6_KT', KT[:, 0:Sj], S.kcT[r0:r0 + 128, base:base + Sj], writes=['KT'])
                s.dma('sp', 'p6_Ktok', Ktok[:, 0:nch, :],
                      S.kc[base:base + Sj, r0:r0 + 128].rearrange("(c p) d -> p c d", p=128), writes=['Ktok'])
                for hh in range(2):
                    s.dma('sp', 'p6_V1', V1[:, 0:nch, hh, 0:64],
                          S.vc[base:base + Sj, r0 + hh * 64:r0 + hh * 64 + 64].rearrange("(c p) e -> p c e", p=128),
                          writes=['V1'])
                s.dma('sp', 'p6_OGS', OGS[:, 0:nch, :],
                      S.ogs[base:base + Sj, r0:r0 + 128].rearrange("(c p) d -> p c d", p=128), writes=['OGS'])
                pend = []

                def flush(n):
                    while len(pend) > n:
                        pend.pop(0)()
                for ci in range(nch):
                    for d in range(2):
                        c = ci if d == 0 else nch - 1 - ci
                        first = (ci == 0)
                        last = (ci == nch - 1)
                        mask = maskF if d == 0 else maskB
                        sl = islot % 4
                        bkp = islot % 2
                        islot += 1
                        col0 = c * 8 + hp * 2
                        cs = slice(c * 128, (c + 1) * 128)
                        spb, Ob, dUb = Bk[bkp], Bk[2 + bkp], Bk[4 + bkp]
                        spk, Ok, dUk = f'B{bkp}', f'B{2 + bkp}', f'B{4 + bkp}'

                        def st1(spb=spb, spk=spk, cs=cs, sl=sl, c=c, d=d, col0=col0, mask=mask):
                            def mm(e):
                                for hh in range(2):
                                    p0 = hh * 64
                                    r = e.matmul(spb[:, hh * 128:(hh + 1) * 128], lhsT=KT[:, cs],
                                                 rhs=QTh[hh][:, cs], start=True, stop=True)
                                return r
                            s.op('pe', mm, reads=['KT', 'QT'], writes=[spk])
                            for hh in range(2):
                                s.op('act', lambda e, hh=hh: e.activation(
                                    out=Kwp[sl][:, hh, :], in_=Ktok[:, c, hh * 64:(hh + 1) * 64], func=AF.Copy,
                                    scale=E[d][:, col0 + hh:col0 + hh + 1]),
                                    reads=['Ktok', f'E{d}'], writes=[f'Kwp{sl}'])
                            for hh in range(2):
                                s.op('dve', lambda e, hh=hh: e.scalar_tensor_tensor(
                                    out=PTp[sl][:, hh, :], in0=spb[:, hh * 128:(hh + 1) * 128],
                                    scalar=E[d][:, col0 + hh:col0 + hh + 1], in1=mask[:], op0=ALU.mult, op1=ALU.mult),
                                    reads=[spk, f'E{d}', 'maskF', 'maskB'], writes=[f'PTp{sl}'])

                        firstw = (ci < nch // 2)

                        def st2(Ob=Ob, Ok=Ok, dUb=dUb, dUk=dUk, cs=cs, sl=sl, c=c, d=d, col0=col0, first=first,
                                last=last, hp=hp, firstw=firstw):
                            wk = f'W{d}'
                            u_ = Upair[sl]
                            if not first:
                                s.op('act', lambda e: e.activation(out=u_[:], in_=Wst[d][:], func=AF.Copy,
                                                                   scale=DECp[d][:, c, hp:hp + 1]),
                                     reads=[wk, f'DECp{d}'], writes=[f'Up{sl}'])

                            def omm(e):
                                for hh in range(2):
                                    p0 = hh * 64
                                    r = e.matmul(Ob[:, hh * 128:hh * 128 + 65], lhsT=PTp[sl][:, hh, :],
                                                 rhs=V1[:, c, hh, :], start=True, stop=first)
                                    if not first:
                                        r = e.matmul(Ob[:, hh * 128:hh * 128 + 65], lhsT=QTh[hh][:, cs],
                                                     rhs=u_[:, :], start=False, stop=True)
                                return r
                            s.op('pe', omm, reads=[f'PTp{sl}', 'V1', 'QT', f'Up{sl}'], writes=[Ok])
                            if not last:
                                def dmm(e):
                                    for hh in range(2):
                                        p0 = hh * 64
                                        r = e.matmul(dUb[p0:p0 + 64, 0:65], lhsT=Kwp[sl][:, hh, :],
                                                     rhs=V1[:, c, hh, :], start=True, stop=True)
                                    return r
                                s.op('pe', dmm, reads=[f'Kwp{sl}', 'V1'], writes=[dUk])
                                if first:
                                    s.op('dve', lambda e: e.tensor_copy(out=Wst[d][:], in_=dUb[:, 0:65]),
                                         reads=[dUk], writes=[wk])
                                else:
                                    s.op('dve', lambda e: e.scalar_tensor_tensor(
                                        out=Wst[d][:], in0=Wst[d][:], scalar=DECp[d][:, c, hp:hp + 1],
                                        in1=dUb[:, 0:65], op0=ALU.mult, op1=ALU.add),
                                        reads=[wk, f'DECp{d}', dUk], writes=[wk])
                            if os.environ.get('P6A'):
                                return
                            r_ = rp[sl]
                            Ov = Ob[:, 0:256].rearrange("p (h x) -> p h x", x=128)
                            s.op('act', lambda e: e.activation(out=r_[:, 0:2].unsqueeze(2), in_=Ov[:, :, 64:65],
                                                               func=AF.Abs), reads=[Ok], writes=[f'rp{sl}'])
                            s.op('dve', lambda e: e.tensor_tensor(out=r_[:, 0:2], in0=r_[:, 0:2],
                                                                  in1=THR[d][:, col0:col0 + 2], op=ALU.max),
                                 reads=[f'rp{sl}', f'THR{d}'], writes=[f'rp{sl}'])
                            s.op('dve', lambda e: e.reciprocal(out=r_[:, 2:4], in_=r_[:, 0:2]),
                                 reads=[f'rp{sl}'], writes=[f'rp{sl}b'])
                            hfv = HF[:, c, :].rearrange("p (h x) -> p h x", x=64)
                            rb = r_[:, 2:4].unsqueeze(2).to_broadcast([128, 2, 64])
                            if firstw:
                                s.op('dve', lambda e: e.tensor_tensor(out=hfv, in0=Ov[:, :, 0:64], in1=rb, op=ALU.mult),
                                     reads=[Ok, f'rp{sl}b'], writes=[f'HF{c}'])
                            else:
                                ht = htmp[sl % 2]
                                s.op('dve', lambda e: e.tensor_tensor(out=ht[:], in0=Ov[:, :, 0:64], in1=rb, op=ALU.mult),
                                     reads=[Ok, f'rp{sl}b'], writes=[f'htmp{sl % 2}'])
                                s.op('pool', lambda e: e.tensor_tensor(out=hfv, in0=hfv, in1=ht[:], op=ALU.add),
                                     reads=[f'htmp{sl % 2}', f'HF{c}'], writes=[f'HF{c}'])
                        st1()
                        pend.append(st2)
                        flush(int(os.environ.get('LAG6', '1')))
                flush(0)
                for cg in range(nch // 4):
                    hv = HF[:, cg * 4:(cg + 1) * 4, :]
                    hkeys = [f'HF{c}' for c in range(cg * 4, cg * 4 + 4)]
                    s.op('act', lambda e, hv=hv: e.activation(out=sqt[:], in_=hv, func=AF.Square),
                         reads=hkeys, writes=['sqt'])
                    s.op('dve', lambda e: e.tensor_reduce(out=ssq[:], in_=sqt[:].rearrange("p c (h e) -> p (c h) e", e=64),
                                                          axis=AX.X, op=ALU.add), reads=['sqt'], writes=['ssq'])
                    rsqrt_act(g, ssq[:], ssq[:], 1.0 / 64, ['ssq'], ['ssq'])
                    s.op('dve', lambda e, hv=hv: e.tensor_tensor(
                        out=hn[:].rearrange("p c (h e) -> p (c h) e", e=64),
                        in0=hv.rearrange("p c (h e) -> p (c h) e", e=64),
                        in1=ssq[:].unsqueeze(2).to_broadcast([128, 8, 64]), op=ALU.mult),
                        reads=hkeys + ['ssq'], writes=['hn'])
                    s.op('pool', lambda e, r0=r0: e.tensor_tensor(out=hn[:], in0=hn[:],
                                                                  in1=bc(mhg[:, r0:r0 + 128], [128, 4, 128]), op=ALU.mult),
                         reads=['hn', 'mhg'], writes=['hn'])
                    s.op('pool', lambda e, cg=cg: e.tensor_tensor(out=hb[:], in0=hn[:], in1=OGS[:, cg * 4:(cg + 1) * 4, :],
                                                                  op=ALU.mult), reads=['hn', 'OGS'], writes=['hb'])

                    def trh(e):
                        for i in range(4):
                            r = e.transpose(out=trP[:, i, :], in_=hb[:, i, :], identity=g.identB[:])
                        return r
                    s.op('pe', trh, reads=['hb', 'identB'], writes=['trP'])
                    bs = istg % 2
                    istg += 1
                    s.op('act', lambda e, bs=bs: e.activation(out=stg[bs][:], in_=trP[:, :, :].rearrange("p c t -> p (c t)"),
                                                              func=AF.Copy), reads=['trP'], writes=[f'stg{bs}'])
                    t0 = base + cg * 512
                    s.dma('sp', f'p6_stg{bs}', S.o2T[r0:r0 + 128, t0:t0 + 512], stg[bs][:], reads=[f'stg{bs}'])
        s.barrier()
```

```python
import numpy as np
import os
from contextlib import ExitStack
import concourse.bass as bass
import concourse.mybir as mybir
from concourse.bass_utils import run_bass_kernel_spmd

F32 = mybir.dt.float32
BF16 = mybir.dt.bfloat16
AF = mybir.ActivationFunctionType
ALU = mybir.AluOpType
AX = mybir.AxisListType
ENGS = ('pe', 'act', 'dve', 'pool', 'sp')
D = 1024
EPS = 1e-6


class Sched:
    def __init__(self, nc, stack):
        self.nc = nc
        self.stack = stack
        self.streams = {e: [] for e in ENGS}
        self.sems = {}
        self.cnt = {}
        self.seen = {e: {} for e in ENGS}
        self.bufs = {}
        self.snap = {}
        self.nwaits = 0
        self.nops = 0

    def sem(self, name):
        if name not in self.sems:
            self.sems[name] = self.stack.enter_context(
                self.nc.semaphore(name.replace(':', '_').replace('/', '_')))
            self.cnt[name] = 0
        return self.sems[name]

    def _need(self, E, ev, waits):
        if ev is None:
            return
        name, val = ev
        if self.seen[E].get(name, 0) >= val:
            return
        if waits.get(name, 0) < val:
            waits[name] = val

    def _collect(self, E, reads, writes):
        waits = {}
        for k in reads:
            b = self.bufs.get(k)
            if b is not None:
                self._need(E, b['w'], waits)
        own = 'e:' + E
        for k in writes:
            b = self.bufs.get(k)
            if b is not None:
                self._need(E, b['w'], waits)
                for ev in b['r']:
                    if ev[0] != own:
                        self._need(E, ev, waits)
        return waits

    def _apply_waits(self, E, waits):
        seen = self.seen[E]
        wl = []
        for name, val in waits.items():
            if seen.get(name, 0) >= val:
                continue
            wl.append((self.sem(name), val))
            sn = self.snap.get((name, val))
            if sn:
                for n2, v2 in sn.items():
                    if seen.get(n2, 0) < v2:
                        seen[n2] = v2
            seen[name] = val
        self.nwaits += len(wl)
        return wl

    def _record(self, ev, reads, writes):
        for k in reads:
            b = self.bufs.get(k)
            if b is None:
                b = self.bufs[k] = {'w': None, 'r': []}
            b['r'].append(ev)
            if len(b['r']) > 64:
                b['r'] = b['r'][-48:]
        for k in writes:
            self.bufs[k] = {'w': ev, 'r': []}

    def op(self, E, fn, reads=(), writes=()):
        waits = self._collect(E, reads, writes)
        if E == 'pe':
            waits.pop('e:pe', None)
        wl = self._apply_waits(E, waits)
        name = 'e:' + E
        s = self.sem(name)
        self.cnt[name] += 1
        val = self.cnt[name]
        ev = (name, val)
        if E == 'pe':
            self.seen[E][name] = val
        self.snap[ev] = dict(self.seen[E])
        self.streams[E].append((wl, fn, s, 1))
        self._record(ev, reads, writes)
        self.nops += 1
        return ev

    def dma(self, Q, semname, out, in_, reads=(), writes=(), **kw):
        waits = self._collect(Q, reads, writes)
        wl = self._apply_waits(Q, waits)
        name = 'd:' + semname
        s = self.sem(name)
        self.cnt[name] += 16
        val = self.cnt[name]
        ev = (name, val)
        self.snap[ev] = dict(self.seen[Q])

        def fn(eng, out=out, in_=in_, kw=kw):
            return eng.dma_start(out=out, in_=in_, **kw)
        self.streams[Q].append((wl, fn, s, 16))
        self._record(ev, reads, writes)
        self.nops += 1
        return ev

    def barrier(self):
        allev = dict(self.cnt)
        for E in ENGS:
            waits = {}
            for name, val in allev.items():
                if val > 0 and self.seen[E].get(name, 0) < val and name != 'e:' + E:
                    waits[name] = val
            wl = self._apply_waits(E, waits)
            if wl:
                self.streams[E].append((wl, None, None, 0))
        self.bufs = {}

    def emit(self):
        nc = self.nc
        self.barrier()
        streams = self.streams
        with nc.Block() as block:
            def run(E):
                def body(eng):
                    for wl, fn, s, inc in streams[E]:
                        for (ws, wv) in wl:
                            eng.wait_ge(ws, wv)
                        if fn is not None:
                            fn(eng).then_inc(s, inc)
                return body
            block.sync(run('sp'))
            block.scalar(run('act'))
            block.vector(run('dve'))
            block.gpsimd(run('pool'))
            block.tensor(run('pe'))


class Ctx:
    pass


def build(seqs, phases, debug=False):
    nc = bass.Bass("TRN2", target_bir_lowering=False)
    NS = len(seqs)
    NT = sum(seqs)
    offs = [sum(seqs[:i]) for i in range(NS)]
    SMAX = max(seqs)
    g = Ctx()
    g.nc, g.seqs, g.NS, g.NT, g.offs, g.debug = nc, seqs, NS, NT, offs, debug

    def din(name, shape, dt=F32):
        return nc.dram_tensor(name, list(shape), dt, kind="ExternalInput").ap()

    def dscr(name, shape, dt):
        return nc.dram_tensor(name, list(shape), dt, kind=("ExternalOutput" if debug else "Internal")).ap()
    g.dscr = dscr
    I = Ctx()
    g.I = I
    I.x = din("x", [NT, D])
    I.cT = din("cT", [128, 8 * NS])
    I.w_in_ab = din("w_in_ab", [D, 2304])
    I.w_out_ab = din("w_out_ab", [D, D])
    I.w_in_cd = din("w_in_cd", [D, 3104])
    I.w_out_cd = din("w_out_cd", [D, D])
    I.w_ada = din("w_ada", [2, D, 6 * D])
    I.b_ada = din("b_ada", [1, 2 * 6 * D])
    I.normgT = din("normgT", [128, 2 * 4 * 8])
    I.norm_g = din("norm_g", [8, D])
    I.w_ff1 = din("w_ff1", [2, D, 4 * D])
    I.w_ff2 = din("w_ff2", [2, 4 * D, D])
    I.qkg = din("qkg", [1, 128])
    I.ropeA = din("ropeA", [SMAX, 64])
    I.ropeB = din("ropeB", [SMAX, 64])
    I.gate_bias = din("gate_bias", [32, 1])
    I.mhg = din("mhg", [1, 512])
    I.sgg = din("sgg", [1, 512])
    I.wsT = din("wsT", [128, 8 * 128])
    I.bsT = din("bsT", [128, 8])
    y = nc.dram_tensor("y", [NT, D], F32, kind="ExternalOutput").ap()
    g.y = y

    Sx = Ctx()
    g.S = Sx
    Sx.gates = dscr("s_gates", [2, 2, NS, D], F32)
    Sx.qaT = dscr("s_qaT", [512, NT], BF16)
    Sx.kaT = dscr("s_kaT", [512, NT], BF16)
    Sx.qbT = dscr("s_qbT", [512, NT], BF16)
    Sx.kbT = dscr("s_kbT", [128, NT], BF16)
    Sx.vA = dscr("s_vA", [NT, 512], BF16)
    Sx.vB = dscr("s_vB", [NT, 128], BF16)
    Sx.oT = dscr("s_oT", [D, NT], BF16)
    Sx.xmid = dscr("s_xmid", [NT, D], F32)
    Sx.x1 = dscr("s_x1", [NT, D], F32)
    Sx.qcT = dscr("s_qcT", [512, NT], BF16)
    Sx.kcT = dscr("s_kcT", [512, NT], BF16)
    Sx.kc = dscr("s_kc", [NT, 512], BF16)
    Sx.vc = dscr("s_vc", [NT, 512], BF16)
    Sx.ogs = dscr("s_ogs", [NT, 512], F32)
    Sx.gtok = dscr("s_gtok", [NT, 32], F32)
    Sx.o2T = dscr("s_o2T", [D, NT], BF16)

    with ExitStack() as st:
        s = Sched(nc, st)
        g.s = s
        g.identB = st.enter_context(nc.sbuf_tensor("identB", [128, 128], BF16))
        g.identF = st.enter_context(nc.sbuf_tensor("identF", [128, 128], F32))
        g.GS = st.enter_context(nc.sbuf_tensor("GS", [128, 2, 2, 8, NS], F32))
        g.SH = st.enter_context(nc.sbuf_tensor("SH", [128, 2, 2, 8, NS], F32))
        g.epsc = st.enter_context(nc.sbuf_tensor("epsc", [128, 2], F32))
        s.op('pool', lambda e: e.memset(g.epsc[:], EPS), writes=['epsc'])
        for t, nm in ((g.identB, 'identB'), (g.identF, 'identF')):
            s.op('pool', lambda e, t=t: e.memset(t[:], 1.0), writes=[nm])
            s.op('pool', lambda e, t=t: e.affine_select(out=t[:], in_=t[:], pattern=[[-1, 128]],
                                                        compare_op=ALU.is_equal, fill=0.0, base=0,
                                                        channel_multiplier=1), reads=[nm], writes=[nm])
        if 0 in phases:
            phase0(g)
        if 1 in phases:
            phase1(g)
        if 2 in phases:
            import os
            if not os.environ.get('SKIPB'):
                phase2(g)
            if not os.environ.get('SKIPA'):
                phase2a(g)
        if 3 in phases:
            phase3(g, 0, Sx.oT, I.w_out_ab, I.x, Sx.xmid)
        if 4 in phases:
            phase4(g, 0, Sx.xmid, Sx.x1 if (5 in phases or debug) else g.y)
        if 5 in phases:
            phase5(g)
        if 6 in phases:
            phase6(g)
        if 7 in phases:
            phase3(g, 1, Sx.o2T, I.w_out_cd, Sx.x1, Sx.xmid)
        if 8 in phases:
            phase4(g, 1, Sx.xmid, g.y)
        s.emit()
    g.stats = (s.nops, s.nwaits)
    return nc, g


def load_w_bf16(g, st_name, wt, w_dram, K, N, n0=0, colmap=None):
    s = g.s
    for k in range(K):
        c = 0
        while c < N:
            w = min(1024, N - c)
            s.dma('pool', st_name, wt[:, k, c:c + w], w_dram[k * 128:(k + 1) * 128, n0 + c:n0 + c + w],
                  writes=[st_name])
            c += w


def phase0(g):
    nc, s, NS, I = g.nc, g.s, g.NS, g.I
    with ExitStack() as st:
        sb = lambda n, sh, dt: st.enter_context(nc.sbuf_tensor(n, sh, dt))
        cTf = sb("p0_cTf", [128, 8 * NS], F32)
        cTb = sb("p0_cTb", [128, 8, NS], BF16)
        wt = [sb(f"p0_w{i}", [128, 8, 3072], BF16) for i in range(2)]
        bada = sb("p0_bada", [NS, 2 * 6 * D], F32)
        modrow = sb("p0_modrow", [NS, 2, 6 * D], F32)
        ngT = sb("p0_ngT", [128, 2, 4, 8], F32)
        modT = sb("p0_modT", [128, 2, 4, 8, NS], F32)
        ps = [st.enter_context(nc.psum_tensor(f"p0_ps{i}", [128, 512], F32)) for i in range(2)]
        pT = st.enter_context(nc.psum_tensor("p0_pT", [128, 4 * 8 * NS], F32))
        s.dma('sp', 'p0_cTf', cTf[:], I.cT[:, :], writes=['cTf'])
        s.dma('sp', 'p0_bada', bada[:], I.b_ada[0:1, :].partition_broadcast(NS), writes=['bada'])
        s.dma('sp', 'p0_ngT', ngT[:], I.normgT[:, :], writes=['ngT'])
        s.op('act', lambda e: e.activation(out=cTb[:].rearrange("p k s -> p (k s)"), in_=cTf[:], func=AF.Silu),
             reads=['cTf'], writes=['cTb'])
        it = 0
        for l in range(2):
            for hf in range(2):
                w = wt[it % 2]
                wn = f"p0_w{it % 2}"
                load_w_bf16(g, wn, w, I.w_ada[l], 8, 3072, n0=hf * 3072)
                for nch in range(6):
                    p = ps[nch % 2]
                    pn = f"p0_ps{nch % 2}"

                    def mm(e, p=p, w=w, nch=nch):
                        for k in range(8):
                            r = e.matmul(p[0:NS, :], lhsT=cTb[:, k, :], rhs=w[:, k, nch * 512:(nch + 1) * 512],
                                         start=(k == 0), stop=(k == 7))
                        return r
                    s.op('pe', mm, reads=['cTb', wn], writes=[pn])
                    c0 = hf * 3072 + nch * 512
                    s.op('dve', lambda e, p=p, l=l, c0=c0: e.tensor_tensor(
                        out=modrow[:, l, c0:c0 + 512], in0=p[0:NS, :],
                        in1=bada[:, l * 6 * D + c0:l * 6 * D + c0 + 512], op=ALU.add),
                        reads=[pn, 'bada'], writes=['modrow'])
                it += 1
        for l in range(2):
            for gi, part in enumerate((2, 5)):
                s.dma('sp', 'p0_gst', g.S.gates[l, gi], modrow[:, l, part * D:(part + 1) * D],
                      reads=['modrow'])
        for l in range(2):
            def tr(e, l=l):
                for pi, part in enumerate((0, 1, 3, 4)):
                    for k in range(8):
                        c0 = part * D + k * 128
                        o = (pi * 8 + k) * NS
                        r = e.transpose(out=pT[:, o:o + NS], in_=modrow[:, l, c0:c0 + 128],
                                        identity=g.identF[0:NS, 0:NS])
                return r
            s.op('pe', tr, reads=['modrow', 'identF'], writes=['p0_pT'])
            s.op('dve', lambda e, l=l: e.tensor_copy(out=modT[:, l].rearrange("p a k s -> p (a k s)"), in_=pT[:, :]),
                 reads=['p0_pT'], writes=['modT'])
        for l in range(2):
            for m in range(2):
                sc = modT[:, l, 2 * m + 1]
                sh = modT[:, l, 2 * m]
                ngb = ngT[:, l, 2 * m, :].unsqueeze(2).to_broadcast([128, 8, NS])
                s.op('dve', lambda e, sc=sc, ngb=ngb, l=l, m=m: e.scalar_tensor_tensor(
                    out=g.GS[:, l, m], in0=sc, scalar=1.0, in1=ngb, op0=ALU.add, op1=ALU.mult),
                    reads=['modT', 'ngT'], writes=['GS'])
                s.op('dve', lambda e, sh=sh, l=l, m=m: e.tensor_copy(out=g.SH[:, l, m], in_=sh),
                     reads=['modT'], writes=['SH'])
        s.barrier()


def bc(ap2d, shape):
    return ap2d.unsqueeze(1).to_broadcast(shape)


def rsqrt_act(g, out, in_, scale, rk, wk):
    s = g.s
    s.op('act', lambda e: e.activation(out=out, in_=in_, func=AF.Ln, scale=scale, bias=g.epsc[:, 0:1]),
         reads=rk + ['epsc'], writes=wk)
    s.op('act', lambda e: e.activation(out=out, in_=out, func=AF.Exp, scale=-0.5), reads=wk, writes=wk)


def rms_prep(g, pre, xt, xn, ss, rstd, junk, nblk, xkey, outkey):
    s = g.s
    for b in range(nblk):
        s.op('act', lambda e, b=b: e.activation(out=junk[:], in_=xt[:, b, :], func=AF.Square,
                                                accum_out=ss[:, b:b + 1]),
             reads=[xkey], writes=[pre + 'junk', pre + 'ss'])
    rsqrt_act(g, rstd[:, 0:nblk], ss[:, 0:nblk], 1.0 / D, [pre + 'ss'], [pre + 'rstd'])
    for b in range(nblk):
        s.op('act', lambda e, b=b: e.activation(out=xn[:, b, :], in_=xt[:, b, :], func=AF.Copy,
                                                scale=rstd[:, b:b + 1]),
             reads=[xkey, pre + 'rstd'], writes=[outkey])


def to_featmajor(g, pre, xn, hmT, tps, l, m, j, nblk, xnkey, hkey):
    s = g.s
    for half in range(nblk // 2):
        tp = tps[half % len(tps)]
        tk = pre + f'tp{half % len(tps)}'

        def tr(e, half=half, tp=tp):
            for k in range(8):
                for bb in range(2):
                    r = e.transpose(out=tp[:, k, bb * 128:(bb + 1) * 128],
                                    in_=xn[:, half * 2 + bb, k * 128:(k + 1) * 128], identity=g.identB[:])
            return r
        s.op('pe', tr, reads=[xnkey, 'identB'], writes=[tk])
        for k in range(8):
            eng = 'dve' if k % 2 == 0 else 'pool'
            eng = 'dve'
            s.op(eng, lambda e, k=k, half=half, tp=tp: e.tensor_scalar(
                out=hmT[:, k, half * 256:(half + 1) * 256], in0=tp[:, k, :],
                scalar1=g.GS[:, l, m, k, j:j + 1], scalar2=g.SH[:, l, m, k, j:j + 1],
                op0=ALU.mult, op1=ALU.add), reads=[tk, 'GS', 'SH'], writes=[hkey])


def rotary(g, eng, out, zin, cos, sin, H, hd, tmp, rk, wk, tk):
    s = g.s
    zv = zin.rearrange("p (h two d) -> p h two d", two=2, d=hd)
    ov = out.rearrange("p (h two d) -> p h two d", two=2, d=hd)
    x1, x2 = zv[:, :, 0, :], zv[:, :, 1, :]
    o1, o2 = ov[:, :, 0, :], ov[:, :, 1, :]
    cb = bc(cos, [128, H, hd])
    sb_ = bc(sin, [128, H, hd])
    t = [tmp[:, i, 0:H * hd].rearrange("p (h d) -> p h d", d=hd) for i in range(4)]
    s.op(eng, lambda e: e.tensor_tensor(out=t[0], in0=x1, in1=cb, op=ALU.mult), reads=rk, writes=[tk + '0'])
    s.op(eng, lambda e: e.tensor_tensor(out=t[1], in0=x2, in1=sb_, op=ALU.mult), reads=rk, writes=[tk + '1'])
    s.op(eng, lambda e: e.tensor_tensor(out=o1, in0=t[0], in1=t[1], op=ALU.subtract),
         reads=[tk + '0', tk + '1'], writes=wk)
    s.op(eng, lambda e: e.tensor_tensor(out=t[2], in0=x2, in1=cb, op=ALU.mult), reads=rk, writes=[tk + '2'])
    s.op(eng, lambda e: e.tensor_tensor(out=t[3], in0=x1, in1=sb_, op=ALU.mult), reads=rk, writes=[tk + '3'])
    s.op(eng, lambda e: e.tensor_tensor(out=o2, in0=t[2], in1=t[3], op=ALU.add),
         reads=[tk + '2', tk + '3'], writes=wk)


def phase1(g):
    nc, s, NS, I, S = g.nc, g.s, g.NS, g.I, g.S
    with ExitStack() as st:
        sb = lambda n, sh, dt: st.enter_context(nc.sbuf_tensor(n, sh, dt))
        pm = lambda n, sh, dt: st.enter_context(nc.psum_tensor(n, sh, dt))
        wab = sb("p1_wab", [128, 8, 2304], BF16)
        xt = [sb(f"p1_xt{i}", [128, 4, D], F32) for i in range(2)]
        xn = [sb(f"p1_xn{i}", [128, 4, D], BF16) for i in range(2)]
        junk = sb("p1_junk", [128, D], BF16)
        ss = [sb(f"p1_ss{i}", [128, 4], F32) for i in range(2)]
        rstd = [sb(f"p1_rstd{i}", [128, 4], F32) for i in range(2)]
        hmT = [sb(f"p1_hmT{i}", [128, 8, 512], BF16) for i in range(2)]
        zs = [sb(f"p1_zs{i}", [128, 2304], F32) for i in range(2)]
        rA = [sb(f"p1_rA{i}", [128, 4, 64], F32) for i in range(2)]
        rB = [sb(f"p1_rB{i}", [128, 4, 64], F32) for i in range(2)]
        qkg = sb("p1_qkg", [128, 128], F32)
        tmpA = sb("p1_tmpA", [128, 4, 256], F32)
        tmpB = sb("p1_tmpB", [128, 4, 256], F32)
        sq = sb("p1_sq", [128, 640], F32)
        ssq = sb("p1_ssq", [128, 10], F32)
        qn = sb("p1_qn", [128, 640], F32)
        qk = [sb(f"p1_qk{i}", [128, 1664], BF16) for i in range(2)]
        stg = [sb(f"p1_stg{i}", [128, 13, 512], BF16) for i in range(2)]
        vst = [sb(f"p1_vst{i}", [128, 4, 640], BF16) for i in range(2)]
        tps = [pm(f"p1_tp{i}", [128, 8, 256], BF16) for i in range(2)]
        zp = [pm(f"p1_zp{i}", [128, 512], F32) for i in range(2)]
        qT = pm("p1_qT", [128, 16, 128], BF16)

        load_w_bf16(g, 'p1_wab', wab, I.w_in_ab, 8, 2304)
        s.dma('sp', 'p1_qkg', qkg[:], I.qkg[0:1, :].partition_broadcast(128), writes=['qkg'])
        tiles = []
        for j, Sj in enumerate(g.seqs):
            for t in range(Sj // 512):
                tiles.append((j, t * 512, g.offs[j] + t * 512))

        def load(i):
            j, p0, t0 = tiles[i]
            b = i % 2
            s.dma('sp', f'p1_xt{b}', xt[b][:], I.x[t0:t0 + 512, :].rearrange("(b p) d -> p b d", p=128),
                  writes=[f'xt{b}'])
            s.dma('sp', f'p1_rA{b}', rA[b][:], I.ropeA[p0:p0 + 512, :].rearrange("(b p) d -> p b d", p=128),
                  writes=[f'rA{b}'])
            s.dma('sp', f'p1_rB{b}', rB[b][:], I.ropeB[p0:p0 + 512, :].rearrange("(b p) d -> p b d", p=128),
                  writes=[f'rB{b}'])
        load(0)
        zi = 0
        pend = []

        def flush():
            while pend:
                pend.pop(0)()

        def prep(i):
            j, p0, t0 = tiles[i]
            b = i % 2
            rms_prep(g, f'p1{b}', xt[b], xn[b], ss[b], rstd[b], junk, 4, f'xt{b}', f'xn{b}')
            to_featmajor(g, 'p1', xn[b], hmT[b], tps, 0, 0, j, 4, f'xn{b}', f'hmT{b}')
        prep(0)
        for i, (j, p0, t0) in enumerate(tiles):
            b = i % 2
            if i + 1 < len(tiles):
                load(i + 1)
            for blk in range(4):
                if blk == 3 and i + 1 < len(tiles):
                    prep(i + 1)
                zb = zs[blk % 2]
                zk = f'zs{blk % 2}'
                for c in range(5):
                    n0 = c * 512
                    nw = min(512, 2304 - n0)
                    p = zp[zi % 2]
                    pn = f'zp{zi % 2}'
                    zi += 1

                    def mm(e, p=p, n0=n0, nw=nw, blk=blk, b=b):
                        for k in range(8):
                            r = e.matmul(p[:, 0:nw], lhsT=hmT[b][:, k, blk * 128:(blk + 1) * 128],
                                         rhs=wab[:, k, n0:n0 + nw], start=(k == 0), stop=(k == 7))
                        return r
                    s.op('pe', mm, reads=[f'hmT{b}', 'p1_wab'], writes=[pn])
                    if c == 2:
                        s.op('act', lambda e, p=p, blk=blk, b=b: e.activation(
                            out=vst[b][:, blk, 0:512], in_=p[:, 0:512], func=AF.Copy),
                            reads=[pn], writes=[f'vst{b}'])
                    else:
                        s.op('act', lambda e, p=p, n0=n0, nw=nw, zb=zb: e.activation(
                            out=zb[:, n0:n0 + nw], in_=p[:, 0:nw], func=AF.Copy),
                            reads=[pn], writes=[zk + f'c{c}'])
                flush()
                q = qk[blk % 2]
                qkk = f'qk{blk % 2}'
                cosA, sinA = rA[b][:, blk, 0:32], rA[b][:, blk, 32:64]
                cosB, sinB = rB[b][:, blk, 0:32], rB[b][:, blk, 32:64]
                rotary(g, 'dve', q[:, 0:512], zb[:, 0:512], cosA, sinA, 8, 32, tmpA,
                       [zk + 'c0', f'rA{b}'], [qkk + 'qa'], 'tmpA')
                rotary(g, 'pool', q[:, 512:1024], zb[:, 512:1024], cosA, sinA, 8, 32, tmpB,
                       [zk + 'c1', f'rA{b}'], [qkk + 'ka'], 'tmpB')
                s.op('act', lambda e, zb=zb: e.activation(out=sq[:], in_=zb[:, 1536:2176], func=AF.Square),
                     reads=[zk + 'c3', zk + 'c4'], writes=['sq'])
                s.op('dve', lambda e: e.tensor_reduce(out=ssq[:], in_=sq[:].rearrange("p (h d) -> p h d", d=64),
                                                      axis=AX.X, op=ALU.add), reads=['sq'], writes=['ssq'])
                rsqrt_act(g, ssq[:], ssq[:], 1.0 / 64, ['ssq'], ['ssq'])
                s.op('dve', lambda e, zb=zb: e.tensor_tensor(
                    out=qn[:].rearrange("p (h d) -> p h d", d=64),
                    in0=zb[:, 1536:2176].rearrange("p (h d) -> p h d", d=64),
                    in1=ssq[:].unsqueeze(2).to_broadcast([128, 10, 64]), op=ALU.mult),
                    reads=[zk + 'c3', zk + 'c4', 'ssq'], writes=['qn'])
                s.op('dve', lambda e: e.tensor_tensor(
                    out=qn[:, 0:512].rearrange("p (h d) -> p h d", d=64),
                    in0=qn[:, 0:512].rearrange("p (h d) -> p h d", d=64),
                    in1=bc(qkg[:, 0:64], [128, 8, 64]), op=ALU.mult), reads=['qn', 'qkg'], writes=['qn'])
                s.op('dve', lambda e: e.tensor_tensor(
                    out=qn[:, 512:640].rearrange("p (h d) -> p h d", d=64),
                    in0=qn[:, 512:640].rearrange("p (h d) -> p h d", d=64),
                    in1=bc(qkg[:, 64:128], [128, 2, 64]), op=ALU.mult), reads=['qn', 'qkg'], writes=['qn'])
                for half in range(2):
                    for (c0, H, o0) in ((0, 8, 1024), (512, 2, 1536)):
                        zin = qn[:, c0:c0 + H * 64].rearrange("p (h x) -> p h x", x=64)[:, :, half * 32:(half + 1) * 32]
                        oo = q[:, o0:o0 + H * 64].rearrange("p (h x) -> p h x", x=64)[:, :, half * 32:(half + 1) * 32]
                        rotary_v(g, 'dve', oo, zin, cosB[:, half * 16:(half + 1) * 16],
                                 sinB[:, half * 16:(half + 1) * 16], H, 16, tmpA, ['qn', f'rB{b}'],
                                 [qkk + 'b'], 'tmpA')
                s.op('pool', lambda e, zb=zb, blk=blk, b=b: e.tensor_copy(out=vst[b][:, blk, 512:640],
                                                                          in_=zb[:, 2176:2304]),
                     reads=[zk + 'c4'], writes=[f'vst{b}'])
                def tail(q=q, qkk=qkk, blk=blk, b=b):
                    def trq(e):
                        for c in range(13):
                            r = e.transpose(out=qT[:, c, :], in_=q[:, c * 128:(c + 1) * 128], identity=g.identB[:])
                        return r
                    s.op('pe', trq, reads=[qkk + 'qa', qkk + 'ka', qkk + 'b', 'identB'], writes=['qT'])
                    s.op('act', lambda e: e.activation(out=stg[b][:, :, blk * 128:(blk + 1) * 128],
                                                       in_=qT[:, 0:13, :], func=AF.Copy),
                         reads=['qT'], writes=[f'stg{b}'])
                pend.append(tail)
            flush()
            for (dst, c0, n) in ((S.qaT, 0, 4), (S.kaT, 4, 4), (S.qbT, 8, 4), (S.kbT, 12, 1)):
                s.dma('sp', f'p1_stg{b}', dst[:, t0:t0 + 512].rearrange("(c p) t -> p c t", p=128),
                      stg[b][:, c0:c0 + n, :], reads=[f'stg{b}'])
            s.dma('sp', f'p1_vst{b}', S.vA[t0:t0 + 512, :].rearrange("(b p) d -> p b d", p=128), vst[b][:, :, 0:512],
                  reads=[f'vst{b}'])
            s.dma('sp', f'p1_vst{b}', S.vB[t0:t0 + 512, :].rearrange("(b p) d -> p b d", p=128), vst[b][:, :, 512:640],
                  reads=[f'vst{b}'])
        s.barrier()


def rotary_v(g, eng, ov, zv, cos, sin, H, hd, tmp, rk, wk, tk):
    s = g.s
    x1, x2 = zv[:, :, 0:hd], zv[:, :, hd:2 * hd]
    o1, o2 = ov[:, :, 0:hd], ov[:, :, hd:2 * hd]
    cb = bc(cos, [128, H, hd])
    sb_ = bc(sin, [128, H, hd])
    t = [tmp[:, i, 0:H * hd].rearrange("p (h d) -> p h d", d=hd) for i in range(4)]
    s.op(eng, lambda e: e.tensor_tensor(out=t[0], in0=x1, in1=cb, op=ALU.mult), reads=rk, writes=[tk + '0'])
    s.op(eng, lambda e: e.tensor_tensor(out=t[1], in0=x2, in1=sb_, op=ALU.mult), reads=rk, writes=[tk + '1'])
    s.op(eng, lambda e: e.tensor_tensor(out=o1, in0=t[0], in1=t[1], op=ALU.subtract),
         reads=[tk + '0', tk + '1'], writes=wk)
    s.op(eng, lambda e: e.tensor_tensor(out=t[2], in0=x2, in1=cb, op=ALU.mult), reads=rk, writes=[tk + '2'])
    s.op(eng, lambda e: e.tensor_tensor(out=t[3], in0=x1, in1=sb_, op=ALU.mult), reads=rk, writes=[tk + '3'])
    s.op(eng, lambda e: e.tensor_tensor(out=o2, in0=t[2], in1=t[3], op=ALU.add),
         reads=[tk + '2', tk + '3'], writes=wk)


def phase2(g):
    nc, s, S = g.nc, g.s, g.S
    SMAX = max(g.seqs)
    with ExitStack() as st:
        sb = lambda n, sh, dt: st.enter_context(nc.sbuf_tensor(n, sh, dt))
        pm = lambda n, sh, dt: st.enter_context(nc.psum_tensor(n, sh, dt))
        kT = [sb(f"p2_kT{i}", [128, SMAX], BF16) for i in range(2)]
        V1 = [sb(f"p2_V1{i}", [128, SMAX // 128, 128], BF16) for i in range(2)]
        qT = [sb(f"p2_qT{i}", [128, SMAX], BF16) for i in range(2)]
        pT = [sb(f"p2_pT{i}", [128, 1024], BF16) for i in range(3)]
        rd = [sb(f"p2_rd{i}", [64, 512], F32) for i in range(2)]
        ob = [sb(f"p2_ob{i}", [64, 512], BF16) for i in range(2)]
        sp_ = [pm(f"p2_sp{i}", [128, 1024], F32) for i in range(3)]
        ot = [pm(f"p2_ot{i}", [128, 512], F32) for i in range(2)]
        for i in range(2):
            s.op('pool', lambda e, i=i: e.memset(V1[i][:, :, 64:128], 1.0), writes=[f'V1{i}'])
            s.op('pool', lambda e, i=i: e.memset(kT[i][64:128, :], 0.0), writes=[f'kT{i}'])
            s.op('pool', lambda e, i=i: e.memset(qT[i][64:128, :], 0.0), writes=[f'qT{i}'])
        ikv = 0
        ih = 0
        ip = 0
        io = 0
        LAG = 2
        pend = []

        def flush(n):
            while len(pend) > n:
                pend.pop(0)()
        for j, Sj in enumerate(g.seqs):
            base = g.offs[j]
            nkb, nqg = Sj // 128, Sj // 512
            for kv in range(2):
                bk = ikv % 2
                ikv += 1
                s.dma('sp', f'p2_kT{bk}', kT[bk][0:64, 0:Sj], S.kbT[kv * 64:(kv + 1) * 64, base:base + Sj],
                      writes=[f'kT{bk}'])
                s.dma('sp', f'p2_V1{bk}', V1[bk][:, 0:nkb, 0:64],
                      S.vB[base:base + Sj, kv * 64:(kv + 1) * 64].rearrange("(b p) d -> p b d", p=128),
                      writes=[f'V1{bk}'])
                for hq in range(4):
                    h = kv * 4 + hq
                    bq = ih % 2
                    ih += 1
                    s.dma('sp', f'p2_qT{bq}', qT[bq][0:64, 0:Sj], S.qbT[h * 64:(h + 1) * 64, base:base + Sj],
                          writes=[f'qT{bq}'])
                    for qg in range(nqg):
                        o = ot[io % 2]
                        on = f'ot{io % 2}'
                        bo = io % 2
                        io += 1
                        for kb in range(0, nkb, 2):
                            p = sp_[ip % 3]
                            pn = f'sp{ip % 3}'
                            pt = pT[ip % 3]
                            ptn = f'pT{ip % 3}'
                            ip += 1

                            def smm(e, p=p, bk=bk, bq=bq, kb=kb, qg=qg):
                                for i in range(2):
                                    r = e.matmul(p[:, i * 512:(i + 1) * 512],
                                                 lhsT=kT[bk][:, (kb + i) * 128:(kb + i + 1) * 128],
                                                 rhs=qT[bq][:, qg * 512:(qg + 1) * 512], start=True, stop=True)
                                return r
                            s.op('pe', smm, reads=[f'kT{bk}', f'qT{bq}'], writes=[pn])
                            s.op('act', lambda e, p=p, pt=pt: e.activation(out=pt[:], in_=p[:], func=AF.Exp,
                                                                           scale=0.125),
                                 reads=[pn], writes=[ptn])

                            def stage2(o=o, on=on, bo=bo, bk=bk, kb=kb, pt=pt, ptn=ptn, nkb=nkb, h=h, qg=qg,
                                       base=base):
                                def pvm(e):
                                    for i in range(2):
                                        r = e.matmul(o[:, :], lhsT=V1[bk][:, kb + i, :], rhs=pt[:, i * 512:(i + 1) * 512],
                                                     start=(kb + i == 0), stop=(kb + i == nkb - 1))
                                    return r
                                s.op('pe', pvm, reads=[f'V1{bk}', ptn], writes=[on])
                                if kb + 2 == nkb:
                                    s.op('dve', lambda e: e.reciprocal(out=rd[bo][:], in_=o[64:128, :]),
                                         reads=[on], writes=[f'rd{bo}'])
                                    s.op('dve', lambda e: e.tensor_tensor(out=ob[bo][:], in0=o[0:64, :],
                                                                          in1=rd[bo][:], op=ALU.mult),
                                         reads=[on, f'rd{bo}'], writes=[f'ob{bo}'])
                                    t0 = base + qg * 512
                                    s.dma('sp', f'p2_ob{bo}', S.oT[512 + h * 64:512 + (h + 1) * 64, t0:t0 + 512],
                                          ob[bo][:], reads=[f'ob{bo}'])
                            pend.append(stage2)
                            flush(LAG)
        flush(0)
        s.barrier()


def phase2a(g):
    nc, s, S = g.nc, g.s, g.S
    with ExitStack() as st:
        sb = lambda n, sh, dt: st.enter_context(nc.sbuf_tensor(n, sh, dt))
        pm = lambda n, sh, dt: st.enter_context(nc.psum_tensor(n, sh, dt))
        kTw = sb("p2a_kTw", [128, 4, 4096], BF16)
        qTsh = [sb(f"p2a_qTs{i}", [128, 4, 2048], BF16) for i in range(2)]
        ACC = sb("p2a_ACC", [128, 8, 2048], F32)
        V1 = [sb(f"p2a_V1{i}", [128, 9, 8, 128], BF16) for i in range(2)]
        band = sb("p2a_band", [128, 3, 256], BF16)
        pT = [sb(f"p2a_pT{i}", [128, 256], BF16) for i in range(4)]
        pTm = [sb(f"p2a_pTm{i}", [128, 256], BF16) for i in range(4)]
        rd = [sb(f"p2a_rd{i}", [64, 2048], F32) for i in range(2)]
        ob = [sb(f"p2a_ob{i}", [64, 2048], BF16) for i in range(2)]
        sp_ = [pm(f"p2a_sp{i}", [128, 512], F32) for i in range(4)]
        ot = [pm(f"p2a_ot{i}", [128, 512], F32) for i in range(4)]
        s.op('pool', lambda e: e.memset(qTsh[0][64:128, :, :], 0.0), writes=['qTs'])
        s.op('pool', lambda e: e.memset(qTsh[1][0:64, :, :], 0.0), writes=['qTs'])
        NEG = -30000.0
        for bi in range(3):
            bt = band[:, bi, :]
            s.op('pool', lambda e, bt=bt: e.memset(bt, 0.0), writes=['band'])
            s.op('pool', lambda e, bt=bt: e.affine_select(out=bt, in_=bt, pattern=[[1, 256]], compare_op=ALU.is_ge,
                                                          fill=NEG, base=0, channel_multiplier=-1),
                 reads=['band'], writes=['band'])
            s.op('pool', lambda e, bt=bt: e.affine_select(out=bt, in_=bt, pattern=[[-1, 256]], compare_op=ALU.is_ge,
                                                          fill=NEG, base=128, channel_multiplier=1),
                 reads=['band'], writes=['band'])
        s.op('pool', lambda e: e.memset(band[0:64, 1, :], NEG), reads=['band'], writes=['band'])
        s.op('pool', lambda e: e.memset(band[64:128, 2, :], NEG), reads=['band'], writes=['band'])
        for i in range(2):
            s.op('pool', lambda e, i=i: e.memset(V1[i][:, :, :, 0:64], 0.0), writes=[f'aV1{i}'])
            s.op('pool', lambda e, i=i: e.memset(V1[i][:, :, :, 64:128], 1.0), writes=[f'aV1{i}'])
        iu = 0
        ip = 0
        import os
        LAG = int(os.environ.get('LAGA', '2'))
        pend = []

        def flush(n):
            while len(pend) > n:
                pend.pop(0)()
        for j, Sj in enumerate(g.seqs):
            base = g.offs[j]
            for seg in range(Sj // 2048):
                seg0 = seg * 2048
                lo, hi = max(0, seg0 - 1024), min(Sj, seg0 + 3072)
                if lo > seg0 - 1024:
                    s.op('pool', lambda e: e.memset(kTw[:, :, 0:1024], 0.0), writes=['kTw'])
                if hi < seg0 + 3072:
                    s.op('pool', lambda e: e.memset(kTw[:, :, 3072:4096], 0.0), writes=['kTw'])
                s.dma('sp', 'p2a_kTw', kTw[:, :, lo - (seg0 - 1024):hi - (seg0 - 1024)],
                      S.kaT[:, base + lo:base + hi].rearrange("(c p) t -> p c t", p=128), writes=['kTw'])
                qsrc = S.qaT[:, base + seg0:base + seg0 + 2048].rearrange("(c p) t -> p c t", p=128)
                s.dma('sp', 'p2a_qTs', qTsh[0][0:64, :, :], qsrc[0:64], writes=['qTs'])
                s.dma('sp', 'p2a_qTs', qTsh[1][64:128, :, :], qsrc[64:128], writes=['qTs'])
                units = [(1, 0, 0, 8), (1, 0, 8, 8)] + [(4, r, 0, 4) for r in range(4)] + \
                        [(16, r, 0, 1) for r in range(16)]
                first_write = {}
                for (d, r, qb0, nqb) in units:
                    L = Sj // d
                    lq0 = seg0 // d + 128 * qb0
                    bv = iu % 2
                    iu += 1
                    vk = f'aV1{bv}'
                    v1 = V1[bv]
                    nkb = nqb + 1
                    m_lo, m_hi = 0, nkb
                    at_start = (lq0 == 0)
                    at_end = (lq0 + 128 * nqb == L)
                    if lq0 == 0:
                        tok = base + 0 * d + r
                        s.dma('sp', f'p2a_V1{bv}', v1[64:128, 0, :, 0:64],
                              S.vA[tok:tok + 63 * d + 1:d, :].rearrange("p (h e) -> p h e", e=64), writes=[vk])
                        m_lo = 1
                    if lq0 + 128 * nqb == L:
                        tok = base + (lq0 - 64 + 128 * nqb) * d + r
                        s.dma('sp', f'p2a_V1{bv}', v1[0:64, nqb, :, 0:64],
                              S.vA[tok:tok + 63 * d + 1:d, :].rearrange("p (h e) -> p h e", e=64), writes=[vk])
                        m_hi = nkb - 1
                    for m in range(m_lo, m_hi):
                        tok = base + (lq0 - 64 + 128 * m) * d + r
                        s.dma('sp', f'p2a_V1{bv}', v1[:, m, :, 0:64],
                              S.vA[tok:tok + 127 * d + 1:d, :].rearrange("p (h e) -> p h e", e=64), writes=[vk])
                    for h in range(8):
                        c, hh = h // 2, (h % 2) * 64
                        for m in range(nkb):
                            n_lo, n_hi = max(m - 1, 0), min(m, nqb - 1)
                            nq = n_hi - n_lo + 1
                            kc0 = 1024 + (128 * (qb0 + m) - 64) * d + r
                            qc0 = 128 * (qb0 + n_lo) * d + r
                            p = sp_[ip % 4][:, 0:256]
                            pn = f'asp{ip % 4}'
                            pt, ptm = pT[ip % 4], pTm[ip % 4]
                            ptn = f'apT{ip % 4}'
                            ip += 1
                            W = nq * 128
                            b0 = 128 if m == 0 else 0
                            bi = 1 if (m == 0 and at_start) else (2 if (m == nkb - 1 and at_end) else 0)

                            def smm(e, p=p, c=c, hh=hh, kc0=kc0, qc0=qc0, W=W, d=d, b0=b0, bi=bi):
                                e.matmul(p[:, 0:W], lhsT=kTw[:, c, kc0:kc0 + 127 * d + 1:d],
                                         rhs=qTsh[hh // 64][:, c, qc0:qc0 + (W - 1) * d + 1:d], start=True, stop=False)
                                return e.matmul(p[:, 0:W], lhsT=g.identB[:], rhs=band[:, bi, b0:b0 + W],
                                                start=False, stop=True)
                            s.op('pe', smm, reads=['kTw', 'qTs', 'band', 'identB'], writes=[pn])
                            s.op('act', lambda e, p=p, ptm=ptm, W=W: e.activation(out=ptm[:, 0:W], in_=p[:, 0:W],
                                                                                 func=AF.Exp, scale=0.125),
                                 reads=[pn], writes=[ptn + 'm'])

                            def stage2(n_lo=n_lo, n_hi=n_hi, h=h, v1=v1, vk=vk, m=m, ptm=ptm, ptn=ptn, d=d, r=r,
                                       qb0=qb0):
                                for n in range(n_lo, n_hi + 1):
                                    o = ot[n % 4][:, 0:128]
                                    on = f'aot{n % 4}'
                                    col = (n - n_lo) * 128
                                    s.op('pe', lambda e, o=o, col=col, n=n: e.matmul(
                                        o, lhsT=v1[:, m, h, :], rhs=ptm[:, col:col + 128],
                                        start=(m == n), stop=(m == n + 1)),
                                        reads=[vk, ptn + 'm'], writes=[on])
                                    if m == n + 1:
                                        a0 = 128 * (qb0 + n) * d + r
                                        av = ACC[:, h, a0:a0 + 127 * d + 1:d]
                                        if d == 1:
                                            s.op('dve', lambda e, o=o, av=av: e.tensor_copy(out=av, in_=o),
                                                 reads=[on], writes=[f'ACC{h}'])
                                        else:
                                            s.op('dve', lambda e, o=o, av=av: e.tensor_tensor(out=av, in0=o, in1=av,
                                                                                              op=ALU.add),
                                                 reads=[on, f'ACC{h}'], writes=[f'ACC{h}'])
                            pend.append(stage2)
                            flush(LAG)
                flush(0)
                for h in range(8):
                    bo = h % 2
                    s.op('act', lambda e, h=h, bo=bo: e.activation(out=rd[bo][:], in_=ACC[64:128, h, :], func=AF.Ln),
                         reads=[f'ACC{h}'], writes=[f'ard{bo}'])
                    s.op('act', lambda e, bo=bo: e.activation(out=rd[bo][:], in_=rd[bo][:], func=AF.Exp, scale=-1.0),
                         reads=[f'ard{bo}'], writes=[f'ard{bo}'])
                    s.op('pool', lambda e, h=h, bo=bo: e.tensor_tensor(out=ob[bo][:], in0=ACC[0:64, h, :],
                                                                      in1=rd[bo][:], op=ALU.mult),
                         reads=[f'ACC{h}', f'ard{bo}'], writes=[f'aob{bo}'])
                    t0 = base + seg0
                    s.dma('sp', f'p2a_ob{bo}', S.oT[h * 64:(h + 1) * 64, t0:t0 + 2048], ob[bo][:],
                          reads=[f'aob{bo}'])
        s.barrier()


def post_norm_residual(g, pre, yp, ypk, xres, xkey, GB, gbk, out, outkey, ss, rstd, junk, tmp, par=0):
    s = g.s
    sp = str(par)
    tm = tmp[par] if isinstance(tmp, (list, tuple)) else tmp
    s.op('act', lambda e: e.activation(out=junk[:], in_=yp[:, :], func=AF.Square, accum_out=ss[:, par:par + 1]),
         reads=[ypk], writes=[pre + 'junk', pre + 'ss' + sp])
    rsqrt_act(g, rstd[:, par:par + 1], ss[:, par:par + 1], 1.0 / D, [pre + 'ss' + sp], [pre + 'rstd' + sp])
    s.op('dve', lambda e: e.scalar_tensor_tensor(out=tm[:], in0=yp[:, :], scalar=rstd[:, par:par + 1], in1=GB[:],
                                                 op0=ALU.mult, op1=ALU.mult),
         reads=[ypk, pre + 'rstd' + sp, gbk], writes=[pre + 'tmp' + sp])
    s.op('pool' if par == 0 else 'dve', lambda e: e.tensor_tensor(out=out, in0=tm[:], in1=xres, op=ALU.add),
         reads=[pre + 'tmp' + sp, xkey], writes=[outkey])


def load_GB(g, pre, GB, gb, ngb, l, gi, j):
    s = g.s
    s.dma('sp', pre + 'gb', gb[:], g.S.gates[l, gi, j:j + 1, :].partition_broadcast(128), writes=[pre + 'gb'])
    s.op('dve', lambda e: e.tensor_tensor(out=GB[:], in0=gb[:], in1=ngb[:], op=ALU.mult),
         reads=[pre + 'gb', pre + 'ngb'], writes=[pre + 'GB'])


def phase3(g, l, oT_d, w_out_d, xin_d, xout_d):
    nc, s = g.nc, g.s
    pre = f'p3{l}'
    with ExitStack() as st:
        sb = lambda n, sh, dt: st.enter_context(nc.sbuf_tensor(pre + n, sh, dt))
        pm = lambda n, sh, dt: st.enter_context(nc.psum_tensor(pre + n, sh, dt))
        wo = sb("wo", [128, 8, D], BF16)
        oT = [sb(f"oT{i}", [128, 8, 512], BF16) for i in range(2)]
        xt = [sb(f"xt{i}", [128, 4, D], F32) for i in range(2)]
        xo = [sb(f"xo{i}", [128, 4, D], F32) for i in range(2)]
        gb = sb("gb", [128, D], F32)
        ngb = sb("ngb", [128, D], F32)
        GB = sb("GB", [128, D], F32)
        junk = sb("junk", [128, D], BF16)
        tmp = [sb(f"tmp{i}", [128, D], F32) for i in range(2)]
        ss = sb("ss", [128, 2], F32)
        rstd = sb("rstd", [128, 2], F32)
        yp = [pm(f"yp{i}", [128, D], F32) for i in range(2)]
        load_w_bf16(g, pre + 'wo', wo, w_out_d, 8, D)
        s.dma('sp', pre + 'ngb', ngb[:], g.I.norm_g[l * 4 + 1:l * 4 + 2, :].partition_broadcast(128),
              writes=[pre + 'ngb'])
        tiles = []
        for j, Sj in enumerate(g.seqs):
            for t in range(Sj // 512):
                tiles.append((j, g.offs[j] + t * 512))

        def load(i):
            j, t0 = tiles[i]
            b = i % 2
            s.dma('sp', pre + f'oT{b}', oT[b][:], oT_d[:, t0:t0 + 512].rearrange("(c p) t -> p c t", p=128),
                  writes=[pre + f'oT{b}'])
            s.dma('sp', pre + f'xt{b}', xt[b][:], xin_d[t0:t0 + 512, :].rearrange("(b p) d -> p b d", p=128),
                  writes=[pre + f'xt{b}'])
        load(0)
        curj = -1
        iy = 0
        for i, (j, t0) in enumerate(tiles):
            b = i % 2
            if i + 1 < len(tiles):
                load(i + 1)
            if j != curj:
                load_GB(g, pre, GB, gb, ngb, l, 0, j)
                curj = j
            for blk in range(4):
                y_ = yp[iy % 2]
                yk = pre + f'yp{iy % 2}'
                iy += 1

                def mm(e, y_=y_, b=b, blk=blk):
                    for nh in range(2):
                        for k in range(8):
                            r = e.matmul(y_[:, nh * 512:(nh + 1) * 512], lhsT=oT[b][:, k, blk * 128:(blk + 1) * 128],
                                         rhs=wo[:, k, nh * 512:(nh + 1) * 512], start=(k == 0), stop=(k == 7))
                    return r
                s.op('pe', mm, reads=[pre + f'oT{b}', pre + 'wo'], writes=[yk])
                post_norm_residual(g, pre, y_, yk, xt[b][:, blk, :], pre + f'xt{b}', GB, pre + 'GB',
                                   xo[b][:, blk, :], pre + f'xo{b}', ss, rstd, junk, tmp, par=(iy % 2))
            s.dma('sp', pre + f'xo{b}', xout_d[t0:t0 + 512, :].rearrange("(b p) d -> p b d", p=128), xo[b][:],
                  reads=[pre + f'xo{b}'])
        s.barrier()


def phase4(g, l, xin_d, xout_d):
    nc, s = g.nc, g.s
    pre = f'p4{l}'
    with ExitStack() as st:
        sb = lambda n, sh, dt: st.enter_context(nc.sbuf_tensor(pre + n, sh, dt))
        pm = lambda n, sh, dt: st.enter_context(nc.psum_tensor(pre + n, sh, dt))
        w1 = sb("w1", [128, 8, 4 * D], BF16)
        w2 = sb("w2", [128, 32, D], BF16)
        xt = [sb(f"xt{i}", [128, 2, D], F32) for i in range(2)]
        xn = sb("xn", [128, 2, D], BF16)
        xo = [sb(f"xo{i}", [128, 2, D], F32) for i in range(2)]
        hfT = sb("hfT", [128, 8, 256], BF16)
        h1a = sb("h1a", [128, 2, 256], BF16)
        h1T = sb("h1T", [128, 32, 256], BF16)
        ngb = sb("ngb", [128, D], F32)
        GB = sb("GB", [128, D], F32)
        junk = sb("junk", [128, D], BF16)
        tmp = [sb(f"tmp{i}", [128, D], F32) for i in range(2)]
        ss = sb("ss", [128, 4], F32)
        rstd = sb("rstd", [128, 4], F32)
        ss2 = sb("ss2", [128, 2], F32)
        rstd2 = sb("rstd2", [128, 2], F32)
        tps = [pm("tp0", [128, 8, 256], BF16)]
        hp = [pm(f"hp{i}", [128, 512], F32) for i in range(2)]
        yp = [pm(f"yp{i}", [128, D], F32) for i in range(2)]
        load_w_bf16(g, pre + 'w1', w1, g.I.w_ff1[l], 8, 4 * D)
        load_w_bf16(g, pre + 'w2', w2, g.I.w_ff2[l], 32, D)
        s.dma('sp', pre + 'ngb', ngb[:], g.I.norm_g[l * 4 + 3:l * 4 + 4, :].partition_broadcast(128),
              writes=[pre + 'ngb'])
        tiles = []
        for j, Sj in enumerate(g.seqs):
            for t in range(Sj // 256):
                tiles.append((j, g.offs[j] + t * 256))

        def load(i):
            j, t0 = tiles[i]
            b = i % 2
            s.dma('sp', pre + f'xt{b}', xt[b][:], xin_d[t0:t0 + 256, :].rearrange("(b p) d -> p b d", p=128),
                  writes=[pre + f'xt{b}'])
        load(0)
        curj = -1
        ih = 0
        iy = 0
        def prep_a(i):
            j, t0 = tiles[i]
            b = i % 2
            rms_prep(g, pre, xt[b], xn, ss, rstd, junk, 2, pre + f'xt{b}', pre + 'xn')

        def prep_b(i):
            j, t0 = tiles[i]
            to_featmajor(g, pre, xn, hfT, tps, l, 1, j, 2, pre + 'xn', pre + 'hfT')
        prep_a(0)
        prep_b(0)
        for i, (j, t0) in enumerate(tiles):
            b = i % 2
            if i + 1 < len(tiles):
                load(i + 1)
            if j != curj:
                s.dma('sp', pre + 'GB', GB[:], g.S.gates[l, 1, j:j + 1, :].partition_broadcast(128),
                      writes=[pre + 'GB'])
                s.op('pool', lambda e: e.tensor_tensor(out=GB[:], in0=GB[:], in1=ngb[:], op=ALU.mult),
                     reads=[pre + 'GB', pre + 'ngb'], writes=[pre + 'GB'])
                curj = j
            for fc in range(32):
                p = hp[ih % 2]
                pn = pre + f'hp{ih % 2}'
                ha = h1a[:, ih % 2, :]
                hak = pre + f'h1a{ih % 2}'
                ih += 1

                def mm(e, p=p, fc=fc):
                    for k in range(8):
                        r = e.matmul(p[:, 0:256], lhsT=w1[:, k, fc * 128:(fc + 1) * 128], rhs=hfT[:, k, :],
                                     start=(k == 0), stop=(k == 7))
                    return r
                s.op('pe', mm, reads=[pre + 'w1', pre + 'hfT'], writes=[pn])
                s.op('act', lambda e, p=p, ha=ha: e.activation(out=ha, in_=p[:, 0:256], func=AF.Relu),
                     reads=[pn], writes=[hak])
                eng = 'pool' if fc % 2 == 0 else 'dve'
                s.op(eng, lambda e, ha=ha, fc=fc: e.tensor_tensor(out=h1T[:, fc, :], in0=ha, in1=ha, op=ALU.mult),
                     reads=[hak], writes=[pre + f'h1T{fc}'])
            if i + 1 < len(tiles):
                prep_a(i + 1)
            for blk in range(2):
                if blk == 1 and i + 1 < len(tiles):
                    prep_b(i + 1)
                y_ = yp[iy % 2]
                yk = pre + f'yp{iy % 2}'
                iy += 1

                def mm2(e, y_=y_, blk=blk):
                    for nh in range(2):
                        for k in range(32):
                            r = e.matmul(y_[:, nh * 512:(nh + 1) * 512], lhsT=h1T[:, k, blk * 128:(blk + 1) * 128],
                                         rhs=w2[:, k, nh * 512:(nh + 1) * 512], start=(k == 0), stop=(k == 31))
                    return r
                s.op('pe', mm2, reads=[pre + f'h1T{fc}' for fc in range(32)] + [pre + 'w2'], writes=[yk])
                post_norm_residual(g, pre + 'b', y_, yk, xt[b][:, blk, :], pre + f'xt{b}', GB, pre + 'GB',
                                   xo[b][:, blk, :], pre + f'xo{b}', ss2, rstd2, junk, tmp, par=(iy % 2))
            s.dma('sp', pre + f'xo{b}', xout_d[t0:t0 + 256, :].rearrange("(b p) d -> p b d", p=128), xo[b][:],
                  reads=[pre + f'xo{b}'])
        s.barrier()


def _rope_angles(pos, dim):
    inv_freq = (np.float32(10000.0) ** (-np.arange(0, dim, 2, dtype=np.float32) / np.float32(dim))).astype(np.float32)
    return (pos.astype(np.float32)[:, None] * inv_freq[None, :]).astype(np.float32)


def rope_tables(S):
    pos = np.arange(S)
    angA = _rope_angles(pos, 64)
    ar = _rope_angles(pos // 64, 32)
    ac = _rope_angles(pos % 64, 32)
    ropeA = np.concatenate([np.cos(angA), np.sin(angA)], axis=1).astype(np.float32)
    ropeB = np.concatenate([np.cos(ar), np.cos(ac), np.sin(ar), np.sin(ac)], axis=1).astype(np.float32)
    return ropeA, ropeB


def core_map(inp, xs, cs):
    NS = len(xs)
    SMAX = max(x.shape[0] for x in xs)
    ropeA, ropeB = rope_tables(SMAX)
    c = np.stack(cs)
    cT = c.reshape(NS, 8, 128).transpose(2, 1, 0).reshape(128, 8 * NS)
    ng = np.asarray(inp['norm_g'])
    m = {
        'x': np.concatenate(xs, 0), 'cT': cT,
        'w_in_ab': inp['w_in_ab'][0], 'w_out_ab': inp['w_out_ab'][0], 'w_in_cd': inp['w_in_cd'][0],
        'w_out_cd': inp['w_out_cd'][0], 'w_ada': inp['w_ada'], 'b_ada': np.asarray(inp['b_ada']).reshape(1, -1),
        'normgT': ng.reshape(2, 4, 8, 128).transpose(3, 0, 1, 2).reshape(128, 64),
        'norm_g': ng.reshape(8, 1024), 'w_ff1': inp['w_ff1'], 'w_ff2': inp['w_ff2'],
        'qkg': np.asarray(inp['qk_norm_g']).reshape(1, 128), 'ropeA': ropeA, 'ropeB': ropeB,
        'gate_bias': np.asarray(inp['gate_bias']).reshape(32, 1), 'mhg': np.asarray(inp['mh_norm_g']).reshape(1, 512),
        'sgg': np.asarray(inp['sg_norm_g']).reshape(1, 512),
        'wsT': np.asarray(inp['w_spatial'])[0].transpose(2, 0, 1).reshape(128, 1024),
        'bsT': np.asarray(inp['b_spatial'])[0].T,
    }
    return {k: np.ascontiguousarray(np.asarray(v), dtype=np.float32) for k, v in m.items()}


ALL_PHASES = (0, 1, 2, 3, 4, 5, 6, 7, 8)
_CACHE = {}


def kernel(**inputs):
    inp = {k: np.asarray(v) for k, v in inputs.items()}
    n = 8
    seqs = [8192, 2048, 2048, 2048, 2048]
    if 'nc' not in _CACHE:
        _CACHE['nc'] = build(seqs, set(ALL_PHASES), debug=False)
    nc, g = _CACHE['nc']
    in_maps = []
    for i in range(n):
        xs = [inp['x_prompt'][i]] + [inp['x_sample'][4 * i + k] for k in range(4)]
        cs = [inp['c_prompt'][i]] + [inp['c_sample'][4 * i + k] for k in range(4)]
        in_maps.append(core_map(inp, xs, cs))
    res = run_bass_kernel_spmd(nc, in_maps, core_ids=list(range(n)))
    y_prompt = np.empty((8, 8192, 1024), np.float32)
    y_sample = np.empty((32, 2048, 1024), np.float32)
    for i in range(n):
        y = np.asarray(res.results[i]['y'])
        y_prompt[i] = y[0:8192]
        for k in range(4):
            y_sample[4 * i + k] = y[8192 + 2048 * k:8192 + 2048 * (k + 1)]
    return (y_prompt, y_sample)


def phase5(g):
    nc, s, NS, I, S = g.nc, g.s, g.NS, g.I, g.S
    with ExitStack() as st:
        sb = lambda n, sh, dt: st.enter_context(nc.sbuf_tensor("p5_" + n, sh, dt))
        pm = lambda n, sh, dt: st.enter_context(nc.psum_tensor("p5_" + n, sh, dt))
        wcd = sb("wcd", [128, 8, 3104], BF16)
        wsT = sb("wsT", [128, 8, 128], BF16)
        bsT = sb("bsT", [128, 8], F32)
        sgg = sb("sgg", [128, 512], F32)
        gbias = sb("gbias", [128, 32], F32)
        xt = [sb(f"xt{i}", [128, 4, D], F32) for i in range(2)]
        xn = sb("xn", [128, 4, D], BF16)
        junk = sb("junk", [128, D], BF16)
        ss = sb("ss", [128, 4], F32)
        rstd = sb("rstd", [128, 4], F32)
        hmT = sb("hmT", [128, 8, 512], BF16)
        qk = [sb(f"qk{i}", [128, 1536], BF16) for i in range(2)]
        vst = [sb(f"vst{i}", [128, 4, 512], BF16) for i in range(2)]
        kst = [sb(f"kst{i}", [128, 4, 512], BF16) for i in range(2)]
        ogst = [sb(f"ogst{i}", [128, 4, 512], F32) for i in range(2)]
        gst = [sb(f"gst{i}", [128, 4, 32], F32) for i in range(2)]
        stg = [sb(f"stg{i}", [128, 12, 512], BF16) for i in range(2)]
        ugs = [sb(f"ug{i}", [128, 512], F32) for i in range(2)]
        vg = sb("vg", [128, 512], F32)
        vc = sb("vc", [128, 512], F32)
        vln = sb("vln", [128, 512], BF16)
        sgt = sb("sgt", [128, 512], F32)
        lns = sb("lns", [128, 4], F32)
        tps = [pm("tp0", [128, 8, 256], BF16)]
        zp = [pm(f"zp{i}", [128, 512], F32) for i in range(2)]
        qT = pm("qT", [128, 16, 128], BF16)
        sgp = pm("sgp", [128, 512], F32)
        for k in range(8):
            for (d0, s0, w) in ((0, 0, 1024), (1024, 1024, 1024), (2048, 2080, 512), (2560, 2592, 512),
                                (3072, 2048, 32)):
                s.dma('pool', 'p5_wcd', wcd[:, k, d0:d0 + w], I.w_in_cd[k * 128:(k + 1) * 128, s0:s0 + w],
                      writes=['wcd'])
        s.dma('pool', 'p5_wsT', wsT[:].rearrange("q g p -> q (g p)"), I.wsT[:, :], writes=['wsT'])
        s.dma('sp', 'p5_bsT', bsT[:], I.bsT[:, :], writes=['bsT'])
        s.dma('sp', 'p5_sgg', sgg[:], I.sgg[0:1, :].partition_broadcast(128), writes=['sgg'])
        s.dma('sp', 'p5_gbias', gbias[:], I.gate_bias.rearrange("a b -> b a")[0:1, :].partition_broadcast(128),
              writes=['gbias'])
        tiles = []
        for j, Sj in enumerate(g.seqs):
            for t in range(Sj // 512):
                tiles.append((j, g.offs[j] + t * 512))

        def load(i):
            j, t0 = tiles[i]
            b = i % 2
            s.dma('sp', f'p5_xt{b}', xt[b][:], S.x1[t0:t0 + 512, :].rearrange("(b p) d -> p b d", p=128),
                  writes=[f'xt{b}'])
        load(0)
        zi = 0
        pend = []
        for i, (j, t0) in enumerate(tiles):
            b = i % 2
            if i + 1 < len(tiles):
                load(i + 1)
            rms_prep(g, 'p5', xt[b], xn, ss, rstd, junk, 4, f'xt{b}', 'xn')
            to_featmajor(g, 'p5', xn, hmT, tps, 1, 0, j, 4, 'xn', 'hmT')
            for blk in range(4):
                q = qk[blk % 2]
                qkk = f'qk{blk % 2}'
                if blk > 0:
                    pass
                for c in range(7):
                    n0 = c * 512
                    nw = min(512, 3104 - n0)
                    p = zp[zi % 2]
                    pn = f'zp{zi % 2}'
                    zi += 1

                    def mm(e, p=p, n0=n0, nw=nw, blk=blk):
                        for k in range(8):
                            r = e.matmul(p[:, 0:nw], lhsT=hmT[:, k, blk * 128:(blk + 1) * 128],
                                         rhs=wcd[:, k, n0:n0 + nw], start=(k == 0), stop=(k == 7))
                        return r
                    s.op('pe', mm, reads=['hmT', 'wcd'], writes=[pn])
                    if c == 0:
                        s.op('act', lambda e, p=p, q=q: e.activation(out=q[:, 0:512], in_=p[:, :], func=AF.Copy),
                             reads=[pn], writes=[qkk + 'q'])
                    elif c == 1:
                        s.op('act', lambda e, p=p, q=q: e.activation(out=q[:, 512:1024], in_=p[:, :], func=AF.Copy,
                                                                     scale=0.125), reads=[pn], writes=[qkk + 'k'])
                        s.op('pool', lambda e, q=q, blk=blk, b=b: e.tensor_copy(out=kst[b][:, blk, :],
                                                                               in_=q[:, 512:1024]),
                             reads=[qkk + 'k'], writes=[f'kst{b}'])
                    elif c == 2:
                        s.op('act', lambda e, p=p, blk=blk, b=b: e.activation(out=vst[b][:, blk, :], in_=p[:, :],
                                                                              func=AF.Copy),
                             reads=[pn], writes=[f'vst{b}'])
                    elif c == 3:
                        s.op('act', lambda e, p=p, blk=blk, b=b: e.activation(out=ogst[b][:, blk, :], in_=p[:, :],
                                                                              func=AF.Sigmoid),
                             reads=[pn], writes=[f'ogst{b}'])
                    elif c == 4:
                        s.op('act', lambda e, p=p, blk=blk: e.activation(out=ugs[blk % 2][:], in_=p[:, :],
                                                                         func=AF.Gelu_apprx_tanh),
                             reads=[pn], writes=[f'ug{blk % 2}'])
                    elif c == 5:
                        s.op('act', lambda e, p=p: e.activation(out=vg[:], in_=p[:, :], func=AF.Gelu_apprx_tanh,
                                                                accum_out=lns[:, 0:1]),
                             reads=[pn], writes=['vg', 'lns0'])
                    else:
                        s.op('dve', lambda e, p=p, blk=blk, b=b: e.tensor_tensor(
                            out=gst[b][:, blk, :], in0=p[:, 0:32], in1=gbias[:], op=ALU.add),
                            reads=[pn, 'gbias'], writes=[f'gst{b}'])
                while pend:
                    pend.pop(0)()
                s.op('dve', lambda e: e.tensor_scalar(out=lns[:, 1:2], in0=lns[:, 0:1], scalar1=-1.0 / 512,
                                                      scalar2=None, op0=ALU.mult), reads=['lns0'], writes=['lns1'])
                s.op('act', lambda e: e.activation(out=vc[:], in_=vg[:], func=AF.Identity, bias=lns[:, 1:2]),
                     reads=['vg', 'lns1'], writes=['vc'])
                s.op('act', lambda e: e.activation(out=junk[:, 0:512], in_=vc[:], func=AF.Square,
                                                   accum_out=lns[:, 2:3]), reads=['vc'], writes=['lns2', 'p5junk'])
                rsqrt_act(g, lns[:, 3:4], lns[:, 2:3], 1.0 / 512, ['lns2'], ['lns3'])
                s.op('dve', lambda e: e.scalar_tensor_tensor(out=vln[:], in0=vc[:], scalar=lns[:, 3:4], in1=sgg[:],
                                                             op0=ALU.mult, op1=ALU.mult),
                     reads=['vc', 'lns3', 'sgg'], writes=['vln'])

                def tail(q=q, qkk=qkk, blk=blk, b=b):
                    def sgm(e):
                        for grp in range(8):
                            r = e.matmul(sgp[:, grp * 64:(grp + 1) * 64], lhsT=wsT[:, grp, :],
                                         rhs=vln[:, grp * 64:(grp + 1) * 64], start=True, stop=True)
                        return r
                    s.op('pe', sgm, reads=['wsT', 'vln'], writes=['sgp'])
                    s.op('dve', lambda e: e.tensor_tensor(out=sgt[:].rearrange("p (g e) -> p g e", e=64),
                                                          in0=sgp[:, :].rearrange("p (g e) -> p g e", e=64),
                                                          in1=bsT[:, :].unsqueeze(2).to_broadcast([128, 8, 64]),
                                                          op=ALU.add), reads=['sgp', 'bsT'], writes=['sgt'])
                    s.op('pool', lambda e, q=q: e.tensor_tensor(out=q[:, 1024:1536], in0=sgt[:], in1=ugs[blk % 2][:],
                                                                op=ALU.mult),
                         reads=['sgt', f'ug{blk % 2}'], writes=[qkk + 'd'])

                    def trq(e, q=q):
                        for c in range(12):
                            r = e.transpose(out=qT[:, c, :], in_=q[:, c * 128:(c + 1) * 128], identity=g.identB[:])
                        return r
                    s.op('pe', trq, reads=[qkk + 'q', qkk + 'k', qkk + 'd', 'identB'], writes=['qT'])
                    s.op('act', lambda e, blk=blk, b=b: e.activation(out=stg[b][:, :, blk * 128:(blk + 1) * 128],
                                                                     in_=qT[:, 0:12, :], func=AF.Copy),
                         reads=['qT'], writes=[f'stg{b}'])

                pend.append(tail)
            while pend:
                pend.pop(0)()
            for (dst, r0, c0, n) in ((S.qcT, 0, 0, 4), (S.kcT, 0, 4, 4), (S.o2T, 512, 8, 4)):
                s.dma('sp', f'p5_stg{b}', dst[r0:r0 + 512, t0:t0 + 512].rearrange("(c p) t -> p c t", p=128),
                      stg[b][:, c0:c0 + n, :], reads=[f'stg{b}'])
            tm = lambda d: d[t0:t0 + 512, :].rearrange("(b p) d -> p b d", p=128)
            s.dma('sp', f'p5_vst{b}', tm(S.vc), vst[b][:], reads=[f'vst{b}'])
            s.dma('sp', f'p5_kst{b}', tm(S.kc), kst[b][:], reads=[f'kst{b}'])
            s.dma('sp', f'p5_ogst{b}', tm(S.ogs), ogst[b][:], reads=[f'ogst{b}'])
            s.dma('sp', f'p5_gst{b}', tm(S.gtok), gst[b][:], reads=[f'gst{b}'])
        s.barrier()


def phase6(g):
    nc, s, I, S = g.nc, g.s, g.I, g.S
    SMAX = max(g.seqs)
    NCH = SMAX // 128
    with ExitStack() as st:
        sb = lambda n, sh, dt: st.enter_context(nc.sbuf_tensor("p6_" + n, sh, dt))
        pm = lambda n, sh, dt: st.enter_context(nc.psum_tensor("p6_" + n, sh, dt))
        SU = sb("SU", [128, 128], F32)
        SL = sb("SL", [128, 128], F32)
        ONES = sb("ONES", [128, 128], F32)
        maskF = sb("maskF", [128, 128], BF16)
        maskB = sb("maskB", [128, 128], BF16)
        mhg = sb("mhg", [128, 512], F32)
        gz = sb("gz", [128, NCH, 32], F32)
        l1 = [sb(f"l1{d}", [128, NCH * 8], F32) for d in range(2)]
        tmpg = sb("tmpg", [128, NCH * 8], F32)
        E = [sb(f"E{d}", [128, NCH * 8], F32) for d in range(2)]
        THR = [sb(f"THR{d}", [128, NCH * 8], F32) for d in range(2)]
        DEC = [sb(f"DEC{d}", [128, NCH * 8], F32) for d in range(2)]
        QTh = [sb(f"QTh{i}", [128, SMAX], BF16) for i in range(2)]
        KT = sb("KT", [128, SMAX], BF16)
        Ktok = sb("Ktok", [128, NCH, 128], BF16)
        V1 = sb("V1", [128, NCH, 2, 65], BF16)
        OGS = sb("OGS", [128, NCH, 128], F32)
        HF = sb("HF", [128, NCH, 128], F32)
        Wst = [sb(f"W{d}", [128, 65], F32) for d in range(2)]
        U = [sb(f"U{i}", [128, 65], BF16) for i in range(4)]
        Kw = [sb(f"Kw{i}", [128, 64], BF16) for i in range(4)]
        PT = [sb(f"PT{i}", [128, 128], BF16) for i in range(4)]
        rr = [sb(f"rr{i}", [128, 2], F32) for i in range(4)]
        sqt = sb("sqt", [128, 4, 128], F32)
        ssq = sb("ssq", [128, 8], F32)
        hn = sb("hn", [128, 4, 128], F32)
        hb = sb("hb", [128, 4, 128], BF16)
        stg = [sb(f"stg{i}", [128, 512], BF16) for i in range(2)]
        Bk = [pm(f"Bk{i}", [128, 512], F32) for i in range(6)]
        GP = Bk[0:4]
        trP = pm("trP", [128, 4, 128], BF16)
        DECp = [sb(f"DECp{d}", [128, NCH, 4], F32) for d in range(2)]
        Upair = [sb(f"Up{i}", [128, 65], BF16) for i in range(4)]
        Kwp = [sb(f"Kwp{i}", [128, 2, 64], BF16) for i in range(4)]
        PTp = [sb(f"PTp{i}", [128, 2, 128], BF16) for i in range(4)]
        rp = [sb(f"rp{i}", [128, 4], F32) for i in range(4)]
        htmp = [sb(f"htmp{i}", [128, 2, 64], F32) for i in range(2)]

        def tri(t, nm, cm, step, base):
            s.op('pool', lambda e: e.memset(t[:], 1.0), writes=[nm])
            s.op('pool', lambda e: e.affine_select(out=t[:], in_=t[:], pattern=[[step, 128]], compare_op=ALU.is_ge,
                                                   fill=0.0, base=base, channel_multiplier=cm),
                 reads=[nm], writes=[nm])
        tri(SU, 'SU', 1, -1, -1)
        tri(SL, 'SL', -1, 1, -1)
        tri(maskF, 'maskF', -1, 1, 0)
        tri(maskB, 'maskB', 1, -1, 0)
        s.op('pool', lambda e: e.memset(ONES[:], 1.0), writes=['ONES'])
        s.op('pool', lambda e: e.memset(QTh[0][64:128, :], 0.0), writes=['QT'])
        s.op('pool', lambda e: e.memset(QTh[1][0:64, :], 0.0), writes=['QT'])
        s.op('pool', lambda e: e.memset(V1[:, :, :, 64:65], 1.0), writes=['V1'])
        s.dma('sp', 'p6_mhg', mhg[:], I.mhg[0:1, :].partition_broadcast(128), writes=['mhg'])
        islot = 0
        istg = 0
        for j, Sj in enumerate(g.seqs):
            base = g.offs[j]
            nch = Sj // 128
            N8 = nch * 8
            s.dma('sp', 'p6_gz', gz[:, 0:nch, :], S.gtok[base:base + Sj, :].rearrange("(c p) d -> p c d", p=128),
                  writes=['gz'])
            for d in range(2):
                fcol = 8 + 16 * d
                icol = 16 * d
                l1v = l1[d][:, 0:N8].rearrange("p (c h) -> p c h", h=8)
                s.op('act', lambda e, nch=nch, N8=N8, l1v=l1v, fcol=fcol: e.activation(out=l1v, in_=gz[:, 0:nch, fcol:fcol + 8],
                                                                       func=AF.Exp, scale=-1.0),
                     reads=['gz'], writes=[f'l1{d}'])
                s.op('act', lambda e, nch=nch, N8=N8, d=d: e.activation(out=l1[d][:, 0:N8], in_=l1[d][:, 0:N8], func=AF.Ln, bias=1.0),
                     reads=[f'l1{d}'], writes=[f'l1{d}'])
                Gp, Tp = GP[2 * d], GP[2 * d + 1]
                tri_m = SU if d == 0 else SL
                s.op('pe', lambda e, nch=nch, N8=N8, Gp=Gp, tri_m=tri_m, d=d: e.matmul(Gp[:, 0:N8], lhsT=tri_m[:], rhs=l1[d][:, 0:N8],
                                                                      start=True, stop=True),
                     reads=['SU', 'SL', f'l1{d}'], writes=[f'B{2 * d}'])
                s.op('pe', lambda e, nch=nch, N8=N8, Tp=Tp, d=d: e.matmul(Tp[:, 0:N8], lhsT=ONES[:], rhs=l1[d][:, 0:N8],
                                                         start=True, stop=True),
                     reads=['ONES', f'l1{d}'], writes=[f'B{2 * d + 1}'])
                s.op('dve', lambda e, nch=nch, N8=N8, Gp=Gp, icol=icol: e.tensor_tensor(
                    out=tmpg[:, 0:N8].rearrange("p (c h) -> p c h", h=8), in0=gz[:, 0:nch, icol:icol + 8],
                    in1=Gp[:, 0:N8].rearrange("p (c h) -> p c h", h=8), op=ALU.subtract),
                    reads=['gz', f'B{2 * d}'], writes=['tmpg'])
                s.op('act', lambda e, nch=nch, N8=N8, d=d: e.activation(out=E[d][:, 0:N8], in_=tmpg[:, 0:N8], func=AF.Exp),
                     reads=['tmpg'], writes=[f'E{d}'])
                s.op('act', lambda e, nch=nch, N8=N8, d=d, Gp=Gp: e.activation(out=THR[d][:, 0:N8], in_=Gp[:, 0:N8], func=AF.Exp,
                                                              scale=-1.0), reads=[f'B{2 * d}'], writes=[f'THR{d}'])
                s.op('act', lambda e, nch=nch, N8=N8, d=d, Tp=Tp: e.activation(out=DEC[d][:, 0:N8], in_=Tp[:, 0:N8], func=AF.Exp,
                                                              scale=-1.0), reads=[f'B{2 * d + 1}'], writes=[f'DEC{d}'])
            for d in range(2):
                for hh in range(2):
                    p0 = hh * 64
                    s.op('pool', lambda e, nch=nch, N8=N8, d=d, hh=hh, p0=p0: e.tensor_copy(
                        out=DECp[d][p0:p0 + 64, 0:nch, :],
                        in_=DEC[d][p0:p0 + 64, 0:N8].rearrange("p (c hp two) -> p c hp two", hp=4, two=2)[:, :, :, hh]),
                        reads=[f'DEC{d}'], writes=[f'DECp{d}'])
            for hp in range(4):
                r0 = hp * 128
                s.dma('sp', 'p6_QT', QTh[0][0:64, 0:Sj], S.qcT[r0:r0 + 64, base:base + Sj], writes=['QT'])
                s.dma('sp', 'p6_QT', QTh[1][64:128, 0:Sj], S.qcT[r0 + 64:r0 + 128, base:base + Sj], writes=['QT'])
                s.dma('sp', 'p6_KT', KT[:, 0:Sj], S.kcT[r0:r0 + 128, base:base + Sj], writes=['KT'])
                s.dma('sp', 'p6_Ktok', Ktok[:, 0:nch, :],
                      S.kc[base:base + Sj, r0:r0 + 128].rearrange("(c p) d -> p c d", p=128), writes=['Ktok'])
                for hh in range(2):
                    s.dma('sp', 'p6_V1', V1[:, 0:nch, hh, 0:64],
                          S.vc[base:base + Sj, r0 + hh * 64:r0 + hh * 64 + 64].rearrange("(c p) e -> p c e", p=128),
                          writes=['V1'])
                s.dma('sp', 'p6_OGS', OGS[:, 0:nch, :],
                      S.ogs[base:base + Sj, r0:r0 + 128].rearrange("(c p) d -> p c d", p=128), writes=['OGS'])
                pend = []

                def flush(n):
                    while len(pend) > n:
                        pend.pop(0)()
                for ci in range(nch):
                    for d in range(2):
                        c = ci if d == 0 else nch - 1 - ci
                        first = (ci == 0)
                        last = (ci == nch - 1)
                        mask = maskF if d == 0 else maskB
                        sl = islot % 4
                        bkp = islot % 2
                        islot += 1
                        col0 = c * 8 + hp * 2
                        cs = slice(c * 128, (c + 1) * 128)
                        spb, Ob, dUb = Bk[bkp], Bk[2 + bkp], Bk[4 + bkp]
                        spk, Ok, dUk = f'B{bkp}', f'B{2 + bkp}', f'B{4 + bkp}'

                        def st1(spb=spb, spk=spk, cs=cs, sl=sl, c=c, d=d, col0=col0, mask=mask):
                            def mm(e):
                                for hh in range(2):
                                    p0 = hh * 64
                                    r = e.matmul(spb[:, hh * 128:(hh + 1) * 128], lhsT=KT[:, cs],
                                                 rhs=QTh[hh][:, cs], start=True, stop=True)
                                return r
                            s.op('pe', mm, reads=['KT', 'QT'], writes=[spk])
                            for hh in range(2):
                                s.op('act', lambda e, hh=hh: e.activation(
                                    out=Kwp[sl][:, hh, :], in_=Ktok[:, c, hh * 64:(hh + 1) * 64], func=AF.Copy,
                                    scale=E[d][:, col0 + hh:col0 + hh + 1]),
                                    reads=['Ktok', f'E{d}'], writes=[f'Kwp{sl}'])
                            for hh in range(2):
                                s.op('dve', lambda e, hh=hh: e.scalar_tensor_tensor(
                                    out=PTp[sl][:, hh, :], in0=spb[:, hh * 128:(hh + 1) * 128],
                                    scalar=E[d][:, col0 + hh:col0 + hh + 1], in1=mask[:], op0=ALU.mult, op1=ALU.mult),
                                    reads=[spk, f'E{d}', 'maskF', 'maskB'], writes=[f'PTp{sl}'])

                        firstw = (ci < nch // 2)

                        def st2(Ob=Ob, Ok=Ok, dUb=dUb, dUk=dUk, cs=cs, sl=sl, c=c, d=d, col0=col0, first=first,
                                last=last, hp=hp, firstw=firstw):
                            wk = f'W{d}'
                            u_ = Upair[sl]
                            if not first:
                                s.op('act', lambda e: e.activation(out=u_[:], in_=Wst[d][:], func=AF.Copy,
                                                                   scale=DECp[d][:, c, hp:hp + 1]),
                                     reads=[wk, f'DECp{d}'], writes=[f'Up{sl}'])

                            def omm(e):
                                for hh in range(2):
                                    p0 = hh * 64
                                    r = e.matmul(Ob[:, hh * 128:hh * 128 + 65], lhsT=PTp[sl][:, hh, :],
                                                 rhs=V1[:, c, hh, :], start=True, stop=first)
                                    if not first:
                                        r = e.matmul(Ob[:, hh * 128:hh * 128 + 65], lhsT=QTh[hh][:, cs],
                                                     rhs=u_[:, :], start=False, stop=True)
                                return r
                            s.op('pe', omm, reads=[f'PTp{sl}', 'V1', 'QT', f'Up{sl}'], writes=[Ok])
                            if not last:
                                def dmm(e):
                                    for hh in range(2):
                                        p0 = hh * 64
                                        r = e.matmul(dUb[p0:p0 + 64, 0:65], lhsT=Kwp[sl][:, hh, :],
                                                     rhs=V1[:, c, hh, :], start=True, stop=True)
                                    return r
                                s.op('pe', dmm, reads=[f'Kwp{sl}', 'V1'], writes=[dUk])
                                if first:
                                    s.op('dve', lambda e: e.tensor_copy(out=Wst[d][:], in_=dUb[:, 0:65]),
                                         reads=[dUk], writes=[wk])
                                else:
                                    s.op('dve', lambda e: e.scalar_tensor_tensor(
                                        out=Wst[d][:], in0=Wst[d][:], scalar=DECp[d][:, c, hp:hp + 1],
                                        in1=dUb[:, 0:65], op0=ALU.mult, op1=ALU.add),
                                        reads=[wk, f'DECp{d}', dUk], writes=[wk])
                            if os.environ.get('P6A'):
                                return
                            r_ = rp[sl]
                            Ov = Ob[:, 0:256].rearrange("p (h x) -> p h x", x=128)
                            s.op('act', lambda e: e.activation(out=r_[:, 0:2].unsqueeze(2), in_=Ov[:, :, 64:65],
                                                               func=AF.Abs), reads=[Ok], writes=[f'rp{sl}'])
                            s.op('dve', lambda e: e.tensor_tensor(out=r_[:, 0:2], in0=r_[:, 0:2],
                                                                  in1=THR[d][:, col0:col0 + 2], op=ALU.max),
                                 reads=[f'rp{sl}', f'THR{d}'], writes=[f'rp{sl}'])
                            s.op('dve', lambda e: e.reciprocal(out=r_[:, 2:4], in_=r_[:, 0:2]),
                                 reads=[f'rp{sl}'], writes=[f'rp{sl}b'])
                            hfv = HF[:, c, :].rearrange("p (h x) -> p h x", x=64)
                            rb = r_[:, 2:4].unsqueeze(2).to_broadcast([128, 2, 64])
                            if firstw:
                                s.op('dve', lambda e: e.tensor_tensor(out=hfv, in0=Ov[:, :, 0:64], in1=rb, op=ALU.mult),
                                     reads=[Ok, f'rp{sl}b'], writes=[f'HF{c}'])
                            else:
                                ht = htmp[sl % 2]
                                s.op('dve', lambda e: e.tensor_tensor(out=ht[:], in0=Ov[:, :, 0:64], in1=rb, op=ALU.mult),
                                     reads=[Ok, f'rp{sl}b'], writes=[f'htmp{sl % 2}'])
                                s.op('pool', lambda e: e.tensor_tensor(out=hfv, in0=hfv, in1=ht[:], op=ALU.add),
                                     reads=[f'htmp{sl % 2}', f'HF{c}'], writes=[f'HF{c}'])
                        st1()
                        pend.append(st2)
                        flush(int(os.environ.get('LAG6', '1')))
                flush(0)
                for cg in range(nch // 4):
                    hv = HF[:, cg * 4:(cg + 1) * 4, :]
                    hkeys = [f'HF{c}' for c in range(cg * 4, cg * 4 + 4)]
                    s.op('act', lambda e, hv=hv: e.activation(out=sqt[:], in_=hv, func=AF.Square),
                         reads=hkeys, writes=['sqt'])
                    s.op('dve', lambda e: e.tensor_reduce(out=ssq[:], in_=sqt[:].rearrange("p c (h e) -> p (c h) e", e=64),
                                                          axis=AX.X, op=ALU.add), reads=['sqt'], writes=['ssq'])
                    rsqrt_act(g, ssq[:], ssq[:], 1.0 / 64, ['ssq'], ['ssq'])
                    s.op('dve', lambda e, hv=hv: e.tensor_tensor(
                        out=hn[:].rearrange("p c (h e) -> p (c h) e", e=64),
                        in0=hv.rearrange("p c (h e) -> p (c h) e", e=64),
                        in1=ssq[:].unsqueeze(2).to_broadcast([128, 8, 64]), op=ALU.mult),
                        reads=hkeys + ['ssq'], writes=['hn'])
                    s.op('pool', lambda e, r0=r0: e.tensor_tensor(out=hn[:], in0=hn[:],
                                                                  in1=bc(mhg[:, r0:r0 + 128], [128, 4, 128]), op=ALU.mult),
                         reads=['hn', 'mhg'], writes=['hn'])
                    s.op('pool', lambda e, cg=cg: e.tensor_tensor(out=hb[:], in0=hn[:], in1=OGS[:, cg * 4:(cg + 1) * 4, :],
                                                                  op=ALU.mult), reads=['hn', 'OGS'], writes=['hb'])

                    def trh(e):
                        for i in range(4):
                            r = e.transpose(out=trP[:, i, :], in_=hb[:, i, :], identity=g.identB[:])
                        return r
                    s.op('pe', trh, reads=['hb', 'identB'], writes=['trP'])
                    bs = istg % 2
                    istg += 1
                    s.op('act', lambda e, bs=bs: e.activation(out=stg[bs][:], in_=trP[:, :, :].rearrange("p c t -> p (c t)"),
                                                              func=AF.Copy), reads=['trP'], writes=[f'stg{bs}'])
                    t0 = base + cg * 512
                    s.dma('sp', f'p6_stg{bs}', S.o2T[r0:r0 + 128, t0:t0 + 512], stg[bs][:], reads=[f'stg{bs}'])
        s.barrier()
```

```python
import numpy as np
import os
from contextlib import ExitStack
import concourse.bass as bass
import concourse.mybir as mybir
from concourse.bass_utils import run_bass_kernel_spmd

F32 = mybir.dt.float32
BF16 = mybir.dt.bfloat16
AF = mybir.ActivationFunctionType
ALU = mybir.AluOpType
AX = mybir.AxisListType
ENGS = ('pe', 'act', 'dve', 'pool', 'sp')
D = 1024
EPS = 1e-6


class Sched:
    def __init__(self, nc, stack):
        self.nc = nc
        self.stack = stack
        self.streams = {e: [] for e in ENGS}
        self.sems = {}
        self.cnt = {}
        self.seen = {e: {} for e in ENGS}
        self.bufs = {}
        self.snap = {}
        self.nwaits = 0
        self.nops = 0

    def sem(self, name):
        if name not in self.sems:
            self.sems[name] = self.stack.enter_context(
                self.nc.semaphore(name.replace(':', '_').replace('/', '_')))
            self.cnt[name] = 0
        return self.sems[name]

    def _need(self, E, ev, waits):
        if ev is None:
            return
        name, val = ev
        if self.seen[E].get(name, 0) >= val:
            return
        if waits.get(name, 0) < val:
            waits[name] = val

    def _collect(self, E, reads, writes):
        waits = {}
        for k in reads:
            b = self.bufs.get(k)
            if b is not None:
                self._need(E, b['w'], waits)
        own = 'e:' + E
        for k in writes:
            b = self.bufs.get(k)
            if b is not None:
                self._need(E, b['w'], waits)
                for ev in b['r']:
                    if ev[0] != own:
                        self._need(E, ev, waits)
        return waits

    def _apply_waits(self, E, waits):
        seen = self.seen[E]
        wl = []
        for name, val in waits.items():
            if seen.get(name, 0) >= val:
                continue
            wl.append((self.sem(name), val))
            sn = self.snap.get((name, val))
            if sn:
                for n2, v2 in sn.items():
                    if seen.get(n2, 0) < v2:
                        seen[n2] = v2
            seen[name] = val
        self.nwaits += len(wl)
        return wl

    def _record(self, ev, reads, writes):
        for k in reads:
            b = self.bufs.get(k)
            if b is None:
                b = self.bufs[k] = {'w': None, 'r': []}
            b['r'].append(ev)
            if len(b['r']) > 64:
                b['r'] = b['r'][-48:]
        for k in writes:
            self.bufs[k] = {'w': ev, 'r': []}

    def op(self, E, fn, reads=(), writes=()):
        waits = self._collect(E, reads, writes)
        if E == 'pe':
            waits.pop('e:pe', None)
        wl = self._apply_waits(E, waits)
        name = 'e:' + E
        s = self.sem(name)
        self.cnt[name] += 1
        val = self.cnt[name]
        ev = (name, val)
        if E == 'pe':
            self.seen[E][name] = val
        self.snap[ev] = dict(self.seen[E])
        self.streams[E].append((wl, fn, s, 1))
        self._record(ev, reads, writes)
        self.nops += 1
        return ev

    def dma(self, Q, semname, out, in_, reads=(), writes=(), **kw):
        waits = self._collect(Q, reads, writes)
        wl = self._apply_waits(Q, waits)
        name = 'd:' + semname
        s = self.sem(name)
        self.cnt[name] += 16
        val = self.cnt[name]
        ev = (name, val)
        self.snap[ev] = dict(self.seen[Q])

        def fn(eng, out=out, in_=in_, kw=kw):
            return eng.dma_start(out=out, in_=in_, **kw)
        self.streams[Q].append((wl, fn, s, 16))
        self._record(ev, reads, writes)
        self.nops += 1
        return ev

    def barrier(self):
        allev = dict(self.cnt)
        for E in ENGS:
            waits = {}
            for name, val in allev.items():
                if val > 0 and self.seen[E].get(name, 0) < val and name != 'e:' + E:
                    waits[name] = val
            wl = self._apply_waits(E, waits)
            if wl:
                self.streams[E].append((wl, None, None, 0))
        self.bufs = {}

    def emit(self):
        nc = self.nc
        self.barrier()
        streams = self.streams
        with nc.Block() as block:
            def run(E):
                def body(eng):
                    for wl, fn, s, inc in streams[E]:
                        for (ws, wv) in wl:
                            eng.wait_ge(ws, wv)
                        if fn is not None:
                            fn(eng).then_inc(s, inc)
                return body
            block.sync(run('sp'))
            block.scalar(run('act'))
            block.vector(run('dve'))
            block.gpsimd(run('pool'))
            block.tensor(run('pe'))


class Ctx:
    pass


def build(seqs, phases, debug=False):
    nc = bass.Bass("TRN2", target_bir_lowering=False)
    NS = len(seqs)
    NT = sum(seqs)
    offs = [sum(seqs[:i]) for i in range(NS)]
    SMAX = max(seqs)
    g = Ctx()
    g.nc, g.seqs, g.NS, g.NT, g.offs, g.debug = nc, seqs, NS, NT, offs, debug

    def din(name, shape, dt=F32):
        return nc.dram_tensor(name, list(shape), dt, kind="ExternalInput").ap()

    def dscr(name, shape, dt):
        return nc.dram_tensor(name, list(shape), dt, kind=("ExternalOutput" if debug else "Internal")).ap()
    g.dscr = dscr
    I = Ctx()
    g.I = I
    I.x = din("x", [NT, D])
    I.cT = din("cT", [128, 8 * NS])
    I.w_in_ab = din("w_in_ab", [D, 2304])
    I.w_out_ab = din("w_out_ab", [D, D])
    I.w_in_cd = din("w_in_cd", [D, 3104])
    I.w_out_cd = din("w_out_cd", [D, D])
    I.w_ada = din("w_ada", [2, D, 6 * D])
    I.b_ada = din("b_ada", [1, 2 * 6 * D])
    I.normgT = din("normgT", [128, 2 * 4 * 8])
    I.norm_g = din("norm_g", [8, D])
    I.w_ff1 = din("w_ff1", [2, D, 4 * D])
    I.w_ff2 = din("w_ff2", [2, 4 * D, D])
    I.qkg = din("qkg", [1, 128])
    I.ropeA = din("ropeA", [SMAX, 64])
    I.ropeB = din("ropeB", [SMAX, 64])
    I.gate_bias = din("gate_bias", [32, 1])
    I.mhg = din("mhg", [1, 512])
    I.sgg = din("sgg", [1, 512])
    I.wsT = din("wsT", [128, 8 * 128])
    I.bsT = din("bsT", [128, 8])
    y = nc.dram_tensor("y", [NT, D], F32, kind="ExternalOutput").ap()
    g.y = y

    Sx = Ctx()
    g.S = Sx
    Sx.gates = dscr("s_gates", [2, 2, NS, D], F32)
    Sx.qaT = dscr("s_qaT", [512, NT], BF16)
    Sx.kaT = dscr("s_kaT", [512, NT], BF16)
    Sx.qbT = dscr("s_qbT", [512, NT], BF16)
    Sx.kbT = dscr("s_kbT", [128, NT], BF16)
    Sx.vA = dscr("s_vA", [NT, 512], BF16)
    Sx.vB = dscr("s_vB", [NT, 128], BF16)
    Sx.oT = dscr("s_oT", [D, NT], BF16)
    Sx.xmid = dscr("s_xmid", [NT, D], F32)
    Sx.x1 = dscr("s_x1", [NT, D], F32)
    Sx.qcT = dscr("s_qcT", [512, NT], BF16)
    Sx.kcT = dscr("s_kcT", [512, NT], BF16)
    Sx.kc = dscr("s_kc", [NT, 512], BF16)
    Sx.vc = dscr("s_vc", [NT, 512], BF16)
    Sx.ogs = dscr("s_ogs", [NT, 512], F32)
    Sx.gtok = dscr("s_gtok", [NT, 32], F32)
    Sx.o2T = dscr("s_o2T", [D, NT], BF16)

    with ExitStack() as st:
        s = Sched(nc, st)
        g.s = s
        g.identB = st.enter_context(nc.sbuf_tensor("identB", [128, 128], BF16))
        g.identF = st.enter_context(nc.sbuf_tensor("identF", [128, 128], F32))
        g.GS = st.enter_context(nc.sbuf_tensor("GS", [128, 2, 2, 8, NS], F32))
        g.SH = st.enter_context(nc.sbuf_tensor("SH", [128, 2, 2, 8, NS], F32))
        g.epsc = st.enter_context(nc.sbuf_tensor("epsc", [128, 2], F32))
        s.op('pool', lambda e: e.memset(g.epsc[:], EPS), writes=['epsc'])
        for t, nm in ((g.identB, 'identB'), (g.identF, 'identF')):
            s.op('pool', lambda e, t=t: e.memset(t[:], 1.0), writes=[nm])
            s.op('pool', lambda e, t=t: e.affine_select(out=t[:], in_=t[:], pattern=[[-1, 128]],
                                                        compare_op=ALU.is_equal, fill=0.0, base=0,
                                                        channel_multiplier=1), reads=[nm], writes=[nm])
        if 0 in phases:
            phase0(g)
        if 1 in phases:
            phase1(g)
        if 2 in phases:
            import os
            if not os.environ.get('SKIPB'):
                phase2(g)
            if not os.environ.get('SKIPA'):
                phase2a(g)
        if 3 in phases:
            phase3(g, 0, Sx.oT, I.w_out_ab, I.x, Sx.xmid)
        if 4 in phases:
            phase4(g, 0, Sx.xmid, Sx.x1 if (5 in phases or debug) else g.y)
        if 5 in phases:
            phase5(g)
        if 6 in phases:
            phase6(g)
        if 7 in phases:
            phase3(g, 1, Sx.o2T, I.w_out_cd, Sx.x1, Sx.xmid)
        if 8 in phases:
            phase4(g, 1, Sx.xmid, g.y)
        s.emit()
    g.stats = (s.nops, s.nwaits)
    return nc, g


def load_w_bf16(g, st_name, wt, w_dram, K, N, n0=0, colmap=None):
    s = g.s
    for k in range(K):
        c = 0
        while c < N:
            w = min(1024, N - c)
            s.dma('pool', st_name, wt[:, k, c:c + w], w_dram[k * 128:(k + 1) * 128, n0 + c:n0 + c + w],
                  writes=[st_name])
            c += w


def phase0(g):
    nc, s, NS, I = g.nc, g.s, g.NS, g.I
    with ExitStack() as st:
        sb = lambda n, sh, dt: st.enter_context(nc.sbuf_tensor(n, sh, dt))
        cTf = sb("p0_cTf", [128, 8 * NS], F32)
        cTb = sb("p0_cTb", [128, 8, NS], BF16)
        wt = [sb(f"p0_w{i}", [128, 8, 3072], BF16) for i in range(2)]
        bada = sb("p0_bada", [NS, 2 * 6 * D], F32)
        modrow = sb("p0_modrow", [NS, 2, 6 * D], F32)
        ngT = sb("p0_ngT", [128, 2, 4, 8], F32)
        modT = sb("p0_modT", [128, 2, 4, 8, NS], F32)
        ps = [st.enter_context(nc.psum_tensor(f"p0_ps{i}", [128, 512], F32)) for i in range(2)]
        pT = st.enter_context(nc.psum_tensor("p0_pT", [128, 4 * 8 * NS], F32))
        s.dma('sp', 'p0_cTf', cTf[:], I.cT[:, :], writes=['cTf'])
        s.dma('sp', 'p0_bada', bada[:], I.b_ada[0:1, :].partition_broadcast(NS), writes=['bada'])
        s.dma('sp', 'p0_ngT', ngT[:], I.normgT[:, :], writes=['ngT'])
        s.op('act', lambda e: e.activation(out=cTb[:].rearrange("p k s -> p (k s)"), in_=cTf[:], func=AF.Silu),
             reads=['cTf'], writes=['cTb'])
        it = 0
        for l in range(2):
            for hf in range(2):
                w = wt[it % 2]
                wn = f"p0_w{it % 2}"
                load_w_bf16(g, wn, w, I.w_ada[l], 8, 3072, n0=hf * 3072)
                for nch in range(6):
                    p = ps[nch % 2]
                    pn = f"p0_ps{nch % 2}"

                    def mm(e, p=p, w=w, nch=nch):
                        for k in range(8):
                            r = e.matmul(p[0:NS, :], lhsT=cTb[:, k, :], rhs=w[:, k, nch * 512:(nch + 1) * 512],
                                         start=(k == 0), stop=(k == 7))
                        return r
                    s.op('pe', mm, reads=['cTb', wn], writes=[pn])
                    c0 = hf * 3072 + nch * 512
                    s.op('dve', lambda e, p=p, l=l, c0=c0: e.tensor_tensor(
                        out=modrow[:, l, c0:c0 + 512], in0=p[0:NS, :],
                        in1=bada[:, l * 6 * D + c0:l * 6 * D + c0 + 512], op=ALU.add),
                        reads=[pn, 'bada'], writes=['modrow'])
                it += 1
        for l in range(2):
            for gi, part in enumerate((2, 5)):
                s.dma('sp', 'p0_gst', g.S.gates[l, gi], modrow[:, l, part * D:(part + 1) * D],
                      reads=['modrow'])
        for l in range(2):
            def tr(e, l=l):
                for pi, part in enumerate((0, 1, 3, 4)):
                    for k in range(8):
                        c0 = part * D + k * 128
                        o = (pi * 8 + k) * NS
                        r = e.transpose(out=pT[:, o:o + NS], in_=modrow[:, l, c0:c0 + 128],
                                        identity=g.identF[0:NS, 0:NS])
                return r
            s.op('pe', tr, reads=['modrow', 'identF'], writes=['p0_pT'])
            s.op('dve', lambda e, l=l: e.tensor_copy(out=modT[:, l].rearrange("p a k s -> p (a k s)"), in_=pT[:, :]),
                 reads=['p0_pT'], writes=['modT'])
        for l in range(2):
            for m in range(2):
                sc = modT[:, l, 2 * m + 1]
                sh = modT[:, l, 2 * m]
                ngb = ngT[:, l, 2 * m, :].unsqueeze(2).to_broadcast([128, 8, NS])
                s.op('dve', lambda e, sc=sc, ngb=ngb, l=l, m=m: e.scalar_tensor_tensor(
                    out=g.GS[:, l, m], in0=sc, scalar=1.0, in1=ngb, op0=ALU.add, op1=ALU.mult),
                    reads=['modT', 'ngT'], writes=['GS'])
                s.op('dve', lambda e, sh=sh, l=l, m=m: e.tensor_copy(out=g.SH[:, l, m], in_=sh),
                     reads=['modT'], writes=['SH'])
        s.barrier()


def bc(ap2d, shape):
    return ap2d.unsqueeze(1).to_broadcast(shape)


def rsqrt_act(g, out, in_, scale, rk, wk):
    s = g.s
    s.op('act', lambda e: e.activation(out=out, in_=in_, func=AF.Ln, scale=scale, bias=g.epsc[:, 0:1]),
         reads=rk + ['epsc'], writes=wk)
    s.op('act', lambda e: e.activation(out=out, in_=out, func=AF.Exp, scale=-0.5), reads=wk, writes=wk)


def rms_prep(g, pre, xt, xn, ss, rstd, junk, nblk, xkey, outkey):
    s = g.s
    for b in range(nblk):
        s.op('act', lambda e, b=b: e.activation(out=junk[:], in_=xt[:, b, :], func=AF.Square,
                                                accum_out=ss[:, b:b + 1]),
             reads=[xkey], writes=[pre + 'junk', pre + 'ss'])
    rsqrt_act(g, rstd[:, 0:nblk], ss[:, 0:nblk], 1.0 / D, [pre + 'ss'], [pre + 'rstd'])
    for b in range(nblk):
        s.op('act', lambda e, b=b: e.activation(out=xn[:, b, :], in_=xt[:, b, :], func=AF.Copy,
                                                scale=rstd[:, b:b + 1]),
             reads=[xkey, pre + 'rstd'], writes=[outkey])


def to_featmajor(g, pre, xn, hmT, tps, l, m, j, nblk, xnkey, hkey):
    s = g.s
    for half in range(nblk // 2):
        tp = tps[half % len(tps)]
        tk = pre + f'tp{half % len(tps)}'

        def tr(e, half=half, tp=tp):
            for k in range(8):
                for bb in range(2):
                    r = e.transpose(out=tp[:, k, bb * 128:(bb + 1) * 128],
                                    in_=xn[:, half * 2 + bb, k * 128:(k + 1) * 128], identity=g.identB[:])
            return r
        s.op('pe', tr, reads=[xnkey, 'identB'], writes=[tk])
        for k in range(8):
            eng = 'dve' if k % 2 == 0 else 'pool'
            eng = 'dve'
            s.op(eng, lambda e, k=k, half=half, tp=tp: e.tensor_scalar(
                out=hmT[:, k, half * 256:(half + 1) * 256], in0=tp[:, k, :],
                scalar1=g.GS[:, l, m, k, j:j + 1], scalar2=g.SH[:, l, m, k, j:j + 1],
                op0=ALU.mult, op1=ALU.add), reads=[tk, 'GS', 'SH'], writes=[hkey])


def rotary(g, eng, out, zin, cos, sin, H, hd, tmp, rk, wk, tk):
    s = g.s
    zv = zin.rearrange("p (h two d) -> p h two d", two=2, d=hd)
    ov = out.rearrange("p (h two d) -> p h two d", two=2, d=hd)
    x1, x2 = zv[:, :, 0, :], zv[:, :, 1, :]
    o1, o2 = ov[:, :, 0, :], ov[:, :, 1, :]
    cb = bc(cos, [128, H, hd])
    sb_ = bc(sin, [128, H, hd])
    t = [tmp[:, i, 0:H * hd].rearrange("p (h d) -> p h d", d=hd) for i in range(4)]
    s.op(eng, lambda e: e.tensor_tensor(out=t[0], in0=x1, in1=cb, op=ALU.mult), reads=rk, writes=[tk + '0'])
    s.op(eng, lambda e: e.tensor_tensor(out=t[1], in0=x2, in1=sb_, op=ALU.mult), reads=rk, writes=[tk + '1'])
    s.op(eng, lambda e: e.tensor_tensor(out=o1, in0=t[0], in1=t[1], op=ALU.subtract),
         reads=[tk + '0', tk + '1'], writes=wk)
    s.op(eng, lambda e: e.tensor_tensor(out=t[2], in0=x2, in1=cb, op=ALU.mult), reads=rk, writes=[tk + '2'])
    s.op(eng, lambda e: e.tensor_tensor(out=t[3], in0=x1, in1=sb_, op=ALU.mult), reads=rk, writes=[tk + '3'])
    s.op(eng, lambda e: e.tensor_tensor(out=o2, in0=t[2], in1=t[3], op=ALU.add),
         reads=[tk + '2', tk + '3'], writes=wk)


def phase1(g):
    nc, s, NS, I, S = g.nc, g.s, g.NS, g.I, g.S
    with ExitStack() as st:
        sb = lambda n, sh, dt: st.enter_context(nc.sbuf_tensor(n, sh, dt))
        pm = lambda n, sh, dt: st.enter_context(nc.psum_tensor(n, sh, dt))
        wab = sb("p1_wab", [128, 8, 2304], BF16)
        xt = [sb(f"p1_xt{i}", [128, 4, D], F32) for i in range(2)]
        xn = [sb(f"p1_xn{i}", [128, 4, D], BF16) for i in range(2)]
        junk = sb("p1_junk", [128, D], BF16)
        ss = [sb(f"p1_ss{i}", [128, 4], F32) for i in range(2)]
        rstd = [sb(f"p1_rstd{i}", [128, 4], F32) for i in range(2)]
        hmT = [sb(f"p1_hmT{i}", [128, 8, 512], BF16) for i in range(2)]
        zs = [sb(f"p1_zs{i}", [128, 2304], F32) for i in range(2)]
        rA = [sb(f"p1_rA{i}", [128, 4, 64], F32) for i in range(2)]
        rB = [sb(f"p1_rB{i}", [128, 4, 64], F32) for i in range(2)]
        qkg = sb("p1_qkg", [128, 128], F32)
        tmpA = sb("p1_tmpA", [128, 4, 256], F32)
        tmpB = sb("p1_tmpB", [128, 4, 256], F32)
        sq = sb("p1_sq", [128, 640], F32)
        ssq = sb("p1_ssq", [128, 10], F32)
        qn = sb("p1_qn", [128, 640], F32)
        qk = [sb(f"p1_qk{i}", [128, 1664], BF16) for i in range(2)]
        stg = [sb(f"p1_stg{i}", [128, 13, 512], BF16) for i in range(2)]
        vst = [sb(f"p1_vst{i}", [128, 4, 640], BF16) for i in range(2)]
        tps = [pm(f"p1_tp{i}", [128, 8, 256], BF16) for i in range(2)]
        zp = [pm(f"p1_zp{i}", [128, 512], F32) for i in range(2)]
        qT = pm("p1_qT", [128, 16, 128], BF16)

        load_w_bf16(g, 'p1_wab', wab, I.w_in_ab, 8, 2304)
        s.dma('sp', 'p1_qkg', qkg[:], I.qkg[0:1, :].partition_broadcast(128), writes=['qkg'])
        tiles = []
        for j, Sj in enumerate(g.seqs):
            for t in range(Sj // 512):
                tiles.append((j, t * 512, g.offs[j] + t * 512))

        def load(i):
            j, p0, t0 = tiles[i]
            b = i % 2
            s.dma('sp', f'p1_xt{b}', xt[b][:], I.x[t0:t0 + 512, :].rearrange("(b p) d -> p b d", p=128),
                  writes=[f'xt{b}'])
            s.dma('sp', f'p1_rA{b}', rA[b][:], I.ropeA[p0:p0 + 512, :].rearrange("(b p) d -> p b d", p=128),
                  writes=[f'rA{b}'])
            s.dma('sp', f'p1_rB{b}', rB[b][:], I.ropeB[p0:p0 + 512, :].rearrange("(b p) d -> p b d", p=128),
                  writes=[f'rB{b}'])
        load(0)
        zi = 0
        pend = []

        def flush():
            while pend:
                pend.pop(0)()

        def prep(i):
            j, p0, t0 = tiles[i]
            b = i % 2
            rms_prep(g, f'p1{b}', xt[b], xn[b], ss[b], rstd[b], junk, 4, f'xt{b}', f'xn{b}')
            to_featmajor(g, 'p1', xn[b], hmT[b], tps, 0, 0, j, 4, f'xn{b}', f'hmT{b}')
        prep(0)
        for i, (j, p0, t0) in enumerate(tiles):
            b = i % 2
            if i + 1 < len(tiles):
                load(i + 1)
            for blk in range(4):
                if blk == 3 and i + 1 < len(tiles):
                    prep(i + 1)
                zb = zs[blk % 2]
                zk = f'zs{blk % 2}'
                for c in range(5):
                    n0 = c * 512
                    nw = min(512, 2304 - n0)
                    p = zp[zi % 2]
                    pn = f'zp{zi % 2}'
                    zi += 1

                    def mm(e, p=p, n0=n0, nw=nw, blk=blk, b=b):
                        for k in range(8):
                            r = e.matmul(p[:, 0:nw], lhsT=hmT[b][:, k, blk * 128:(blk + 1) * 128],
                                         rhs=wab[:, k, n0:n0 + nw], start=(k == 0), stop=(k == 7))
                        return r
                    s.op('pe', mm, reads=[f'hmT{b}', 'p1_wab'], writes=[pn])
                    if c == 2:
                        s.op('act', lambda e, p=p, blk=blk, b=b: e.activation(
                            out=vst[b][:, blk, 0:512], in_=p[:, 0:512], func=AF.Copy),
                            reads=[pn], writes=[f'vst{b}'])
                    else:
                        s.op('act', lambda e, p=p, n0=n0, nw=nw, zb=zb: e.activation(
                            out=zb[:, n0:n0 + nw], in_=p[:, 0:nw], func=AF.Copy),
                            reads=[pn], writes=[zk + f'c{c}'])
                flush()
                q = qk[blk % 2]
                qkk = f'qk{blk % 2}'
                cosA, sinA = rA[b][:, blk, 0:32], rA[b][:, blk, 32:64]
                cosB, sinB = rB[b][:, blk, 0:32], rB[b][:, blk, 32:64]
                rotary(g, 'dve', q[:, 0:512], zb[:, 0:512], cosA, sinA, 8, 32, tmpA,
                       [zk + 'c0', f'rA{b}'], [qkk + 'qa'], 'tmpA')
                rotary(g, 'pool', q[:, 512:1024], zb[:, 512:1024], cosA, sinA, 8, 32, tmpB,
                       [zk + 'c1', f'rA{b}'], [qkk + 'ka'], 'tmpB')
                s.op('act', lambda e, zb=zb: e.activation(out=sq[:], in_=zb[:, 1536:2176], func=AF.Square),
                     reads=[zk + 'c3', zk + 'c4'], writes=['sq'])
                s.op('dve', lambda e: e.tensor_reduce(out=ssq[:], in_=sq[:].rearrange("p (h d) -> p h d", d=64),
                                                      axis=AX.X, op=ALU.add), reads=['sq'], writes=['ssq'])
                rsqrt_act(g, ssq[:], ssq[:], 1.0 / 64, ['ssq'], ['ssq'])
                s.op('dve', lambda e, zb=zb: e.tensor_tensor(
                    out=qn[:].rearrange("p (h d) -> p h d", d=64),
                    in0=zb[:, 1536:2176].rearrange("p (h d) -> p h d", d=64),
                    in1=ssq[:].unsqueeze(2).to_broadcast([128, 10, 64]), op=ALU.mult),
                    reads=[zk + 'c3', zk + 'c4', 'ssq'], writes=['qn'])
                s.op('dve', lambda e: e.tensor_tensor(
                    out=qn[:, 0:512].rearrange("p (h d) -> p h d", d=64),
                    in0=qn[:, 0:512].rearrange("p (h d) -> p h d", d=64),
                    in1=bc(qkg[:, 0:64], [128, 8, 64]), op=ALU.mult), reads=['qn', 'qkg'], writes=['qn'])
                s.op('dve', lambda e: e.tensor_tensor(
                    out=qn[:, 512:640].rearrange("p (h d) -> p h d", d=64),
                    in0=qn[:, 512:640].rearrange("p (h d) -> p h d", d=64),
                    in1=bc(qkg[:, 64:128], [128, 2, 64]), op=ALU.mult), reads=['qn', 'qkg'], writes=['qn'])
                for half in range(2):
                    for (c0, H, o0) in ((0, 8, 1024), (512, 2, 1536)):
                        zin = qn[:, c0:c0 + H * 64].rearrange("p (h x) -> p h x", x=64)[:, :, half * 32:(half + 1) * 32]
                        oo = q[:, o0:o0 + H * 64].rearrange("p (h x) -> p h x", x=64)[:, :, half * 32:(half + 1) * 32]
                        rotary_v(g, 'dve', oo, zin, cosB[:, half * 16:(half + 1) * 16],
                                 sinB[:, half * 16:(half + 1) * 16], H, 16, tmpA, ['qn', f'rB{b}'],
                                 [qkk + 'b'], 'tmpA')
                s.op('pool', lambda e, zb=zb, blk=blk, b=b: e.tensor_copy(out=vst[b][:, blk, 512:640],
                                                                          in_=zb[:, 2176:2304]),
                     reads=[zk + 'c4'], writes=[f'vst{b}'])
                def tail(q=q, qkk=qkk, blk=blk, b=b):
                    def trq(e):
                        for c in range(13):
                            r = e.transpose(out=qT[:, c, :], in_=q[:, c * 128:(c + 1) * 128], identity=g.identB[:])
                        return r
                    s.op('pe', trq, reads=[qkk + 'qa', qkk + 'ka', qkk + 'b', 'identB'], writes=['qT'])
                    s.op('act', lambda e: e.activation(out=stg[b][:, :, blk * 128:(blk + 1) * 128],
                                                       in_=qT[:, 0:13, :], func=AF.Copy),
                         reads=['qT'], writes=[f'stg{b}'])
                pend.append(tail)
            flush()
            for (dst, c0, n) in ((S.qaT, 0, 4), (S.kaT, 4, 4), (S.qbT, 8, 4), (S.kbT, 12, 1)):
                s.dma('sp', f'p1_stg{b}', dst[:, t0:t0 + 512].rearrange("(c p) t -> p c t", p=128),
                      stg[b][:, c0:c0 + n, :], reads=[f'stg{b}'])
            s.dma('sp', f'p1_vst{b}', S.vA[t0:t0 + 512, :].rearrange("(b p) d -> p b d", p=128), vst[b][:, :, 0:512],
                  reads=[f'vst{b}'])
            s.dma('sp', f'p1_vst{b}', S.vB[t0:t0 + 512, :].rearrange("(b p) d -> p b d", p=128), vst[b][:, :, 512:640],
                  reads=[f'vst{b}'])
        s.barrier()


def rotary_v(g, eng, ov, zv, cos, sin, H, hd, tmp, rk, wk, tk):
    s = g.s
    x1, x2 = zv[:, :, 0:hd], zv[:, :, hd:2 * hd]
    o1, o2 = ov[:, :, 0:hd], ov[:, :, hd:2 * hd]
    cb = bc(cos, [128, H, hd])
    sb_ = bc(sin, [128, H, hd])
    t = [tmp[:, i, 0:H * hd].rearrange("p (h d) -> p h d", d=hd) for i in range(4)]
    s.op(eng, lambda e: e.tensor_tensor(out=t[0], in0=x1, in1=cb, op=ALU.mult), reads=rk, writes=[tk + '0'])
    s.op(eng, lambda e: e.tensor_tensor(out=t[1], in0=x2, in1=sb_, op=ALU.mult), reads=rk, writes=[tk + '1'])
    s.op(eng, lambda e: e.tensor_tensor(out=o1, in0=t[0], in1=t[1], op=ALU.subtract),
         reads=[tk + '0', tk + '1'], writes=wk)
    s.op(eng, lambda e: e.tensor_tensor(out=t[2], in0=x2, in1=cb, op=ALU.mult), reads=rk, writes=[tk + '2'])
    s.op(eng, lambda e: e.tensor_tensor(out=t[3], in0=x1, in1=sb_, op=ALU.mult), reads=rk, writes=[tk + '3'])
    s.op(eng, lambda e: e.tensor_tensor(out=o2, in0=t[2], in1=t[3], op=ALU.add),
         reads=[tk + '2', tk + '3'], writes=wk)


def phase2(g):
    nc, s, S = g.nc, g.s, g.S
    SMAX = max(g.seqs)
    with ExitStack() as st:
        sb = lambda n, sh, dt: st.enter_context(nc.sbuf_tensor(n, sh, dt))
        pm = lambda n, sh, dt: st.enter_context(nc.psum_tensor(n, sh, dt))
        kT = [sb(f"p2_kT{i}", [128, SMAX], BF16) for i in range(2)]
        V1 = [sb(f"p2_V1{i}", [128, SMAX // 128, 128], BF16) for i in range(2)]
        qT = [sb(f"p2_qT{i}", [128, SMAX], BF16) for i in range(2)]
        pT = [sb(f"p2_pT{i}", [128, 1024], BF16) for i in range(3)]
        rd = [sb(f"p2_rd{i}", [64, 512], F32) for i in range(2)]
        ob = [sb(f"p2_ob{i}", [64, 512], BF16) for i in range(2)]
        sp_ = [pm(f"p2_sp{i}", [128, 1024], F32) for i in range(3)]
        ot = [pm(f"p2_ot{i}", [128, 512], F32) for i in range(2)]
        for i in range(2):
            s.op('pool', lambda e, i=i: e.memset(V1[i][:, :, 64:128], 1.0), writes=[f'V1{i}'])
            s.op('pool', lambda e, i=i: e.memset(kT[i][64:128, :], 0.0), writes=[f'kT{i}'])
            s.op('pool', lambda e, i=i: e.memset(qT[i][64:128, :], 0.0), writes=[f'qT{i}'])
        ikv = 0
        ih = 0
        ip = 0
        io = 0
        LAG = 2
        pend = []

        def flush(n):
            while len(pend) > n:
                pend.pop(0)()
        for j, Sj in enumerate(g.seqs):
            base = g.offs[j]
            nkb, nqg = Sj // 128, Sj // 512
            for kv in range(2):
                bk = ikv % 2
                ikv += 1
                s.dma('sp', f'p2_kT{bk}', kT[bk][0:64, 0:Sj], S.kbT[kv * 64:(kv + 1) * 64, base:base + Sj],
                      writes=[f'kT{bk}'])
                s.dma('sp', f'p2_V1{bk}', V1[bk][:, 0:nkb, 0:64],
                      S.vB[base:base + Sj, kv * 64:(kv + 1) * 64].rearrange("(b p) d -> p b d", p=128),
                      writes=[f'V1{bk}'])
                for hq in range(4):
                    h = kv * 4 + hq
                    bq = ih % 2
                    ih += 1
                    s.dma('sp', f'p2_qT{bq}', qT[bq][0:64, 0:Sj], S.qbT[h * 64:(h + 1) * 64, base:base + Sj],
                          writes=[f'qT{bq}'])
                    for qg in range(nqg):
                        o = ot[io % 2]
                        on = f'ot{io % 2}'
                        bo = io % 2
                        io += 1
                        for kb in range(0, nkb, 2):
                            p = sp_[ip % 3]
                            pn = f'sp{ip % 3}'
                            pt = pT[ip % 3]
                            ptn = f'pT{ip % 3}'
                            ip += 1

                            def smm(e, p=p, bk=bk, bq=bq, kb=kb, qg=qg):
                                for i in range(2):
                                    r = e.matmul(p[:, i * 512:(i + 1) * 512],
                                                 lhsT=kT[bk][:, (kb + i) * 128:(kb + i + 1) * 128],
                                                 rhs=qT[bq][:, qg * 512:(qg + 1) * 512], start=True, stop=True)
                                return r
                            s.op('pe', smm, reads=[f'kT{bk}', f'qT{bq}'], writes=[pn])
                            s.op('act', lambda e, p=p, pt=pt: e.activation(out=pt[:], in_=p[:], func=AF.Exp,
                                                                           scale=0.125),
                                 reads=[pn], writes=[ptn])

                            def stage2(o=o, on=on, bo=bo, bk=bk, kb=kb, pt=pt, ptn=ptn, nkb=nkb, h=h, qg=qg,
                                       base=base):
                                def pvm(e):
                                    for i in range(2):
                                        r = e.matmul(o[:, :], lhsT=V1[bk][:, kb + i, :], rhs=pt[:, i * 512:(i + 1) * 512],
                                                     start=(kb + i == 0), stop=(kb + i == nkb - 1))
                                    return r
                                s.op('pe', pvm, reads=[f'V1{bk}', ptn], writes=[on])
                                if kb + 2 == nkb:
                                    s.op('dve', lambda e: e.reciprocal(out=rd[bo][:], in_=o[64:128, :]),
                                         reads=[on], writes=[f'rd{bo}'])
                                    s.op('dve', lambda e: e.tensor_tensor(out=ob[bo][:], in0=o[0:64, :],
                                                                          in1=rd[bo][:], op=ALU.mult),
                                         reads=[on, f'rd{bo}'], writes=[f'ob{bo}'])
                                    t0 = base + qg * 512
                                    s.dma('sp', f'p2_ob{bo}', S.oT[512 + h * 64:512 + (h + 1) * 64, t0:t0 + 512],
                                          ob[bo][:], reads=[f'ob{bo}'])
                            pend.append(stage2)
                            flush(LAG)
        flush(0)
        s.barrier()


def phase2a(g):
    nc, s, S = g.nc, g.s, g.S
    with ExitStack() as st:
        sb = lambda n, sh, dt: st.enter_context(nc.sbuf_tensor(n, sh, dt))
        pm = lambda n, sh, dt: st.enter_context(nc.psum_tensor(n, sh, dt))
        kTw = sb("p2a_kTw", [128, 4, 4096], BF16)
        qTsh = [sb(f"p2a_qTs{i}", [128, 4, 2048], BF16) for i in range(2)]
        ACC = sb("p2a_ACC", [128, 8, 2048], F32)
        V1 = [sb(f"p2a_V1{i}", [128, 9, 8, 128], BF16) for i in range(2)]
        band = sb("p2a_band", [128, 3, 256], BF16)
        pT = [sb(f"p2a_pT{i}", [128, 256], BF16) for i in range(4)]
        pTm = [sb(f"p2a_pTm{i}", [128, 256], BF16) for i in range(4)]
        rd = [sb(f"p2a_rd{i}", [64, 2048], F32) for i in range(2)]
        ob = [sb(f"p2a_ob{i}", [64, 2048], BF16) for i in range(2)]
        sp_ = [pm(f"p2a_sp{i}", [128, 512], F32) for i in range(4)]
        ot = [pm(f"p2a_ot{i}", [128, 512], F32) for i in range(4)]
        s.op('pool', lambda e: e.memset(qTsh[0][64:128, :, :], 0.0), writes=['qTs'])
        s.op('pool', lambda e: e.memset(qTsh[1][0:64, :, :], 0.0), writes=['qTs'])
        NEG = -30000.0
        for bi in range(3):
            bt = band[:, bi, :]
            s.op('pool', lambda e, bt=bt: e.memset(bt, 0.0), writes=['band'])
            s.op('pool', lambda e, bt=bt: e.affine_select(out=bt, in_=bt, pattern=[[1, 256]], compare_op=ALU.is_ge,
                                                          fill=NEG, base=0, channel_multiplier=-1),
                 reads=['band'], writes=['band'])
            s.op('pool', lambda e, bt=bt: e.affine_select(out=bt, in_=bt, pattern=[[-1, 256]], compare_op=ALU.is_ge,
                                                          fill=NEG, base=128, channel_multiplier=1),
                 reads=['band'], writes=['band'])
        s.op('pool', lambda e: e.memset(band[0:64, 1, :], NEG), reads=['band'], writes=['band'])
        s.op('pool', lambda e: e.memset(band[64:128, 2, :], NEG), reads=['band'], writes=['band'])
        for i in range(2):
            s.op('pool', lambda e, i=i: e.memset(V1[i][:, :, :, 0:64], 0.0), writes=[f'aV1{i}'])
            s.op('pool', lambda e, i=i: e.memset(V1[i][:, :, :, 64:128], 1.0), writes=[f'aV1{i}'])
        iu = 0
        ip = 0
        import os
        LAG = int(os.environ.get('LAGA', '3'))
        pend = []

        def flush(n):
            while len(pend) > n:
                pend.pop(0)()
        for j, Sj in enumerate(g.seqs):
            base = g.offs[j]
            for seg in range(Sj // 2048):
                seg0 = seg * 2048
                lo, hi = max(0, seg0 - 1024), min(Sj, seg0 + 3072)
                if lo > seg0 - 1024:
                    s.op('pool', lambda e: e.memset(kTw[:, :, 0:1024], 0.0), writes=['kTw'])
                if hi < seg0 + 3072:
                    s.op('pool', lambda e: e.memset(kTw[:, :, 3072:4096], 0.0), writes=['kTw'])
                s.dma('sp', 'p2a_kTw', kTw[:, :, lo - (seg0 - 1024):hi - (seg0 - 1024)],
                      S.kaT[:, base + lo:base + hi].rearrange("(c p) t -> p c t", p=128), writes=['kTw'])
                qsrc = S.qaT[:, base + seg0:base + seg0 + 2048].rearrange("(c p) t -> p c t", p=128)
                s.dma('sp', 'p2a_qTs', qTsh[0][0:64, :, :], qsrc[0:64], writes=['qTs'])
                s.dma('sp', 'p2a_qTs', qTsh[1][64:128, :, :], qsrc[64:128], writes=['qTs'])
                units = [(1, 0, 0, 8), (1, 0, 8, 8)] + [(4, r, 0, 4) for r in range(4)] + \
                        [(16, r, 0, 1) for r in range(16)]
                first_write = {}
                for (d, r, qb0, nqb) in units:
                    L = Sj // d
                    lq0 = seg0 // d + 128 * qb0
                    bv = iu % 2
                    iu += 1
                    vk = f'aV1{bv}'
                    v1 = V1[bv]
                    nkb = nqb + 1
                    m_lo, m_hi = 0, nkb
                    at_start = (lq0 == 0)
                    at_end = (lq0 + 128 * nqb == L)
                    if lq0 == 0:
                        tok = base + 0 * d + r
                        s.dma('sp', f'p2a_V1{bv}', v1[64:128, 0, :, 0:64],
                              S.vA[tok:tok + 63 * d + 1:d, :].rearrange("p (h e) -> p h e", e=64), writes=[vk])
                        m_lo = 1
                    if lq0 + 128 * nqb == L:
                        tok = base + (lq0 - 64 + 128 * nqb) * d + r
                        s.dma('sp', f'p2a_V1{bv}', v1[0:64, nqb, :, 0:64],
                              S.vA[tok:tok + 63 * d + 1:d, :].rearrange("p (h e) -> p h e", e=64), writes=[vk])
                        m_hi = nkb - 1
                    for m in range(m_lo, m_hi):
                        tok = base + (lq0 - 64 + 128 * m) * d + r
                        s.dma('sp', f'p2a_V1{bv}', v1[:, m, :, 0:64],
                              S.vA[tok:tok + 127 * d + 1:d, :].rearrange("p (h e) -> p h e", e=64), writes=[vk])
                    for h in range(8):
                        c, hh = h // 2, (h % 2) * 64
                        for m in range(nkb):
                            n_lo, n_hi = max(m - 1, 0), min(m, nqb - 1)
                            nq = n_hi - n_lo + 1
                            kc0 = 1024 + (128 * (qb0 + m) - 64) * d + r
                            qc0 = 128 * (qb0 + n_lo) * d + r
                            p = sp_[ip % 4][:, 0:256]
                            pn = f'asp{ip % 4}'
                            pt, ptm = pT[ip % 4], pTm[ip % 4]
                            ptn = f'apT{ip % 4}'
                            ip += 1
                            W = nq * 128
                            b0 = 128 if m == 0 else 0
                            bi = 1 if (m == 0 and at_start) else (2 if (m == nkb - 1 and at_end) else 0)

                            def smm(e, p=p, c=c, hh=hh, kc0=kc0, qc0=qc0, W=W, d=d, b0=b0, bi=bi):
                                e.matmul(p[:, 0:W], lhsT=kTw[:, c, kc0:kc0 + 127 * d + 1:d],
                                         rhs=qTsh[hh // 64][:, c, qc0:qc0 + (W - 1) * d + 1:d], start=True, stop=False)
                                return e.matmul(p[:, 0:W], lhsT=g.identB[:], rhs=band[:, bi, b0:b0 + W],
                                                start=False, stop=True)
                            s.op('pe', smm, reads=['kTw', 'qTs', 'band', 'identB'], writes=[pn])
                            s.op('act', lambda e, p=p, ptm=ptm, W=W: e.activation(out=ptm[:, 0:W], in_=p[:, 0:W],
                                                                                 func=AF.Exp, scale=0.125),
                                 reads=[pn], writes=[ptn + 'm'])

                            def stage2(n_lo=n_lo, n_hi=n_hi, h=h, v1=v1, vk=vk, m=m, ptm=ptm, ptn=ptn, d=d, r=r,
                                       qb0=qb0):
                                for n in range(n_lo, n_hi + 1):
                                    o = ot[n % 4][:, 0:128]
                                    on = f'aot{n % 4}'
                                    col = (n - n_lo) * 128
                                    s.op('pe', lambda e, o=o, col=col, n=n: e.matmul(
                                        o, lhsT=v1[:, m, h, :], rhs=ptm[:, col:col + 128],
                                        start=(m == n), stop=(m == n + 1)),
                                        reads=[vk, ptn + 'm'], writes=[on])
                                    if m == n + 1:
                                        a0 = 128 * (qb0 + n) * d + r
                                        av = ACC[:, h, a0:a0 + 127 * d + 1:d]
                                        if d == 1:
                                            s.op('dve', lambda e, o=o, av=av: e.tensor_copy(out=av, in_=o),
                                                 reads=[on], writes=[f'ACC{h}'])
                                        else:
                                            s.op('dve', lambda e, o=o, av=av: e.tensor_tensor(out=av, in0=o, in1=av,
                                                                                              op=ALU.add),
                                                 reads=[on, f'ACC{h}'], writes=[f'ACC{h}'])
                            pend.append(stage2)
                            flush(LAG)
                flush(0)
                for h in range(8):
                    bo = h % 2
                    s.op('act', lambda e, h=h, bo=bo: e.activation(out=rd[bo][:], in_=ACC[64:128, h, :], func=AF.Ln),
                         reads=[f'ACC{h}'], writes=[f'ard{bo}'])
                    s.op('act', lambda e, bo=bo: e.activation(out=rd[bo][:], in_=rd[bo][:], func=AF.Exp, scale=-1.0),
                         reads=[f'ard{bo}'], writes=[f'ard{bo}'])
                    s.op('pool', lambda e, h=h, bo=bo: e.tensor_tensor(out=ob[bo][:], in0=ACC[0:64, h, :],
                                                                      in1=rd[bo][:], op=ALU.mult),
                         reads=[f'ACC{h}', f'ard{bo}'], writes=[f'aob{bo}'])
                    t0 = base + seg0
                    s.dma('sp', f'p2a_ob{bo}', S.oT[h * 64:(h + 1) * 64, t0:t0 + 2048], ob[bo][:],
                          reads=[f'aob{bo}'])
        s.barrier()


def post_norm_residual(g, pre, yp, ypk, xres, xkey, GB, gbk, out, outkey, ss, rstd, junk, tmp, par=0):
    s = g.s
    sp = str(par)
    tm = tmp[par] if isinstance(tmp, (list, tuple)) else tmp
    s.op('act', lambda e: e.activation(out=junk[:], in_=yp[:, :], func=AF.Square, accum_out=ss[:, par:par + 1]),
         reads=[ypk], writes=[pre + 'junk', pre + 'ss' + sp])
    rsqrt_act(g, rstd[:, par:par + 1], ss[:, par:par + 1], 1.0 / D, [pre + 'ss' + sp], [pre + 'rstd' + sp])
    s.op('dve', lambda e: e.scalar_tensor_tensor(out=tm[:], in0=yp[:, :], scalar=rstd[:, par:par + 1], in1=GB[:],
                                                 op0=ALU.mult, op1=ALU.mult),
         reads=[ypk, pre + 'rstd' + sp, gbk], writes=[pre + 'tmp' + sp])
    s.op('pool' if par == 0 else 'dve', lambda e: e.tensor_tensor(out=out, in0=tm[:], in1=xres, op=ALU.add),
         reads=[pre + 'tmp' + sp, xkey], writes=[outkey])


def load_GB(g, pre, GB, gb, ngb, l, gi, j):
    s = g.s
    s.dma('sp', pre + 'gb', gb[:], g.S.gates[l, gi, j:j + 1, :].partition_broadcast(128), writes=[pre + 'gb'])
    s.op('dve', lambda e: e.tensor_tensor(out=GB[:], in0=gb[:], in1=ngb[:], op=ALU.mult),
         reads=[pre + 'gb', pre + 'ngb'], writes=[pre + 'GB'])


def phase3(g, l, oT_d, w_out_d, xin_d, xout_d):
    nc, s = g.nc, g.s
    pre = f'p3{l}'
    with ExitStack() as st:
        sb = lambda n, sh, dt: st.enter_context(nc.sbuf_tensor(pre + n, sh, dt))
        pm = lambda n, sh, dt: st.enter_context(nc.psum_tensor(pre + n, sh, dt))
        wo = sb("wo", [128, 8, D], BF16)
        oT = [sb(f"oT{i}", [128, 8, 512], BF16) for i in range(2)]
        xt = [sb(f"xt{i}", [128, 4, D], F32) for i in range(2)]
        xo = [sb(f"xo{i}", [128, 4, D], F32) for i in range(2)]
        gb = sb("gb", [128, D], F32)
        ngb = sb("ngb", [128, D], F32)
        GB = sb("GB", [128, D], F32)
        junk = sb("junk", [128, D], BF16)
        tmp = [sb(f"tmp{i}", [128, D], F32) for i in range(2)]
        ss = sb("ss", [128, 2], F32)
        rstd = sb("rstd", [128, 2], F32)
        yp = [pm(f"yp{i}", [128, D], F32) for i in range(2)]
        load_w_bf16(g, pre + 'wo', wo, w_out_d, 8, D)
        s.dma('sp', pre + 'ngb', ngb[:], g.I.norm_g[l * 4 + 1:l * 4 + 2, :].partition_broadcast(128),
              writes=[pre + 'ngb'])
        tiles = []
        for j, Sj in enumerate(g.seqs):
            for t in range(Sj // 512):
                tiles.append((j, g.offs[j] + t * 512))

        def load(i):
            j, t0 = tiles[i]
            b = i % 2
            s.dma('sp', pre + f'oT{b}', oT[b][:], oT_d[:, t0:t0 + 512].rearrange("(c p) t -> p c t", p=128),
                  writes=[pre + f'oT{b}'])
            s.dma('sp', pre + f'xt{b}', xt[b][:], xin_d[t0:t0 + 512, :].rearrange("(b p) d -> p b d", p=128),
                  writes=[pre + f'xt{b}'])
        load(0)
        curj = -1
        iy = 0
        for i, (j, t0) in enumerate(tiles):
            b = i % 2
            if i + 1 < len(tiles):
                load(i + 1)
            if j != curj:
                load_GB(g, pre, GB, gb, ngb, l, 0, j)
                curj = j
            for blk in range(4):
                y_ = yp[iy % 2]
                yk = pre + f'yp{iy % 2}'
                iy += 1

                def mm(e, y_=y_, b=b, blk=blk):
                    for nh in range(2):
                        for k in range(8):
                            r = e.matmul(y_[:, nh * 512:(nh + 1) * 512], lhsT=oT[b][:, k, blk * 128:(blk + 1) * 128],
                                         rhs=wo[:, k, nh * 512:(nh + 1) * 512], start=(k == 0), stop=(k == 7))
                    return r
                s.op('pe', mm, reads=[pre + f'oT{b}', pre + 'wo'], writes=[yk])
                post_norm_residual(g, pre, y_, yk, xt[b][:, blk, :], pre + f'xt{b}', GB, pre + 'GB',
                                   xo[b][:, blk, :], pre + f'xo{b}', ss, rstd, junk, tmp, par=(iy % 2))
            s.dma('sp', pre + f'xo{b}', xout_d[t0:t0 + 512, :].rearrange("(b p) d -> p b d", p=128), xo[b][:],
                  reads=[pre + f'xo{b}'])
        s.barrier()


def phase4(g, l, xin_d, xout_d):
    nc, s = g.nc, g.s
    pre = f'p4{l}'
    with ExitStack() as st:
        sb = lambda n, sh, dt: st.enter_context(nc.sbuf_tensor(pre + n, sh, dt))
        pm = lambda n, sh, dt: st.enter_context(nc.psum_tensor(pre + n, sh, dt))
        w1 = sb("w1", [128, 8, 4 * D], BF16)
        w2 = sb("w2", [128, 32, D], BF16)
        xt = [sb(f"xt{i}", [128, 2, D], F32) for i in range(2)]
        xn = sb("xn", [128, 2, D], BF16)
        xo = [sb(f"xo{i}", [128, 2, D], F32) for i in range(2)]
        hfT = sb("hfT", [128, 8, 256], BF16)
        h1a = sb("h1a", [128, 2, 256], BF16)
        h1T = sb("h1T", [128, 32, 256], BF16)
        ngb = sb("ngb", [128, D], F32)
        GB = sb("GB", [128, D], F32)
        junk = sb("junk", [128, D], BF16)
        tmp = [sb(f"tmp{i}", [128, D], F32) for i in range(2)]
        ss = sb("ss", [128, 4], F32)
        rstd = sb("rstd", [128, 4], F32)
        ss2 = sb("ss2", [128, 2], F32)
        rstd2 = sb("rstd2", [128, 2], F32)
        tps = [pm("tp0", [128, 8, 256], BF16)]
        hp = [pm(f"hp{i}", [128, 512], F32) for i in range(2)]
        yp = [pm(f"yp{i}", [128, D], F32) for i in range(2)]
        load_w_bf16(g, pre + 'w1', w1, g.I.w_ff1[l], 8, 4 * D)
        load_w_bf16(g, pre + 'w2', w2, g.I.w_ff2[l], 32, D)
        s.dma('sp', pre + 'ngb', ngb[:], g.I.norm_g[l * 4 + 3:l * 4 + 4, :].partition_broadcast(128),
              writes=[pre + 'ngb'])
        tiles = []
        for j, Sj in enumerate(g.seqs):
            for t in range(Sj // 256):
                tiles.append((j, g.offs[j] + t * 256))

        def load(i):
            j, t0 = tiles[i]
            b = i % 2
            s.dma('sp', pre + f'xt{b}', xt[b][:], xin_d[t0:t0 + 256, :].rearrange("(b p) d -> p b d", p=128),
                  writes=[pre + f'xt{b}'])
        load(0)
        curj = -1
        ih = 0
        iy = 0
        def prep_a(i):
            j, t0 = tiles[i]
            b = i % 2
            rms_prep(g, pre, xt[b], xn, ss, rstd, junk, 2, pre + f'xt{b}', pre + 'xn')

        def prep_b(i):
            j, t0 = tiles[i]
            to_featmajor(g, pre, xn, hfT, tps, l, 1, j, 2, pre + 'xn', pre + 'hfT')
        prep_a(0)
        prep_b(0)
        for i, (j, t0) in enumerate(tiles):
            b = i % 2
            if i + 1 < len(tiles):
                load(i + 1)
            if j != curj:
                s.dma('sp', pre + 'GB', GB[:], g.S.gates[l, 1, j:j + 1, :].partition_broadcast(128),
                      writes=[pre + 'GB'])
                s.op('pool', lambda e: e.tensor_tensor(out=GB[:], in0=GB[:], in1=ngb[:], op=ALU.mult),
                     reads=[pre + 'GB', pre + 'ngb'], writes=[pre + 'GB'])
                curj = j
            for fc in range(32):
                p = hp[ih % 2]
                pn = pre + f'hp{ih % 2}'
                ha = h1a[:, ih % 2, :]
                hak = pre + f'h1a{ih % 2}'
                ih += 1

                def mm(e, p=p, fc=fc):
                    for k in range(8):
                        r = e.matmul(p[:, 0:256], lhsT=w1[:, k, fc * 128:(fc + 1) * 128], rhs=hfT[:, k, :],
                                     start=(k == 0), stop=(k == 7))
                    return r
                s.op('pe', mm, reads=[pre + 'w1', pre + 'hfT'], writes=[pn])
                s.op('act', lambda e, p=p, ha=ha: e.activation(out=ha, in_=p[:, 0:256], func=AF.Relu),
                     reads=[pn], writes=[hak])
                eng = 'pool' if fc % 2 == 0 else 'dve'
                s.op(eng, lambda e, ha=ha, fc=fc: e.tensor_tensor(out=h1T[:, fc, :], in0=ha, in1=ha, op=ALU.mult),
                     reads=[hak], writes=[pre + f'h1T{fc}'])
            if i + 1 < len(tiles):
                prep_a(i + 1)
            for blk in range(2):
                if blk == 1 and i + 1 < len(tiles):
                    prep_b(i + 1)
                y_ = yp[iy % 2]
                yk = pre + f'yp{iy % 2}'
                iy += 1

                for kh in range(2):
                    def mm2(e, y_=y_, blk=blk, kh=kh):
                        for nh in range(2):
                            for k in range(kh * 16, kh * 16 + 16):
                                r = e.matmul(y_[:, nh * 512:(nh + 1) * 512], lhsT=h1T[:, k, blk * 128:(blk + 1) * 128],
                                             rhs=w2[:, k, nh * 512:(nh + 1) * 512], start=(k == 0), stop=(k == 31))
                        return r
                    s.op('pe', mm2, reads=[pre + f'h1T{fc}' for fc in range(kh * 16, kh * 16 + 16)] + [pre + 'w2'],
                         writes=[yk])
                post_norm_residual(g, pre + 'b', y_, yk, xt[b][:, blk, :], pre + f'xt{b}', GB, pre + 'GB',
                                   xo[b][:, blk, :], pre + f'xo{b}', ss2, rstd2, junk, tmp, par=(iy % 2))
            s.dma('sp', pre + f'xo{b}', xout_d[t0:t0 + 256, :].rearrange("(b p) d -> p b d", p=128), xo[b][:],
                  reads=[pre + f'xo{b}'])
        s.barrier()


def _rope_angles(pos, dim):
    inv_freq = (np.float32(10000.0) ** (-np.arange(0, dim, 2, dtype=np.float32) / np.float32(dim))).astype(np.float32)
    return (pos.astype(np.float32)[:, None] * inv_freq[None, :]).astype(np.float32)


def rope_tables(S):
    pos = np.arange(S)
    angA = _rope_angles(pos, 64)
    ar = _rope_angles(pos // 64, 32)
    ac = _rope_angles(pos % 64, 32)
    ropeA = np.concatenate([np.cos(angA), np.sin(angA)], axis=1).astype(np.float32)
    ropeB = np.concatenate([np.cos(ar), np.cos(ac), np.sin(ar), np.sin(ac)], axis=1).astype(np.float32)
    return ropeA, ropeB


def core_map(inp, xs, cs):
    NS = len(xs)
    SMAX = max(x.shape[0] for x in xs)
    ropeA, ropeB = rope_tables(SMAX)
    c = np.stack(cs)
    cT = c.reshape(NS, 8, 128).transpose(2, 1, 0).reshape(128, 8 * NS)
    ng = np.asarray(inp['norm_g'])
    m = {
        'x': np.concatenate(xs, 0), 'cT': cT,
        'w_in_ab': inp['w_in_ab'][0], 'w_out_ab': inp['w_out_ab'][0], 'w_in_cd': inp['w_in_cd'][0],
        'w_out_cd': inp['w_out_cd'][0], 'w_ada': inp['w_ada'], 'b_ada': np.asarray(inp['b_ada']).reshape(1, -1),
        'normgT': ng.reshape(2, 4, 8, 128).transpose(3, 0, 1, 2).reshape(128, 64),
        'norm_g': ng.reshape(8, 1024), 'w_ff1': inp['w_ff1'], 'w_ff2': inp['w_ff2'],
        'qkg': np.asarray(inp['qk_norm_g']).reshape(1, 128), 'ropeA': ropeA, 'ropeB': ropeB,
        'gate_bias': np.asarray(inp['gate_bias']).reshape(32, 1), 'mhg': np.asarray(inp['mh_norm_g']).reshape(1, 512),
        'sgg': np.asarray(inp['sg_norm_g']).reshape(1, 512),
        'wsT': np.asarray(inp['w_spatial'])[0].transpose(2, 0, 1).reshape(128, 1024),
        'bsT': np.asarray(inp['b_spatial'])[0].T,
    }
    return {k: np.ascontiguousarray(np.asarray(v), dtype=np.float32) for k, v in m.items()}


ALL_PHASES = (0, 1, 2, 3, 4, 5, 6, 7, 8)
_CACHE = {}


def kernel(**inputs):
    inp = {k: np.asarray(v) for k, v in inputs.items()}
    n = 8
    seqs = [8192, 2048, 2048, 2048, 2048]
    if 'nc' not in _CACHE:
        _CACHE['nc'] = build(seqs, set(ALL_PHASES), debug=False)
    nc, g = _CACHE['nc']
    in_maps = []
    for i in range(n):
        xs = [inp['x_prompt'][i]] + [inp['x_sample'][4 * i + k] for k in range(4)]
        cs = [inp['c_prompt'][i]] + [inp['c_sample'][4 * i + k] for k in range(4)]
        in_maps.append(core_map(inp, xs, cs))
    res = run_bass_kernel_spmd(nc, in_maps, core_ids=list(range(n)))
    y_prompt = np.empty((8, 8192, 1024), np.float32)
    y_sample = np.empty((32, 2048, 1024), np.float32)
    for i in range(n):
        y = np.asarray(res.results[i]['y'])
        y_prompt[i] = y[0:8192]
        for k in range(4):
            y_sample[4 * i + k] = y[8192 + 2048 * k:8192 + 2048 * (k + 1)]
    return (y_prompt, y_sample)


def phase5(g):
    nc, s, NS, I, S = g.nc, g.s, g.NS, g.I, g.S
    with ExitStack() as st:
        sb = lambda n, sh, dt: st.enter_context(nc.sbuf_tensor("p5_" + n, sh, dt))
        pm = lambda n, sh, dt: st.enter_context(nc.psum_tensor("p5_" + n, sh, dt))
        wcd = sb("wcd", [128, 8, 3104], BF16)
        wsT = sb("wsT", [128, 8, 128], BF16)
        bsT = sb("bsT", [128, 8], F32)
        sgg = sb("sgg", [128, 512], F32)
        gbias = sb("gbias", [128, 32], F32)
        xt = [sb(f"xt{i}", [128, 4, D], F32) for i in range(2)]
        xn = sb("xn", [128, 4, D], BF16)
        junk = sb("junk", [128, D], BF16)
        ss = sb("ss", [128, 4], F32)
        rstd = sb("rstd", [128, 4], F32)
        hmT = sb("hmT", [128, 8, 512], BF16)
        qk = [sb(f"qk{i}", [128, 1536], BF16) for i in range(2)]
        vst = [sb(f"vst{i}", [128, 4, 512], BF16) for i in range(2)]
        kst = [sb(f"kst{i}", [128, 4, 512], BF16) for i in range(2)]
        ogst = [sb(f"ogst{i}", [128, 4, 512], F32) for i in range(2)]
        gst = [sb(f"gst{i}", [128, 4, 32], F32) for i in range(2)]
        stg = [sb(f"stg{i}", [128, 12, 512], BF16) for i in range(2)]
        ugs = [sb(f"ug{i}", [128, 512], F32) for i in range(2)]
        vg = sb("vg", [128, 512], F32)
        vc = sb("vc", [128, 512], F32)
        vln = sb("vln", [128, 512], BF16)
        sgt = sb("sgt", [128, 512], F32)
        lns = sb("lns", [128, 4], F32)
        tps = [pm("tp0", [128, 8, 256], BF16)]
        zp = [pm(f"zp{i}", [128, 512], F32) for i in range(2)]
        qT = pm("qT", [128, 16, 128], BF16)
        sgp = pm("sgp", [128, 512], F32)
        for k in range(8):
            for (d0, s0, w) in ((0, 0, 1024), (1024, 1024, 1024), (2048, 2080, 512), (2560, 2592, 512),
                                (3072, 2048, 32)):
                s.dma('pool', 'p5_wcd', wcd[:, k, d0:d0 + w], I.w_in_cd[k * 128:(k + 1) * 128, s0:s0 + w],
                      writes=['wcd'])
        s.dma('pool', 'p5_wsT', wsT[:].rearrange("q g p -> q (g p)"), I.wsT[:, :], writes=['wsT'])
        s.dma('sp', 'p5_bsT', bsT[:], I.bsT[:, :], writes=['bsT'])
        s.dma('sp', 'p5_sgg', sgg[:], I.sgg[0:1, :].partition_broadcast(128), writes=['sgg'])
        s.dma('sp', 'p5_gbias', gbias[:], I.gate_bias.rearrange("a b -> b a")[0:1, :].partition_broadcast(128),
              writes=['gbias'])
        tiles = []
        for j, Sj in enumerate(g.seqs):
            for t in range(Sj // 512):
                tiles.append((j, g.offs[j] + t * 512))

        def load(i):
            j, t0 = tiles[i]
            b = i % 2
            s.dma('sp', f'p5_xt{b}', xt[b][:], S.x1[t0:t0 + 512, :].rearrange("(b p) d -> p b d", p=128),
                  writes=[f'xt{b}'])
        load(0)
        zi = 0
        pend = []
        for i, (j, t0) in enumerate(tiles):
            b = i % 2
            if i + 1 < len(tiles):
                load(i + 1)
            rms_prep(g, 'p5', xt[b], xn, ss, rstd, junk, 4, f'xt{b}', 'xn')
            to_featmajor(g, 'p5', xn, hmT, tps, 1, 0, j, 4, 'xn', 'hmT')
            for blk in range(4):
                q = qk[blk % 2]
                qkk = f'qk{blk % 2}'
                if blk > 0:
                    pass
                for c in range(7):
                    n0 = c * 512
                    nw = min(512, 3104 - n0)
                    p = zp[zi % 2]
                    pn = f'zp{zi % 2}'
                    zi += 1

                    def mm(e, p=p, n0=n0, nw=nw, blk=blk):
                        for k in range(8):
                            r = e.matmul(p[:, 0:nw], lhsT=hmT[:, k, blk * 128:(blk + 1) * 128],
                                         rhs=wcd[:, k, n0:n0 + nw], start=(k == 0), stop=(k == 7))
                        return r
                    s.op('pe', mm, reads=['hmT', 'wcd'], writes=[pn])
                    if c == 0:
                        s.op('act', lambda e, p=p, q=q: e.activation(out=q[:, 0:512], in_=p[:, :], func=AF.Copy),
                             reads=[pn], writes=[qkk + 'q'])
                    elif c == 1:
                        s.op('act', lambda e, p=p, q=q: e.activation(out=q[:, 512:1024], in_=p[:, :], func=AF.Copy,
                                                                     scale=0.125), reads=[pn], writes=[qkk + 'k'])
                        s.op('pool', lambda e, q=q, blk=blk, b=b: e.tensor_copy(out=kst[b][:, blk, :],
                                                                               in_=q[:, 512:1024]),
                             reads=[qkk + 'k'], writes=[f'kst{b}'])
                    elif c == 2:
                        s.op('act', lambda e, p=p, blk=blk, b=b: e.activation(out=vst[b][:, blk, :], in_=p[:, :],
                                                                              func=AF.Copy),
                             reads=[pn], writes=[f'vst{b}'])
                    elif c == 3:
                        s.op('act', lambda e, p=p, blk=blk, b=b: e.activation(out=ogst[b][:, blk, :], in_=p[:, :],
                                                                              func=AF.Sigmoid),
                             reads=[pn], writes=[f'ogst{b}'])
                    elif c == 4:
                        s.op('act', lambda e, p=p, blk=blk: e.activation(out=ugs[blk % 2][:], in_=p[:, :],
                                                                         func=AF.Gelu_apprx_tanh),
                             reads=[pn], writes=[f'ug{blk % 2}'])
                    elif c == 5:
                        s.op('act', lambda e, p=p: e.activation(out=vg[:], in_=p[:, :], func=AF.Gelu_apprx_tanh,
                                                                accum_out=lns[:, 0:1]),
                             reads=[pn], writes=['vg', 'lns0'])
                    else:
                        s.op('dve', lambda e, p=p, blk=blk, b=b: e.tensor_tensor(
                            out=gst[b][:, blk, :], in0=p[:, 0:32], in1=gbias[:], op=ALU.add),
                            reads=[pn, 'gbias'], writes=[f'gst{b}'])
                while pend:
                    pend.pop(0)()
                s.op('dve', lambda e: e.tensor_scalar(out=lns[:, 1:2], in0=lns[:, 0:1], scalar1=-1.0 / 512,
                                                      scalar2=None, op0=ALU.mult), reads=['lns0'], writes=['lns1'])
                s.op('act', lambda e: e.activation(out=vc[:], in_=vg[:], func=AF.Identity, bias=lns[:, 1:2]),
                     reads=['vg', 'lns1'], writes=['vc'])
                s.op('act', lambda e: e.activation(out=junk[:, 0:512], in_=vc[:], func=AF.Square,
                                                   accum_out=lns[:, 2:3]), reads=['vc'], writes=['lns2', 'p5junk'])
                rsqrt_act(g, lns[:, 3:4], lns[:, 2:3], 1.0 / 512, ['lns2'], ['lns3'])
                s.op('dve', lambda e: e.scalar_tensor_tensor(out=vln[:], in0=vc[:], scalar=lns[:, 3:4], in1=sgg[:],
                                                             op0=ALU.mult, op1=ALU.mult),
                     reads=['vc', 'lns3', 'sgg'], writes=['vln'])

                def tail(q=q, qkk=qkk, blk=blk, b=b):
                    def sgm(e):
                        for grp in range(8):
                            r = e.matmul(sgp[:, grp * 64:(grp + 1) * 64], lhsT=wsT[:, grp, :],
                                         rhs=vln[:, grp * 64:(grp + 1) * 64], start=True, stop=True)
                        return r
                    s.op('pe', sgm, reads=['wsT', 'vln'], writes=['sgp'])
                    s.op('dve', lambda e: e.tensor_tensor(out=sgt[:].rearrange("p (g e) -> p g e", e=64),
                                                          in0=sgp[:, :].rearrange("p (g e) -> p g e", e=64),
                                                          in1=bsT[:, :].unsqueeze(2).to_broadcast([128, 8, 64]),
                                                          op=ALU.add), reads=['sgp', 'bsT'], writes=['sgt'])
                    s.op('pool', lambda e, q=q: e.tensor_tensor(out=q[:, 1024:1536], in0=sgt[:], in1=ugs[blk % 2][:],
                                                                op=ALU.mult),
                         reads=['sgt', f'ug{blk % 2}'], writes=[qkk + 'd'])

                    def trq(e, q=q):
                        for c in range(12):
                            r = e.transpose(out=qT[:, c, :], in_=q[:, c * 128:(c + 1) * 128], identity=g.identB[:])
                        return r
                    s.op('pe', trq, reads=[qkk + 'q', qkk + 'k', qkk + 'd', 'identB'], writes=['qT'])
                    s.op('act', lambda e, blk=blk, b=b: e.activation(out=stg[b][:, :, blk * 128:(blk + 1) * 128],
                                                                     in_=qT[:, 0:12, :], func=AF.Copy),
                         reads=['qT'], writes=[f'stg{b}'])

                pend.append(tail)
            while pend:
                pend.pop(0)()
            for (dst, r0, c0, n) in ((S.qcT, 0, 0, 4), (S.kcT, 0, 4, 4), (S.o2T, 512, 8, 4)):
                s.dma('sp', f'p5_stg{b}', dst[r0:r0 + 512, t0:t0 + 512].rearrange("(c p) t -> p c t", p=128),
                      stg[b][:, c0:c0 + n, :], reads=[f'stg{b}'])
            tm = lambda d: d[t0:t0 + 512, :].rearrange("(b p) d -> p b d", p=128)
            s.dma('sp', f'p5_vst{b}', tm(S.vc), vst[b][:], reads=[f'vst{b}'])
            s.dma('sp', f'p5_kst{b}', tm(S.kc), kst[b][:], reads=[f'kst{b}'])
            s.dma('sp', f'p5_ogst{b}', tm(S.ogs), ogst[b][:], reads=[f'ogst{b}'])
            s.dma('sp', f'p5_gst{b}', tm(S.gtok), gst[b][:], reads=[f'gst{b}'])
        s.barrier()


def phase6(g):
    nc, s, I, S = g.nc, g.s, g.I, g.S
    SMAX = max(g.seqs)
    NCH = SMAX // 128
    with ExitStack() as st:
        sb = lambda n, sh, dt: st.enter_context(nc.sbuf_tensor("p6_" + n, sh, dt))
        pm = lambda n, sh, dt: st.enter_context(nc.psum_tensor("p6_" + n, sh, dt))
        SU = sb("SU", [128, 128], F32)
        SL = sb("SL", [128, 128], F32)
        ONES = sb("ONES", [128, 128], F32)
        maskF = sb("maskF", [128, 128], BF16)
        maskB = sb("maskB", [128, 128], BF16)
        mhg = sb("mhg", [128, 512], F32)
        gz = sb("gz", [128, NCH, 32], F32)
        l1 = [sb(f"l1{d}", [128, NCH * 8], F32) for d in range(2)]
        tmpg = sb("tmpg", [128, NCH * 8], F32)
        E = [sb(f"E{d}", [128, NCH * 8], F32) for d in range(2)]
        THR = [sb(f"THR{d}", [128, NCH * 8], F32) for d in range(2)]
        DEC = [sb(f"DEC{d}", [128, NCH * 8], F32) for d in range(2)]
        QTh = [sb(f"QTh{i}", [128, SMAX], BF16) for i in range(2)]
        KT = sb("KT", [128, SMAX], BF16)
        Ktok = sb("Ktok", [128, NCH, 128], BF16)
        V1 = sb("V1", [128, NCH, 2, 65], BF16)
        OGS = sb("OGS", [128, NCH, 128], F32)
        HF = sb("HF", [128, NCH, 128], F32)
        Wst = [sb(f"W{d}", [128, 65], F32) for d in range(2)]
        U = [sb(f"U{i}", [128, 65], BF16) for i in range(4)]
        Kw = [sb(f"Kw{i}", [128, 64], BF16) for i in range(4)]
        PT = [sb(f"PT{i}", [128, 128], BF16) for i in range(4)]
        rr = [sb(f"rr{i}", [128, 2], F32) for i in range(4)]
        sqt = sb("sqt", [128, 4, 128], F32)
        ssq = sb("ssq", [128, 8], F32)
        hn = sb("hn", [128, 4, 128], F32)
        hb = sb("hb", [128, 4, 128], BF16)
        stg = [sb(f"stg{i}", [128, 512], BF16) for i in range(2)]
        Bk = [pm(f"Bk{i}", [128, 512], F32) for i in range(6)]
        GP = Bk[0:4]
        trP = pm("trP", [128, 4, 128], BF16)
        DECp = [sb(f"DECp{d}", [128, NCH, 4], F32) for d in range(2)]
        Upair = [sb(f"Up{i}", [128, 65], BF16) for i in range(4)]
        Kwp = [sb(f"Kwp{i}", [128, 2, 64], BF16) for i in range(4)]
        PTp = [sb(f"PTp{i}", [128, 2, 128], BF16) for i in range(4)]
        rp = [sb(f"rp{i}", [128, 4], F32) for i in range(4)]
        htmp = [sb(f"htmp{i}", [128, 2, 64], F32) for i in range(2)]

        def tri(t, nm, cm, step, base):
            s.op('pool', lambda e: e.memset(t[:], 1.0), writes=[nm])
            s.op('pool', lambda e: e.affine_select(out=t[:], in_=t[:], pattern=[[step, 128]], compare_op=ALU.is_ge,
                                                   fill=0.0, base=base, channel_multiplier=cm),
                 reads=[nm], writes=[nm])
        tri(SU, 'SU', 1, -1, -1)
        tri(SL, 'SL', -1, 1, -1)
        tri(maskF, 'maskF', -1, 1, 0)
        tri(maskB, 'maskB', 1, -1, 0)
        s.op('pool', lambda e: e.memset(ONES[:], 1.0), writes=['ONES'])
        s.op('pool', lambda e: e.memset(QTh[0][64:128, :], 0.0), writes=['QT'])
        s.op('pool', lambda e: e.memset(QTh[1][0:64, :], 0.0), writes=['QT'])
        s.op('pool', lambda e: e.memset(V1[:, :, :, 64:65], 1.0), writes=['V1'])
        s.dma('sp', 'p6_mhg', mhg[:], I.mhg[0:1, :].partition_broadcast(128), writes=['mhg'])
        islot = 0
        istg = 0
        for j, Sj in enumerate(g.seqs):
            base = g.offs[j]
            nch = Sj // 128
            N8 = nch * 8
            s.dma('sp', 'p6_gz', gz[:, 0:nch, :], S.gtok[base:base + Sj, :].rearrange("(c p) d -> p c d", p=128),
                  writes=['gz'])
            for d in range(2):
                fcol = 8 + 16 * d
                icol = 16 * d
                l1v = l1[d][:, 0:N8].rearrange("p (c h) -> p c h", h=8)
                s.op('act', lambda e, nch=nch, N8=N8, l1v=l1v, fcol=fcol: e.activation(out=l1v, in_=gz[:, 0:nch, fcol:fcol + 8],
                                                                       func=AF.Exp, scale=-1.0),
                     reads=['gz'], writes=[f'l1{d}'])
                s.op('act', lambda e, nch=nch, N8=N8, d=d: e.activation(out=l1[d][:, 0:N8], in_=l1[d][:, 0:N8], func=AF.Ln, bias=1.0),
                     reads=[f'l1{d}'], writes=[f'l1{d}'])
                Gp, Tp = GP[2 * d], GP[2 * d + 1]
                tri_m = SU if d == 0 else SL
                s.op('pe', lambda e, nch=nch, N8=N8, Gp=Gp, tri_m=tri_m, d=d: e.matmul(Gp[:, 0:N8], lhsT=tri_m[:], rhs=l1[d][:, 0:N8],
                                                                      start=True, stop=True),
                     reads=['SU', 'SL', f'l1{d}'], writes=[f'B{2 * d}'])
                s.op('pe', lambda e, nch=nch, N8=N8, Tp=Tp, d=d: e.matmul(Tp[:, 0:N8], lhsT=ONES[:], rhs=l1[d][:, 0:N8],
                                                         start=True, stop=True),
                     reads=['ONES', f'l1{d}'], writes=[f'B{2 * d + 1}'])
                s.op('dve', lambda e, nch=nch, N8=N8, Gp=Gp, icol=icol: e.tensor_tensor(
                    out=tmpg[:, 0:N8].rearrange("p (c h) -> p c h", h=8), in0=gz[:, 0:nch, icol:icol + 8],
                    in1=Gp[:, 0:N8].rearrange("p (c h) -> p c h", h=8), op=ALU.subtract),
                    reads=['gz', f'B{2 * d}'], writes=['tmpg'])
                s.op('act', lambda e, nch=nch, N8=N8, d=d: e.activation(out=E[d][:, 0:N8], in_=tmpg[:, 0:N8], func=AF.Exp),
                     reads=['tmpg'], writes=[f'E{d}'])
                s.op('act', lambda e, nch=nch, N8=N8, d=d, Gp=Gp: e.activation(out=THR[d][:, 0:N8], in_=Gp[:, 0:N8], func=AF.Exp,
                                                              scale=-1.0), reads=[f'B{2 * d}'], writes=[f'THR{d}'])
                s.op('act', lambda e, nch=nch, N8=N8, d=d, Tp=Tp: e.activation(out=DEC[d][:, 0:N8], in_=Tp[:, 0:N8], func=AF.Exp,
                                                              scale=-1.0), reads=[f'B{2 * d + 1}'], writes=[f'DEC{d}'])
            for d in range(2):
                for hh in range(2):
                    p0 = hh * 64
                    s.op('pool', lambda e, nch=nch, N8=N8, d=d, hh=hh, p0=p0: e.tensor_copy(
                        out=DECp[d][p0:p0 + 64, 0:nch, :],
                        in_=DEC[d][p0:p0 + 64, 0:N8].rearrange("p (c hp two) -> p c hp two", hp=4, two=2)[:, :, :, hh]),
                        reads=[f'DEC{d}'], writes=[f'DECp{d}'])
            for hp in range(4):
                r0 = hp * 128
                s.dma('sp', 'p6_QT', QTh[0][0:64, 0:Sj], S.qcT[r0:r0 + 64, base:base + Sj], writes=['QT'])
                s.dma('sp', 'p6_QT', QTh[1][64:128, 0:Sj], S.qcT[r0 + 64:r0 + 128, base:base + Sj], writes=['QT'])
                s.dma('sp', 'p6_KT', KT[:, 0:Sj], S.kcT[r0:r0 + 128, base:base + Sj], writes=['KT'])
                s.dma('sp', 'p6_Ktok', Ktok[:, 0:nch, :],
                      S.kc[base:base + Sj, r0:r0 + 128].rearrange("(c p) d -> p c d", p=128), writes=['Ktok'])
                for hh in range(2):
                    s.dma('sp', 'p6_V1', V1[:, 0:nch, hh, 0:64],
                          S.vc[base:base + Sj, r0 + hh * 64:r0 + hh * 64 + 64].rearrange("(c p) e -> p c e", p=128),
                          writes=['V1'])
                s.dma('sp', 'p6_OGS', OGS[:, 0:nch, :],
                      S.ogs[base:base + Sj, r0:r0 + 128].rearrange("(c p) d -> p c d", p=128), writes=['OGS'])
                pend = []

                def flush(n):
                    while len(pend) > n:
                        pend.pop(0)()
                for ci in range(nch):
                    for d in range(2):
                        c = ci if d == 0 else nch - 1 - ci
                        first = (ci == 0)
                        last = (ci == nch - 1)
                        mask = maskF if d == 0 else maskB
                        sl = islot % 4
                        bkp = islot % 2
                        islot += 1
                        col0 = c * 8 + hp * 2
                        cs = slice(c * 128, (c + 1) * 128)
                        spb, Ob, dUb = Bk[bkp], Bk[2 + bkp], Bk[4 + bkp]
                        spk, Ok, dUk = f'B{bkp}', f'B{2 + bkp}', f'B{4 + bkp}'

                        def st1(spb=spb, spk=spk, cs=cs, sl=sl, c=c, d=d, col0=col0, mask=mask):
                            def mm(e):
                                for hh in range(2):
                                    p0 = hh * 64
                                    r = e.matmul(spb[:, hh * 128:(hh + 1) * 128], lhsT=KT[:, cs],
                                                 rhs=QTh[hh][:, cs], start=True, stop=True)
                                return r
                            s.op('pe', mm, reads=['KT', 'QT'], writes=[spk])
                            for hh in range(2):
                                s.op('act', lambda e, hh=hh: e.activation(
                                    out=Kwp[sl][:, hh, :], in_=Ktok[:, c, hh * 64:(hh + 1) * 64], func=AF.Copy,
                                    scale=E[d][:, col0 + hh:col0 + hh + 1]),
                                    reads=['Ktok', f'E{d}'], writes=[f'Kwp{sl}'])
                            for hh in range(2):
                                s.op('dve', lambda e, hh=hh: e.scalar_tensor_tensor(
                                    out=PTp[sl][:, hh, :], in0=spb[:, hh * 128:(hh + 1) * 128],
                                    scalar=E[d][:, col0 + hh:col0 + hh + 1], in1=mask[:], op0=ALU.mult, op1=ALU.mult),
                                    reads=[spk, f'E{d}', 'maskF', 'maskB'], writes=[f'PTp{sl}'])

                        firstw = (ci < nch // 2)

                        def st2(Ob=Ob, Ok=Ok, dUb=dUb, dUk=dUk, cs=cs, sl=sl, c=c, d=d, col0=col0, first=first,
                                last=last, hp=hp, firstw=firstw):
                            wk = f'W{d}'
                            u_ = Upair[sl]
                            if not first:
                                s.op('act', lambda e: e.activation(out=u_[:], in_=Wst[d][:], func=AF.Copy,
                                                                   scale=DECp[d][:, c, hp:hp + 1]),
                                     reads=[wk, f'DECp{d}'], writes=[f'Up{sl}'])

                            def omm(e):
                                for hh in range(2):
                                    p0 = hh * 64
                                    r = e.matmul(Ob[:, hh * 128:hh * 128 + 65], lhsT=PTp[sl][:, hh, :],
                                                 rhs=V1[:, c, hh, :], start=True, stop=first)
                                    if not first:
                                        r = e.matmul(Ob[:, hh * 128:hh * 128 + 65], lhsT=QTh[hh][:, cs],
                                                     rhs=u_[:, :], start=False, stop=True)
                                return r
                            s.op('pe', omm, reads=[f'PTp{sl}', 'V1', 'QT', f'Up{sl}'], writes=[Ok])
                            if not last:
                                def dmm(e):
                                    for hh in range(2):
                                        p0 = hh * 64
                                        r = e.matmul(dUb[p0:p0 + 64, 0:65], lhsT=Kwp[sl][:, hh, :],
                                                     rhs=V1[:, c, hh, :], start=True, stop=True)
                                    return r
                                s.op('pe', dmm, reads=[f'Kwp{sl}', 'V1'], writes=[dUk])
                                if first:
                                    s.op('dve', lambda e: e.tensor_copy(out=Wst[d][:], in_=dUb[:, 0:65]),
                                         reads=[dUk], writes=[wk])
                                else:
                                    s.op('dve', lambda e: e.scalar_tensor_tensor(
                                        out=Wst[d][:], in0=Wst[d][:], scalar=DECp[d][:, c, hp:hp + 1],
                                        in1=dUb[:, 0:65], op0=ALU.mult, op1=ALU.add),
                                        reads=[wk, f'DECp{d}', dUk], writes=[wk])
                            if os.environ.get('P6A'):
                                return
                            r_ = rp[sl]
                            Ov = Ob[:, 0:256].rearrange("p (h x) -> p h x", x=128)
                            s.op('act', lambda e: e.activation(out=r_[:, 0:2].unsqueeze(2), in_=Ov[:, :, 64:65],
                                                               func=AF.Abs), reads=[Ok], writes=[f'rp{sl}'])
                            s.op('dve', lambda e: e.tensor_tensor(out=r_[:, 0:2], in0=r_[:, 0:2],
                                                                  in1=THR[d][:, col0:col0 + 2], op=ALU.max),
                                 reads=[f'rp{sl}', f'THR{d}'], writes=[f'rp{sl}'])
                            s.op('dve', lambda e: e.reciprocal(out=r_[:, 2:4], in_=r_[:, 0:2]),
                                 reads=[f'rp{sl}'], writes=[f'rp{sl}b'])
                            hfv = HF[:, c, :].rearrange("p (h x) -> p h x", x=64)
                            rb = r_[:, 2:4].unsqueeze(2).to_broadcast([128, 2, 64])
                            if firstw:
                                s.op('dve', lambda e: e.tensor_tensor(out=hfv, in0=Ov[:, :, 0:64], in1=rb, op=ALU.mult),
                                     reads=[Ok, f'rp{sl}b'], writes=[f'HF{c}'])
                            else:
                                ht = htmp[sl % 2]
                                s.op('dve', lambda e: e.tensor_tensor(out=ht[:], in0=Ov[:, :, 0:64], in1=rb, op=ALU.mult),
                                     reads=[Ok, f'rp{sl}b'], writes=[f'htmp{sl % 2}'])
                                s.op('pool', lambda e: e.tensor_tensor(out=hfv, in0=hfv, in1=ht[:], op=ALU.add),
                                     reads=[f'htmp{sl % 2}', f'HF{c}'], writes=[f'HF{c}'])
                        st1()
                        pend.append(st2)
                        flush(int(os.environ.get('LAG6', '1')))
                flush(0)
                for cg in range(nch // 4):
                    hv = HF[:, cg * 4:(cg + 1) * 4, :]
                    hkeys = [f'HF{c}' for c in range(cg * 4, cg * 4 + 4)]
                    s.op('act', lambda e, hv=hv: e.activation(out=sqt[:], in_=hv, func=AF.Square),
                         reads=hkeys, writes=['sqt'])
                    s.op('dve', lambda e: e.tensor_reduce(out=ssq[:], in_=sqt[:].rearrange("p c (h e) -> p (c h) e", e=64),
                                                          axis=AX.X, op=ALU.add), reads=['sqt'], writes=['ssq'])
                    rsqrt_act(g, ssq[:], ssq[:], 1.0 / 64, ['ssq'], ['ssq'])
                    s.op('dve', lambda e, hv=hv: e.tensor_tensor(
                        out=hn[:].rearrange("p c (h e) -> p (c h) e", e=64),
                        in0=hv.rearrange("p c (h e) -> p (c h) e", e=64),
                        in1=ssq[:].unsqueeze(2).to_broadcast([128, 8, 64]), op=ALU.mult),
                        reads=hkeys + ['ssq'], writes=['hn'])
                    s.op('pool', lambda e, r0=r0: e.tensor_tensor(out=hn[:], in0=hn[:],
                                                                  in1=bc(mhg[:, r0:r0 + 128], [128, 4, 128]), op=ALU.mult),
                         reads=['hn', 'mhg'], writes=['hn'])
                    s.op('pool', lambda e, cg=cg: e.tensor_tensor(out=hb[:], in0=hn[:], in1=OGS[:, cg * 4:(cg + 1) * 4, :],
                                                                  op=ALU.mult), reads=['hn', 'OGS'], writes=['hb'])

                    def trh(e):
                        for i in range(4):
                            r = e.transpose(out=trP[:, i, :], in_=hb[:, i, :], identity=g.identB[:])
                        return r
                    s.op('pe', trh, reads=['hb', 'identB'], writes=['trP'])
                    bs = istg % 2
                    istg += 1
                    s.op('act', lambda e, bs=bs: e.activation(out=stg[bs][:], in_=trP[:, :, :].rearrange("p c t -> p (c t)"),
                                                              func=AF.Copy), reads=['trP'], writes=[f'stg{bs}'])
                    t0 = base + cg * 512
                    s.dma('sp', f'p6_stg{bs}', S.o2T[r0:r0 + 128, t0:t0 + 512], stg[bs][:], reads=[f'stg{bs}'])
        s.barrier()
```
